# Optimizing a Trainium2 kernel written in Bass

```python
import jax, jax.numpy as jnp
from jax import lax
import numpy as np

D_MODEL = 1024
BATCH = 8
SEQ = 4096
DEPTH = 2

CHUNK = 64
Q_BLOCK = 2 * CHUNK
HEAD_DIM = 64
CONV_CH = 256
CONV_W = 3
SB_HEADS = 6
FOX_HEADS = 6
SB_DIM = SB_HEADS * HEAD_DIM
FOX_DIM = FOX_HEADS * HEAD_DIM
D_MIX = CONV_CH + SB_DIM + FOX_DIM
D_IN = 3 * CONV_CH + 3 * SB_DIM + 3 * FOX_DIM + FOX_HEADS
N_MOD = 6
N_EXPERTS = 32
TOP_K = 4
D_EXPERT = D_MODEL
SWIGLU_ALPHA = 1.702
SWIGLU_LIMIT = 7.0
EPS = 1e-6

kernel_name = 'hybrid_conv_stickbreak_fox_moe_adaln'


def _rms(x):
    xf = x.astype(jnp.float32)
    return (xf * lax.rsqrt(jnp.mean(xf * xf, axis=-1, keepdims=True) + EPS)).astype(x.dtype)


def rms_norm(x, g):
    return _rms(x) * g


def short_conv_mixer(gb, gc, u, conv_w):
    v = gc * u
    y = lax.conv_general_dilated(v, conv_w[:, None, :], window_strides=(1,), padding=[(CONV_W - 1, 0)],
                                 dimension_numbers=('NWC', 'WIO', 'NWC'), feature_group_count=CONV_CH)
    return gb * y


def _query_blocks(t):
    b, h, s = t.shape[:3]
    t = t.reshape((b, h, s // Q_BLOCK, Q_BLOCK) + t.shape[3:])
    return jnp.moveaxis(t, 2, 0)


def _merge_blocks(o):
    nb, b, h, qb, d = o.shape
    return o.transpose(1, 0, 3, 2, 4).reshape(b, nb * qb, h * d)


def stick_breaking_attention(q, k, v):
    s = q.shape[2]
    scale = q.shape[-1] ** -0.5
    kpos = jnp.arange(s)

    def block(args):
        qb, start = args
        z = jnp.einsum('bhqd,bhkd->bhqk', qb, k, preferred_element_type=jnp.float32) * scale
        qpos = start + jnp.arange(Q_BLOCK)
        strict = kpos[None, :] < qpos[:, None]
        log_beta = jax.nn.log_sigmoid(z)
        log_1mb = jnp.where(strict, jax.nn.log_sigmoid(-z), 0.0)
        rest = lax.cumsum(log_1mb, axis=3, reverse=True) - log_1mb
        w = jnp.where(strict, jnp.exp(log_beta + rest), 0.0)
        return jnp.einsum('bhqk,bhkd->bhqd', w.astype(v.dtype), v)

    starts = jnp.arange(s // Q_BLOCK, dtype=jnp.int32) * Q_BLOCK
    return _merge_blocks(lax.map(block, (_query_blocks(q), starts)))


def forgetting_attention(q, k, v, log_f):
    s = q.shape[2]
    scale = q.shape[-1] ** -0.5
    kpos = jnp.arange(s)
    cum = lax.cumsum(log_f, axis=2)

    def block(args):
        qb, fq, start = args
        logits = jnp.einsum('bhqd,bhkd->bhqk', qb, k, preferred_element_type=jnp.float32) * scale
        logits = logits + (fq[..., None] - cum[:, :, None, :])
        qpos = start + jnp.arange(Q_BLOCK)
        causal = kpos[None, :] <= qpos[:, None]
        p = jax.nn.softmax(jnp.where(causal, logits, -jnp.inf), axis=-1)
        return jnp.einsum('bhqk,bhkd->bhqd', p.astype(v.dtype), v)

    starts = jnp.arange(s // Q_BLOCK, dtype=jnp.int32) * Q_BLOCK
    return _merge_blocks(lax.map(block, (_query_blocks(q), _query_blocks(cum), starts)))


def moe_ffn(h, router_w, router_b, w_mlp1, b_mlp1, w_mlp2, b_mlp2):
    b, s, d = h.shape
    xt = h.reshape(b * s, d)
    logits = (xt @ router_w + router_b).astype(jnp.float32)
    top_vals, top_idx = lax.top_k(logits, TOP_K)
    top_w = jax.nn.softmax(top_vals, axis=-1)
    combine = jnp.einsum('tk,tke->et', top_w, jax.nn.one_hot(top_idx, N_EXPERTS, dtype=jnp.float32))

    def expert_step(acc, params):
        w1, b1, w2, b2, gate_e = params
        u = xt @ w1 + b1
        glu = jnp.minimum(u[:, ::2], SWIGLU_LIMIT)
        lin = jnp.clip(u[:, 1::2], -SWIGLU_LIMIT, SWIGLU_LIMIT)
        a = glu * jax.nn.sigmoid(SWIGLU_ALPHA * glu) * (lin + 1)
        out = a @ w2 + b2
        return acc + gate_e[:, None] * out.astype(jnp.float32), None

    acc0 = jnp.zeros((b * s, d), jnp.float32)
    acc, _ = lax.scan(expert_step, acc0, (w_mlp1, b_mlp1, w_mlp2, b_mlp2, combine))
    return acc.astype(h.dtype).reshape(b, s, d)


def hybrid_layer(x, c, norm1_g, w_ada, b_ada, w_in, conv_w, sb_q_g, sb_k_g, fox_q_g, fox_k_g, fox_f_b,
                 out_norm_g, w_out, norm2_g, router_w, router_b, w_mlp1, b_mlp1, w_mlp2, b_mlp2):
    b, s, d = x.shape
    mod = (jax.nn.silu(c) @ w_ada + b_ada).reshape(b, N_MOD, 1, d)
    shift1, scale1, gate1, shift2, scale2, gate2 = (mod[:, i] for i in range(N_MOD))

    h = rms_norm(x, norm1_g) * (1 + scale1) + shift1
    proj = h @ w_in
    cuts = [int(v) for v in np.cumsum([CONV_CH] * 3 + [SB_DIM] * 3 + [FOX_DIM] * 3)]
    cb, cc, cu, sq, sk, sv, fq, fk, fv, flog = jnp.split(proj, cuts, axis=-1)

    def heads(t, n):
        return t.reshape(b, s, n, HEAD_DIM).transpose(0, 2, 1, 3)

    y_conv = short_conv_mixer(cb, cc, cu, conv_w)
    y_sb = stick_breaking_attention(rms_norm(heads(sq, SB_HEADS), sb_q_g),
                                    rms_norm(heads(sk, SB_HEADS), sb_k_g), heads(sv, SB_HEADS))
    log_f = jax.nn.log_sigmoid((flog + fox_f_b).astype(jnp.float32)).transpose(0, 2, 1)
    y_fox = forgetting_attention(rms_norm(heads(fq, FOX_HEADS), fox_q_g),
                                 rms_norm(heads(fk, FOX_HEADS), fox_k_g), heads(fv, FOX_HEADS), log_f)
    y = jnp.concatenate([y_conv, y_sb, y_fox], axis=-1)
    y = _rms(y.reshape(b, s, D_MIX // HEAD_DIM, HEAD_DIM)).reshape(b, s, D_MIX) * out_norm_g
    x = x + gate1 * (y @ w_out)

    h2 = rms_norm(x, norm2_g) * (1 + scale2) + shift2
    return x + gate2 * moe_ffn(h2, router_w, router_b, w_mlp1, b_mlp1, w_mlp2, b_mlp2)


def setup_inputs(seed: int = 0) -> dict:
    key = jax.random.key(seed)
    ks = jax.random.split(key, 24)
    nrm = jax.random.normal
    f32 = jnp.float32
    L, D, E = DEPTH, D_MODEL, N_EXPERTS
    return {
        'x': nrm(ks[0], (BATCH, SEQ, D), f32),
        'c': nrm(ks[1], (BATCH, D), f32),
        'norm1_g': 1.0 + 0.05 * nrm(ks[2], (L, D), f32),
        'w_ada': 0.5 * D ** -0.5 * nrm(ks[3], (L, D, N_MOD * D), f32),
        'b_ada': 0.02 * nrm(ks[4], (L, N_MOD * D), f32),
        'w_in': D ** -0.5 * nrm(ks[5], (L, D, D_IN), f32),
        'conv_w': CONV_W ** -0.5 * nrm(ks[6], (L, CONV_W, CONV_CH), f32),
        'sb_q_g': 1.0 + 0.05 * nrm(ks[7], (L, HEAD_DIM), f32),
        'sb_k_g': 1.0 + 0.05 * nrm(ks[8], (L, HEAD_DIM), f32),
        'fox_q_g': 1.0 + 0.05 * nrm(ks[9], (L, HEAD_DIM), f32),
        'fox_k_g': 1.0 + 0.05 * nrm(ks[10], (L, HEAD_DIM), f32),
        'fox_f_b': 2.0 + 0.5 * nrm(ks[11], (L, FOX_HEADS), f32),
        'out_norm_g': 1.0 + 0.05 * nrm(ks[12], (L, D_MIX), f32),
        'w_out': D_MIX ** -0.5 * nrm(ks[13], (L, D_MIX, D), f32),
        'norm2_g': 1.0 + 0.05 * nrm(ks[14], (L, D), f32),
        'router_w': D ** -0.5 * nrm(ks[15], (L, D, E), f32),
        'router_b': 0.01 * nrm(ks[16], (L, E), f32),
        'w_mlp1': D ** -0.5 * nrm(ks[17], (L, E, D, 2 * D_EXPERT), f32),
        'b_mlp1': 0.01 * nrm(ks[18], (L, E, 2 * D_EXPERT), f32),
        'w_mlp2': D_EXPERT ** -0.5 * nrm(ks[19], (L, E, D_EXPERT, D), f32),
        'b_mlp2': 0.01 * nrm(ks[20], (L, E, D), f32),
    }


def reference(x, c, norm1_g, w_ada, b_ada, w_in, conv_w, sb_q_g, sb_k_g, fox_q_g, fox_k_g, fox_f_b,
              out_norm_g, w_out, norm2_g, router_w, router_b, w_mlp1, b_mlp1, w_mlp2, b_mlp2):
    for l in range(DEPTH):
        x = hybrid_layer(x, c, norm1_g[l], w_ada[l], b_ada[l], w_in[l], conv_w[l], sb_q_g[l], sb_k_g[l],
                         fox_q_g[l], fox_k_g[l], fox_f_b[l], out_norm_g[l], w_out[l], norm2_g[l],
                         router_w[l], router_b[l], w_mlp1[l], b_mlp1[l], w_mlp2[l], b_mlp2[l])
    return x
```

```python
import numpy as np
import ml_dtypes
from contextlib import ExitStack
import concourse.bass as bass
import concourse.mybir as mybir
from concourse.bass_utils import run_bass_kernel_spmd

F32 = mybir.dt.float32
BF16 = mybir.dt.bfloat16
I32 = mybir.dt.int32
AF = mybir.ActivationFunctionType
ALU = mybir.AluOpType
AX = mybir.AxisListType

D = 1024
NL = 2
NE = 32
DIN = 3078
EPS = 1e-6
TS = 512
NBLK_MAX = 64
ENGS = ['sync', 'scalar', 'vector', 'gpsimd', 'tensor']


class Sem:
    def __init__(self, h):
        self.h = h
        self.v = 0
        self.nobarrier = False


class Buf:
    def __init__(self, t, name):
        self.t = t
        self.name = name
        self.w = {}
        self.r = {}
        self.dsem = None

    def __getitem__(self, idx):
        return self.t[idx]


class Prog:
    def __init__(self, nc, stack):
        self.nc = nc
        self.stack = stack
        self.q = {e: [] for e in ENGS}
        self.esem = {}
        self.allsems = []
        self.waited = {e: {} for e in ENGS}
        self.nsem = 0
        self.free_dsems = []
        self.phase_bufs = []
        for e in ['scalar', 'vector', 'gpsimd', 'tensor']:
            self.esem[e] = self.newsem('e_' + e)

    def newsem(self, name):
        h = self.stack.enter_context(self.nc.semaphore(name + '_%d' % self.nsem))
        self.nsem += 1
        s = Sem(h)
        self.allsems.append(s)
        return s

    def uname(self, name):
        self.nsem += 1
        return '%s_u%d' % (name, self.nsem)

    def sbuf(self, stack, name, shape, dt):
        name = self.uname(name)
        t = stack.enter_context(self.nc.sbuf_tensor(name, list(shape), dt))
        return Buf(t, name)

    def psum(self, stack, name, shape, dt=F32):
        name = self.uname(name)
        t = stack.enter_context(self.nc.psum_tensor(name, list(shape), dt))
        return Buf(t, name)

    def dram(self, name, shape, dt, kind="Internal"):
        t = self.nc.dram_tensor(name, list(shape), dt, kind=kind)
        return Buf(t, name)

    def op(self, eng, fn, r=(), w=(), dsem=None):
        waits = {}

        def addw(d):
            for s, v in d.items():
                if waits.get(s, 0) < v:
                    waits[s] = v
        for b in r:
            addw(b.w)
        for b in w:
            addw(b.w)
            addw(b.r)
        if dsem is not None:
            if dsem.dsem is None:
                dsem.dsem = self.free_dsems.pop() if self.free_dsems else self.newsem('d')
                self.phase_bufs.append(dsem)
            sem = dsem.dsem
            amt = 16
        else:
            sem = self.esem[eng]
            amt = 1
        wl = []
        for s, v in waits.items():
            if self.waited[eng].get(s, 0) >= v:
                continue
            if s not in self.esem.values():
                v = s.v
            if self.waited[eng].get(s, 0) < v:
                self.waited[eng][s] = v
                wl.append((s, v))
        sem.v += amt
        tok = (sem, sem.v)
        self.q[eng].append((fn, wl, sem, amt))
        for b in r:
            if b.r.get(sem, 0) < sem.v:
                b.r[sem] = sem.v
        for b in w:
            b.w = dict(b.w)
            b.w[sem] = sem.v
            b.r = {}
        return tok

    def barrier(self):
        for e in ENGS:
            wl = []
            for s in self.allsems:
                if s.nobarrier:
                    continue
                if s.v > 0 and self.waited[e].get(s, 0) < s.v:
                    self.waited[e][s] = s.v
                    wl.append((s, s.v))
            self.q[e].append((None, wl, None, 0))
        for b in self.phase_bufs:
            self.free_dsems.append(b.dsem)
            b.dsem = None
        self.phase_bufs = []

    def flush(self):
        with self.nc.Block() as block:
            for e in ENGS:
                items = self.q[e]

                def body(eng, items=items):
                    for fn, wl, sem, amt in items:
                        for s, v in wl:
                            eng.wait_ge(s.h, v)
                        if fn is not None:
                            ins = fn(eng)
                            ins.then_inc(sem.h, amt)
                getattr(block, e)(body)
        self.q = {e: [] for e in ENGS}


def bcast_ap(ap1d_tensor, offset, n, parts=128):
    return bass.AP(ap1d_tensor, offset, [[0, parts], [1, n]])


def make_consts():
    c = {}
    c['ident_bf'] = np.eye(128, dtype=np.float32).astype(ml_dtypes.bfloat16)
    c['ident_f'] = np.eye(128, dtype=np.float32)
    blk = np.zeros((128, 128), np.float32)
    blk[:64, :64] = 1.0 / 64
    blk[64:, 64:] = 1.0 / 64
    c['blk64'] = blk
    wn = np.full((65, 64), 1.0 / 64, np.float32)
    wn[64, :] = EPS
    c['wn65'] = wn
    j = np.arange(128)[:, None]
    k = np.arange(128)[None, :]
    c['negtri'] = np.where(j >= k, -1.0, 0.0).astype(np.float32).astype(ml_dtypes.bfloat16)
    c['negones'] = np.full((128, 128), -1.0, np.float32).astype(ml_dtypes.bfloat16)
    p = np.arange(128)[:, None]
    col = np.arange(512)[None, :]
    ms = np.zeros((4, 128, 512), np.float32)
    mn = np.zeros((4, 128, 512), np.float32)
    for jj in range(4):
        bc = col // 128
        ms[jj] = np.where(bc < jj, 0.0, np.where(bc == jj, (p < (col % 128)), 1.0))
        mn[jj] = np.where(bc < jj, 0.0, np.where(bc == jj, (p <= (col % 128)), 1.0))
    c['stri_f'] = (j < k).astype(np.float32)
    c['pidx'] = np.stack([np.arange(128), 8 * np.arange(128), np.minimum(np.arange(128), 1)],
                         1).astype(np.float32)
    c['blkstart'] = np.broadcast_to((np.arange(NBLK_MAX, dtype=np.float32) * TS)[None, :], (128, NBLK_MAX)).copy()
    c['mask_s'] = ms.transpose(1, 0, 2).copy().astype(ml_dtypes.bfloat16)
    c['mask_n'] = mn.transpose(1, 0, 2).copy().astype(ml_dtypes.bfloat16)
    return c


CONST_SPECS = [('ident_bf', [128, 128], BF16), ('ident_f', [128, 128], F32), ('blk64', [128, 128], F32),
               ('wn65', [65, 64], F32), ('negtri', [128, 128], BF16), ('negones', [128, 128], BF16),
               ('mask_s', [128, 4, 512], BF16), ('mask_n', [128, 4, 512], BF16),
               ('stri_f', [128, 128], F32), ('blkstart', [128, NBLK_MAX], F32), ('pidx', [128, 3], F32)]

INPUT_SPECS = [
    ('x', lambda S: [S, D]), ('c', lambda S: [D]), ('norm1_g', lambda S: [NL, D]),
    ('w_ada', lambda S: [NL, D, 6 * D]), ('b_ada', lambda S: [NL, 6 * D]), ('w_in', lambda S: [NL, D, DIN]),
    ('conv_w', lambda S: [NL, 3, 256]), ('sb_q_g', lambda S: [NL, 64]), ('sb_k_g', lambda S: [NL, 64]),
    ('fox_q_g', lambda S: [NL, 64]), ('fox_k_g', lambda S: [NL, 64]), ('fox_f_b', lambda S: [NL, 6]),
    ('out_norm_g', lambda S: [NL, D]), ('w_out', lambda S: [NL, D, D]), ('norm2_g', lambda S: [NL, D]),
    ('router_w', lambda S: [NL, D, NE]), ('router_b', lambda S: [NL, NE]),
    ('w_mlp1', lambda S: [NL, NE, D, 2 * D]), ('b_mlp1', lambda S: [NL, NE, 2 * D]),
    ('w_mlp2', lambda S: [NL, NE, D, D]), ('b_mlp2', lambda S: [NL, NE, D]),
]


class K:
    def __init__(self, S, nlayers=NL, debug=False, stop_after=None, sparse=True):
        self.sparse = sparse
        self.S = S
        self.NT = S // 128
        self.NC = S // 512
        self.debug = debug
        self.nlayers = nlayers
        self.stop_after = stop_after
        nc = bass.Bass("TRN2", target_bir_lowering=False)
        self.nc = nc
        self.stack = ExitStack()
        self.P = Prog(nc, self.stack)
        P = self.P
        self.inp = {}
        for name, shp in INPUT_SPECS:
            self.inp[name] = Buf(nc.dram_tensor(name, shp(S), F32, kind="ExternalInput"), name)
        self.cst = {}
        for name, shp, dt in CONST_SPECS:
            self.cst[name] = Buf(nc.dram_tensor('c_' + name, shp, dt, kind="ExternalInput"), name)
        self.out = Buf(nc.dram_tensor("out", [S, D], F32, kind="ExternalOutput"), "out")
        sk = "ExternalOutput" if debug else "Internal"
        self.sk = sk
        self.modv = P.dram("modv", [NL, 6 * D], F32, sk)
        self.qT_sb = P.dram("qT_sb", [384, S], BF16, sk)
        self.kT_sb = P.dram("kT_sb", [384, S], BF16, sk)
        self.v_sb = P.dram("v_sb", [S, 384], BF16, sk)
        self.qTa = P.dram("qTa", [6, 70, S], BF16, sk)
        self.kTa = P.dram("kTa", [6, 70, S], BF16, sk)
        self.v_fox = P.dram("v_fox", [S, 384], BF16, sk)
        self.yT = P.dram("yT", [D, S], BF16, sk)
        self.x1 = P.dram("x1", [S, D], F32, sk)
        self.x2 = P.dram("x2", [S, D], F32, sk)

    def load_consts(self):
        P = self.P
        st = self.stack
        self.c = {}
        for name, shp, dt in CONST_SPECS:
            b = P.sbuf(st, 'k_' + name, shp, dt)
            src = self.cst[name]
            P.op('sync', lambda e, b=b, src=src: e.dma_start(out=b[:], in_=src.t.ap()), r=[src], w=[b], dsem=b)
            self.c[name] = b
        ones = P.sbuf(st, 'k_ones', [128, 512], F32)
        P.op('vector', lambda e: e.memset(ones[:], 1.0), w=[ones])
        self.c['ones'] = ones
        onesb = P.sbuf(st, 'k_onesb', [128, 512], BF16)
        P.op('vector', lambda e: e.memset(onesb[:], 1.0), w=[onesb])
        self.c['onesb'] = onesb


    def precast_weights(self):
        P = self.P
        inp = self.inp
        self.wc1, self.wc2 = [], []
        for l in range(self.nlayers):
            b1 = P.dram("wc1_%d" % l, [NE * D, 2 * D], BF16)
            b2 = P.dram("wc2_%d" % l, [NE * D, D], BF16)
            for b in (b1, b2):
                b.dsem = P.newsem('wc')
                b.dsem.nobarrier = True
            self.wc1.append(b1)
            self.wc2.append(b2)
        self.pending_casts = [[] for _ in range(self.nlayers)]
        for l in range(self.nlayers):
            for e_ in range(NE):
                src1 = inp['w_mlp1'].t.ap()[l, e_].rearrange("(a b) n -> a (b n)", b=4)
                dst1 = self.wc1[l].t.ap()[e_ * D:(e_ + 1) * D, :].rearrange("(a b) n -> a (b n)", b=4)
                self.pending_casts[l].append((src1, dst1, self.wc1[l]))
                src2 = inp['w_mlp2'].t.ap()[l, e_].rearrange("(a b) n -> a (b n)", b=8)
                dst2 = self.wc2[l].t.ap()[e_ * D:(e_ + 1) * D, :].rearrange("(a b) n -> a (b n)", b=8)
                self.pending_casts[l].append((src2, dst2, self.wc2[l]))

    def issue_casts(self, l, n):
        P = self.P
        for _ in range(n):
            if not self.pending_casts[l]:
                return
            src, dst, buf = self.pending_casts[l].pop(0)
            P.op('gpsimd', lambda e, src=src, dst=dst: e.dma_start(out=dst, in_=src), w=[buf], dsem=buf)

    def phase_ada(self):
        P = self.P
        inp = self.inp
        with ExitStack() as st:
            cT = P.sbuf(st, 'a_cT', [128, 8], F32)
            sc = P.sbuf(st, 'a_sc', [128, 8], F32)
            wb = [P.sbuf(st, 'a_w%d' % i, [128, 3072], F32) for i in range(2)]
            bada = P.sbuf(st, 'a_b', [1, 6 * D], F32)
            modrow = P.sbuf(st, 'a_mod', [1, 6 * D], F32)
            ps = [P.psum(st, 'a_ps%d' % i, [128, 512]) for i in range(6)]
            cap = inp['c'].t.ap().rearrange("(p j) -> p j", j=8)
            P.op('sync', lambda e: e.dma_start(out=cT[:], in_=cap), w=[cT], dsem=cT)
            P.op('scalar', lambda e: e.activation(out=sc[:], in_=cT[:], func=AF.Silu), r=[cT], w=[sc])
            it = 0
            for l in range(self.nlayers):
                bsrc = inp['b_ada'].t.ap()[l:l + 1, :]
                P.op('sync', lambda e, bsrc=bsrc: e.dma_start(out=bada[:], in_=bsrc), w=[bada], dsem=bada)
                wv = inp['w_ada'].t.ap()[l].rearrange("(p j) n -> p j n", j=8)
                for half in range(2):
                    for j in range(8):
                        b = wb[it % 2]
                        it += 1
                        src = wv[:, j, half * 3072:(half + 1) * 3072]
                        P.op('sync' if it % 2 else 'gpsimd',
                             lambda e, b=b, src=src: e.dma_start(out=b[:], in_=src), w=[b], dsem=b)

                        def mm(e, b=b, j=j):
                            ins = None
                            for n in range(6):
                                ins = e.matmul(ps[n][0:1, :], sc[:, j:j + 1], b[:, n * 512:(n + 1) * 512],
                                               start=(j == 0), stop=(j == 7))
                            return ins
                        P.op('tensor', mm, r=[b, sc], w=ps)
                    for n in range(6):
                        o = half * 3072 + n * 512
                        P.op('vector', lambda e, n=n, o=o: e.tensor_tensor(
                            out=modrow[0:1, o:o + 512], in0=ps[n][0:1, :], in1=bada[0:1, o:o + 512], op=ALU.add),
                            r=[ps[n], bada], w=[modrow])
                dst = self.modv.t.ap()[l:l + 1, :]
                P.op('sync', lambda e, dst=dst: e.dma_start(out=dst, in_=modrow[:]), r=[modrow], w=[self.modv],
                     dsem=modrow)
            P.barrier()
            P.flush()

    def load_bcast(self, st, name, srcbuf, offset, n=D, eng='sync'):
        P = self.P
        b = P.sbuf(st, name, [128, n], F32)
        ap = bcast_ap(srcbuf.t, offset, n)
        P.op(eng, lambda e: e.dma_start(out=b[:], in_=ap), r=[srcbuf], w=[b], dsem=b)
        return b

    def mod_tiles(self, st, l, which, gname):
        P = self.P
        gb = self.load_bcast(st, 'm_g', self.inp[gname], l * D)
        sb = self.load_bcast(st, 'm_s', self.modv, l * 6 * D + (3 * which + 1) * D, eng='gpsimd')
        tb = self.load_bcast(st, 'm_t', self.modv, l * 6 * D + (3 * which + 0) * D)
        P.op('vector', lambda e: e.scalar_tensor_tensor(out=gb[:], in0=sb[:], scalar=1.0, in1=gb[:],
                                                       op0=ALU.add, op1=ALU.mult), r=[sb, gb], w=[gb])
        return gb, tb

    def rstd_from_ssq(self, ssq, lnv, rstd, n):
        P = self.P
        P.op('scalar', lambda e: e.activation(out=lnv[:], in_=ssq[:], func=AF.Ln, bias=EPS, scale=1.0 / n),
             r=[ssq], w=[lnv])
        P.op('scalar', lambda e: e.activation(out=rstd[:], in_=lnv[:], func=AF.Exp, scale=-0.5),
             r=[lnv], w=[rstd])

    def phase_norm_T(self, st, l, xsrc, which, gname, hT, hT_dram=None, router=False, h2b_all=None):
        P = self.P
        inp = self.inp
        with ExitStack() as s2:
            if hT_dram is not None:
                stg = [P.sbuf(s2, 'n_stg%d' % i, [128, 8, 512], BF16) for i in range(2)]
            if router:
                rw = P.sbuf(s2, 'n_rw', [128, 8, NE], F32)
                src_rw = inp['router_w'].t.ap()[l].rearrange("(k p) n -> p k n", p=128)
                P.op('sync', lambda e: e.dma_start(out=rw[:], in_=src_rw), w=[rw], dsem=rw)
                rb = self.load_bcast(s2, 'n_rb', inp['router_b'], l * NE, n=NE)
                h32 = [P.sbuf(s2, 'n_h32%d' % i, [128, D], F32) for i in range(2)]
                h32T = P.sbuf(s2, 'n_h32T', [128, 8, 128], F32)
                pR = P.psum(s2, 'n_pR', [128, 8, 128], F32)
                pL = P.psum(s2, 'n_pL', [128, 512], F32)
                lg = P.sbuf(s2, 'n_lg', [128, NE], F32)
                ex = P.sbuf(s2, 'n_ex', [128, NE], F32)
                t8 = P.sbuf(s2, 'n_t8', [128, 8], F32)
                nmx = P.sbuf(s2, 'n_nmx', [128, 1], F32)
                ssm = P.sbuf(s2, 'n_ssm', [128, 1], F32)
                identf = self.c['ident_f']
            G, T = self.mod_tiles(s2, l, which, gname)
            xt = [P.sbuf(s2, 'n_x%d' % i, [128, D], F32) for i in range(2)]
            junk = P.sbuf(s2, 'n_junk', [128, D], BF16)
            hn = [P.sbuf(s2, 'n_hn%d' % i, [128, D], F32) for i in range(2)]
            hb = [P.sbuf(s2, 'n_hb%d' % i, [128, D], BF16) for i in range(2)]
            ssq = [P.sbuf(s2, 'n_ssq%d' % i, [128, 1], F32) for i in range(2)]
            lnv = [P.sbuf(s2, 'n_ln%d' % i, [128, 1], F32) for i in range(2)]
            rstd = [P.sbuf(s2, 'n_rs%d' % i, [128, 1], F32) for i in range(2)]
            if h2b_all is None:
                pT = [P.psum(s2, 'n_pT%d' % i, [128, 8, 128], BF16) for i in range(2)]
            ident = self.c['ident_bf']
            for t in range(self.NT):
                i = t % 2
                src = xsrc.t.ap()[t * 128:(t + 1) * 128, :]
                P.op('sync', lambda e, i=i, src=src: e.dma_start(out=xt[i][:], in_=src), r=[xsrc], w=[xt[i]],
                     dsem=xt[i])
                P.op('scalar', lambda e, i=i: e.activation(out=junk[:], in_=xt[i][:], func=AF.Square,
                                                          accum_out=ssq[i][:]), r=[xt[i]], w=[junk, ssq[i]])
                self.rstd_from_ssq(ssq[i], lnv[i], rstd[i], D)
                P.op('vector', lambda e, i=i: e.scalar_tensor_tensor(
                    out=hn[i][:], in0=xt[i][:], scalar=rstd[i][:, 0:1], in1=G[:], op0=ALU.mult, op1=ALU.mult),
                    r=[xt[i], rstd[i], G], w=[hn[i]])
                if not router:
                    P.op('gpsimd', lambda e, i=i: e.tensor_tensor(out=hb[i][:], in0=hn[i][:], in1=T[:], op=ALU.add),
                         r=[hn[i], T], w=[hb[i]])
                else:
                    P.op('gpsimd', lambda e, i=i: e.tensor_tensor(out=h32[i][:], in0=hn[i][:], in1=T[:], op=ALU.add),
                         r=[hn[i], T], w=[h32[i]])
                    if h2b_all is None:
                        P.op('scalar', lambda e, i=i: e.copy(out=hb[i][:], in_=h32[i][:]), r=[h32[i]], w=[hb[i]])
                    else:
                        P.op('scalar', lambda e, i=i, t=t: e.copy(out=h2b_all[:, t, :], in_=h32[i][:]),
                             r=[h32[i]], w=[h2b_all])

                    def trf(e, i=i):
                        ins = None
                        for k in range(8):
                            ins = e.transpose(pR[:, k, :], h32[i][:, k * 128:(k + 1) * 128], identf[:])
                        return ins
                    P.op('tensor', trf, r=[h32[i], identf], w=[pR])
                    P.op('scalar', lambda e: e.copy(out=h32T[:], in_=pR[:]), r=[pR], w=[h32T])

                    def mmr(e):
                        ins = None
                        for k in range(8):
                            ins = e.matmul(pL[:, 0:NE], h32T[:, k, :], rw[:, k, :], start=(k == 0), stop=(k == 7))
                        return ins
                    P.op('tensor', mmr, r=[h32T, rw], w=[pL])
                    P.op('vector', lambda e: e.tensor_tensor(out=lg[:], in0=pL[:, 0:NE], in1=rb[:], op=ALU.add),
                         r=[pL, rb], w=[lg])
                    P.op('vector', lambda e: e.max(out=t8[:], in_=lg[:]), r=[lg], w=[t8])
                    mb = self.memb
                    cb = self.comb
                    P.op('vector', lambda e, t=t: e.tensor_scalar(out=mb[:, t, :], in0=lg[:], scalar1=t8[:, 3:4],
                                                                 scalar2=None, op0=ALU.is_ge), r=[lg, t8], w=[mb])
                    P.op('vector', lambda e: e.tensor_scalar(out=nmx[:], in0=t8[:, 0:1], scalar1=-1.0, scalar2=None,
                                                             op0=ALU.mult), r=[t8], w=[nmx])
                    P.op('scalar', lambda e: e.activation(out=ex[:], in_=lg[:], func=AF.Exp, bias=nmx[:, 0:1]),
                         r=[lg, nmx], w=[ex])
                    P.op('vector', lambda e, t=t: e.tensor_tensor(out=ex[:], in0=ex[:], in1=mb[:, t, :], op=ALU.mult),
                         r=[ex, mb], w=[ex])
                    P.op('vector', lambda e: e.reduce_sum(out=ssm[:], in_=ex[:], axis=AX.X), r=[ex], w=[ssm])
                    P.op('vector', lambda e: e.reciprocal(out=ssm[:], in_=ssm[:]), r=[ssm], w=[ssm])
                    P.op('vector', lambda e, t=t: e.tensor_scalar(out=cb[:, t, :], in0=ex[:], scalar1=ssm[:, 0:1],
                                                                 scalar2=None, op0=ALU.mult), r=[ex, ssm], w=[cb])

                if h2b_all is not None:
                    continue

                def tr(e, i=i):
                    ins = None
                    for k in range(8):
                        ins = e.transpose(pT[i][:, k, :], hb[i][:, k * 128:(k + 1) * 128], ident[:])
                    return ins
                P.op('tensor', tr, r=[hb[i], ident], w=[pT[i]])
                if hT_dram is None:
                    P.op('vector', lambda e, i=i, t=t: e.tensor_copy(out=hT[:, :, t * 128:(t + 1) * 128],
                                                                    in_=pT[i][:]), r=[pT[i]], w=[hT])
                else:
                    sg_ = stg[(t // 4) % 2]
                    tt = t % 4
                    P.op('vector', lambda e, i=i, tt=tt, sg_=sg_: e.tensor_copy(
                        out=sg_[:, :, tt * 128:(tt + 1) * 128], in_=pT[i][:]), r=[pT[i]], w=[sg_])
                    if tt == 3:
                        cc_ = t // 4
                        d_ap = hT_dram.t.ap().rearrange("(k p) s -> p k s", p=128)[:, :, cc_ * 512:(cc_ + 1) * 512]
                        P.op('sync', lambda e, sg_=sg_, d_ap=d_ap: e.dma_start(out=d_ap, in_=sg_[:]),
                             r=[sg_], w=[hT_dram], dsem=sg_)
            if h2b_all is not None:
                self.slots_and_scatter(s2, l, h2b_all)
            P.barrier()
            P.flush()

    def phase_proj(self, l, hT):
        P = self.P
        S = self.S
        inp = self.inp
        with ExitStack() as st:
            wbf = P.sbuf(st, 'p_w', [128, 8, DIN], BF16)
            wv = inp['w_in'].t.ap()[l].rearrange("(k p) n -> p k n", p=128)
            for k in range(8):
                P.op('gpsimd', lambda e, k=k: e.dma_start(out=wbf[:, k, :], in_=wv[:, k, :]), w=[wbf], dsem=wbf)
            gcol = {}
            for nm in ['sb_q_g', 'sb_k_g', 'fox_q_g', 'fox_k_g']:
                g = P.sbuf(st, 'p_' + nm, [128, 1], F32)
                for hh in range(2):
                    src = inp[nm].t.ap()[l].rearrange("(d o) -> d o", o=1)
                    P.op('sync', lambda e, g=g, hh=hh, src=src: e.dma_start(out=g[hh * 64:(hh + 1) * 64, :], in_=src),
                         w=[g], dsem=g)
                if nm.endswith('q_g'):
                    P.op('vector', lambda e, g=g: e.tensor_scalar(out=g[:], in0=g[:], scalar1=0.125, scalar2=None,
                                                                 op0=ALU.mult), r=[g], w=[g])
                gcol[nm] = g
            cw = P.sbuf(st, 'p_cw', [128, 2, 3], F32)
            for j in range(2):
                for i in range(3):
                    src = inp['conv_w'].t.ap()[l, i, j * 128:(j + 1) * 128].rearrange("(d o) -> d o", o=1)
                    P.op('sync', lambda e, j=j, i=i, src=src: e.dma_start(out=cw[:, j, i:i + 1], in_=src),
                         w=[cw], dsem=cw)
            og = P.sbuf(st, 'p_og', [128, 8], F32)
            src_og = inp['out_norm_g'].t.ap()[l].rearrange("(k p) -> p k", p=128)
            P.op('sync', lambda e: e.dma_start(out=og[:], in_=src_og, allow_slow_non_contiguous=True), w=[og], dsem=og)
            self.og_keep = None
            fb = P.sbuf(st, 'p_fb', [6, 1], F32)
            src_fb = inp['fox_f_b'].t.ap()[l].rearrange("(d o) -> d o", o=1)
            P.op('sync', lambda e: e.dma_start(out=fb[:], in_=src_fb), w=[fb], dsem=fb)
            P.op('vector', lambda e: e.tensor_scalar(out=fb[:], in0=fb[:], scalar1=-1.0, scalar2=None, op0=ALU.mult),
                 r=[fb], w=[fb])

            blk = self.c['blk64']
            pq = [P.psum(st, 'p_pq%d' % i, [128, 512]) for i in range(2)]
            pm = [P.psum(st, 'p_pm%d' % i, [128, 512]) for i in range(2)]
            sq = [P.sbuf(st, 'p_sq%d' % i, [128, 512], F32) for i in range(2)]
            rs = [P.sbuf(st, 'p_rs%d' % i, [128, 512], F32) for i in range(2)]
            qo = [P.sbuf(st, 'p_qo%d' % i, [128, 512], BF16) for i in range(2)]
            it = 0

            def proj_mm(ps, c0, ncols, n):
                def mm(e):
                    ins = None
                    for k in range(8):
                        ins = e.matmul(ps[0:ncols, :], wbf[:, k, c0:c0 + ncols], hT[:, k, n * 512:(n + 1) * 512],
                                       start=(k == 0), stop=(k == 7))
                    return ins
                P.op('tensor', mm, r=[wbf, hT], w=[ps])

            qk_tiles = []
            for m in range(3):
                qk_tiles.append((768 + m * 128, 'sb_q_g', self.qT_sb, m * 128, None))
                qk_tiles.append((1152 + m * 128, 'sb_k_g', self.kT_sb, m * 128, None))
                qk_tiles.append((1920 + m * 128, 'fox_q_g', self.qTa, None, 2 * m))
                qk_tiles.append((2304 + m * 128, 'fox_k_g', self.kTa, None, 2 * m))
            for (c0, gname, dst, row0, fh) in qk_tiles:
                for n in range(self.NC):
                    i = it % 2
                    it += 1
                    proj_mm(pq[i], c0, 128, n)
                    P.op('scalar', lambda e, i=i: e.activation(out=sq[i][:], in_=pq[i][:], func=AF.Square),
                         r=[pq[i]], w=[sq[i]])
                    P.op('tensor', lambda e, i=i: e.matmul(pm[i][:], blk[:], sq[i][:], start=True, stop=True),
                         r=[blk, sq[i]], w=[pm[i]])
                    P.op('scalar', lambda e, i=i: e.activation(out=rs[i][:], in_=pm[i][:], func=AF.Ln, bias=EPS),
                         r=[pm[i]], w=[rs[i]])
                    P.op('scalar', lambda e, i=i: e.activation(out=rs[i][:], in_=rs[i][:], func=AF.Exp, scale=-0.5),
                         r=[rs[i]], w=[rs[i]])
                    g = gcol[gname]
                    P.op('vector', lambda e, i=i, g=g: e.scalar_tensor_tensor(
                        out=qo[i][:], in0=pq[i][:], scalar=g[:, 0:1], in1=rs[i][:], op0=ALU.mult, op1=ALU.mult),
                        r=[pq[i], g, rs[i]], w=[qo[i]])
                    if row0 is not None:
                        d_ap = dst.t.ap()[row0:row0 + 128, n * 512:(n + 1) * 512]
                        P.op('sync', lambda e, i=i, d_ap=d_ap: e.dma_start(out=d_ap, in_=qo[i][:]),
                             r=[qo[i]], w=[dst], dsem=qo[i])
                    else:
                        for hh in range(2):
                            d_ap = dst.t.ap()[fh + hh, 0:64, n * 512:(n + 1) * 512]
                            P.op('sync', lambda e, i=i, hh=hh, d_ap=d_ap: e.dma_start(
                                out=d_ap, in_=qo[i][hh * 64:(hh + 1) * 64, :]), r=[qo[i]], w=[dst], dsem=qo[i])

            pc = [P.psum(st, 'p_pc%d' % i, [128, 512]) for i in range(3)]
            vb = P.sbuf(st, 'p_vb', [128, 514], F32)
            ccs = P.sbuf(st, 'p_ccs', [128, 512], F32)
            yc = P.sbuf(st, 'p_yc', [128, 512], F32)
            for j in range(2):
                P.op('vector', lambda e: e.memset(vb[:, 0:2], 0.0), w=[vb])
                for n in range(self.NC):
                    i = it % 2
                    it += 1
                    for q3 in range(3):
                        proj_mm(pc[q3], q3 * 256 + j * 128, 128, n)
                    if n > 0:
                        P.op('vector', lambda e: e.tensor_copy(out=vb[:, 0:2], in_=vb[:, 512:514]), r=[vb], w=[vb])
                    P.op('scalar', lambda e: e.copy(out=ccs[:], in_=pc[1][:]), r=[pc[1]], w=[ccs])
                    P.op('vector', lambda e: e.tensor_tensor(out=vb[:, 2:514], in0=pc[2][:], in1=ccs[:], op=ALU.mult),
                         r=[pc[2], ccs], w=[vb])
                    P.op('vector', lambda e, j=j: e.tensor_scalar(out=yc[:], in0=vb[:, 0:512], scalar1=cw[:, j, 0:1],
                                                                 scalar2=None, op0=ALU.mult), r=[vb, cw], w=[yc])
                    P.op('vector', lambda e, j=j: e.scalar_tensor_tensor(
                        out=yc[:], in0=vb[:, 1:513], scalar=cw[:, j, 1:2], in1=yc[:], op0=ALU.mult, op1=ALU.add),
                        r=[vb, cw, yc], w=[yc])
                    P.op('vector', lambda e, j=j: e.scalar_tensor_tensor(
                        out=yc[:], in0=vb[:, 2:514], scalar=cw[:, j, 2:3], in1=yc[:], op0=ALU.mult, op1=ALU.add),
                        r=[vb, cw, yc], w=[yc])
                    P.op('vector', lambda e: e.tensor_tensor(out=yc[:], in0=pc[0][:], in1=yc[:], op=ALU.mult),
                         r=[pc[0], yc], w=[yc])
                    P.op('scalar', lambda e, i=i: e.activation(out=sq[i][:], in_=yc[:], func=AF.Square),
                         r=[yc], w=[sq[i]])
                    P.op('tensor', lambda e, i=i: e.matmul(pm[i][:], blk[:], sq[i][:], start=True, stop=True),
                         r=[blk, sq[i]], w=[pm[i]])
                    P.op('scalar', lambda e, i=i: e.activation(out=rs[i][:], in_=pm[i][:], func=AF.Ln, bias=EPS),
                         r=[pm[i]], w=[rs[i]])
                    P.op('scalar', lambda e, i=i: e.activation(out=rs[i][:], in_=rs[i][:], func=AF.Exp, scale=-0.5),
                         r=[rs[i]], w=[rs[i]])
                    P.op('vector', lambda e, i=i, j=j: e.scalar_tensor_tensor(
                        out=qo[i][:], in0=yc[:], scalar=og[:, j:j + 1], in1=rs[i][:], op0=ALU.mult, op1=ALU.mult),
                        r=[yc, og, rs[i]], w=[qo[i]])
                    d_ap = self.yT.t.ap()[j * 128:(j + 1) * 128, n * 512:(n + 1) * 512]
                    P.op('sync', lambda e, i=i, d_ap=d_ap: e.dma_start(out=d_ap, in_=qo[i][:]),
                         r=[qo[i]], w=[self.yT], dsem=qo[i])

            pf = pc[0]
            fe = P.sbuf(st, 'p_fe', [6, 512], F32)
            fc = P.sbuf(st, 'p_fc', [6, 512 + 1], F32)
            r1 = P.sbuf(st, 'p_r1', [6, 512], F32)
            pcs = [P.sbuf(st, 'p_pc%d' % i, [6, 512], BF16) for i in range(3)]
            npcs = [P.sbuf(st, 'p_npc%d' % i, [6, 512], BF16) for i in range(3)]
            ones = self.c['ones']
            onesb = self.c['onesb']
            P.op('vector', lambda e: e.memset(fc[:, 0:1], 0.0), w=[fc])
            for n in range(self.NC):
                proj_mm(pf, 3072, 6, n)
                P.op('scalar', lambda e: e.activation(out=fe[:], in_=pf[0:6, :], func=AF.Exp, bias=fb[:, 0:1],
                                                      scale=-1.0), r=[pf, fb], w=[fe])
                P.op('scalar', lambda e: e.activation(out=fe[:], in_=fe[:], func=AF.Ln, bias=1.0), r=[fe], w=[fe])
                if n > 0:
                    P.op('vector', lambda e: e.tensor_copy(out=fc[:, 0:1], in_=fc[:, 512:513]), r=[fc], w=[fc])
                P.op('vector', lambda e: e.tensor_tensor_scan(out=fc[:, 1:513], data0=ones[0:6, 0:512], data1=fe[:],
                                                              initial=fc[:, 0:1], op0=ALU.mult, op1=ALU.add),
                     r=[ones, fe, fc], w=[fc])
                P.op('vector', lambda e: e.tensor_copy(out=pcs[0][:], in_=fc[:, 1:513]), r=[fc], w=[pcs[0]])
                P.op('vector', lambda e: e.tensor_tensor(out=r1[:], in0=fc[:, 1:513], in1=pcs[0][:], op=ALU.subtract),
                     r=[fc, pcs[0]], w=[r1])
                P.op('vector', lambda e: e.tensor_copy(out=pcs[1][:], in_=r1[:]), r=[r1], w=[pcs[1]])
                P.op('vector', lambda e: e.tensor_tensor(out=r1[:], in0=r1[:], in1=pcs[1][:], op=ALU.subtract),
                     r=[r1, pcs[1]], w=[r1])
                P.op('vector', lambda e: e.tensor_copy(out=pcs[2][:], in_=r1[:]), r=[r1], w=[pcs[2]])
                for q3 in range(3):
                    P.op('vector', lambda e, q3=q3: e.tensor_scalar(out=npcs[q3][:], in0=pcs[q3][:], scalar1=-1.0,
                                                                   scalar2=None, op0=ALU.mult),
                         r=[pcs[q3]], w=[npcs[q3]])
                    sl = slice(n * 512, (n + 1) * 512)
                    dq = self.qTa.t.ap()[:, 64 + q3, sl]
                    dk = self.kTa.t.ap()[:, 67 + q3, sl]
                    P.op('sync', lambda e, q3=q3, dq=dq: e.dma_start(out=dq, in_=npcs[q3][:]),
                         r=[npcs[q3]], w=[self.qTa], dsem=npcs[q3])
                    P.op('sync', lambda e, q3=q3, dk=dk: e.dma_start(out=dk, in_=pcs[q3][:]),
                         r=[pcs[q3]], w=[self.kTa], dsem=pcs[q3])
                    dq1 = self.qTa.t.ap()[:, 67 + q3, sl]
                    dk1 = self.kTa.t.ap()[:, 64 + q3, sl]
                    P.op('sync', lambda e, dq1=dq1: e.dma_start(out=dq1, in_=onesb[0:6, 0:512]),
                         r=[onesb], w=[self.qTa], dsem=onesb)
                    P.op('sync', lambda e, dk1=dk1: e.dma_start(out=dk1, in_=onesb[0:6, 0:512]),
                         r=[onesb], w=[self.kTa], dsem=onesb)

            vo = [P.sbuf(st, 'p_vo%d' % i, [128, 768], BF16) for i in range(2)]
            for t in range(self.NT):
                i = t % 2

                def mmv(e, t=t, i=i):
                    ins = None
                    for (ps, c0) in ((pq[i], 1536), (pm[i], 2688)):
                        for k in range(8):
                            ins = e.matmul(ps[:, 0:384], hT[:, k, t * 128:(t + 1) * 128], wbf[:, k, c0:c0 + 384],
                                           start=(k == 0), stop=(k == 7))
                    return ins
                P.op('tensor', mmv, r=[wbf, hT], w=[pq[i], pm[i]])
                P.op('scalar', lambda e, i=i: e.copy(out=vo[i][:, 0:384], in_=pq[i][:, 0:384]), r=[pq[i]], w=[vo[i]])
                P.op('vector', lambda e, i=i: e.tensor_copy(out=vo[i][:, 384:768], in_=pm[i][:, 0:384]),
                     r=[pm[i]], w=[vo[i]])
                d1 = self.v_sb.t.ap()[t * 128:(t + 1) * 128, :]
                d2 = self.v_fox.t.ap()[t * 128:(t + 1) * 128, :]
                P.op('sync', lambda e, i=i, d1=d1: e.dma_start(out=d1, in_=vo[i][:, 0:384]), r=[vo[i]],
                     w=[self.v_sb], dsem=vo[i])
                P.op('sync', lambda e, i=i, d2=d2: e.dma_start(out=d2, in_=vo[i][:, 384:768]), r=[vo[i]],
                     w=[self.v_fox], dsem=vo[i])
            P.barrier()
            P.flush()


    def head_norm_store(self, st_bufs, po, nrow, lhsT_ap, gcol_ap, drow, c, eps_bias):
        P = self.P
        osb, sq, pn, rs, yo = st_bufs
        P.op('scalar', lambda e: e.copy(out=osb[0:64, :], in_=po[0:64, :]), r=[po], w=[osb])
        P.op('scalar', lambda e: e.activation(out=sq[0:nrow, :], in_=po[0:nrow, :], func=AF.Square), r=[po], w=[sq])
        P.op('tensor', lambda e: e.matmul(pn[0:64, :], lhsT_ap, sq[0:nrow, :], start=True, stop=True),
             r=[sq], w=[pn])
        P.op('scalar', lambda e: e.activation(out=rs[0:64, :], in_=pn[0:64, :], func=AF.Ln, bias=eps_bias),
             r=[pn], w=[rs])
        P.op('scalar', lambda e: e.activation(out=rs[0:64, :], in_=rs[0:64, :], func=AF.Exp, scale=-0.5),
             r=[rs], w=[rs])
        P.op('vector', lambda e: e.scalar_tensor_tensor(out=yo[0:64, :], in0=osb[0:64, :], scalar=gcol_ap,
                                                       in1=rs[0:64, :], op0=ALU.mult, op1=ALU.mult),
             r=[osb, rs], w=[yo])
        d_ap = self.yT.t.ap()[drow:drow + 64, c * 512:(c + 1) * 512]
        P.op('sync', lambda e: e.dma_start(out=d_ap, in_=yo[0:64, :]), r=[yo], w=[self.yT], dsem=yo)

    def phase_attn(self, l):
        P = self.P
        S = self.S
        NT = self.NT
        inp = self.inp
        with ExitStack() as st:
            ogs = P.sbuf(st, 't_og', [64, 12], F32)
            src_og = inp['out_norm_g'].t.ap()[l, 256:1024].rearrange("(h d) -> d h", d=64)
            P.op('sync', lambda e: e.dma_start(out=ogs[:], in_=src_og, allow_slow_non_contiguous=True),
                 w=[ogs], dsem=ogs)
            kinds = ('sb', 'fox')
            kT = {k: [P.sbuf(st, 't_kT%s%d' % (k, i), [70, S], BF16) for i in range(2)] for k in kinds}
            qT = {k: [P.sbuf(st, 't_qT%s%d' % (k, i), [70, S], BF16) for i in range(2)] for k in kinds}
            vv = {k: [P.sbuf(st, 't_v%s%d' % (k, i), [128, NT, 65], BF16) for i in range(2)] for k in kinds}
            for i in range(2):
                P.op('vector', lambda e, i=i: e.memset(vv['fox'][i][:], 1.0), w=[vv['fox'][i]])
            pzs = [P.psum(st, 't_pzs%d' % i, [128, 512]) for i in range(2)]
            pbs = [P.psum(st, 't_pbs%d' % i, [128, 512]) for i in range(2)]
            pzf = P.psum(st, 't_pzf', [128, 512])
            po = {k: P.psum(st, 't_po' + k, [128, 512]) for k in kinds}
            pn = P.psum(st, 't_pn', [128, 512])
            e32 = [P.sbuf(st, 't_e%d' % i, [128, 512], F32) for i in range(2)]
            e32f = P.sbuf(st, 't_ef', [128, 512], F32)
            ec = [P.sbuf(st, 't_ec%d' % i, [128, 512], BF16) for i in range(2)]
            sp = [P.sbuf(st, 't_sp%d' % i, [128, 512], BF16) for i in range(2)]
            wt = {k: [P.sbuf(st, 't_w%s%d' % (k, i), [128, 512], BF16) for i in range(2)] for k in kinds}
            lacc = P.sbuf(st, 't_lacc', [128, 512], F32)
            laccb = [P.sbuf(st, 't_laccb%d' % i, [128, 512], BF16) for i in range(2)]
            nbufs = {}
            for k in kinds:
                nbufs[k] = (P.sbuf(st, 't_osb' + k, [64, 512], F32), P.sbuf(st, 't_sq' + k, [65, 512], F32), pn,
                            P.sbuf(st, 't_rs' + k, [64, 512], F32), P.sbuf(st, 't_yo' + k, [64, 512], BF16))
            negtri = self.c['negtri']
            negones = self.c['negones']
            mask_s = self.c['mask_s']
            mask_n = self.c['mask_n']
            blk = self.c['blk64']
            wn65 = self.c['wn65']
            its = []
            for c in range(self.NC):
                nkb = 4 * c + 4
                for idx, kb in enumerate(reversed(range(nkb))):
                    its.append((c, kb, idx == 0, kb == 0))
            def head_loads(h):
                slot = h % 2
                for kind in kinds:
                    K_ = 64 if kind == 'sb' else 70
                    if kind == 'sb':
                        ksrc = self.kT_sb.t.ap()[h * 64:(h + 1) * 64, :]
                        qsrc = self.qT_sb.t.ap()[h * 64:(h + 1) * 64, :]
                        vsrc = self.v_sb.t.ap()[:, h * 64:(h + 1) * 64].rearrange("(t p) d -> p t d", p=128)
                        srcs = (self.kT_sb, self.qT_sb, self.v_sb)
                    else:
                        ksrc = self.kTa.t.ap()[h]
                        qsrc = self.qTa.t.ap()[h]
                        vsrc = self.v_fox.t.ap()[:, h * 64:(h + 1) * 64].rearrange("(t p) d -> p t d", p=128)
                        srcs = (self.kTa, self.qTa, self.v_fox)
                    kt_, qt_, v_ = kT[kind][slot], qT[kind][slot], vv[kind][slot]
                    P.op('sync', lambda e, kt_=kt_, ksrc=ksrc, K_=K_: e.dma_start(out=kt_[0:K_, :], in_=ksrc),
                         r=[srcs[0]], w=[kt_], dsem=kt_)
                    P.op('sync', lambda e, qt_=qt_, qsrc=qsrc, K_=K_: e.dma_start(out=qt_[0:K_, :], in_=qsrc),
                         r=[srcs[1]], w=[qt_], dsem=qt_)
                    P.op('sync', lambda e, v_=v_, vsrc=vsrc: e.dma_start(out=v_[:, :, 0:64], in_=vsrc),
                         r=[srcs[2]], w=[v_], dsem=v_)

            head_loads(0)
            for h in range(6):
                slot = h % 2
                if h + 1 < 6:
                    head_loads(h + 1)
                if self.sparse:
                    self.issue_casts(l, 64 if h == 5 else 11)

                def stageA(kind, n, slot=slot):
                    c, kb, first, last = its[n]
                    i = n % 2
                    j = kb - 4 * c
                    kt_, qt_ = kT[kind][slot], qT[kind][slot]
                    if kind == 'sb':
                        pz = pzs[i]
                        P.op('tensor', lambda e: e.matmul(pz[:], kt_[0:64, kb * 128:(kb + 1) * 128],
                                                          qt_[0:64, c * 512:(c + 1) * 512], start=True, stop=True),
                             r=[kt_, qt_], w=[pz])
                        P.op('scalar', lambda e: e.activation(out=e32[i][:], in_=pz[:], func=AF.Exp),
                             r=[pz], w=[e32[i]])
                        P.op('scalar', lambda e: e.activation(out=sp[i][:], in_=e32[i][:], func=AF.Ln, bias=1.0),
                             r=[e32[i]], w=[sp[i]])
                        if j >= 0:
                            P.op('gpsimd', lambda e: e.tensor_tensor(out=sp[i][:], in0=sp[i][:], in1=mask_s[:, j, :],
                                                                    op=ALU.mult), r=[sp[i], mask_s], w=[sp[i]])
                        if not last:
                            ln_ = laccb[(n + 1) % 2]
                            if first:
                                P.op('vector', lambda e: e.tensor_copy(out=lacc[:], in_=sp[i][:]), r=[sp[i]], w=[lacc])
                            else:
                                P.op('vector', lambda e: e.tensor_tensor(out=lacc[:], in0=lacc[:], in1=sp[i][:],
                                                                        op=ALU.add), r=[sp[i], lacc], w=[lacc])
                            P.op('vector', lambda e: e.tensor_copy(out=ln_[:], in_=lacc[:]), r=[lacc], w=[ln_])
                    else:
                        w_ = wt['fox'][i]
                        P.op('tensor', lambda e: e.matmul(pzf[:], kt_[0:70, kb * 128:(kb + 1) * 128],
                                                          qt_[0:70, c * 512:(c + 1) * 512], start=True, stop=True),
                             r=[kt_, qt_], w=[pzf])
                        if j >= 0:
                            P.op('vector', lambda e: e.tensor_scalar(out=e32f[:], in0=pzf[:], scalar1=60.0,
                                                                    scalar2=None, op0=ALU.min), r=[pzf], w=[e32f])
                            P.op('scalar', lambda e: e.activation(out=w_[:], in_=e32f[:], func=AF.Exp),
                                 r=[e32f], w=[w_])
                            P.op('gpsimd', lambda e: e.tensor_tensor(out=w_[:], in0=w_[:], in1=mask_n[:, j, :],
                                                                    op=ALU.mult), r=[w_, mask_n], w=[w_])
                        else:
                            P.op('scalar', lambda e: e.activation(out=w_[:], in_=pzf[:], func=AF.Exp),
                                 r=[pzf], w=[w_])

                def stageB1(n, slot=slot, h=h):
                    kind = 'sb'
                    c, kb, first, last = its[n]
                    i = n % 2
                    j = kb - 4 * c
                    w_ = wt[kind][i]
                    if True:
                        lb = laccb[n % 2]
                        pb = pbs[i]

                        def mm2(e):
                            ins = e.matmul(pb[:], negtri[:], sp[i][:], start=True, stop=first)
                            if not first:
                                ins = e.matmul(pb[:], negones[:], lb[:], start=False, stop=True)
                            return ins
                        P.op('tensor', mm2, r=[sp[i], negtri, negones] + ([] if first else [lb]), w=[pb])
                        P.op('scalar', lambda e: e.activation(out=ec[i][:], in_=pb[:], func=AF.Exp), r=[pb], w=[ec[i]])
                        if j >= 0:
                            P.op('gpsimd', lambda e: e.tensor_tensor(out=ec[i][:], in0=ec[i][:], in1=mask_s[:, j, :],
                                                                    op=ALU.mult), r=[ec[i], mask_s], w=[ec[i]])
                        P.op('vector', lambda e: e.tensor_tensor(out=w_[:], in0=e32[i][:], in1=ec[i][:], op=ALU.mult),
                             r=[e32[i], ec[i]], w=[w_])

                def stageB2(kind, n, slot=slot, h=h):
                    c, kb, first, last = its[n]
                    i = n % 2
                    v_ = vv[kind][slot]
                    pc_ = po[kind]
                    w_ = wt[kind][i]
                    nr = 64 if kind == 'sb' else 65
                    P.op('tensor', lambda e: e.matmul(pc_[0:nr, :], v_[:, kb, 0:nr], w_[:], start=first, stop=last),
                         r=[v_, w_], w=[pc_])
                    if last:
                        if kind == 'sb':
                            self.head_norm_store(nbufs[kind], pc_, 64, blk[0:64, 0:64], ogs[:, h:h + 1],
                                                 256 + 64 * h, c, EPS)
                        else:
                            self.head_norm_store(nbufs[kind], pc_, 65, wn65[:, :], ogs[:, 6 + h:7 + h],
                                                 640 + 64 * h, c, 0.0)

                stageA('sb', 0)
                stageA('fox', 0)
                for n in range(len(its)):
                    stageB1(n)
                    if n + 1 < len(its):
                        stageA('sb', n + 1)
                        stageA('fox', n + 1)
                    stageB2('fox', n)
                    stageB2('sb', n)
            P.barrier()
            P.flush()

    def phase_wout(self, l, xsrc, xdst):
        P = self.P
        inp = self.inp
        with ExitStack() as st:
            wo = P.sbuf(st, 'o_w', [128, 8, D], BF16)
            wv = inp['w_out'].t.ap()[l].rearrange("(k p) n -> p k n", p=128)
            for k in range(8):
                P.op('gpsimd', lambda e, k=k: e.dma_start(out=wo[:, k, :], in_=wv[:, k, :]), w=[wo], dsem=wo)
            g1 = self.load_bcast(st, 'o_g1', self.modv, l * 6 * D + 2 * D)
            yt = [P.sbuf(st, 'o_y%d' % i, [128, 8, 512], BF16) for i in range(2)]
            xt = [P.sbuf(st, 'o_x%d' % i, [128, D], F32) for i in range(2)]
            xo = [P.sbuf(st, 'o_xo%d' % i, [128, D], F32) for i in range(2)]
            py = [P.psum(st, 'o_p%d' % i, [128, 512]) for i in range(4)]
            yv = self.yT.t.ap().rearrange("(k p) s -> p k s", p=128)
            for c in range(self.NC):
                yb = yt[c % 2]
                P.op('sync', lambda e, yb=yb, c=c: e.dma_start(out=yb[:], in_=yv[:, :, c * 512:(c + 1) * 512]),
                     r=[self.yT], w=[yb], dsem=yb)
                for tt in range(4):
                    t = c * 4 + tt
                    i = t % 2
                    src = xsrc.t.ap()[t * 128:(t + 1) * 128, :]
                    P.op('sync', lambda e, i=i, src=src: e.dma_start(out=xt[i][:], in_=src), r=[xsrc], w=[xt[i]],
                         dsem=xt[i])
                    for half in range(2):
                        pp = py[i * 2 + half]

                        def mm(e, yb=yb, tt=tt, half=half, pp=pp):
                            ins = None
                            for k in range(8):
                                ins = e.matmul(pp[:], yb[:, k, tt * 128:(tt + 1) * 128],
                                               wo[:, k, half * 512:(half + 1) * 512], start=(k == 0), stop=(k == 7))
                            return ins
                        P.op('tensor', mm, r=[yb, wo], w=[pp])
                        sl = slice(half * 512, (half + 1) * 512)
                        P.op('vector', lambda e, i=i, pp=pp, sl=sl: e.tensor_tensor(
                            out=xo[i][:, sl], in0=pp[:], in1=g1[:, sl], op=ALU.mult), r=[pp, g1], w=[xo[i]])
                    P.op('gpsimd', lambda e, i=i: e.tensor_tensor(out=xo[i][:], in0=xo[i][:], in1=xt[i][:], op=ALU.add),
                         r=[xo[i], xt[i]], w=[xo[i]])
                    dst = xdst.t.ap()[t * 128:(t + 1) * 128, :]
                    P.op('sync', lambda e, i=i, dst=dst: e.dma_start(out=dst, in_=xo[i][:]), r=[xo[i]], w=[xdst],
                         dsem=xo[i])
            P.barrier()
            P.flush()

    def load_expert(self, l, e_, w1b, w2b, b1c, b1s):
        P = self.P
        inp = self.inp
        w1v = inp['w_mlp1'].t.ap()[l, e_].rearrange("(k p) n -> p k n", p=128)
        w2v = inp['w_mlp2'].t.ap()[l, e_].rearrange("(p i) n -> p i n", i=8)
        for k in range(8):
            P.op('gpsimd', lambda e, k=k: e.dma_start(out=w1b[:, k, :], in_=w1v[:, k, :]), w=[w1b], dsem=w1b)
        for k in range(0, 8, 2):
            P.op('gpsimd', lambda e, k=k: e.dma_start(out=w2b[:, k:k + 2, :], in_=w2v[:, k:k + 2, :]),
                 w=[w2b], dsem=w2b)
        b1v = inp['b_mlp1'].t.ap()[l, e_].rearrange("(p m) -> p m", m=16)
        P.op('sync', lambda e: e.dma_start(out=b1c[:], in_=b1v), w=[b1c], dsem=b1c)

    def ffn_block(self, B, xT, w1b, w2b, b1c, b1s, evac, mid_hook=None):
        P = self.P
        aT = B['aT']
        CSIG = float(1.0 / (1.0 + np.exp(-1.702 * 7.0)))
        for i in range(8):
            q = B['it'] % 2
            B['it'] += 1
            pg, pl = B['pg'][q], B['pl'][q]
            sg, g, ll, tt = B['sg'][q], B['g'][q], B['l'][q], B['t'][q]

            def mm1(e, i=i, pg=pg, pl=pl):
                ins = None
                for (pp, j) in ((pg, 0), (pl, 1)):
                    for k in range(8):
                        ins = e.matmul(pp[:], w1b[:, k, 2 * i + j:2 * D:16], xT[:, k, :],
                                       start=(k == 0), stop=(k == 7))
                return ins
            P.op('tensor', mm1, r=[w1b, xT], w=[pg, pl])
            P.op('vector', lambda e, i=i, g=g, pg=pg: e.tensor_scalar(out=g[:], in0=pg[:], scalar1=b1c[:, 2 * i:2 * i + 1],
                                                                     scalar2=7.0, op0=ALU.add, op1=ALU.min),
                 r=[pg, b1c], w=[g])
            P.op('scalar', lambda e, sg=sg, g=g: e.activation(out=sg[:], in_=g[:], func=AF.Sigmoid, scale=1.702),
                 r=[g], w=[sg])
            P.op('vector', lambda e, i=i, ll=ll, pl=pl: e.tensor_scalar(out=ll[:], in0=pl[:], scalar1=b1c[:, 2 * i + 1:2 * i + 2],
                                                                       scalar2=7.0, op0=ALU.add, op1=ALU.min),
                 r=[pl, b1c], w=[ll])
            P.op('vector', lambda e, ll=ll: e.tensor_scalar(out=ll[:], in0=ll[:], scalar1=-7.0, scalar2=1.0,
                                                           op0=ALU.max, op1=ALU.add), r=[ll], w=[ll])
            P.op('gpsimd', lambda e, sg=sg, g=g, tt=tt: e.tensor_tensor(out=tt[:], in0=sg[:], in1=g[:], op=ALU.mult),
                 r=[sg, g], w=[tt])
            P.op('vector', lambda e, i=i, tt=tt, ll=ll: e.tensor_tensor(out=aT[:, i, :], in0=tt[:], in1=ll[:],
                                                                       op=ALU.mult), r=[tt, ll], w=[aT])
            if i == 4 and mid_hook is not None:
                mid_hook()
        for r_ in range(4):
            for half in range(2):
                pp = B['py'][B['ity'] % len(B['py'])]
                B['ity'] += 1

                def mm2(e, r_=r_, half=half, pp=pp):
                    ins = None
                    for k in range(8):
                        ins = e.matmul(pp[:], aT[:, k, r_ * 128:(r_ + 1) * 128],
                                       w2b[:, k, half * 512:(half + 1) * 512], start=(k == 0), stop=(k == 7))
                    return ins
                P.op('tensor', mm2, r=[aT, w2b], w=[pp])
                evac(r_, half, pp)

    def ffn_bufs(self, st, npy=4):
        P = self.P
        B = {'it': 0, 'ity': 0}
        B['aT'] = P.sbuf(st, 'f_aT', [128, 8, 512], BF16)
        B['pg'] = [P.psum(st, 'f_pg%d' % i, [128, 512]) for i in range(2)]
        B['pl'] = [P.psum(st, 'f_pl%d' % i, [128, 512]) for i in range(2)]
        B['py'] = [P.psum(st, 'f_py%d' % i, [128, 512]) for i in range(npy)]
        for nm in ('sg', 'g', 'l', 't'):
            B[nm] = [P.sbuf(st, 'f_%s%d' % (nm, i), [128, 512], F32) for i in range(2)]
        return B

    def phase_moe_dense(self, l, xsrc, xdst):
        P = self.P
        inp = self.inp
        NT = self.NT
        with ExitStack() as st:
            B = self.ffn_bufs(st)
            w1b = [P.sbuf(st, 'd_w1%d' % i, [128, 8, 2 * D], BF16) for i in range(2)]
            w2b = [P.sbuf(st, 'd_w2%d' % i, [128, 8, D], BF16) for i in range(2)]
            b1c = [P.sbuf(st, 'd_b1c%d' % i, [128, 16], F32) for i in range(2)]
            b1s = [P.sbuf(st, 'd_b1s%d' % i, [128, 8], F32) for i in range(2)]
            xT = [P.sbuf(st, 'd_xT%d' % i, [128, 8, 512], BF16) for i in range(2)]
            stage = [P.sbuf(st, 'd_st%d' % i, [128, D], F32) for i in range(4)]
            hv = self.h2T_d.t.ap().rearrange("(k p) s -> p k s", p=128)
            accd = self.accd
            with ExitStack() as s2:
                b2s = P.sbuf(s2, 'd_b2', [NE, D], F32)
                P.op('sync', lambda e: e.dma_start(out=b2s[:], in_=inp['b_mlp2'].t.ap()[l]), w=[b2s], dsem=b2s)
                cT = P.sbuf(s2, 'd_cT', [NE, 128], F32)
                identf = self.c['ident_f']
                pt = B['pg'][0]
                for t in range(NT):
                    P.op('tensor', lambda e, t=t: e.transpose(pt[0:NE, 0:128], self.comb[:, t, :], identf[:]),
                         r=[self.comb, identf], w=[pt])
                    P.op('scalar', lambda e: e.copy(out=cT[:], in_=pt[0:NE, 0:128]), r=[pt], w=[cT])
                    sg_ = stage[t % 4]
                    for half in range(2):
                        pp = B['py'][half]
                        P.op('tensor', lambda e, pp=pp, half=half: e.matmul(
                            pp[:], cT[:], b2s[:, half * 512:(half + 1) * 512], start=True, stop=True),
                            r=[cT, b2s], w=[pp])
                        P.op('vector', lambda e, pp=pp, half=half, sg_=sg_: e.tensor_copy(
                            out=sg_[:, half * 512:(half + 1) * 512], in_=pp[:]), r=[pp], w=[sg_])
                    dst = accd.t.ap()[t * 128:(t + 1) * 128, :]
                    P.op('gpsimd', lambda e, sg_=sg_, dst=dst: e.dma_start(out=dst, in_=sg_[:]), r=[sg_], w=[accd],
                         dsem=sg_)
            blk = 0
            for e_ in range(NE):
                ws = e_ % 2
                self.load_expert(l, e_, w1b[ws], w2b[ws], b1c[ws], b1s[ws])
                for c in range(self.NC):
                    xb = xT[blk % 2]
                    blk += 1
                    P.op('sync', lambda e, xb=xb, c=c: e.dma_start(out=xb[:], in_=hv[:, :, c * 512:(c + 1) * 512]),
                         r=[self.h2T_d], w=[xb], dsem=xb)

                    def evac(r_, half, pp, c=c, e_=e_):
                        t = c * 4 + r_
                        sg_ = stage[r_]
                        sl = slice(half * 512, (half + 1) * 512)
                        P.op('vector', lambda e: e.tensor_scalar(out=sg_[:, sl], in0=pp[:],
                                                                 scalar1=self.comb[:, t, e_:e_ + 1], scalar2=None,
                                                                 op0=ALU.mult), r=[pp, self.comb], w=[sg_])
                        if half == 1:
                            dst = accd.t.ap()[t * 128:(t + 1) * 128, :]
                            P.op('gpsimd', lambda e: e.dma_start(out=dst, in_=sg_[:], accum_op=ALU.add),
                                 r=[sg_], w=[accd], dsem=sg_)
                    self.ffn_block(B, xb, w1b[ws], w2b[ws], b1c[ws], b1s[ws], evac)
            P.barrier()
            P.flush()
        self.phase_final(l, xsrc, xdst)

    def phase_final(self, l, xsrc, xdst):
        P = self.P
        with ExitStack() as st:
            g2 = self.load_bcast(st, 'z_g2', self.modv, l * 6 * D + 5 * D)
            xt = [P.sbuf(st, 'z_x%d' % i, [128, D], F32) for i in range(2)]
            at = [P.sbuf(st, 'z_a%d' % i, [128, D], F32) for i in range(2)]
            for t in range(self.NT):
                i = t % 2
                rows = slice(t * 128, (t + 1) * 128)
                P.op('sync', lambda e, i=i, rows=rows: e.dma_start(out=xt[i][:], in_=xsrc.t.ap()[rows, :]),
                     r=[xsrc], w=[xt[i]], dsem=xt[i])
                P.op('sync', lambda e, i=i, rows=rows: e.dma_start(out=at[i][:], in_=self.accd.t.ap()[rows, :]),
                     r=[self.accd], w=[at[i]], dsem=at[i])
                P.op('vector', lambda e, i=i: e.tensor_tensor(out=at[i][:], in0=at[i][:], in1=g2[:], op=ALU.mult),
                     r=[at[i], g2], w=[at[i]])
                P.op('gpsimd', lambda e, i=i: e.tensor_tensor(out=at[i][:], in0=at[i][:], in1=xt[i][:], op=ALU.add),
                     r=[at[i], xt[i]], w=[at[i]])
                P.op('sync', lambda e, i=i, rows=rows: e.dma_start(out=xdst.t.ap()[rows, :], in_=at[i][:]),
                     r=[at[i]], w=[xdst], dsem=at[i])
            P.barrier()
            P.flush()


    def slots_and_scatter(self, st, l, h2b_all):
        P = self.P
        NT = self.NT
        NBLK = self.NBLK
        memb, comb = self.memb, self.comb
        onesf = self.c['ones']
        stri = self.c['stri_f']
        blkstart = self.c['blkstart']
        pcn = P.psum(st, 'q_pcn', [128, 512])
        cnt = P.sbuf(st, 'q_cnt', [128, NE], F32)
        nb = P.sbuf(st, 'q_nb', [128, NE], F32)
        incl = P.sbuf(st, 'q_incl', [128, NE], F32)
        off = P.sbuf(st, 'q_off', [128, NE], F32)
        texp = P.sbuf(st, 'q_texp', [128, NBLK], F32)

        def mmc(e):
            ins = None
            for t in range(NT):
                ins = e.matmul(pcn[:, 0:NE], onesf[:, 0:128], memb[:, t, :], start=(t == 0), stop=(t == NT - 1))
            return ins
        P.op('tensor', mmc, r=[onesf, memb], w=[pcn])
        P.op('vector', lambda e: e.tensor_copy(out=cnt[:], in_=pcn[:, 0:NE]), r=[pcn], w=[cnt])
        P.op('vector', lambda e: e.tensor_scalar(out=nb[:], in0=cnt[:], scalar1=0.0, scalar2=None, op0=ALU.is_gt),
             r=[cnt], w=[nb])
        for k in range(1, self.S // TS):
            P.op('vector', lambda e, k=k: e.scalar_tensor_tensor(out=nb[:], in0=cnt[:], scalar=float(k * TS),
                                                                in1=nb[:], op0=ALU.is_gt, op1=ALU.add),
                 r=[cnt, nb], w=[nb])
        P.op('vector', lambda e: e.tensor_scalar(out=nb[:], in0=nb[:], scalar1=float(TS), scalar2=None, op0=ALU.mult),
             r=[nb], w=[nb])
        P.op('vector', lambda e: e.tensor_tensor_scan(out=incl[:], data0=onesf[:, 0:NE], data1=nb[:], initial=0.0,
                                                      op0=ALU.mult, op1=ALU.add), r=[onesf, nb], w=[incl])
        P.op('vector', lambda e: e.tensor_tensor(out=off[:], in0=incl[:], in1=nb[:], op=ALU.subtract),
             r=[incl, nb], w=[off])
        P.op('vector', lambda e: e.tensor_scalar(out=texp[:], in0=blkstart[:, 0:NBLK], scalar1=incl[:, 0:1],
                                                 scalar2=None, op0=ALU.is_ge), r=[blkstart, incl], w=[texp])
        for e_ in range(1, NE):
            P.op('vector', lambda e, e_=e_: e.scalar_tensor_tensor(out=texp[:], in0=blkstart[:, 0:NBLK],
                                                                  scalar=incl[:, e_:e_ + 1], in1=texp[:],
                                                                  op0=ALU.is_ge, op1=ALU.add),
                 r=[blkstart, incl, texp], w=[texp])
        gar = P.sbuf(st, 'q_gar', [128, NBLK], F32)
        P.op('vector', lambda e: e.tensor_scalar(out=gar[:], in0=texp[:], scalar1=float(NE) - 0.5,
                                                 scalar2=self.c['pidx'][:, 2:3], op0=ALU.is_gt, op1=ALU.mult),
             r=[texp, self.c['pidx']], w=[gar])
        P.op('vector', lambda e: e.tensor_scalar(out=texp[:], in0=texp[:], scalar1=float(NE - 1), scalar2=None,
                                                 op0=ALU.min), r=[texp], w=[texp])
        pidx = self.c['pidx']
        t1 = P.sbuf(st, 'q_t1', [128, NBLK], F32)
        ixf = P.sbuf(st, 'q_ixf', [128, NBLK, 5], F32)
        BIG = float(1 << 15)
        P.op('vector', lambda e: e.tensor_scalar(out=t1[:], in0=texp[:], scalar1=128.0, scalar2=None,
                                                 op0=ALU.mult), r=[texp], w=[t1])
        P.op('vector', lambda e: e.scalar_tensor_tensor(out=t1[:], in0=gar[:], scalar=BIG, in1=t1[:], op0=ALU.mult,
                                                       op1=ALU.add), r=[gar, t1], w=[t1])
        P.op('vector', lambda e: e.tensor_scalar(out=ixf[:, :, 4], in0=t1[:], scalar1=pidx[:, 0:1], scalar2=None,
                                                 op0=ALU.add), r=[t1, pidx], w=[ixf])
        P.op('vector', lambda e: e.tensor_scalar(out=ixf[:, :, 1], in0=ixf[:, :, 4], scalar1=2.0, scalar2=None,
                                                 op0=ALU.mult), r=[ixf], w=[ixf])
        P.op('vector', lambda e: e.tensor_scalar(out=ixf[:, :, 2], in0=ixf[:, :, 4], scalar1=2.0, scalar2=1.0,
                                                 op0=ALU.mult, op1=ALU.add), r=[ixf], w=[ixf])
        P.op('vector', lambda e: e.tensor_scalar(out=ixf[:, :, 0], in0=ixf[:, :, 4], scalar1=float(l * NE * 128),
                                                 scalar2=None, op0=ALU.add), r=[ixf], w=[ixf])
        P.op('vector', lambda e: e.tensor_scalar(out=t1[:], in0=texp[:], scalar1=float(l * NE), scalar2=None,
                                                 op0=ALU.add), r=[texp], w=[t1])
        P.op('vector', lambda e: e.scalar_tensor_tensor(out=ixf[:, :, 3], in0=gar[:], scalar=BIG, in1=t1[:],
                                                       op0=ALU.mult, op1=ALU.add), r=[gar, t1], w=[ixf])
        widx = self.widx
        P.op('vector', lambda e: e.tensor_copy(out=widx[:], in_=ixf[:]), r=[ixf], w=[widx])
        macc = P.sbuf(st, 'q_macc', [128, NE], F32)
        pp = [P.psum(st, 'q_pp%d' % i, [128, 512]) for i in range(2)]
        tmp = P.sbuf(st, 'q_tmp', [128, NE], F32)
        val = P.sbuf(st, 'q_val', [128, NE], F32)
        oh = P.sbuf(st, 'q_oh', [128, NE], F32)
        t8 = P.sbuf(st, 'q_t8', [128, 8], F32)
        sl4, g4 = self.sl4, self.g4
        for t in range(NT):
            ppt = pp[t % 2]

            def mmp(e, t=t, ppt=ppt):
                ins = e.matmul(ppt[:, 0:NE], stri[:], memb[:, t, :], start=True, stop=(t == 0))
                if t > 0:
                    ins = e.matmul(ppt[:, 0:NE], onesf[:, 0:128], macc[:], start=False, stop=True)
                return ins
            P.op('tensor', mmp, r=[stri, memb, onesf] + ([macc] if t > 0 else []), w=[ppt])
            P.op('vector', lambda e, ppt=ppt: e.tensor_tensor(out=tmp[:], in0=ppt[:, 0:NE], in1=off[:], op=ALU.add),
                 r=[ppt, off], w=[tmp])
            P.op('vector', lambda e, t=t: e.scalar_tensor_tensor(out=val[:], in0=tmp[:], scalar=1.0, in1=memb[:, t, :],
                                                                op0=ALU.add, op1=ALU.mult), r=[tmp, memb], w=[val])
            P.op('vector', lambda e: e.max(out=t8[:], in_=val[:]), r=[val], w=[t8])
            P.op('vector', lambda e, t=t: e.tensor_scalar(out=sl4[:, t, :], in0=t8[:, 0:4], scalar1=-1.0, scalar2=None,
                                                         op0=ALU.add), r=[t8], w=[sl4])
            for j in range(4):
                P.op('vector', lambda e, j=j: e.tensor_scalar(out=oh[:], in0=val[:], scalar1=t8[:, j:j + 1],
                                                             scalar2=None, op0=ALU.is_equal), r=[val, t8], w=[oh])
                P.op('vector', lambda e, t=t: e.tensor_tensor(out=oh[:], in0=oh[:], in1=comb[:, t, :], op=ALU.mult),
                     r=[oh, comb], w=[oh])
                P.op('vector', lambda e, t=t, j=j: e.reduce_sum(out=g4[:, t, j:j + 1], in_=oh[:], axis=AX.X),
                     r=[oh], w=[g4])
            if t == 0:
                P.op('vector', lambda e: e.tensor_copy(out=macc[:], in_=memb[:, 0, :]), r=[memb], w=[macc])
            else:
                P.op('vector', lambda e, t=t: e.tensor_tensor(out=macc[:], in0=macc[:], in1=memb[:, t, :], op=ALU.add),
                     r=[macc, memb], w=[macc])
            Xs = self.Xs
            for j in range(4):
                P.op('gpsimd', lambda e, t=t, j=j: e.indirect_dma_start(
                    out=Xs.t.ap(), out_offset=bass.IndirectOffsetOnAxis(ap=sl4[:, t, j:j + 1], axis=0),
                    in_=h2b_all[:, t, :], in_offset=None), r=[sl4, h2b_all], w=[Xs], dsem=h2b_all)

    def phase_moe_sparse(self, l, xsrc, xdst):
        P = self.P
        inp = self.inp
        NT = self.NT
        widx = self.widx
        wc1, wc2 = self.wc1[l], self.wc2[l]
        w1rows = wc1.t.ap().rearrange("(e p h k) n -> (e p h) (k n)", p=128, h=2, k=4)
        w2rows = wc2.t.ap().rearrange("(e p i) n -> (e p) (i n)", p=128, i=8)
        b1rows = inp['b_mlp1'].t.ap().rearrange("l e (p m) -> (l e p) m", m=16)
        b2rows = inp['b_mlp2'].t.ap().rearrange("l e n -> (l e) n")
        with ExitStack() as st:
            B = self.ffn_bufs(st, npy=3)
            w1b = [P.sbuf(st, 'd_w1%d' % i, [128, 8, 2 * D], BF16) for i in range(2)]
            w2b = [P.sbuf(st, 'd_w2%d' % i, [128, 8, D], BF16) for i in range(2)]
            b1c = [P.sbuf(st, 'd_b1c%d' % i, [128, 16], F32) for i in range(2)]
            b2bc = [P.sbuf(st, 'd_b2%d' % i, [128, D], F32) for i in range(2)]
            xs = [P.sbuf(st, 'd_xs%d' % i, [128, D], BF16) for i in range(2)]
            xT = [P.sbuf(st, 'd_xT%d' % i, [128, 8, 512], BF16) for i in range(2)]
            stage = [P.sbuf(st, 'd_st%d' % i, [128, D], F32) for i in range(4)]
            ident = self.c['ident_bf']
            pTbuf = P.psum(st, 'pTb', [128, 8, 128], BF16)
            Xs, Ys = self.Xs, self.Ys
            rg = [self.nc.gpsimd.alloc_register('bc%d_%d' % (q, l)) for q in range(4)]
            rtile = P.sbuf(st, 'd_rt', [128, 1], F32)

            def setregs(e):
                e.reg_mov(rg[0], NE * 128 * 2 - 1)
                e.reg_mov(rg[1], NL * NE * 128 - 1)
                e.reg_mov(rg[2], NL * NE - 1)
                e.reg_mov(rg[3], NE * 128 - 1)
                return e.memset(rtile[:], 0.0)
            P.op('gpsimd', setregs, w=[rtile])
            nxs = [0]

            def gathers(i):
                ws = i % 2
                wa, wb_, bc, b2 = w1b[ws], w2b[ws], b1c[ws], b2bc[ws]
                for h in range(2):
                    P.op('gpsimd', lambda e, h=h: e.indirect_dma_start(
                        out=wa[:, 4 * h:4 * h + 4, :].rearrange("p k n -> p (k n)"), out_offset=None, in_=w1rows,
                        in_offset=bass.IndirectOffsetOnAxis(ap=widx[:, i, 1 + h:2 + h], axis=0),
                        bounds_check=rg[0], oob_is_err=False), r=[widx, wc1], w=[wa], dsem=wa)
                P.op('gpsimd', lambda e: e.indirect_dma_start(
                    out=wb_[:].rearrange("p k n -> p (k n)"), out_offset=None, in_=w2rows,
                    in_offset=bass.IndirectOffsetOnAxis(ap=widx[:, i, 4:5], axis=0),
                    bounds_check=rg[3], oob_is_err=False), r=[widx, wc2], w=[wb_], dsem=wb_)
                P.op('gpsimd', lambda e: e.indirect_dma_start(
                    out=bc[:], out_offset=None, in_=b1rows,
                    in_offset=bass.IndirectOffsetOnAxis(ap=widx[:, i, 0:1], axis=0),
                    bounds_check=rg[1], oob_is_err=False), r=[widx], w=[bc], dsem=bc)
                P.op('gpsimd', lambda e: e.indirect_dma_start(
                    out=b2[:], out_offset=None, in_=b2rows,
                    in_offset=bass.IndirectOffsetOnAxis(ap=widx[:, i, 3:4], axis=0),
                    bounds_check=rg[2], oob_is_err=False), r=[widx], w=[b2], dsem=b2)

            def prep_x(i):
                xb = xT[i % 2]
                for r_ in range(4):
                    xq = xs[nxs[0] % 2]
                    nxs[0] += 1
                    rows = slice(i * TS + r_ * 128, i * TS + (r_ + 1) * 128)
                    P.op('sync', lambda e, xq=xq, rows=rows: e.dma_start(out=xq[:], in_=Xs.t.ap()[rows, :]),
                         r=[Xs], w=[xq], dsem=xq)

                    def tr(e, xq=xq):
                        ins = None
                        for k in range(8):
                            ins = e.transpose(pTbuf[:, k, :], xq[:, k:D:8], ident[:])
                        return ins
                    P.op('tensor', tr, r=[xq, ident], w=[pTbuf])
                    P.op('scalar', lambda e, xb=xb, r_=r_: e.copy(out=xb[:, :, r_ * 128:(r_ + 1) * 128],
                                                                  in_=pTbuf[:]), r=[pTbuf], w=[xb])

            gathers(0)
            prep_x(0)
            for i in range(self.NBLK):
                ws = i % 2
                wa, wb_, bc, b2 = w1b[ws], w2b[ws], b1c[ws], b2bc[ws]
                xb = xT[i % 2]
                if i + 1 < self.NBLK:
                    gathers(i + 1)

                def evac(r_, half, pp, i=i, b2=b2):
                    sg_ = stage[r_]
                    sl = slice(half * 512, (half + 1) * 512)
                    P.op('vector', lambda e: e.tensor_tensor(out=sg_[:, sl], in0=pp[:], in1=b2[:, sl], op=ALU.add),
                         r=[pp, b2], w=[sg_])
                    if half == 1:
                        rows = slice(i * TS + r_ * 128, i * TS + (r_ + 1) * 128)
                        P.op('sync', lambda e: e.dma_start(out=Ys.t.ap()[rows, :], in_=sg_[:]),
                             r=[sg_], w=[Ys], dsem=sg_)
                hook = (lambda i=i: prep_x(i + 1)) if i + 1 < self.NBLK else None
                self.ffn_block(B, xb, wa, wb_, bc, None, evac, mid_hook=hook)
            P.barrier()
            P.flush()
        self.phase_combine(l, xsrc, xdst)

    def phase_combine(self, l, xsrc, xdst):
        P = self.P
        Ys = self.Ys
        sl4, g4 = self.sl4, self.g4
        with ExitStack() as st:
            g2 = self.load_bcast(st, 'z_g2', self.modv, l * 6 * D + 5 * D)
            xt = [P.sbuf(st, 'z_x%d' % i, [128, D], F32) for i in range(2)]
            yg = [[P.sbuf(st, 'z_y%d_%d' % (i, j), [128, D], F32) for j in range(4)] for i in range(2)]
            acc = [P.sbuf(st, 'z_a%d' % i, [128, D], F32) for i in range(2)]
            for t in range(self.NT):
                i = t % 2
                rows = slice(t * 128, (t + 1) * 128)
                P.op('sync', lambda e, i=i, rows=rows: e.dma_start(out=xt[i][:], in_=xsrc.t.ap()[rows, :]),
                     r=[xsrc], w=[xt[i]], dsem=xt[i])
                for j in range(4):
                    P.op('gpsimd', lambda e, i=i, j=j, t=t: e.indirect_dma_start(
                        out=yg[i][j][:], out_offset=None, in_=Ys.t.ap(),
                        in_offset=bass.IndirectOffsetOnAxis(ap=sl4[:, t, j:j + 1], axis=0)),
                        r=[Ys, sl4], w=[yg[i][j]], dsem=yg[i][j])
                P.op('vector', lambda e, i=i, t=t: e.tensor_scalar(out=acc[i][:], in0=yg[i][0][:],
                                                                  scalar1=g4[:, t, 0:1], scalar2=None, op0=ALU.mult),
                     r=[yg[i][0], g4], w=[acc[i]])
                for j in range(1, 4):
                    P.op('vector', lambda e, i=i, t=t, j=j: e.scalar_tensor_tensor(
                        out=acc[i][:], in0=yg[i][j][:], scalar=g4[:, t, j:j + 1], in1=acc[i][:], op0=ALU.mult,
                        op1=ALU.add), r=[yg[i][j], g4, acc[i]], w=[acc[i]])
                P.op('vector', lambda e, i=i: e.tensor_tensor(out=acc[i][:], in0=acc[i][:], in1=g2[:], op=ALU.mult),
                     r=[acc[i], g2], w=[acc[i]])
                P.op('gpsimd', lambda e, i=i: e.tensor_tensor(out=acc[i][:], in0=acc[i][:], in1=xt[i][:], op=ALU.add),
                     r=[acc[i], xt[i]], w=[acc[i]])
                P.op('sync', lambda e, i=i, rows=rows: e.dma_start(out=xdst.t.ap()[rows, :], in_=acc[i][:]),
                     r=[acc[i]], w=[xdst], dsem=acc[i])
            P.barrier()
            P.flush()

    def build(self):
        P = self.P
        self.load_consts()
        self.comb = P.sbuf(self.stack, 'comb', [128, self.NT, NE], F32)
        self.memb = P.sbuf(self.stack, 'memb', [128, self.NT, NE], F32)
        self.h2T_d = P.dram("h2T_d", [D, self.S], BF16, self.sk)
        self.accd = P.dram("accd", [self.S, D], F32, self.sk)
        self.comb_d = P.dram("comb_d", [128, self.NT, NE], F32, self.sk)
        self.NBLK = NE + 4 * self.S // TS
        self.sl4 = P.sbuf(self.stack, 'sl4', [128, self.NT, 4], I32)
        self.g4 = P.sbuf(self.stack, 'g4', [128, self.NT, 4], F32)
        self.widx = P.sbuf(self.stack, 'widx', [128, self.NBLK, 5], I32)
        self.Xs = P.dram("Xs", [self.NBLK * TS, D], BF16, self.sk)
        self.Ys = P.dram("Ys", [self.NBLK * TS, D], F32, self.sk)
        self.phase_ada()
        if self.sparse:
            self.precast_weights()
        xsrc = self.inp['x']
        for l in range(self.nlayers):
            last = (l == self.nlayers - 1)
            with ExitStack() as st:
                hT = P.sbuf(st, 'hT', [128, 8, self.S], BF16)
                self.phase_norm_T(st, l, xsrc, 0, 'norm1_g', hT)
                self.phase_proj(l, hT)
            self.phase_attn(l)
            self.phase_wout(l, xsrc, self.x1)
            xdst = self.out if last else self.x2
            if self.sparse:
                with ExitStack() as st:
                    h2b_all = P.sbuf(st, 'h2b_all', [128, self.NT, D], BF16)
                    self.phase_norm_T(None, l, self.x1, 1, 'norm2_g', None, router=True, h2b_all=h2b_all)
                self.phase_moe_sparse(l, self.x1, xdst)
            else:
                self.phase_norm_T(None, l, self.x1, 1, 'norm2_g', None, hT_dram=self.h2T_d, router=True)
                self.phase_moe_dense(l, self.x1, xdst)
            xsrc = xdst
        P.barrier()
        P.flush()
        self.stack.close()
        return self.nc


_CACHE = {}


def kernel(**inputs):
    S = inputs['x'].shape[1]
    nb = inputs['x'].shape[0]
    if S not in _CACHE:
        _CACHE[S] = K(S).build()
    nc = _CACHE[S]
    consts = make_consts()
    shared = {}
    for name, _ in INPUT_SPECS:
        if name in ('x', 'c'):
            continue
        shared[name] = np.ascontiguousarray(inputs[name], dtype=np.float32)
    for k, v in consts.items():
        shared['c_' + k] = v
    in_maps = []
    for b in range(nb):
        m = dict(shared)
        m['x'] = np.ascontiguousarray(inputs['x'][b], dtype=np.float32)
        m['c'] = np.ascontiguousarray(inputs['c'][b], dtype=np.float32)
        in_maps.append(m)
    res = run_bass_kernel_spmd(nc, in_maps, core_ids=list(range(nb)))
    return np.stack([np.asarray(r['out']) for r in res.results], 0).astype(np.float32)
```

```python
import numpy as np
import ml_dtypes
from contextlib import ExitStack
import concourse.bass as bass
import concourse.mybir as mybir
from concourse.bass_utils import run_bass_kernel_spmd

F32 = mybir.dt.float32
BF16 = mybir.dt.bfloat16
I32 = mybir.dt.int32
AF = mybir.ActivationFunctionType
ALU = mybir.AluOpType
AX = mybir.AxisListType

D = 1024
NL = 2
NE = 32
DIN = 3078
EPS = 1e-6
TS = 512
NBLK_MAX = 64
ENGS = ['sync', 'scalar', 'vector', 'gpsimd', 'tensor']


class Sem:
    def __init__(self, h):
        self.h = h
        self.v = 0
        self.nobarrier = False


class Buf:
    def __init__(self, t, name):
        self.t = t
        self.name = name
        self.w = {}
        self.r = {}
        self.dsem = None

    def __getitem__(self, idx):
        return self.t[idx]


class Prog:
    def __init__(self, nc, stack):
        self.nc = nc
        self.stack = stack
        self.q = {e: [] for e in ENGS}
        self.esem = {}
        self.allsems = []
        self.waited = {e: {} for e in ENGS}
        self.nsem = 0
        self.free_dsems = []
        self.phase_bufs = []
        for e in ['scalar', 'vector', 'gpsimd', 'tensor']:
            self.esem[e] = self.newsem('e_' + e)

    def newsem(self, name):
        h = self.stack.enter_context(self.nc.semaphore(name + '_%d' % self.nsem))
        self.nsem += 1
        s = Sem(h)
        self.allsems.append(s)
        return s

    def uname(self, name):
        self.nsem += 1
        return '%s_u%d' % (name, self.nsem)

    def sbuf(self, stack, name, shape, dt):
        name = self.uname(name)
        t = stack.enter_context(self.nc.sbuf_tensor(name, list(shape), dt))
        return Buf(t, name)

    def psum(self, stack, name, shape, dt=F32):
        name = self.uname(name)
        t = stack.enter_context(self.nc.psum_tensor(name, list(shape), dt))
        return Buf(t, name)

    def dram(self, name, shape, dt, kind="Internal"):
        t = self.nc.dram_tensor(name, list(shape), dt, kind=kind)
        return Buf(t, name)

    def op(self, eng, fn, r=(), w=(), dsem=None):
        waits = {}

        def addw(d):
            for s, v in d.items():
                if waits.get(s, 0) < v:
                    waits[s] = v
        for b in r:
            addw(b.w)
        for b in w:
            addw(b.w)
            addw(b.r)
        if dsem is not None:
            if dsem.dsem is None:
                dsem.dsem = self.free_dsems.pop() if self.free_dsems else self.newsem('d')
                self.phase_bufs.append(dsem)
            sem = dsem.dsem
            amt = 16
        else:
            sem = self.esem[eng]
            amt = 1
        wl = []
        for s, v in waits.items():
            if self.waited[eng].get(s, 0) >= v:
                continue
            if s not in self.esem.values():
                v = s.v
            if self.waited[eng].get(s, 0) < v:
                self.waited[eng][s] = v
                wl.append((s, v))
        sem.v += amt
        tok = (sem, sem.v)
        self.q[eng].append((fn, wl, sem, amt))
        for b in r:
            if b.r.get(sem, 0) < sem.v:
                b.r[sem] = sem.v
        for b in w:
            b.w = dict(b.w)
            b.w[sem] = sem.v
            b.r = {}
        return tok

    def barrier(self):
        for e in ENGS:
            wl = []
            for s in self.allsems:
                if s.nobarrier:
                    continue
                if s.v > 0 and self.waited[e].get(s, 0) < s.v:
                    self.waited[e][s] = s.v
                    wl.append((s, s.v))
            self.q[e].append((None, wl, None, 0))
        for b in self.phase_bufs:
            self.free_dsems.append(b.dsem)
            b.dsem = None
        self.phase_bufs = []

    def flush(self):
        with self.nc.Block() as block:
            for e in ENGS:
                items = self.q[e]

                def body(eng, items=items):
                    for fn, wl, sem, amt in items:
                        for s, v in wl:
                            eng.wait_ge(s.h, v)
                        if fn is not None:
                            ins = fn(eng)
                            ins.then_inc(sem.h, amt)
                getattr(block, e)(body)
        self.q = {e: [] for e in ENGS}


def bcast_ap(ap1d_tensor, offset, n, parts=128):
    return bass.AP(ap1d_tensor, offset, [[0, parts], [1, n]])


def make_consts():
    c = {}
    c['ident_bf'] = np.eye(128, dtype=np.float32).astype(ml_dtypes.bfloat16)
    c['ident_f'] = np.eye(128, dtype=np.float32)
    blk = np.zeros((128, 128), np.float32)
    blk[:64, :64] = 1.0 / 64
    blk[64:, 64:] = 1.0 / 64
    c['blk64'] = blk
    wn = np.full((65, 64), 1.0 / 64, np.float32)
    wn[64, :] = EPS
    c['wn65'] = wn
    j = np.arange(128)[:, None]
    k = np.arange(128)[None, :]
    c['negtri'] = np.where(j >= k, -1.0, 0.0).astype(np.float32).astype(ml_dtypes.bfloat16)
    c['negones'] = np.full((128, 128), -1.0, np.float32).astype(ml_dtypes.bfloat16)
    p = np.arange(128)[:, None]
    col = np.arange(512)[None, :]
    ms = np.zeros((4, 128, 512), np.float32)
    mn = np.zeros((4, 128, 512), np.float32)
    for jj in range(4):
        bc = col // 128
        ms[jj] = np.where(bc < jj, 0.0, np.where(bc == jj, (p < (col % 128)), 1.0))
        mn[jj] = np.where(bc < jj, 0.0, np.where(bc == jj, (p <= (col % 128)), 1.0))
    c['stri_f'] = (j < k).astype(np.float32)
    c['pidx'] = np.stack([np.arange(128), 8 * np.arange(128), np.minimum(np.arange(128), 1)],
                         1).astype(np.float32)
    c['blkstart'] = np.broadcast_to((np.arange(NBLK_MAX, dtype=np.float32) * TS)[None, :], (128, NBLK_MAX)).copy()
    c['mask_s'] = ms.transpose(1, 0, 2).copy().astype(ml_dtypes.bfloat16)
    c['mask_n'] = mn.transpose(1, 0, 2).copy().astype(ml_dtypes.bfloat16)
    return c


CONST_SPECS = [('ident_bf', [128, 128], BF16), ('ident_f', [128, 128], F32), ('blk64', [128, 128], F32),
               ('wn65', [65, 64], F32), ('negtri', [128, 128], BF16), ('negones', [128, 128], BF16),
               ('mask_s', [128, 4, 512], BF16), ('mask_n', [128, 4, 512], BF16),
               ('stri_f', [128, 128], F32), ('blkstart', [128, NBLK_MAX], F32), ('pidx', [128, 3], F32)]

INPUT_SPECS = [
    ('x', lambda S: [S, D]), ('c', lambda S: [D]), ('norm1_g', lambda S: [NL, D]),
    ('w_ada', lambda S: [NL, D, 6 * D]), ('b_ada', lambda S: [NL, 6 * D]), ('w_in', lambda S: [NL, D, DIN]),
    ('conv_w', lambda S: [NL, 3, 256]), ('sb_q_g', lambda S: [NL, 64]), ('sb_k_g', lambda S: [NL, 64]),
    ('fox_q_g', lambda S: [NL, 64]), ('fox_k_g', lambda S: [NL, 64]), ('fox_f_b', lambda S: [NL, 6]),
    ('out_norm_g', lambda S: [NL, D]), ('w_out', lambda S: [NL, D, D]), ('norm2_g', lambda S: [NL, D]),
    ('router_w', lambda S: [NL, D, NE]), ('router_b', lambda S: [NL, NE]),
    ('w_mlp1', lambda S: [NL, NE, D, 2 * D]), ('b_mlp1', lambda S: [NL, NE, 2 * D]),
    ('w_mlp2', lambda S: [NL, NE, D, D]), ('b_mlp2', lambda S: [NL, NE, D]),
]


class K:
    def __init__(self, S, nlayers=NL, debug=False, stop_after=None, sparse=True):
        self.sparse = sparse
        self.S = S
        self.NT = S // 128
        self.NC = S // 512
        self.debug = debug
        self.nlayers = nlayers
        self.stop_after = stop_after
        nc = bass.Bass("TRN2", target_bir_lowering=False)
        self.nc = nc
        self.stack = ExitStack()
        self.P = Prog(nc, self.stack)
        P = self.P
        self.inp = {}
        for name, shp in INPUT_SPECS:
            self.inp[name] = Buf(nc.dram_tensor(name, shp(S), F32, kind="ExternalInput"), name)
        self.cst = {}
        for name, shp, dt in CONST_SPECS:
            self.cst[name] = Buf(nc.dram_tensor('c_' + name, shp, dt, kind="ExternalInput"), name)
        self.out = Buf(nc.dram_tensor("out", [S, D], F32, kind="ExternalOutput"), "out")
        sk = "ExternalOutput" if debug else "Internal"
        self.sk = sk
        self.modv = P.dram("modv", [NL, 6 * D], F32, sk)
        self.qT_sb = P.dram("qT_sb", [384, S], BF16, sk)
        self.kT_sb = P.dram("kT_sb", [384, S], BF16, sk)
        self.v_sb = P.dram("v_sb", [S, 384], BF16, sk)
        self.qTa = P.dram("qTa", [6, 70, S], BF16, sk)
        self.kTa = P.dram("kTa", [6, 70, S], BF16, sk)
        self.v_fox = P.dram("v_fox", [S, 384], BF16, sk)
        self.yT = P.dram("yT", [D, S], BF16, sk)
        self.x1 = P.dram("x1", [S, D], F32, sk)
        self.x2 = P.dram("x2", [S, D], F32, sk)

    def load_consts(self):
        P = self.P
        st = self.stack
        self.c = {}
        for name, shp, dt in CONST_SPECS:
            b = P.sbuf(st, 'k_' + name, shp, dt)
            src = self.cst[name]
            P.op('sync', lambda e, b=b, src=src: e.dma_start(out=b[:], in_=src.t.ap()), r=[src], w=[b], dsem=b)
            self.c[name] = b
        ones = P.sbuf(st, 'k_ones', [128, 512], F32)
        P.op('vector', lambda e: e.memset(ones[:], 1.0), w=[ones])
        self.c['ones'] = ones
        onesb = P.sbuf(st, 'k_onesb', [128, 512], BF16)
        P.op('vector', lambda e: e.memset(onesb[:], 1.0), w=[onesb])
        self.c['onesb'] = onesb


    def precast_weights(self):
        P = self.P
        inp = self.inp
        self.wc1, self.wc2 = [], []
        for l in range(self.nlayers):
            b1 = P.dram("wc1_%d" % l, [NE * D, 2 * D], BF16)
            b2 = P.dram("wc2_%d" % l, [NE * D, D], BF16)
            for b in (b1, b2):
                b.dsem = P.newsem('wc')
                b.dsem.nobarrier = True
            self.wc1.append(b1)
            self.wc2.append(b2)
        self.pending_casts = [[] for _ in range(self.nlayers)]
        for l in range(self.nlayers):
            for e_ in range(NE):
                src1 = inp['w_mlp1'].t.ap()[l, e_].rearrange("(a b) n -> a (b n)", b=4)
                dst1 = self.wc1[l].t.ap()[e_ * D:(e_ + 1) * D, :].rearrange("(a b) n -> a (b n)", b=4)
                self.pending_casts[l].append((src1, dst1, self.wc1[l]))
                src2 = inp['w_mlp2'].t.ap()[l, e_].rearrange("(a b) n -> a (b n)", b=8)
                dst2 = self.wc2[l].t.ap()[e_ * D:(e_ + 1) * D, :].rearrange("(a b) n -> a (b n)", b=8)
                self.pending_casts[l].append((src2, dst2, self.wc2[l]))

    def issue_casts(self, l, n):
        P = self.P
        for _ in range(n):
            if not self.pending_casts[l]:
                return
            src, dst, buf = self.pending_casts[l].pop(0)
            P.op('gpsimd', lambda e, src=src, dst=dst: e.dma_start(out=dst, in_=src), w=[buf], dsem=buf)

    def phase_ada(self):
        P = self.P
        inp = self.inp
        with ExitStack() as st:
            cT = P.sbuf(st, 'a_cT', [128, 8], F32)
            sc = P.sbuf(st, 'a_sc', [128, 8], F32)
            wb = [P.sbuf(st, 'a_w%d' % i, [128, 3072], F32) for i in range(2)]
            bada = P.sbuf(st, 'a_b', [1, 6 * D], F32)
            modrow = P.sbuf(st, 'a_mod', [1, 6 * D], F32)
            ps = [P.psum(st, 'a_ps%d' % i, [128, 512]) for i in range(6)]
            cap = inp['c'].t.ap().rearrange("(p j) -> p j", j=8)
            P.op('sync', lambda e: e.dma_start(out=cT[:], in_=cap), w=[cT], dsem=cT)
            P.op('scalar', lambda e: e.activation(out=sc[:], in_=cT[:], func=AF.Silu), r=[cT], w=[sc])
            it = 0
            for l in range(self.nlayers):
                bsrc = inp['b_ada'].t.ap()[l:l + 1, :]
                P.op('sync', lambda e, bsrc=bsrc: e.dma_start(out=bada[:], in_=bsrc), w=[bada], dsem=bada)
                wv = inp['w_ada'].t.ap()[l].rearrange("(p j) n -> p j n", j=8)
                for half in range(2):
                    for j in range(8):
                        b = wb[it % 2]
                        it += 1
                        src = wv[:, j, half * 3072:(half + 1) * 3072]
                        P.op('sync' if it % 2 else 'gpsimd',
                             lambda e, b=b, src=src: e.dma_start(out=b[:], in_=src), w=[b], dsem=b)

                        def mm(e, b=b, j=j):
                            ins = None
                            for n in range(6):
                                ins = e.matmul(ps[n][0:1, :], sc[:, j:j + 1], b[:, n * 512:(n + 1) * 512],
                                               start=(j == 0), stop=(j == 7))
                            return ins
                        P.op('tensor', mm, r=[b, sc], w=ps)
                    for n in range(6):
                        o = half * 3072 + n * 512
                        P.op('vector', lambda e, n=n, o=o: e.tensor_tensor(
                            out=modrow[0:1, o:o + 512], in0=ps[n][0:1, :], in1=bada[0:1, o:o + 512], op=ALU.add),
                            r=[ps[n], bada], w=[modrow])
                dst = self.modv.t.ap()[l:l + 1, :]
                P.op('sync', lambda e, dst=dst: e.dma_start(out=dst, in_=modrow[:]), r=[modrow], w=[self.modv],
                     dsem=modrow)
            P.barrier()
            P.flush()

    def load_bcast(self, st, name, srcbuf, offset, n=D, eng='sync'):
        P = self.P
        b = P.sbuf(st, name, [128, n], F32)
        ap = bcast_ap(srcbuf.t, offset, n)
        P.op(eng, lambda e: e.dma_start(out=b[:], in_=ap), r=[srcbuf], w=[b], dsem=b)
        return b

    def mod_tiles(self, st, l, which, gname):
        P = self.P
        gb = self.load_bcast(st, 'm_g', self.inp[gname], l * D)
        sb = self.load_bcast(st, 'm_s', self.modv, l * 6 * D + (3 * which + 1) * D, eng='gpsimd')
        tb = self.load_bcast(st, 'm_t', self.modv, l * 6 * D + (3 * which + 0) * D)
        P.op('vector', lambda e: e.scalar_tensor_tensor(out=gb[:], in0=sb[:], scalar=1.0, in1=gb[:],
                                                       op0=ALU.add, op1=ALU.mult), r=[sb, gb], w=[gb])
        return gb, tb

    def rstd_from_ssq(self, ssq, lnv, rstd, n):
        P = self.P
        P.op('scalar', lambda e: e.activation(out=lnv[:], in_=ssq[:], func=AF.Ln, bias=EPS, scale=1.0 / n),
             r=[ssq], w=[lnv])
        P.op('scalar', lambda e: e.activation(out=rstd[:], in_=lnv[:], func=AF.Exp, scale=-0.5),
             r=[lnv], w=[rstd])

    def phase_norm_T(self, st, l, xsrc, which, gname, hT, hT_dram=None, router=False, h2b_all=None):
        P = self.P
        inp = self.inp
        with ExitStack() as s2:
            if hT_dram is not None:
                stg = [P.sbuf(s2, 'n_stg%d' % i, [128, 8, 512], BF16) for i in range(2)]
            if router:
                rw = P.sbuf(s2, 'n_rw', [128, 8, NE], F32)
                src_rw = inp['router_w'].t.ap()[l].rearrange("(k p) n -> p k n", p=128)
                P.op('sync', lambda e: e.dma_start(out=rw[:], in_=src_rw), w=[rw], dsem=rw)
                rb = self.load_bcast(s2, 'n_rb', inp['router_b'], l * NE, n=NE)
                h32 = [P.sbuf(s2, 'n_h32%d' % i, [128, D], F32) for i in range(2)]
                h32T = P.sbuf(s2, 'n_h32T', [128, 8, 128], F32)
                pR = P.psum(s2, 'n_pR', [128, 8, 128], F32)
                pL = P.psum(s2, 'n_pL', [128, 512], F32)
                lg = P.sbuf(s2, 'n_lg', [128, NE], F32)
                ex = P.sbuf(s2, 'n_ex', [128, NE], F32)
                t8 = P.sbuf(s2, 'n_t8', [128, 8], F32)
                nmx = P.sbuf(s2, 'n_nmx', [128, 1], F32)
                ssm = P.sbuf(s2, 'n_ssm', [128, 1], F32)
                identf = self.c['ident_f']
            G, T = self.mod_tiles(s2, l, which, gname)
            xt = [P.sbuf(s2, 'n_x%d' % i, [128, D], F32) for i in range(2)]
            junk = P.sbuf(s2, 'n_junk', [128, D], BF16)
            hn = [P.sbuf(s2, 'n_hn%d' % i, [128, D], F32) for i in range(2)]
            hb = [P.sbuf(s2, 'n_hb%d' % i, [128, D], BF16) for i in range(2)]
            ssq = [P.sbuf(s2, 'n_ssq%d' % i, [128, 1], F32) for i in range(2)]
            lnv = [P.sbuf(s2, 'n_ln%d' % i, [128, 1], F32) for i in range(2)]
            rstd = [P.sbuf(s2, 'n_rs%d' % i, [128, 1], F32) for i in range(2)]
            if h2b_all is None:
                pT = [P.psum(s2, 'n_pT%d' % i, [128, 8, 128], BF16) for i in range(2)]
            ident = self.c['ident_bf']
            for t in range(self.NT):
                i = t % 2
                src = xsrc.t.ap()[t * 128:(t + 1) * 128, :]
                P.op('sync', lambda e, i=i, src=src: e.dma_start(out=xt[i][:], in_=src), r=[xsrc], w=[xt[i]],
                     dsem=xt[i])
                P.op('scalar', lambda e, i=i: e.activation(out=junk[:], in_=xt[i][:], func=AF.Square,
                                                          accum_out=ssq[i][:]), r=[xt[i]], w=[junk, ssq[i]])
                self.rstd_from_ssq(ssq[i], lnv[i], rstd[i], D)
                P.op('vector', lambda e, i=i: e.scalar_tensor_tensor(
                    out=hn[i][:], in0=xt[i][:], scalar=rstd[i][:, 0:1], in1=G[:], op0=ALU.mult, op1=ALU.mult),
                    r=[xt[i], rstd[i], G], w=[hn[i]])
                if not router:
                    P.op('gpsimd', lambda e, i=i: e.tensor_tensor(out=hb[i][:], in0=hn[i][:], in1=T[:], op=ALU.add),
                         r=[hn[i], T], w=[hb[i]])
                else:
                    P.op('gpsimd', lambda e, i=i: e.tensor_tensor(out=h32[i][:], in0=hn[i][:], in1=T[:], op=ALU.add),
                         r=[hn[i], T], w=[h32[i]])
                    if h2b_all is None:
                        P.op('scalar', lambda e, i=i: e.copy(out=hb[i][:], in_=h32[i][:]), r=[h32[i]], w=[hb[i]])
                    else:
                        P.op('scalar', lambda e, i=i, t=t: e.copy(out=h2b_all[:, t, :], in_=h32[i][:]),
                             r=[h32[i]], w=[h2b_all])

                    def trf(e, i=i):
                        ins = None
                        for k in range(8):
                            ins = e.transpose(pR[:, k, :], h32[i][:, k * 128:(k + 1) * 128], identf[:])
                        return ins
                    P.op('tensor', trf, r=[h32[i], identf], w=[pR])
                    P.op('scalar', lambda e: e.copy(out=h32T[:], in_=pR[:]), r=[pR], w=[h32T])

                    def mmr(e):
                        ins = None
                        for k in range(8):
                            ins = e.matmul(pL[:, 0:NE], h32T[:, k, :], rw[:, k, :], start=(k == 0), stop=(k == 7))
                        return ins
                    P.op('tensor', mmr, r=[h32T, rw], w=[pL])
                    P.op('vector', lambda e: e.tensor_tensor(out=lg[:], in0=pL[:, 0:NE], in1=rb[:], op=ALU.add),
                         r=[pL, rb], w=[lg])
                    P.op('vector', lambda e: e.max(out=t8[:], in_=lg[:]), r=[lg], w=[t8])
                    mb = self.memb
                    cb = self.comb
                    P.op('vector', lambda e, t=t: e.tensor_scalar(out=mb[:, t, :], in0=lg[:], scalar1=t8[:, 3:4],
                                                                 scalar2=None, op0=ALU.is_ge), r=[lg, t8], w=[mb])
                    P.op('vector', lambda e: e.tensor_scalar(out=nmx[:], in0=t8[:, 0:1], scalar1=-1.0, scalar2=None,
                                                             op0=ALU.mult), r=[t8], w=[nmx])
                    P.op('scalar', lambda e: e.activation(out=ex[:], in_=lg[:], func=AF.Exp, bias=nmx[:, 0:1]),
                         r=[lg, nmx], w=[ex])
                    P.op('vector', lambda e, t=t: e.tensor_tensor(out=ex[:], in0=ex[:], in1=mb[:, t, :], op=ALU.mult),
                         r=[ex, mb], w=[ex])
                    P.op('vector', lambda e: e.reduce_sum(out=ssm[:], in_=ex[:], axis=AX.X), r=[ex], w=[ssm])
                    P.op('vector', lambda e: e.reciprocal(out=ssm[:], in_=ssm[:]), r=[ssm], w=[ssm])
                    P.op('vector', lambda e, t=t: e.tensor_scalar(out=cb[:, t, :], in0=ex[:], scalar1=ssm[:, 0:1],
                                                                 scalar2=None, op0=ALU.mult), r=[ex, ssm], w=[cb])

                if h2b_all is not None:
                    continue

                def tr(e, i=i):
                    ins = None
                    for k in range(8):
                        ins = e.transpose(pT[i][:, k, :], hb[i][:, k * 128:(k + 1) * 128], ident[:])
                    return ins
                P.op('tensor', tr, r=[hb[i], ident], w=[pT[i]])
                if hT_dram is None:
                    P.op('vector', lambda e, i=i, t=t: e.tensor_copy(out=hT[:, :, t * 128:(t + 1) * 128],
                                                                    in_=pT[i][:]), r=[pT[i]], w=[hT])
                else:
                    sg_ = stg[(t // 4) % 2]
                    tt = t % 4
                    P.op('vector', lambda e, i=i, tt=tt, sg_=sg_: e.tensor_copy(
                        out=sg_[:, :, tt * 128:(tt + 1) * 128], in_=pT[i][:]), r=[pT[i]], w=[sg_])
                    if tt == 3:
                        cc_ = t // 4
                        d_ap = hT_dram.t.ap().rearrange("(k p) s -> p k s", p=128)[:, :, cc_ * 512:(cc_ + 1) * 512]
                        P.op('sync', lambda e, sg_=sg_, d_ap=d_ap: e.dma_start(out=d_ap, in_=sg_[:]),
                             r=[sg_], w=[hT_dram], dsem=sg_)
            if h2b_all is not None:
                self.slots_and_scatter(s2, l, h2b_all)
            P.barrier()
            P.flush()

    def phase_proj(self, l, hT):
        P = self.P
        S = self.S
        inp = self.inp
        with ExitStack() as st:
            wbf = P.sbuf(st, 'p_w', [128, 8, DIN], BF16)
            wv = inp['w_in'].t.ap()[l].rearrange("(k p) n -> p k n", p=128)
            for k in range(8):
                P.op('gpsimd', lambda e, k=k: e.dma_start(out=wbf[:, k, :], in_=wv[:, k, :]), w=[wbf], dsem=wbf)
            gcol = {}
            for nm in ['sb_q_g', 'sb_k_g', 'fox_q_g', 'fox_k_g']:
                g = P.sbuf(st, 'p_' + nm, [128, 1], F32)
                for hh in range(2):
                    src = inp[nm].t.ap()[l].rearrange("(d o) -> d o", o=1)
                    P.op('sync', lambda e, g=g, hh=hh, src=src: e.dma_start(out=g[hh * 64:(hh + 1) * 64, :], in_=src),
                         w=[g], dsem=g)
                if nm.endswith('q_g'):
                    P.op('vector', lambda e, g=g: e.tensor_scalar(out=g[:], in0=g[:], scalar1=0.125, scalar2=None,
                                                                 op0=ALU.mult), r=[g], w=[g])
                gcol[nm] = g
            cw = P.sbuf(st, 'p_cw', [128, 2, 3], F32)
            for j in range(2):
                for i in range(3):
                    src = inp['conv_w'].t.ap()[l, i, j * 128:(j + 1) * 128].rearrange("(d o) -> d o", o=1)
                    P.op('sync', lambda e, j=j, i=i, src=src: e.dma_start(out=cw[:, j, i:i + 1], in_=src),
                         w=[cw], dsem=cw)
            og = P.sbuf(st, 'p_og', [128, 8], F32)
            src_og = inp['out_norm_g'].t.ap()[l].rearrange("(k p) -> p k", p=128)
            P.op('sync', lambda e: e.dma_start(out=og[:], in_=src_og, allow_slow_non_contiguous=True), w=[og], dsem=og)
            self.og_keep = None
            fb = P.sbuf(st, 'p_fb', [6, 1], F32)
            src_fb = inp['fox_f_b'].t.ap()[l].rearrange("(d o) -> d o", o=1)
            P.op('sync', lambda e: e.dma_start(out=fb[:], in_=src_fb), w=[fb], dsem=fb)
            P.op('vector', lambda e: e.tensor_scalar(out=fb[:], in0=fb[:], scalar1=-1.0, scalar2=None, op0=ALU.mult),
                 r=[fb], w=[fb])

            blk = self.c['blk64']
            pq = [P.psum(st, 'p_pq%d' % i, [128, 512]) for i in range(2)]
            pm = [P.psum(st, 'p_pm%d' % i, [128, 512]) for i in range(2)]
            sq = [P.sbuf(st, 'p_sq%d' % i, [128, 512], F32) for i in range(2)]
            rs = [P.sbuf(st, 'p_rs%d' % i, [128, 512], F32) for i in range(2)]
            qo = [P.sbuf(st, 'p_qo%d' % i, [128, 512], BF16) for i in range(2)]
            it = 0

            def proj_mm(ps, c0, ncols, n):
                def mm(e):
                    ins = None
                    for k in range(8):
                        ins = e.matmul(ps[0:ncols, :], wbf[:, k, c0:c0 + ncols], hT[:, k, n * 512:(n + 1) * 512],
                                       start=(k == 0), stop=(k == 7))
                    return ins
                P.op('tensor', mm, r=[wbf, hT], w=[ps])

            qk_tiles = []
            for m in range(3):
                qk_tiles.append((768 + m * 128, 'sb_q_g', self.qT_sb, m * 128, None))
                qk_tiles.append((1152 + m * 128, 'sb_k_g', self.kT_sb, m * 128, None))
                qk_tiles.append((1920 + m * 128, 'fox_q_g', self.qTa, None, 2 * m))
                qk_tiles.append((2304 + m * 128, 'fox_k_g', self.kTa, None, 2 * m))
            for (c0, gname, dst, row0, fh) in qk_tiles:
                for n in range(self.NC):
                    i = it % 2
                    it += 1
                    proj_mm(pq[i], c0, 128, n)
                    P.op('scalar', lambda e, i=i: e.activation(out=sq[i][:], in_=pq[i][:], func=AF.Square),
                         r=[pq[i]], w=[sq[i]])
                    P.op('tensor', lambda e, i=i: e.matmul(pm[i][:], blk[:], sq[i][:], start=True, stop=True),
                         r=[blk, sq[i]], w=[pm[i]])
                    P.op('scalar', lambda e, i=i: e.activation(out=rs[i][:], in_=pm[i][:], func=AF.Ln, bias=EPS),
                         r=[pm[i]], w=[rs[i]])
                    P.op('scalar', lambda e, i=i: e.activation(out=rs[i][:], in_=rs[i][:], func=AF.Exp, scale=-0.5),
                         r=[rs[i]], w=[rs[i]])
                    g = gcol[gname]
                    P.op('vector', lambda e, i=i, g=g: e.scalar_tensor_tensor(
                        out=qo[i][:], in0=pq[i][:], scalar=g[:, 0:1], in1=rs[i][:], op0=ALU.mult, op1=ALU.mult),
                        r=[pq[i], g, rs[i]], w=[qo[i]])
                    if row0 is not None:
                        d_ap = dst.t.ap()[row0:row0 + 128, n * 512:(n + 1) * 512]
                        P.op('sync', lambda e, i=i, d_ap=d_ap: e.dma_start(out=d_ap, in_=qo[i][:]),
                             r=[qo[i]], w=[dst], dsem=qo[i])
                    else:
                        for hh in range(2):
                            d_ap = dst.t.ap()[fh + hh, 0:64, n * 512:(n + 1) * 512]
                            P.op('sync', lambda e, i=i, hh=hh, d_ap=d_ap: e.dma_start(
                                out=d_ap, in_=qo[i][hh * 64:(hh + 1) * 64, :]), r=[qo[i]], w=[dst], dsem=qo[i])

            pc = [P.psum(st, 'p_pc%d' % i, [128, 512]) for i in range(3)]
            vb = P.sbuf(st, 'p_vb', [128, 514], F32)
            ccs = P.sbuf(st, 'p_ccs', [128, 512], F32)
            yc = P.sbuf(st, 'p_yc', [128, 512], F32)
            for j in range(2):
                P.op('vector', lambda e: e.memset(vb[:, 0:2], 0.0), w=[vb])
                for n in range(self.NC):
                    i = it % 2
                    it += 1
                    for q3 in range(3):
                        proj_mm(pc[q3], q3 * 256 + j * 128, 128, n)
                    if n > 0:
                        P.op('vector', lambda e: e.tensor_copy(out=vb[:, 0:2], in_=vb[:, 512:514]), r=[vb], w=[vb])
                    P.op('scalar', lambda e: e.copy(out=ccs[:], in_=pc[1][:]), r=[pc[1]], w=[ccs])
                    P.op('vector', lambda e: e.tensor_tensor(out=vb[:, 2:514], in0=pc[2][:], in1=ccs[:], op=ALU.mult),
                         r=[pc[2], ccs], w=[vb])
                    P.op('vector', lambda e, j=j: e.tensor_scalar(out=yc[:], in0=vb[:, 0:512], scalar1=cw[:, j, 0:1],
                                                                 scalar2=None, op0=ALU.mult), r=[vb, cw], w=[yc])
                    P.op('vector', lambda e, j=j: e.scalar_tensor_tensor(
                        out=yc[:], in0=vb[:, 1:513], scalar=cw[:, j, 1:2], in1=yc[:], op0=ALU.mult, op1=ALU.add),
                        r=[vb, cw, yc], w=[yc])
                    P.op('vector', lambda e, j=j: e.scalar_tensor_tensor(
                        out=yc[:], in0=vb[:, 2:514], scalar=cw[:, j, 2:3], in1=yc[:], op0=ALU.mult, op1=ALU.add),
                        r=[vb, cw, yc], w=[yc])
                    P.op('vector', lambda e: e.tensor_tensor(out=yc[:], in0=pc[0][:], in1=yc[:], op=ALU.mult),
                         r=[pc[0], yc], w=[yc])
                    P.op('scalar', lambda e, i=i: e.activation(out=sq[i][:], in_=yc[:], func=AF.Square),
                         r=[yc], w=[sq[i]])
                    P.op('tensor', lambda e, i=i: e.matmul(pm[i][:], blk[:], sq[i][:], start=True, stop=True),
                         r=[blk, sq[i]], w=[pm[i]])
                    P.op('scalar', lambda e, i=i: e.activation(out=rs[i][:], in_=pm[i][:], func=AF.Ln, bias=EPS),
                         r=[pm[i]], w=[rs[i]])
                    P.op('scalar', lambda e, i=i: e.activation(out=rs[i][:], in_=rs[i][:], func=AF.Exp, scale=-0.5),
                         r=[rs[i]], w=[rs[i]])
                    P.op('vector', lambda e, i=i, j=j: e.scalar_tensor_tensor(
                        out=qo[i][:], in0=yc[:], scalar=og[:, j:j + 1], in1=rs[i][:], op0=ALU.mult, op1=ALU.mult),
                        r=[yc, og, rs[i]], w=[qo[i]])
                    d_ap = self.yT.t.ap()[j * 128:(j + 1) * 128, n * 512:(n + 1) * 512]
                    P.op('sync', lambda e, i=i, d_ap=d_ap: e.dma_start(out=d_ap, in_=qo[i][:]),
                         r=[qo[i]], w=[self.yT], dsem=qo[i])

            pf = pc[0]
            fe = P.sbuf(st, 'p_fe', [6, 512], F32)
            fc = P.sbuf(st, 'p_fc', [6, 512 + 1], F32)
            r1 = P.sbuf(st, 'p_r1', [6, 512], F32)
            pcs = [P.sbuf(st, 'p_pc%d' % i, [6, 512], BF16) for i in range(3)]
            npcs = [P.sbuf(st, 'p_npc%d' % i, [6, 512], BF16) for i in range(3)]
            ones = self.c['ones']
            onesb = self.c['onesb']
            P.op('vector', lambda e: e.memset(fc[:, 0:1], 0.0), w=[fc])
            for n in range(self.NC):
                proj_mm(pf, 3072, 6, n)
                P.op('scalar', lambda e: e.activation(out=fe[:], in_=pf[0:6, :], func=AF.Exp, bias=fb[:, 0:1],
                                                      scale=-1.0), r=[pf, fb], w=[fe])
                P.op('scalar', lambda e: e.activation(out=fe[:], in_=fe[:], func=AF.Ln, bias=1.0), r=[fe], w=[fe])
                if n > 0:
                    P.op('vector', lambda e: e.tensor_copy(out=fc[:, 0:1], in_=fc[:, 512:513]), r=[fc], w=[fc])
                P.op('vector', lambda e: e.tensor_tensor_scan(out=fc[:, 1:513], data0=ones[0:6, 0:512], data1=fe[:],
                                                              initial=fc[:, 0:1], op0=ALU.mult, op1=ALU.add),
                     r=[ones, fe, fc], w=[fc])
                P.op('vector', lambda e: e.tensor_copy(out=pcs[0][:], in_=fc[:, 1:513]), r=[fc], w=[pcs[0]])
                P.op('vector', lambda e: e.tensor_tensor(out=r1[:], in0=fc[:, 1:513], in1=pcs[0][:], op=ALU.subtract),
                     r=[fc, pcs[0]], w=[r1])
                P.op('vector', lambda e: e.tensor_copy(out=pcs[1][:], in_=r1[:]), r=[r1], w=[pcs[1]])
                P.op('vector', lambda e: e.tensor_tensor(out=r1[:], in0=r1[:], in1=pcs[1][:], op=ALU.subtract),
                     r=[r1, pcs[1]], w=[r1])
                P.op('vector', lambda e: e.tensor_copy(out=pcs[2][:], in_=r1[:]), r=[r1], w=[pcs[2]])
                for q3 in range(3):
                    P.op('vector', lambda e, q3=q3: e.tensor_scalar(out=npcs[q3][:], in0=pcs[q3][:], scalar1=-1.0,
                                                                   scalar2=None, op0=ALU.mult),
                         r=[pcs[q3]], w=[npcs[q3]])
                    sl = slice(n * 512, (n + 1) * 512)
                    dq = self.qTa.t.ap()[:, 64 + q3, sl]
                    dk = self.kTa.t.ap()[:, 67 + q3, sl]
                    P.op('sync', lambda e, q3=q3, dq=dq: e.dma_start(out=dq, in_=npcs[q3][:]),
                         r=[npcs[q3]], w=[self.qTa], dsem=npcs[q3])
                    P.op('sync', lambda e, q3=q3, dk=dk: e.dma_start(out=dk, in_=pcs[q3][:]),
                         r=[pcs[q3]], w=[self.kTa], dsem=pcs[q3])
                    dq1 = self.qTa.t.ap()[:, 67 + q3, sl]
                    dk1 = self.kTa.t.ap()[:, 64 + q3, sl]
                    P.op('sync', lambda e, dq1=dq1: e.dma_start(out=dq1, in_=onesb[0:6, 0:512]),
                         r=[onesb], w=[self.qTa], dsem=onesb)
                    P.op('sync', lambda e, dk1=dk1: e.dma_start(out=dk1, in_=onesb[0:6, 0:512]),
                         r=[onesb], w=[self.kTa], dsem=onesb)

            vo = [P.sbuf(st, 'p_vo%d' % i, [128, 768], BF16) for i in range(2)]
            for t in range(self.NT):
                i = t % 2

                def mmv(e, t=t, i=i):
                    ins = None
                    for (ps, c0) in ((pq[i], 1536), (pm[i], 2688)):
                        for k in range(8):
                            ins = e.matmul(ps[:, 0:384], hT[:, k, t * 128:(t + 1) * 128], wbf[:, k, c0:c0 + 384],
                                           start=(k == 0), stop=(k == 7))
                    return ins
                P.op('tensor', mmv, r=[wbf, hT], w=[pq[i], pm[i]])
                P.op('scalar', lambda e, i=i: e.copy(out=vo[i][:, 0:384], in_=pq[i][:, 0:384]), r=[pq[i]], w=[vo[i]])
                P.op('vector', lambda e, i=i: e.tensor_copy(out=vo[i][:, 384:768], in_=pm[i][:, 0:384]),
                     r=[pm[i]], w=[vo[i]])
                d1 = self.v_sb.t.ap()[t * 128:(t + 1) * 128, :]
                d2 = self.v_fox.t.ap()[t * 128:(t + 1) * 128, :]
                P.op('sync', lambda e, i=i, d1=d1: e.dma_start(out=d1, in_=vo[i][:, 0:384]), r=[vo[i]],
                     w=[self.v_sb], dsem=vo[i])
                P.op('sync', lambda e, i=i, d2=d2: e.dma_start(out=d2, in_=vo[i][:, 384:768]), r=[vo[i]],
                     w=[self.v_fox], dsem=vo[i])
            P.barrier()
            P.flush()


    def head_norm_part1(self, st_bufs, po, nrow):
        P = self.P
        osb, sq, pn, rs, yo = st_bufs
        P.op('scalar', lambda e: e.copy(out=osb[0:64, :], in_=po[0:64, :]), r=[po], w=[osb])
        P.op('scalar', lambda e: e.activation(out=sq[0:nrow, :], in_=po[0:nrow, :], func=AF.Square), r=[po], w=[sq])

    def head_norm_part2(self, st_bufs, nrow, lhsT_ap, gcol_ap, drow, c, eps_bias):
        P = self.P
        osb, sq, pn, rs, yo = st_bufs
        P.op('tensor', lambda e: e.matmul(pn[0:64, :], lhsT_ap, sq[0:nrow, :], start=True, stop=True),
             r=[sq], w=[pn])
        P.op('scalar', lambda e: e.activation(out=rs[0:64, :], in_=pn[0:64, :], func=AF.Ln, bias=eps_bias),
             r=[pn], w=[rs])
        P.op('scalar', lambda e: e.activation(out=rs[0:64, :], in_=rs[0:64, :], func=AF.Exp, scale=-0.5),
             r=[rs], w=[rs])
        P.op('vector', lambda e: e.scalar_tensor_tensor(out=yo[0:64, :], in0=osb[0:64, :], scalar=gcol_ap,
                                                       in1=rs[0:64, :], op0=ALU.mult, op1=ALU.mult),
             r=[osb, rs], w=[yo])
        d_ap = self.yT.t.ap()[drow:drow + 64, c * 512:(c + 1) * 512]
        P.op('sync', lambda e: e.dma_start(out=d_ap, in_=yo[0:64, :]), r=[yo], w=[self.yT], dsem=yo)

    def phase_attn(self, l):
        P = self.P
        S = self.S
        NT = self.NT
        inp = self.inp
        with ExitStack() as st:
            ogs = P.sbuf(st, 't_og', [64, 12], F32)
            src_og = inp['out_norm_g'].t.ap()[l, 256:1024].rearrange("(h d) -> d h", d=64)
            P.op('sync', lambda e: e.dma_start(out=ogs[:], in_=src_og, allow_slow_non_contiguous=True),
                 w=[ogs], dsem=ogs)
            kinds = ('sb', 'fox')
            kT = {k: [P.sbuf(st, 't_kT%s%d' % (k, i), [70, S], BF16) for i in range(2)] for k in kinds}
            qT = {k: [P.sbuf(st, 't_qT%s%d' % (k, i), [70, S], BF16) for i in range(2)] for k in kinds}
            vv = {k: [P.sbuf(st, 't_v%s%d' % (k, i), [128, NT, 65], BF16) for i in range(2)] for k in kinds}
            for i in range(2):
                P.op('vector', lambda e, i=i: e.memset(vv['fox'][i][:], 1.0), w=[vv['fox'][i]])
            pzs = [P.psum(st, 't_pzs%d' % i, [128, 512]) for i in range(2)]
            pbs = [P.psum(st, 't_pbs%d' % i, [128, 512]) for i in range(2)]
            pzf = P.psum(st, 't_pzf', [128, 512])
            po = {k: P.psum(st, 't_po' + k, [128, 512]) for k in kinds}
            pn = P.psum(st, 't_pn', [128, 512])
            e32 = [P.sbuf(st, 't_e%d' % i, [128, 512], F32) for i in range(2)]
            e32f = P.sbuf(st, 't_ef', [128, 512], F32)
            ec = [P.sbuf(st, 't_ec%d' % i, [128, 512], BF16) for i in range(2)]
            sp = [P.sbuf(st, 't_sp%d' % i, [128, 512], BF16) for i in range(2)]
            wt = {k: [P.sbuf(st, 't_w%s%d' % (k, i), [128, 512], BF16) for i in range(2)] for k in kinds}
            lacc = P.sbuf(st, 't_lacc', [128, 512], F32)
            laccb = [P.sbuf(st, 't_laccb%d' % i, [128, 512], BF16) for i in range(2)]
            nbufs = {}
            for k in kinds:
                nbufs[k] = (P.sbuf(st, 't_osb' + k, [64, 512], F32), P.sbuf(st, 't_sq' + k, [65, 512], F32), pn,
                            P.sbuf(st, 't_rs' + k, [64, 512], F32), P.sbuf(st, 't_yo' + k, [64, 512], BF16))
            negtri = self.c['negtri']
            negones = self.c['negones']
            mask_s = self.c['mask_s']
            mask_n = self.c['mask_n']
            blk = self.c['blk64']
            wn65 = self.c['wn65']
            its = []
            for c in range(self.NC):
                nkb = 4 * c + 4
                for idx, kb in enumerate(reversed(range(nkb))):
                    its.append((c, kb, idx == 0, kb == 0))
            def head_loads(h):
                slot = h % 2
                for kind in kinds:
                    K_ = 64 if kind == 'sb' else 70
                    if kind == 'sb':
                        ksrc = self.kT_sb.t.ap()[h * 64:(h + 1) * 64, :]
                        qsrc = self.qT_sb.t.ap()[h * 64:(h + 1) * 64, :]
                        vsrc = self.v_sb.t.ap()[:, h * 64:(h + 1) * 64].rearrange("(t p) d -> p t d", p=128)
                        srcs = (self.kT_sb, self.qT_sb, self.v_sb)
                    else:
                        ksrc = self.kTa.t.ap()[h]
                        qsrc = self.qTa.t.ap()[h]
                        vsrc = self.v_fox.t.ap()[:, h * 64:(h + 1) * 64].rearrange("(t p) d -> p t d", p=128)
                        srcs = (self.kTa, self.qTa, self.v_fox)
                    kt_, qt_, v_ = kT[kind][slot], qT[kind][slot], vv[kind][slot]
                    P.op('sync', lambda e, kt_=kt_, ksrc=ksrc, K_=K_: e.dma_start(out=kt_[0:K_, :], in_=ksrc),
                         r=[srcs[0]], w=[kt_], dsem=kt_)
                    P.op('sync', lambda e, qt_=qt_, qsrc=qsrc, K_=K_: e.dma_start(out=qt_[0:K_, :], in_=qsrc),
                         r=[srcs[1]], w=[qt_], dsem=qt_)
                    P.op('sync', lambda e, v_=v_, vsrc=vsrc: e.dma_start(out=v_[:, :, 0:64], in_=vsrc),
                         r=[srcs[2]], w=[v_], dsem=v_)

            head_loads(0)
            for h in range(6):
                slot = h % 2
                if h + 1 < 6:
                    head_loads(h + 1)
                if self.sparse:
                    self.issue_casts(l, 64 if h == 5 else 11)

                def stageA(kind, n, slot=slot):
                    c, kb, first, last = its[n]
                    i = n % 2
                    j = kb - 4 * c
                    kt_, qt_ = kT[kind][slot], qT[kind][slot]
                    if kind == 'sb':
                        pz = pzs[i]
                        P.op('tensor', lambda e: e.matmul(pz[:], kt_[0:64, kb * 128:(kb + 1) * 128],
                                                          qt_[0:64, c * 512:(c + 1) * 512], start=True, stop=True),
                             r=[kt_, qt_], w=[pz])
                        P.op('scalar', lambda e: e.activation(out=e32[i][:], in_=pz[:], func=AF.Exp),
                             r=[pz], w=[e32[i]])
                        P.op('scalar', lambda e: e.activation(out=sp[i][:], in_=e32[i][:], func=AF.Ln, bias=1.0),
                             r=[e32[i]], w=[sp[i]])
                        if j >= 0:
                            P.op('gpsimd', lambda e: e.tensor_tensor(out=sp[i][:], in0=sp[i][:], in1=mask_s[:, j, :],
                                                                    op=ALU.mult), r=[sp[i], mask_s], w=[sp[i]])
                        if not last:
                            ln_ = laccb[(n + 1) % 2]
                            if first:
                                P.op('vector', lambda e: e.tensor_copy(out=lacc[:], in_=sp[i][:]), r=[sp[i]], w=[lacc])
                            else:
                                P.op('vector', lambda e: e.tensor_tensor(out=lacc[:], in0=lacc[:], in1=sp[i][:],
                                                                        op=ALU.add), r=[sp[i], lacc], w=[lacc])
                            P.op('vector', lambda e: e.tensor_copy(out=ln_[:], in_=lacc[:]), r=[lacc], w=[ln_])
                    else:
                        w_ = wt['fox'][i]
                        P.op('tensor', lambda e: e.matmul(pzf[:], kt_[0:70, kb * 128:(kb + 1) * 128],
                                                          qt_[0:70, c * 512:(c + 1) * 512], start=True, stop=True),
                             r=[kt_, qt_], w=[pzf])
                        if j >= 0:
                            P.op('vector', lambda e: e.tensor_scalar(out=e32f[:], in0=pzf[:], scalar1=60.0,
                                                                    scalar2=None, op0=ALU.min), r=[pzf], w=[e32f])
                            P.op('scalar', lambda e: e.activation(out=w_[:], in_=e32f[:], func=AF.Exp),
                                 r=[e32f], w=[w_])
                            P.op('gpsimd', lambda e: e.tensor_tensor(out=w_[:], in0=w_[:], in1=mask_n[:, j, :],
                                                                    op=ALU.mult), r=[w_, mask_n], w=[w_])
                        else:
                            P.op('scalar', lambda e: e.activation(out=w_[:], in_=pzf[:], func=AF.Exp),
                                 r=[pzf], w=[w_])

                def stageB1(n, slot=slot, h=h):
                    kind = 'sb'
                    c, kb, first, last = its[n]
                    i = n % 2
                    j = kb - 4 * c
                    w_ = wt[kind][i]
                    if True:
                        lb = laccb[n % 2]
                        pb = pbs[i]

                        def mm2(e):
                            ins = e.matmul(pb[:], negtri[:], sp[i][:], start=True, stop=first)
                            if not first:
                                ins = e.matmul(pb[:], negones[:], lb[:], start=False, stop=True)
                            return ins
                        P.op('tensor', mm2, r=[sp[i], negtri, negones] + ([] if first else [lb]), w=[pb])
                        P.op('scalar', lambda e: e.activation(out=ec[i][:], in_=pb[:], func=AF.Exp), r=[pb], w=[ec[i]])
                        if j >= 0:
                            P.op('gpsimd', lambda e: e.tensor_tensor(out=ec[i][:], in0=ec[i][:], in1=mask_s[:, j, :],
                                                                    op=ALU.mult), r=[ec[i], mask_s], w=[ec[i]])
                        P.op('vector', lambda e: e.tensor_tensor(out=w_[:], in0=e32[i][:], in1=ec[i][:], op=ALU.mult),
                             r=[e32[i], ec[i]], w=[w_])

                def stageB2(kind, n, slot=slot, h=h):
                    c, kb, first, last = its[n]
                    i = n % 2
                    v_ = vv[kind][slot]
                    pc_ = po[kind]
                    w_ = wt[kind][i]
                    nr = 64 if kind == 'sb' else 65
                    P.op('tensor', lambda e: e.matmul(pc_[0:nr, :], v_[:, kb, 0:nr], w_[:], start=first, stop=last),
                         r=[v_, w_], w=[pc_])
                    if last:
                        if kind == 'sb':
                            self.head_norm_part1(nbufs[kind], pc_, 64)
                            pending.append(lambda: self.head_norm_part2(nbufs['sb'], 64, blk[0:64, 0:64],
                                                                        ogs[:, h:h + 1], 256 + 64 * h, c, EPS))
                        else:
                            self.head_norm_part1(nbufs[kind], pc_, 65)
                            pending.append(lambda: self.head_norm_part2(nbufs['fox'], 65, wn65[:, :],
                                                                        ogs[:, 6 + h:7 + h], 640 + 64 * h, c, 0.0))

                stageA('sb', 0)
                stageA('fox', 0)
                pending = []
                for n in range(len(its)):
                    stageB1(n)
                    if n + 1 < len(its):
                        stageA('sb', n + 1)
                        stageA('fox', n + 1)
                    todo, pending[:] = list(pending), []
                    for f in todo:
                        f()
                    stageB2('fox', n)
                    stageB2('sb', n)
                for f in pending:
                    f()
            P.barrier()
            P.flush()

    def phase_wout(self, l, xsrc, xdst):
        P = self.P
        inp = self.inp
        with ExitStack() as st:
            wo = P.sbuf(st, 'o_w', [128, 8, D], BF16)
            wv = inp['w_out'].t.ap()[l].rearrange("(k p) n -> p k n", p=128)
            for k in range(8):
                P.op('gpsimd', lambda e, k=k: e.dma_start(out=wo[:, k, :], in_=wv[:, k, :]), w=[wo], dsem=wo)
            g1 = self.load_bcast(st, 'o_g1', self.modv, l * 6 * D + 2 * D)
            yt = [P.sbuf(st, 'o_y%d' % i, [128, 8, 512], BF16) for i in range(2)]
            xt = [P.sbuf(st, 'o_x%d' % i, [128, D], F32) for i in range(2)]
            xo = [P.sbuf(st, 'o_xo%d' % i, [128, D], F32) for i in range(2)]
            py = [P.psum(st, 'o_p%d' % i, [128, 512]) for i in range(4)]
            yv = self.yT.t.ap().rearrange("(k p) s -> p k s", p=128)
            for c in range(self.NC):
                yb = yt[c % 2]
                P.op('sync', lambda e, yb=yb, c=c: e.dma_start(out=yb[:], in_=yv[:, :, c * 512:(c + 1) * 512]),
                     r=[self.yT], w=[yb], dsem=yb)
                for tt in range(4):
                    t = c * 4 + tt
                    i = t % 2
                    src = xsrc.t.ap()[t * 128:(t + 1) * 128, :]
                    P.op('sync', lambda e, i=i, src=src: e.dma_start(out=xt[i][:], in_=src), r=[xsrc], w=[xt[i]],
                         dsem=xt[i])
                    for half in range(2):
                        pp = py[i * 2 + half]

                        def mm(e, yb=yb, tt=tt, half=half, pp=pp):
                            ins = None
                            for k in range(8):
                                ins = e.matmul(pp[:], yb[:, k, tt * 128:(tt + 1) * 128],
                                               wo[:, k, half * 512:(half + 1) * 512], start=(k == 0), stop=(k == 7))
                            return ins
                        P.op('tensor', mm, r=[yb, wo], w=[pp])
                        sl = slice(half * 512, (half + 1) * 512)
                        P.op('vector', lambda e, i=i, pp=pp, sl=sl: e.tensor_tensor(
                            out=xo[i][:, sl], in0=pp[:], in1=g1[:, sl], op=ALU.mult), r=[pp, g1], w=[xo[i]])
                    P.op('gpsimd', lambda e, i=i: e.tensor_tensor(out=xo[i][:], in0=xo[i][:], in1=xt[i][:], op=ALU.add),
                         r=[xo[i], xt[i]], w=[xo[i]])
                    dst = xdst.t.ap()[t * 128:(t + 1) * 128, :]
                    P.op('sync', lambda e, i=i, dst=dst: e.dma_start(out=dst, in_=xo[i][:]), r=[xo[i]], w=[xdst],
                         dsem=xo[i])
            P.barrier()
            P.flush()

    def load_expert(self, l, e_, w1b, w2b, b1c, b1s):
        P = self.P
        inp = self.inp
        w1v = inp['w_mlp1'].t.ap()[l, e_].rearrange("(k p) n -> p k n", p=128)
        w2v = inp['w_mlp2'].t.ap()[l, e_].rearrange("(p i) n -> p i n", i=8)
        for k in range(8):
            P.op('gpsimd', lambda e, k=k: e.dma_start(out=w1b[:, k, :], in_=w1v[:, k, :]), w=[w1b], dsem=w1b)
        for k in range(0, 8, 2):
            P.op('gpsimd', lambda e, k=k: e.dma_start(out=w2b[:, k:k + 2, :], in_=w2v[:, k:k + 2, :]),
                 w=[w2b], dsem=w2b)
        b1v = inp['b_mlp1'].t.ap()[l, e_].rearrange("(p m) -> p m", m=16)
        P.op('sync', lambda e: e.dma_start(out=b1c[:], in_=b1v), w=[b1c], dsem=b1c)

    def ffn_block(self, B, xT, w1b, w2b, b1c, b1s, evac, mid_hook=None):
        P = self.P
        aT = B['aT']
        CSIG = float(1.0 / (1.0 + np.exp(-1.702 * 7.0)))
        for i in range(8):
            q = B['it'] % 2
            B['it'] += 1
            pg, pl = B['pg'][q], B['pl'][q]
            sg, g, ll, tt = B['sg'][q], B['g'][q], B['l'][q], B['t'][q]

            def mm1(e, i=i, pg=pg, pl=pl):
                ins = None
                for (pp, j) in ((pg, 0), (pl, 1)):
                    for k in range(8):
                        ins = e.matmul(pp[:], w1b[:, k, 2 * i + j:2 * D:16], xT[:, k, :],
                                       start=(k == 0), stop=(k == 7))
                return ins
            P.op('tensor', mm1, r=[w1b, xT], w=[pg, pl])
            P.op('vector', lambda e, i=i, g=g, pg=pg: e.tensor_scalar(out=g[:], in0=pg[:], scalar1=b1c[:, 2 * i:2 * i + 1],
                                                                     scalar2=7.0, op0=ALU.add, op1=ALU.min),
                 r=[pg, b1c], w=[g])
            P.op('scalar', lambda e, sg=sg, g=g: e.activation(out=sg[:], in_=g[:], func=AF.Sigmoid, scale=1.702),
                 r=[g], w=[sg])
            P.op('vector', lambda e, i=i, ll=ll, pl=pl: e.tensor_scalar(out=ll[:], in0=pl[:], scalar1=b1c[:, 2 * i + 1:2 * i + 2],
                                                                       scalar2=7.0, op0=ALU.add, op1=ALU.min),
                 r=[pl, b1c], w=[ll])
            P.op('vector', lambda e, ll=ll: e.tensor_scalar(out=ll[:], in0=ll[:], scalar1=-7.0, scalar2=1.0,
                                                           op0=ALU.max, op1=ALU.add), r=[ll], w=[ll])
            P.op('gpsimd', lambda e, sg=sg, g=g, tt=tt: e.tensor_tensor(out=tt[:], in0=sg[:], in1=g[:], op=ALU.mult),
                 r=[sg, g], w=[tt])
            P.op('vector', lambda e, i=i, tt=tt, ll=ll: e.tensor_tensor(out=aT[:, i, :], in0=tt[:], in1=ll[:],
                                                                       op=ALU.mult), r=[tt, ll], w=[aT])
            if i == 4 and mid_hook is not None:
                mid_hook()
        for r_ in range(4):
            for half in range(2):
                pp = B['py'][B['ity'] % len(B['py'])]
                B['ity'] += 1

                def mm2(e, r_=r_, half=half, pp=pp):
                    ins = None
                    for k in range(8):
                        ins = e.matmul(pp[:], aT[:, k, r_ * 128:(r_ + 1) * 128],
                                       w2b[:, k, half * 512:(half + 1) * 512], start=(k == 0), stop=(k == 7))
                    return ins
                P.op('tensor', mm2, r=[aT, w2b], w=[pp])
                evac(r_, half, pp)

    def ffn_bufs(self, st, npy=4):
        P = self.P
        B = {'it': 0, 'ity': 0}
        B['aT'] = P.sbuf(st, 'f_aT', [128, 8, 512], BF16)
        B['pg'] = [P.psum(st, 'f_pg%d' % i, [128, 512]) for i in range(2)]
        B['pl'] = [P.psum(st, 'f_pl%d' % i, [128, 512]) for i in range(2)]
        B['py'] = [P.psum(st, 'f_py%d' % i, [128, 512]) for i in range(npy)]
        for nm in ('sg', 'g', 'l', 't'):
            B[nm] = [P.sbuf(st, 'f_%s%d' % (nm, i), [128, 512], F32) for i in range(2)]
        return B

    def phase_moe_dense(self, l, xsrc, xdst):
        P = self.P
        inp = self.inp
        NT = self.NT
        with ExitStack() as st:
            B = self.ffn_bufs(st)
            w1b = [P.sbuf(st, 'd_w1%d' % i, [128, 8, 2 * D], BF16) for i in range(2)]
            w2b = [P.sbuf(st, 'd_w2%d' % i, [128, 8, D], BF16) for i in range(2)]
            b1c = [P.sbuf(st, 'd_b1c%d' % i, [128, 16], F32) for i in range(2)]
            b1s = [P.sbuf(st, 'd_b1s%d' % i, [128, 8], F32) for i in range(2)]
            xT = [P.sbuf(st, 'd_xT%d' % i, [128, 8, 512], BF16) for i in range(2)]
            stage = [P.sbuf(st, 'd_st%d' % i, [128, D], F32) for i in range(4)]
            hv = self.h2T_d.t.ap().rearrange("(k p) s -> p k s", p=128)
            accd = self.accd
            with ExitStack() as s2:
                b2s = P.sbuf(s2, 'd_b2', [NE, D], F32)
                P.op('sync', lambda e: e.dma_start(out=b2s[:], in_=inp['b_mlp2'].t.ap()[l]), w=[b2s], dsem=b2s)
                cT = P.sbuf(s2, 'd_cT', [NE, 128], F32)
                identf = self.c['ident_f']
                pt = B['pg'][0]
                for t in range(NT):
                    P.op('tensor', lambda e, t=t: e.transpose(pt[0:NE, 0:128], self.comb[:, t, :], identf[:]),
                         r=[self.comb, identf], w=[pt])
                    P.op('scalar', lambda e: e.copy(out=cT[:], in_=pt[0:NE, 0:128]), r=[pt], w=[cT])
                    sg_ = stage[t % 4]
                    for half in range(2):
                        pp = B['py'][half]
                        P.op('tensor', lambda e, pp=pp, half=half: e.matmul(
                            pp[:], cT[:], b2s[:, half * 512:(half + 1) * 512], start=True, stop=True),
                            r=[cT, b2s], w=[pp])
                        P.op('vector', lambda e, pp=pp, half=half, sg_=sg_: e.tensor_copy(
                            out=sg_[:, half * 512:(half + 1) * 512], in_=pp[:]), r=[pp], w=[sg_])
                    dst = accd.t.ap()[t * 128:(t + 1) * 128, :]
                    P.op('gpsimd', lambda e, sg_=sg_, dst=dst: e.dma_start(out=dst, in_=sg_[:]), r=[sg_], w=[accd],
                         dsem=sg_)
            blk = 0
            for e_ in range(NE):
                ws = e_ % 2
                self.load_expert(l, e_, w1b[ws], w2b[ws], b1c[ws], b1s[ws])
                for c in range(self.NC):
                    xb = xT[blk % 2]
                    blk += 1
                    P.op('sync', lambda e, xb=xb, c=c: e.dma_start(out=xb[:], in_=hv[:, :, c * 512:(c + 1) * 512]),
                         r=[self.h2T_d], w=[xb], dsem=xb)

                    def evac(r_, half, pp, c=c, e_=e_):
                        t = c * 4 + r_
                        sg_ = stage[r_]
                        sl = slice(half * 512, (half + 1) * 512)
                        P.op('vector', lambda e: e.tensor_scalar(out=sg_[:, sl], in0=pp[:],
                                                                 scalar1=self.comb[:, t, e_:e_ + 1], scalar2=None,
                                                                 op0=ALU.mult), r=[pp, self.comb], w=[sg_])
                        if half == 1:
                            dst = accd.t.ap()[t * 128:(t + 1) * 128, :]
                            P.op('gpsimd', lambda e: e.dma_start(out=dst, in_=sg_[:], accum_op=ALU.add),
                                 r=[sg_], w=[accd], dsem=sg_)
                    self.ffn_block(B, xb, w1b[ws], w2b[ws], b1c[ws], b1s[ws], evac)
            P.barrier()
            P.flush()
        self.phase_final(l, xsrc, xdst)

    def phase_final(self, l, xsrc, xdst):
        P = self.P
        with ExitStack() as st:
            g2 = self.load_bcast(st, 'z_g2', self.modv, l * 6 * D + 5 * D)
            xt = [P.sbuf(st, 'z_x%d' % i, [128, D], F32) for i in range(2)]
            at = [P.sbuf(st, 'z_a%d' % i, [128, D], F32) for i in range(2)]
            for t in range(self.NT):
                i = t % 2
                rows = slice(t * 128, (t + 1) * 128)
                P.op('sync', lambda e, i=i, rows=rows: e.dma_start(out=xt[i][:], in_=xsrc.t.ap()[rows, :]),
                     r=[xsrc], w=[xt[i]], dsem=xt[i])
                P.op('sync', lambda e, i=i, rows=rows: e.dma_start(out=at[i][:], in_=self.accd.t.ap()[rows, :]),
                     r=[self.accd], w=[at[i]], dsem=at[i])
                P.op('vector', lambda e, i=i: e.tensor_tensor(out=at[i][:], in0=at[i][:], in1=g2[:], op=ALU.mult),
                     r=[at[i], g2], w=[at[i]])
                P.op('gpsimd', lambda e, i=i: e.tensor_tensor(out=at[i][:], in0=at[i][:], in1=xt[i][:], op=ALU.add),
                     r=[at[i], xt[i]], w=[at[i]])
                P.op('sync', lambda e, i=i, rows=rows: e.dma_start(out=xdst.t.ap()[rows, :], in_=at[i][:]),
                     r=[at[i]], w=[xdst], dsem=at[i])
            P.barrier()
            P.flush()


    def slots_and_scatter(self, st, l, h2b_all):
        P = self.P
        NT = self.NT
        NBLK = self.NBLK
        memb, comb = self.memb, self.comb
        onesf = self.c['ones']
        stri = self.c['stri_f']
        blkstart = self.c['blkstart']
        pcn = P.psum(st, 'q_pcn', [128, 512])
        cnt = P.sbuf(st, 'q_cnt', [128, NE], F32)
        nb = P.sbuf(st, 'q_nb', [128, NE], F32)
        incl = P.sbuf(st, 'q_incl', [128, NE], F32)
        off = P.sbuf(st, 'q_off', [128, NE], F32)
        texp = P.sbuf(st, 'q_texp', [128, NBLK], F32)

        def mmc(e):
            ins = None
            for t in range(NT):
                ins = e.matmul(pcn[:, 0:NE], onesf[:, 0:128], memb[:, t, :], start=(t == 0), stop=(t == NT - 1))
            return ins
        P.op('tensor', mmc, r=[onesf, memb], w=[pcn])
        P.op('vector', lambda e: e.tensor_copy(out=cnt[:], in_=pcn[:, 0:NE]), r=[pcn], w=[cnt])
        P.op('vector', lambda e: e.tensor_scalar(out=nb[:], in0=cnt[:], scalar1=0.0, scalar2=None, op0=ALU.is_gt),
             r=[cnt], w=[nb])
        for k in range(1, self.S // TS):
            P.op('vector', lambda e, k=k: e.scalar_tensor_tensor(out=nb[:], in0=cnt[:], scalar=float(k * TS),
                                                                in1=nb[:], op0=ALU.is_gt, op1=ALU.add),
                 r=[cnt, nb], w=[nb])
        P.op('vector', lambda e: e.tensor_scalar(out=nb[:], in0=nb[:], scalar1=float(TS), scalar2=None, op0=ALU.mult),
             r=[nb], w=[nb])
        P.op('vector', lambda e: e.tensor_tensor_scan(out=incl[:], data0=onesf[:, 0:NE], data1=nb[:], initial=0.0,
                                                      op0=ALU.mult, op1=ALU.add), r=[onesf, nb], w=[incl])
        P.op('vector', lambda e: e.tensor_tensor(out=off[:], in0=incl[:], in1=nb[:], op=ALU.subtract),
             r=[incl, nb], w=[off])
        P.op('vector', lambda e: e.tensor_scalar(out=texp[:], in0=blkstart[:, 0:NBLK], scalar1=incl[:, 0:1],
                                                 scalar2=None, op0=ALU.is_ge), r=[blkstart, incl], w=[texp])
        for e_ in range(1, NE):
            P.op('vector', lambda e, e_=e_: e.scalar_tensor_tensor(out=texp[:], in0=blkstart[:, 0:NBLK],
                                                                  scalar=incl[:, e_:e_ + 1], in1=texp[:],
                                                                  op0=ALU.is_ge, op1=ALU.add),
                 r=[blkstart, incl, texp], w=[texp])
        gar = P.sbuf(st, 'q_gar', [128, NBLK], F32)
        P.op('vector', lambda e: e.tensor_scalar(out=gar[:], in0=texp[:], scalar1=float(NE) - 0.5,
                                                 scalar2=self.c['pidx'][:, 2:3], op0=ALU.is_gt, op1=ALU.mult),
             r=[texp, self.c['pidx']], w=[gar])
        P.op('vector', lambda e: e.tensor_scalar(out=texp[:], in0=texp[:], scalar1=float(NE - 1), scalar2=None,
                                                 op0=ALU.min), r=[texp], w=[texp])
        pidx = self.c['pidx']
        t1 = P.sbuf(st, 'q_t1', [128, NBLK], F32)
        ixf = P.sbuf(st, 'q_ixf', [128, NBLK, 5], F32)
        BIG = float(1 << 15)
        P.op('vector', lambda e: e.tensor_scalar(out=t1[:], in0=texp[:], scalar1=128.0, scalar2=None,
                                                 op0=ALU.mult), r=[texp], w=[t1])
        P.op('vector', lambda e: e.scalar_tensor_tensor(out=t1[:], in0=gar[:], scalar=BIG, in1=t1[:], op0=ALU.mult,
                                                       op1=ALU.add), r=[gar, t1], w=[t1])
        P.op('vector', lambda e: e.tensor_scalar(out=ixf[:, :, 4], in0=t1[:], scalar1=pidx[:, 0:1], scalar2=None,
                                                 op0=ALU.add), r=[t1, pidx], w=[ixf])
        P.op('vector', lambda e: e.tensor_scalar(out=ixf[:, :, 1], in0=ixf[:, :, 4], scalar1=2.0, scalar2=None,
                                                 op0=ALU.mult), r=[ixf], w=[ixf])
        P.op('vector', lambda e: e.tensor_scalar(out=ixf[:, :, 2], in0=ixf[:, :, 4], scalar1=2.0, scalar2=1.0,
                                                 op0=ALU.mult, op1=ALU.add), r=[ixf], w=[ixf])
        P.op('vector', lambda e: e.tensor_scalar(out=ixf[:, :, 0], in0=ixf[:, :, 4], scalar1=float(l * NE * 128),
                                                 scalar2=None, op0=ALU.add), r=[ixf], w=[ixf])
        P.op('vector', lambda e: e.tensor_scalar(out=t1[:], in0=texp[:], scalar1=float(l * NE), scalar2=None,
                                                 op0=ALU.add), r=[texp], w=[t1])
        P.op('vector', lambda e: e.scalar_tensor_tensor(out=ixf[:, :, 3], in0=gar[:], scalar=BIG, in1=t1[:],
                                                       op0=ALU.mult, op1=ALU.add), r=[gar, t1], w=[ixf])
        widx = self.widx
        P.op('vector', lambda e: e.tensor_copy(out=widx[:], in_=ixf[:]), r=[ixf], w=[widx])
        macc = P.sbuf(st, 'q_macc', [128, NE], F32)
        pp = [P.psum(st, 'q_pp%d' % i, [128, 512]) for i in range(2)]
        tmp = P.sbuf(st, 'q_tmp', [128, NE], F32)
        val = P.sbuf(st, 'q_val', [128, NE], F32)
        oh = P.sbuf(st, 'q_oh', [128, NE], F32)
        t8 = P.sbuf(st, 'q_t8', [128, 8], F32)
        sl4, g4 = self.sl4, self.g4
        for t in range(NT):
            ppt = pp[t % 2]

            def mmp(e, t=t, ppt=ppt):
                ins = e.matmul(ppt[:, 0:NE], stri[:], memb[:, t, :], start=True, stop=(t == 0))
                if t > 0:
                    ins = e.matmul(ppt[:, 0:NE], onesf[:, 0:128], macc[:], start=False, stop=True)
                return ins
            P.op('tensor', mmp, r=[stri, memb, onesf] + ([macc] if t > 0 else []), w=[ppt])
            P.op('vector', lambda e, ppt=ppt: e.tensor_tensor(out=tmp[:], in0=ppt[:, 0:NE], in1=off[:], op=ALU.add),
                 r=[ppt, off], w=[tmp])
            P.op('vector', lambda e, t=t: e.scalar_tensor_tensor(out=val[:], in0=tmp[:], scalar=1.0, in1=memb[:, t, :],
                                                                op0=ALU.add, op1=ALU.mult), r=[tmp, memb], w=[val])
            P.op('vector', lambda e: e.max(out=t8[:], in_=val[:]), r=[val], w=[t8])
            P.op('vector', lambda e, t=t: e.tensor_scalar(out=sl4[:, t, :], in0=t8[:, 0:4], scalar1=-1.0, scalar2=None,
                                                         op0=ALU.add), r=[t8], w=[sl4])
            for j in range(4):
                P.op('vector', lambda e, j=j: e.tensor_scalar(out=oh[:], in0=val[:], scalar1=t8[:, j:j + 1],
                                                             scalar2=None, op0=ALU.is_equal), r=[val, t8], w=[oh])
                P.op('vector', lambda e, t=t: e.tensor_tensor(out=oh[:], in0=oh[:], in1=comb[:, t, :], op=ALU.mult),
                     r=[oh, comb], w=[oh])
                P.op('vector', lambda e, t=t, j=j: e.reduce_sum(out=g4[:, t, j:j + 1], in_=oh[:], axis=AX.X),
                     r=[oh], w=[g4])
            if t == 0:
                P.op('vector', lambda e: e.tensor_copy(out=macc[:], in_=memb[:, 0, :]), r=[memb], w=[macc])
            else:
                P.op('vector', lambda e, t=t: e.tensor_tensor(out=macc[:], in0=macc[:], in1=memb[:, t, :], op=ALU.add),
                     r=[macc, memb], w=[macc])
            Xs = self.Xs
            for j in range(4):
                P.op('gpsimd', lambda e, t=t, j=j: e.indirect_dma_start(
                    out=Xs.t.ap(), out_offset=bass.IndirectOffsetOnAxis(ap=sl4[:, t, j:j + 1], axis=0),
                    in_=h2b_all[:, t, :], in_offset=None), r=[sl4, h2b_all], w=[Xs], dsem=h2b_all)

    def phase_moe_sparse(self, l, xsrc, xdst):
        P = self.P
        inp = self.inp
        NT = self.NT
        widx = self.widx
        wc1, wc2 = self.wc1[l], self.wc2[l]
        w1rows = wc1.t.ap().rearrange("(e p h k) n -> (e p h) (k n)", p=128, h=2, k=4)
        w2rows = wc2.t.ap().rearrange("(e p i) n -> (e p) (i n)", p=128, i=8)
        b1rows = inp['b_mlp1'].t.ap().rearrange("l e (p m) -> (l e p) m", m=16)
        b2rows = inp['b_mlp2'].t.ap().rearrange("l e n -> (l e) n")
        with ExitStack() as st:
            B = self.ffn_bufs(st, npy=3)
            w1b = [P.sbuf(st, 'd_w1%d' % i, [128, 8, 2 * D], BF16) for i in range(2)]
            w2b = [P.sbuf(st, 'd_w2%d' % i, [128, 8, D], BF16) for i in range(2)]
            b1c = [P.sbuf(st, 'd_b1c%d' % i, [128, 16], F32) for i in range(2)]
            b2bc = [P.sbuf(st, 'd_b2%d' % i, [128, D], F32) for i in range(2)]
            xs = [P.sbuf(st, 'd_xs%d' % i, [128, D], BF16) for i in range(2)]
            xT = [P.sbuf(st, 'd_xT%d' % i, [128, 8, 512], BF16) for i in range(2)]
            stage = [P.sbuf(st, 'd_st%d' % i, [128, D], F32) for i in range(4)]
            ident = self.c['ident_bf']
            pTbuf = P.psum(st, 'pTb', [128, 8, 128], BF16)
            Xs, Ys = self.Xs, self.Ys
            rg = [self.nc.gpsimd.alloc_register('bc%d_%d' % (q, l)) for q in range(4)]
            rtile = P.sbuf(st, 'd_rt', [128, 1], F32)

            def setregs(e):
                e.reg_mov(rg[0], NE * 128 * 2 - 1)
                e.reg_mov(rg[1], NL * NE * 128 - 1)
                e.reg_mov(rg[2], NL * NE - 1)
                e.reg_mov(rg[3], NE * 128 - 1)
                return e.memset(rtile[:], 0.0)
            P.op('gpsimd', setregs, w=[rtile])
            nxs = [0]

            def gathers(i):
                ws = i % 2
                wa, wb_, bc, b2 = w1b[ws], w2b[ws], b1c[ws], b2bc[ws]
                for h in range(2):
                    P.op('gpsimd', lambda e, h=h: e.indirect_dma_start(
                        out=wa[:, 4 * h:4 * h + 4, :].rearrange("p k n -> p (k n)"), out_offset=None, in_=w1rows,
                        in_offset=bass.IndirectOffsetOnAxis(ap=widx[:, i, 1 + h:2 + h], axis=0),
                        bounds_check=rg[0], oob_is_err=False), r=[widx, wc1], w=[wa], dsem=wa)
                P.op('gpsimd', lambda e: e.indirect_dma_start(
                    out=wb_[:].rearrange("p k n -> p (k n)"), out_offset=None, in_=w2rows,
                    in_offset=bass.IndirectOffsetOnAxis(ap=widx[:, i, 4:5], axis=0),
                    bounds_check=rg[3], oob_is_err=False), r=[widx, wc2], w=[wb_], dsem=wb_)
                P.op('gpsimd', lambda e: e.indirect_dma_start(
                    out=bc[:], out_offset=None, in_=b1rows,
                    in_offset=bass.IndirectOffsetOnAxis(ap=widx[:, i, 0:1], axis=0),
                    bounds_check=rg[1], oob_is_err=False), r=[widx], w=[bc], dsem=bc)
                P.op('gpsimd', lambda e: e.indirect_dma_start(
                    out=b2[:], out_offset=None, in_=b2rows,
                    in_offset=bass.IndirectOffsetOnAxis(ap=widx[:, i, 3:4], axis=0),
                    bounds_check=rg[2], oob_is_err=False), r=[widx], w=[b2], dsem=b2)

            def prep_x(i):
                xb = xT[i % 2]
                for r_ in range(4):
                    xq = xs[nxs[0] % 2]
                    nxs[0] += 1
                    rows = slice(i * TS + r_ * 128, i * TS + (r_ + 1) * 128)
                    P.op('sync', lambda e, xq=xq, rows=rows: e.dma_start(out=xq[:], in_=Xs.t.ap()[rows, :]),
                         r=[Xs], w=[xq], dsem=xq)

                    def tr(e, xq=xq):
                        ins = None
                        for k in range(8):
                            ins = e.transpose(pTbuf[:, k, :], xq[:, k:D:8], ident[:])
                        return ins
                    P.op('tensor', tr, r=[xq, ident], w=[pTbuf])
                    P.op('scalar', lambda e, xb=xb, r_=r_: e.copy(out=xb[:, :, r_ * 128:(r_ + 1) * 128],
                                                                  in_=pTbuf[:]), r=[pTbuf], w=[xb])

            gathers(0)
            prep_x(0)
            for i in range(self.NBLK):
                ws = i % 2
                wa, wb_, bc, b2 = w1b[ws], w2b[ws], b1c[ws], b2bc[ws]
                xb = xT[i % 2]
                if i + 1 < self.NBLK:
                    gathers(i + 1)

                def evac(r_, half, pp, i=i, b2=b2):
                    sg_ = stage[r_]
                    sl = slice(half * 512, (half + 1) * 512)
                    P.op('vector', lambda e: e.tensor_tensor(out=sg_[:, sl], in0=pp[:], in1=b2[:, sl], op=ALU.add),
                         r=[pp, b2], w=[sg_])
                    if half == 1:
                        rows = slice(i * TS + r_ * 128, i * TS + (r_ + 1) * 128)
                        P.op('sync', lambda e: e.dma_start(out=Ys.t.ap()[rows, :], in_=sg_[:]),
                             r=[sg_], w=[Ys], dsem=sg_)
                hook = (lambda i=i: prep_x(i + 1)) if i + 1 < self.NBLK else None
                self.ffn_block(B, xb, wa, wb_, bc, None, evac, mid_hook=hook)
            P.barrier()
            P.flush()
        self.phase_combine(l, xsrc, xdst)

    def phase_combine(self, l, xsrc, xdst):
        P = self.P
        Ys = self.Ys
        sl4, g4 = self.sl4, self.g4
        with ExitStack() as st:
            g2 = self.load_bcast(st, 'z_g2', self.modv, l * 6 * D + 5 * D)
            xt = [P.sbuf(st, 'z_x%d' % i, [128, D], F32) for i in range(2)]
            yg = [[P.sbuf(st, 'z_y%d_%d' % (i, j), [128, D], F32) for j in range(4)] for i in range(2)]
            acc = [P.sbuf(st, 'z_a%d' % i, [128, D], F32) for i in range(2)]
            for t in range(self.NT):
                i = t % 2
                rows = slice(t * 128, (t + 1) * 128)
                P.op('sync', lambda e, i=i, rows=rows: e.dma_start(out=xt[i][:], in_=xsrc.t.ap()[rows, :]),
                     r=[xsrc], w=[xt[i]], dsem=xt[i])
                for j in range(4):
                    P.op('gpsimd', lambda e, i=i, j=j, t=t: e.indirect_dma_start(
                        out=yg[i][j][:], out_offset=None, in_=Ys.t.ap(),
                        in_offset=bass.IndirectOffsetOnAxis(ap=sl4[:, t, j:j + 1], axis=0)),
                        r=[Ys, sl4], w=[yg[i][j]], dsem=yg[i][j])
                P.op('vector', lambda e, i=i, t=t: e.tensor_scalar(out=acc[i][:], in0=yg[i][0][:],
                                                                  scalar1=g4[:, t, 0:1], scalar2=None, op0=ALU.mult),
                     r=[yg[i][0], g4], w=[acc[i]])
                for j in range(1, 4):
                    P.op('vector', lambda e, i=i, t=t, j=j: e.scalar_tensor_tensor(
                        out=acc[i][:], in0=yg[i][j][:], scalar=g4[:, t, j:j + 1], in1=acc[i][:], op0=ALU.mult,
                        op1=ALU.add), r=[yg[i][j], g4, acc[i]], w=[acc[i]])
                P.op('vector', lambda e, i=i: e.tensor_tensor(out=acc[i][:], in0=acc[i][:], in1=g2[:], op=ALU.mult),
                     r=[acc[i], g2], w=[acc[i]])
                P.op('gpsimd', lambda e, i=i: e.tensor_tensor(out=acc[i][:], in0=acc[i][:], in1=xt[i][:], op=ALU.add),
                     r=[acc[i], xt[i]], w=[acc[i]])
                P.op('sync', lambda e, i=i, rows=rows: e.dma_start(out=xdst.t.ap()[rows, :], in_=acc[i][:]),
                     r=[acc[i]], w=[xdst], dsem=acc[i])
            P.barrier()
            P.flush()

    def build(self):
        P = self.P
        self.load_consts()
        self.comb = P.sbuf(self.stack, 'comb', [128, self.NT, NE], F32)
        self.memb = P.sbuf(self.stack, 'memb', [128, self.NT, NE], F32)
        self.h2T_d = P.dram("h2T_d", [D, self.S], BF16, self.sk)
        self.accd = P.dram("accd", [self.S, D], F32, self.sk)
        self.comb_d = P.dram("comb_d", [128, self.NT, NE], F32, self.sk)
        self.NBLK = NE + 4 * self.S // TS
        self.sl4 = P.sbuf(self.stack, 'sl4', [128, self.NT, 4], I32)
        self.g4 = P.sbuf(self.stack, 'g4', [128, self.NT, 4], F32)
        self.widx = P.sbuf(self.stack, 'widx', [128, self.NBLK, 5], I32)
        self.Xs = P.dram("Xs", [self.NBLK * TS, D], BF16, self.sk)
        self.Ys = P.dram("Ys", [self.NBLK * TS, D], F32, self.sk)
        self.phase_ada()
        if self.sparse:
            self.precast_weights()
        xsrc = self.inp['x']
        for l in range(self.nlayers):
            last = (l == self.nlayers - 1)
            with ExitStack() as st:
                hT = P.sbuf(st, 'hT', [128, 8, self.S], BF16)
                self.phase_norm_T(st, l, xsrc, 0, 'norm1_g', hT)
                self.phase_proj(l, hT)
            self.phase_attn(l)
            self.phase_wout(l, xsrc, self.x1)
            xdst = self.out if last else self.x2
            if self.sparse:
                with ExitStack() as st:
                    h2b_all = P.sbuf(st, 'h2b_all', [128, self.NT, D], BF16)
                    self.phase_norm_T(None, l, self.x1, 1, 'norm2_g', None, router=True, h2b_all=h2b_all)
                self.phase_moe_sparse(l, self.x1, xdst)
            else:
                self.phase_norm_T(None, l, self.x1, 1, 'norm2_g', None, hT_dram=self.h2T_d, router=True)
                self.phase_moe_dense(l, self.x1, xdst)
            xsrc = xdst
        P.barrier()
        P.flush()
        self.stack.close()
        return self.nc


_CACHE = {}


def kernel(**inputs):
    S = inputs['x'].shape[1]
    nb = inputs['x'].shape[0]
    if S not in _CACHE:
        _CACHE[S] = K(S).build()
    nc = _CACHE[S]
    consts = make_consts()
    shared = {}
    for name, _ in INPUT_SPECS:
        if name in ('x', 'c'):
            continue
        shared[name] = np.ascontiguousarray(inputs[name], dtype=np.float32)
    for k, v in consts.items():
        shared['c_' + k] = v
    in_maps = []
    for b in range(nb):
        m = dict(shared)
        m['x'] = np.ascontiguousarray(inputs['x'][b], dtype=np.float32)
        m['c'] = np.ascontiguousarray(inputs['c'][b], dtype=np.float32)
        in_maps.append(m)
    res = run_bass_kernel_spmd(nc, in_maps, core_ids=list(range(nb)))
    return np.stack([np.asarray(r['out']) for r in res.results], 0).astype(np.float32)
```

```python
import numpy as np
import ml_dtypes
from contextlib import ExitStack
import concourse.bass as bass
import concourse.mybir as mybir
from concourse.bass_utils import run_bass_kernel_spmd

F32 = mybir.dt.float32
BF16 = mybir.dt.bfloat16
I32 = mybir.dt.int32
AF = mybir.ActivationFunctionType
ALU = mybir.AluOpType
AX = mybir.AxisListType

D = 1024
NL = 2
NE = 32
DIN = 3078
EPS = 1e-6
TS = 512
NBLK_MAX = 64
ENGS = ['sync', 'scalar', 'vector', 'gpsimd', 'tensor']


class Sem:
    def __init__(self, h):
        self.h = h
        self.v = 0
        self.nobarrier = False


class Buf:
    def __init__(self, t, name):
        self.t = t
        self.name = name
        self.w = {}
        self.r = {}
        self.dsem = None

    def __getitem__(self, idx):
        return self.t[idx]


class Prog:
    def __init__(self, nc, stack):
        self.nc = nc
        self.stack = stack
        self.q = {e: [] for e in ENGS}
        self.esem = {}
        self.allsems = []
        self.waited = {e: {} for e in ENGS}
        self.nsem = 0
        self.free_dsems = []
        self.phase_bufs = []
        for e in ['scalar', 'vector', 'gpsimd', 'tensor']:
            self.esem[e] = self.newsem('e_' + e)

    def newsem(self, name):
        h = self.stack.enter_context(self.nc.semaphore(name + '_%d' % self.nsem))
        self.nsem += 1
        s = Sem(h)
        self.allsems.append(s)
        return s

    def uname(self, name):
        self.nsem += 1
        return '%s_u%d' % (name, self.nsem)

    def sbuf(self, stack, name, shape, dt):
        name = self.uname(name)
        t = stack.enter_context(self.nc.sbuf_tensor(name, list(shape), dt))
        return Buf(t, name)

    def psum(self, stack, name, shape, dt=F32):
        name = self.uname(name)
        t = stack.enter_context(self.nc.psum_tensor(name, list(shape), dt))
        return Buf(t, name)

    def dram(self, name, shape, dt, kind="Internal"):
        t = self.nc.dram_tensor(name, list(shape), dt, kind=kind)
        return Buf(t, name)

    def op(self, eng, fn, r=(), w=(), dsem=None):
        waits = {}

        def addw(d):
            for s, v in d.items():
                if waits.get(s, 0) < v:
                    waits[s] = v
        for b in r:
            addw(b.w)
        for b in w:
            addw(b.w)
            addw(b.r)
        if dsem is not None:
            if dsem.dsem is None:
                dsem.dsem = self.free_dsems.pop() if self.free_dsems else self.newsem('d')
                self.phase_bufs.append(dsem)
            sem = dsem.dsem
            amt = 16
        else:
            sem = self.esem[eng]
            amt = 1
        wl = []
        for s, v in waits.items():
            if self.waited[eng].get(s, 0) >= v:
                continue
            if s not in self.esem.values():
                v = s.v
            if self.waited[eng].get(s, 0) < v:
                self.waited[eng][s] = v
                wl.append((s, v))
        sem.v += amt
        tok = (sem, sem.v)
        self.q[eng].append((fn, wl, sem, amt))
        for b in r:
            if b.r.get(sem, 0) < sem.v:
                b.r[sem] = sem.v
        for b in w:
            b.w = dict(b.w)
            b.w[sem] = sem.v
            b.r = {}
        return tok

    def barrier(self):
        for e in ENGS:
            wl = []
            for s in self.allsems:
                if s.nobarrier:
                    continue
                if s.v > 0 and self.waited[e].get(s, 0) < s.v:
                    self.waited[e][s] = s.v
                    wl.append((s, s.v))
            self.q[e].append((None, wl, None, 0))
        for b in self.phase_bufs:
            self.free_dsems.append(b.dsem)
            b.dsem = None
        self.phase_bufs = []

    def flush(self):
        with self.nc.Block() as block:
            for e in ENGS:
                items = self.q[e]

                def body(eng, items=items):
                    for fn, wl, sem, amt in items:
                        for s, v in wl:
                            eng.wait_ge(s.h, v)
                        if fn is not None:
                            ins = fn(eng)
                            ins.then_inc(sem.h, amt)
                getattr(block, e)(body)
        self.q = {e: [] for e in ENGS}


def bcast_ap(ap1d_tensor, offset, n, parts=128):
    return bass.AP(ap1d_tensor, offset, [[0, parts], [1, n]])


def make_consts():
    c = {}
    c['ident_bf'] = np.eye(128, dtype=np.float32).astype(ml_dtypes.bfloat16)
    c['ident_f'] = np.eye(128, dtype=np.float32)
    blk = np.zeros((128, 128), np.float32)
    blk[:64, :64] = 1.0 / 64
    blk[64:, 64:] = 1.0 / 64
    c['blk64'] = blk
    wn = np.full((65, 64), 1.0 / 64, np.float32)
    wn[64, :] = EPS
    c['wn65'] = wn
    j = np.arange(128)[:, None]
    k = np.arange(128)[None, :]
    c['negtri'] = np.where(j >= k, -1.0, 0.0).astype(np.float32).astype(ml_dtypes.bfloat16)
    c['negones'] = np.full((128, 128), -1.0, np.float32).astype(ml_dtypes.bfloat16)
    p = np.arange(128)[:, None]
    col = np.arange(512)[None, :]
    ms = np.zeros((4, 128, 512), np.float32)
    mn = np.zeros((4, 128, 512), np.float32)
    for jj in range(4):
        bc = col // 128
        ms[jj] = np.where(bc < jj, 0.0, np.where(bc == jj, (p < (col % 128)), 1.0))
        mn[jj] = np.where(bc < jj, 0.0, np.where(bc == jj, (p <= (col % 128)), 1.0))
    c['stri_f'] = (j < k).astype(np.float32)
    c['pidx'] = np.stack([np.arange(128), 8 * np.arange(128), np.minimum(np.arange(128), 1)],
                         1).astype(np.float32)
    c['blkstart'] = np.broadcast_to((np.arange(NBLK_MAX, dtype=np.float32) * TS)[None, :], (128, NBLK_MAX)).copy()
    c['mask_s'] = ms.transpose(1, 0, 2).copy().astype(ml_dtypes.bfloat16)
    c['mask_n'] = mn.transpose(1, 0, 2).copy().astype(ml_dtypes.bfloat16)
    return c


CONST_SPECS = [('ident_bf', [128, 128], BF16), ('ident_f', [128, 128], F32), ('blk64', [128, 128], F32),
               ('wn65', [65, 64], F32), ('negtri', [128, 128], BF16), ('negones', [128, 128], BF16),
               ('mask_s', [128, 4, 512], BF16), ('mask_n', [128, 4, 512], BF16),
               ('stri_f', [128, 128], F32), ('blkstart', [128, NBLK_MAX], F32), ('pidx', [128, 3], F32)]

INPUT_SPECS = [
    ('x', lambda S: [S, D]), ('c', lambda S: [D]), ('norm1_g', lambda S: [NL, D]),
    ('w_ada', lambda S: [NL, D, 6 * D]), ('b_ada', lambda S: [NL, 6 * D]), ('w_in', lambda S: [NL, D, DIN]),
    ('conv_w', lambda S: [NL, 3, 256]), ('sb_q_g', lambda S: [NL, 64]), ('sb_k_g', lambda S: [NL, 64]),
    ('fox_q_g', lambda S: [NL, 64]), ('fox_k_g', lambda S: [NL, 64]), ('fox_f_b', lambda S: [NL, 6]),
    ('out_norm_g', lambda S: [NL, D]), ('w_out', lambda S: [NL, D, D]), ('norm2_g', lambda S: [NL, D]),
    ('router_w', lambda S: [NL, D, NE]), ('router_b', lambda S: [NL, NE]),
    ('w_mlp1', lambda S: [NL, NE, D, 2 * D]), ('b_mlp1', lambda S: [NL, NE, 2 * D]),
    ('w_mlp2', lambda S: [NL, NE, D, D]), ('b_mlp2', lambda S: [NL, NE, D]),
]


class K:
    def __init__(self, S, nlayers=NL, debug=False, stop_after=None, sparse=True):
        self.sparse = sparse
        self.S = S
        self.NT = S // 128
        self.NC = S // 512
        self.debug = debug
        self.nlayers = nlayers
        self.stop_after = stop_after
        nc = bass.Bass("TRN2", target_bir_lowering=False)
        self.nc = nc
        self.stack = ExitStack()
        self.P = Prog(nc, self.stack)
        P = self.P
        self.inp = {}
        for name, shp in INPUT_SPECS:
            self.inp[name] = Buf(nc.dram_tensor(name, shp(S), F32, kind="ExternalInput"), name)
        self.cst = {}
        for name, shp, dt in CONST_SPECS:
            self.cst[name] = Buf(nc.dram_tensor('c_' + name, shp, dt, kind="ExternalInput"), name)
        self.out = Buf(nc.dram_tensor("out", [S, D], F32, kind="ExternalOutput"), "out")
        sk = "ExternalOutput" if debug else "Internal"
        self.sk = sk
        self.modv = P.dram("modv", [NL, 6 * D], F32, sk)
        self.qT_sb = P.dram("qT_sb", [384, S], BF16, sk)
        self.kT_sb = P.dram("kT_sb", [384, S], BF16, sk)
        self.v_sb = P.dram("v_sb", [S, 384], BF16, sk)
        self.qTa = P.dram("qTa", [6, 70, S], BF16, sk)
        self.kTa = P.dram("kTa", [6, 70, S], BF16, sk)
        self.v_fox = P.dram("v_fox", [S, 384], BF16, sk)
        self.yT = P.dram("yT", [D, S], BF16, sk)
        self.x1 = P.dram("x1", [S, D], F32, sk)
        self.x2 = P.dram("x2", [S, D], F32, sk)

    def load_consts(self):
        P = self.P
        st = self.stack
        self.c = {}
        for name, shp, dt in CONST_SPECS:
            b = P.sbuf(st, 'k_' + name, shp, dt)
            src = self.cst[name]
            P.op('sync', lambda e, b=b, src=src: e.dma_start(out=b[:], in_=src.t.ap()), r=[src], w=[b], dsem=b)
            self.c[name] = b
        ones = P.sbuf(st, 'k_ones', [128, 512], F32)
        P.op('vector', lambda e: e.memset(ones[:], 1.0), w=[ones])
        self.c['ones'] = ones
        onesb = P.sbuf(st, 'k_onesb', [128, 512], BF16)
        P.op('vector', lambda e: e.memset(onesb[:], 1.0), w=[onesb])
        self.c['onesb'] = onesb


    def precast_weights(self):
        P = self.P
        inp = self.inp
        self.wc1, self.wc2 = [], []
        for l in range(self.nlayers):
            b1 = P.dram("wc1_%d" % l, [NE * D, 2 * D], BF16)
            b2 = P.dram("wc2_%d" % l, [NE * D, D], BF16)
            for b in (b1, b2):
                b.dsem = P.newsem('wc')
                b.dsem.nobarrier = True
            self.wc1.append(b1)
            self.wc2.append(b2)
        self.pending_casts = [[] for _ in range(self.nlayers)]
        for l in range(self.nlayers):
            for e_ in range(NE):
                src1 = inp['w_mlp1'].t.ap()[l, e_].rearrange("(a b) n -> a (b n)", b=4)
                dst1 = self.wc1[l].t.ap()[e_ * D:(e_ + 1) * D, :].rearrange("(a b) n -> a (b n)", b=4)
                self.pending_casts[l].append((src1, dst1, self.wc1[l]))
                src2 = inp['w_mlp2'].t.ap()[l, e_].rearrange("(a b) n -> a (b n)", b=8)
                dst2 = self.wc2[l].t.ap()[e_ * D:(e_ + 1) * D, :].rearrange("(a b) n -> a (b n)", b=8)
                self.pending_casts[l].append((src2, dst2, self.wc2[l]))

    def issue_casts(self, l, n):
        P = self.P
        for _ in range(n):
            if not self.pending_casts[l]:
                return
            src, dst, buf = self.pending_casts[l].pop(0)
            P.op('gpsimd', lambda e, src=src, dst=dst: e.dma_start(out=dst, in_=src), w=[buf], dsem=buf)

    def phase_ada(self):
        P = self.P
        inp = self.inp
        with ExitStack() as st:
            cT = P.sbuf(st, 'a_cT', [128, 8], F32)
            sc = P.sbuf(st, 'a_sc', [128, 8], F32)
            wb = [P.sbuf(st, 'a_w%d' % i, [128, 3072], F32) for i in range(2)]
            bada = P.sbuf(st, 'a_b', [1, 6 * D], F32)
            modrow = P.sbuf(st, 'a_mod', [1, 6 * D], F32)
            ps = [P.psum(st, 'a_ps%d' % i, [128, 512]) for i in range(6)]
            cap = inp['c'].t.ap().rearrange("(p j) -> p j", j=8)
            P.op('sync', lambda e: e.dma_start(out=cT[:], in_=cap), w=[cT], dsem=cT)
            P.op('scalar', lambda e: e.activation(out=sc[:], in_=cT[:], func=AF.Silu), r=[cT], w=[sc])
            it = 0
            for l in range(self.nlayers):
                bsrc = inp['b_ada'].t.ap()[l:l + 1, :]
                P.op('sync', lambda e, bsrc=bsrc: e.dma_start(out=bada[:], in_=bsrc), w=[bada], dsem=bada)
                wv = inp['w_ada'].t.ap()[l].rearrange("(p j) n -> p j n", j=8)
                for half in range(2):
                    for j in range(8):
                        b = wb[it % 2]
                        it += 1
                        src = wv[:, j, half * 3072:(half + 1) * 3072]
                        P.op('sync' if it % 2 else 'gpsimd',
                             lambda e, b=b, src=src: e.dma_start(out=b[:], in_=src), w=[b], dsem=b)

                        def mm(e, b=b, j=j):
                            ins = None
                            for n in range(6):
                                ins = e.matmul(ps[n][0:1, :], sc[:, j:j + 1], b[:, n * 512:(n + 1) * 512],
                                               start=(j == 0), stop=(j == 7))
                            return ins
                        P.op('tensor', mm, r=[b, sc], w=ps)
                    for n in range(6):
                        o = half * 3072 + n * 512
                        P.op('vector', lambda e, n=n, o=o: e.tensor_tensor(
                            out=modrow[0:1, o:o + 512], in0=ps[n][0:1, :], in1=bada[0:1, o:o + 512], op=ALU.add),
                            r=[ps[n], bada], w=[modrow])
                dst = self.modv.t.ap()[l:l + 1, :]
                P.op('sync', lambda e, dst=dst: e.dma_start(out=dst, in_=modrow[:]), r=[modrow], w=[self.modv],
                     dsem=modrow)
            P.barrier()
            P.flush()

    def load_bcast(self, st, name, srcbuf, offset, n=D, eng='sync'):
        P = self.P
        b = P.sbuf(st, name, [128, n], F32)
        ap = bcast_ap(srcbuf.t, offset, n)
        P.op(eng, lambda e: e.dma_start(out=b[:], in_=ap), r=[srcbuf], w=[b], dsem=b)
        return b

    def mod_tiles(self, st, l, which, gname):
        P = self.P
        gb = self.load_bcast(st, 'm_g', self.inp[gname], l * D)
        sb = self.load_bcast(st, 'm_s', self.modv, l * 6 * D + (3 * which + 1) * D, eng='gpsimd')
        tb = self.load_bcast(st, 'm_t', self.modv, l * 6 * D + (3 * which + 0) * D)
        P.op('vector', lambda e: e.scalar_tensor_tensor(out=gb[:], in0=sb[:], scalar=1.0, in1=gb[:],
                                                       op0=ALU.add, op1=ALU.mult), r=[sb, gb], w=[gb])
        return gb, tb

    def rstd_from_ssq(self, ssq, lnv, rstd, n):
        P = self.P
        P.op('scalar', lambda e: e.activation(out=lnv[:], in_=ssq[:], func=AF.Ln, bias=EPS, scale=1.0 / n),
             r=[ssq], w=[lnv])
        P.op('scalar', lambda e: e.activation(out=rstd[:], in_=lnv[:], func=AF.Exp, scale=-0.5),
             r=[lnv], w=[rstd])

    def phase_norm_T(self, st, l, xsrc, which, gname, hT, hT_dram=None, router=False, h2b_all=None):
        P = self.P
        inp = self.inp
        with ExitStack() as s2:
            if hT_dram is not None:
                stg = [P.sbuf(s2, 'n_stg%d' % i, [128, 8, 512], BF16) for i in range(2)]
            if router:
                rw = P.sbuf(s2, 'n_rw', [128, 8, NE], F32)
                src_rw = inp['router_w'].t.ap()[l].rearrange("(k p) n -> p k n", p=128)
                P.op('sync', lambda e: e.dma_start(out=rw[:], in_=src_rw), w=[rw], dsem=rw)
                rb = self.load_bcast(s2, 'n_rb', inp['router_b'], l * NE, n=NE)
                h32 = [P.sbuf(s2, 'n_h32%d' % i, [128, D], F32) for i in range(2)]
                h32T = P.sbuf(s2, 'n_h32T', [128, 8, 128], F32)
                pR = P.psum(s2, 'n_pR', [128, 8, 128], F32)
                pL = P.psum(s2, 'n_pL', [128, 512], F32)
                lg = P.sbuf(s2, 'n_lg', [128, NE], F32)
                ex = P.sbuf(s2, 'n_ex', [128, NE], F32)
                t8 = P.sbuf(s2, 'n_t8', [128, 8], F32)
                nmx = P.sbuf(s2, 'n_nmx', [128, 1], F32)
                ssm = P.sbuf(s2, 'n_ssm', [128, 1], F32)
                identf = self.c['ident_f']
            G, T = self.mod_tiles(s2, l, which, gname)
            xt = [P.sbuf(s2, 'n_x%d' % i, [128, D], F32) for i in range(2)]
            junk = P.sbuf(s2, 'n_junk', [128, D], BF16)
            hn = [P.sbuf(s2, 'n_hn%d' % i, [128, D], F32) for i in range(2)]
            hb = [P.sbuf(s2, 'n_hb%d' % i, [128, D], BF16) for i in range(2)]
            ssq = [P.sbuf(s2, 'n_ssq%d' % i, [128, 1], F32) for i in range(2)]
            lnv = [P.sbuf(s2, 'n_ln%d' % i, [128, 1], F32) for i in range(2)]
            rstd = [P.sbuf(s2, 'n_rs%d' % i, [128, 1], F32) for i in range(2)]
            if h2b_all is None:
                pT = [P.psum(s2, 'n_pT%d' % i, [128, 8, 128], BF16) for i in range(2)]
            ident = self.c['ident_bf']
            for t in range(self.NT):
                i = t % 2
                src = xsrc.t.ap()[t * 128:(t + 1) * 128, :]
                P.op('sync', lambda e, i=i, src=src: e.dma_start(out=xt[i][:], in_=src), r=[xsrc], w=[xt[i]],
                     dsem=xt[i])
                P.op('scalar', lambda e, i=i: e.activation(out=junk[:], in_=xt[i][:], func=AF.Square,
                                                          accum_out=ssq[i][:]), r=[xt[i]], w=[junk, ssq[i]])
                self.rstd_from_ssq(ssq[i], lnv[i], rstd[i], D)
                P.op('vector', lambda e, i=i: e.scalar_tensor_tensor(
                    out=hn[i][:], in0=xt[i][:], scalar=rstd[i][:, 0:1], in1=G[:], op0=ALU.mult, op1=ALU.mult),
                    r=[xt[i], rstd[i], G], w=[hn[i]])
                if not router:
                    P.op('gpsimd', lambda e, i=i: e.tensor_tensor(out=hb[i][:], in0=hn[i][:], in1=T[:], op=ALU.add),
                         r=[hn[i], T], w=[hb[i]])
                else:
                    P.op('gpsimd', lambda e, i=i: e.tensor_tensor(out=h32[i][:], in0=hn[i][:], in1=T[:], op=ALU.add),
                         r=[hn[i], T], w=[h32[i]])
                    if h2b_all is None:
                        P.op('scalar', lambda e, i=i: e.copy(out=hb[i][:], in_=h32[i][:]), r=[h32[i]], w=[hb[i]])
                    else:
                        P.op('scalar', lambda e, i=i, t=t: e.copy(out=h2b_all[:, t, :], in_=h32[i][:]),
                             r=[h32[i]], w=[h2b_all])

                    def trf(e, i=i):
                        ins = None
                        for k in range(8):
                            ins = e.transpose(pR[:, k, :], h32[i][:, k * 128:(k + 1) * 128], identf[:])
                        return ins
                    P.op('tensor', trf, r=[h32[i], identf], w=[pR])
                    P.op('scalar', lambda e: e.copy(out=h32T[:], in_=pR[:]), r=[pR], w=[h32T])

                    def mmr(e):
                        ins = None
                        for k in range(8):
                            ins = e.matmul(pL[:, 0:NE], h32T[:, k, :], rw[:, k, :], start=(k == 0), stop=(k == 7))
                        return ins
                    P.op('tensor', mmr, r=[h32T, rw], w=[pL])
                    P.op('vector', lambda e: e.tensor_tensor(out=lg[:], in0=pL[:, 0:NE], in1=rb[:], op=ALU.add),
                         r=[pL, rb], w=[lg])
                    P.op('vector', lambda e: e.max(out=t8[:], in_=lg[:]), r=[lg], w=[t8])
                    mb = self.memb
                    cb = self.comb
                    P.op('vector', lambda e, t=t: e.tensor_scalar(out=mb[:, t, :], in0=lg[:], scalar1=t8[:, 3:4],
                                                                 scalar2=None, op0=ALU.is_ge), r=[lg, t8], w=[mb])
                    P.op('vector', lambda e: e.tensor_scalar(out=nmx[:], in0=t8[:, 0:1], scalar1=-1.0, scalar2=None,
                                                             op0=ALU.mult), r=[t8], w=[nmx])
                    P.op('scalar', lambda e: e.activation(out=ex[:], in_=lg[:], func=AF.Exp, bias=nmx[:, 0:1]),
                         r=[lg, nmx], w=[ex])
                    P.op('vector', lambda e, t=t: e.tensor_tensor(out=ex[:], in0=ex[:], in1=mb[:, t, :], op=ALU.mult),
                         r=[ex, mb], w=[ex])
                    P.op('vector', lambda e: e.reduce_sum(out=ssm[:], in_=ex[:], axis=AX.X), r=[ex], w=[ssm])
                    P.op('vector', lambda e: e.reciprocal(out=ssm[:], in_=ssm[:]), r=[ssm], w=[ssm])
                    P.op('vector', lambda e, t=t: e.tensor_scalar(out=cb[:, t, :], in0=ex[:], scalar1=ssm[:, 0:1],
                                                                 scalar2=None, op0=ALU.mult), r=[ex, ssm], w=[cb])

                if h2b_all is not None:
                    continue

                def tr(e, i=i):
                    ins = None
                    for k in range(8):
                        ins = e.transpose(pT[i][:, k, :], hb[i][:, k * 128:(k + 1) * 128], ident[:])
                    return ins
                P.op('tensor', tr, r=[hb[i], ident], w=[pT[i]])
                if hT_dram is None:
                    P.op('vector', lambda e, i=i, t=t: e.tensor_copy(out=hT[:, :, t * 128:(t + 1) * 128],
                                                                    in_=pT[i][:]), r=[pT[i]], w=[hT])
                else:
                    sg_ = stg[(t // 4) % 2]
                    tt = t % 4
                    P.op('vector', lambda e, i=i, tt=tt, sg_=sg_: e.tensor_copy(
                        out=sg_[:, :, tt * 128:(tt + 1) * 128], in_=pT[i][:]), r=[pT[i]], w=[sg_])
                    if tt == 3:
                        cc_ = t // 4
                        d_ap = hT_dram.t.ap().rearrange("(k p) s -> p k s", p=128)[:, :, cc_ * 512:(cc_ + 1) * 512]
                        P.op('sync', lambda e, sg_=sg_, d_ap=d_ap: e.dma_start(out=d_ap, in_=sg_[:]),
                             r=[sg_], w=[hT_dram], dsem=sg_)
            if h2b_all is not None:
                self.slots_and_scatter(s2, l, h2b_all)
            P.barrier()
            P.flush()

    def phase_proj(self, l, hT):
        P = self.P
        S = self.S
        inp = self.inp
        with ExitStack() as st:
            wbf = P.sbuf(st, 'p_w', [128, 8, DIN], BF16)
            wv = inp['w_in'].t.ap()[l].rearrange("(k p) n -> p k n", p=128)
            for k in range(8):
                P.op('gpsimd', lambda e, k=k: e.dma_start(out=wbf[:, k, :], in_=wv[:, k, :]), w=[wbf], dsem=wbf)
            gcol = {}
            for nm in ['sb_q_g', 'sb_k_g', 'fox_q_g', 'fox_k_g']:
                g = P.sbuf(st, 'p_' + nm, [128, 1], F32)
                for hh in range(2):
                    src = inp[nm].t.ap()[l].rearrange("(d o) -> d o", o=1)
                    P.op('sync', lambda e, g=g, hh=hh, src=src: e.dma_start(out=g[hh * 64:(hh + 1) * 64, :], in_=src),
                         w=[g], dsem=g)
                if nm.endswith('q_g'):
                    P.op('vector', lambda e, g=g: e.tensor_scalar(out=g[:], in0=g[:], scalar1=0.125, scalar2=None,
                                                                 op0=ALU.mult), r=[g], w=[g])
                gcol[nm] = g
            cw = P.sbuf(st, 'p_cw', [128, 2, 3], F32)
            for j in range(2):
                for i in range(3):
                    src = inp['conv_w'].t.ap()[l, i, j * 128:(j + 1) * 128].rearrange("(d o) -> d o", o=1)
                    P.op('sync', lambda e, j=j, i=i, src=src: e.dma_start(out=cw[:, j, i:i + 1], in_=src),
                         w=[cw], dsem=cw)
            og = P.sbuf(st, 'p_og', [128, 8], F32)
            src_og = inp['out_norm_g'].t.ap()[l].rearrange("(k p) -> p k", p=128)
            P.op('sync', lambda e: e.dma_start(out=og[:], in_=src_og, allow_slow_non_contiguous=True), w=[og], dsem=og)
            self.og_keep = None
            fb = P.sbuf(st, 'p_fb', [6, 1], F32)
            src_fb = inp['fox_f_b'].t.ap()[l].rearrange("(d o) -> d o", o=1)
            P.op('sync', lambda e: e.dma_start(out=fb[:], in_=src_fb), w=[fb], dsem=fb)
            P.op('vector', lambda e: e.tensor_scalar(out=fb[:], in0=fb[:], scalar1=-1.0, scalar2=None, op0=ALU.mult),
                 r=[fb], w=[fb])

            blk = self.c['blk64']
            pq = [P.psum(st, 'p_pq%d' % i, [128, 512]) for i in range(2)]
            pm = [P.psum(st, 'p_pm%d' % i, [128, 512]) for i in range(2)]
            sq = [P.sbuf(st, 'p_sq%d' % i, [128, 512], F32) for i in range(2)]
            rs = [P.sbuf(st, 'p_rs%d' % i, [128, 512], F32) for i in range(2)]
            qo = [P.sbuf(st, 'p_qo%d' % i, [128, 512], BF16) for i in range(2)]
            it = 0

            def proj_mm(ps, c0, ncols, n):
                def mm(e):
                    ins = None
                    for k in range(8):
                        ins = e.matmul(ps[0:ncols, :], wbf[:, k, c0:c0 + ncols], hT[:, k, n * 512:(n + 1) * 512],
                                       start=(k == 0), stop=(k == 7))
                    return ins
                P.op('tensor', mm, r=[wbf, hT], w=[ps])

            qk_tiles = []
            for m in range(3):
                qk_tiles.append((768 + m * 128, 'sb_q_g', self.qT_sb, m * 128, None))
                qk_tiles.append((1152 + m * 128, 'sb_k_g', self.kT_sb, m * 128, None))
                qk_tiles.append((1920 + m * 128, 'fox_q_g', self.qTa, None, 2 * m))
                qk_tiles.append((2304 + m * 128, 'fox_k_g', self.kTa, None, 2 * m))
            for (c0, gname, dst, row0, fh) in qk_tiles:
                for n in range(self.NC):
                    i = it % 2
                    it += 1
                    proj_mm(pq[i], c0, 128, n)
                    P.op('scalar', lambda e, i=i: e.activation(out=sq[i][:], in_=pq[i][:], func=AF.Square),
                         r=[pq[i]], w=[sq[i]])
                    P.op('tensor', lambda e, i=i: e.matmul(pm[i][:], blk[:], sq[i][:], start=True, stop=True),
                         r=[blk, sq[i]], w=[pm[i]])
                    P.op('scalar', lambda e, i=i: e.activation(out=rs[i][:], in_=pm[i][:], func=AF.Ln, bias=EPS),
                         r=[pm[i]], w=[rs[i]])
                    P.op('scalar', lambda e, i=i: e.activation(out=rs[i][:], in_=rs[i][:], func=AF.Exp, scale=-0.5),
                         r=[rs[i]], w=[rs[i]])
                    g = gcol[gname]
                    P.op('vector', lambda e, i=i, g=g: e.scalar_tensor_tensor(
                        out=qo[i][:], in0=pq[i][:], scalar=g[:, 0:1], in1=rs[i][:], op0=ALU.mult, op1=ALU.mult),
                        r=[pq[i], g, rs[i]], w=[qo[i]])
                    if row0 is not None:
                        d_ap = dst.t.ap()[row0:row0 + 128, n * 512:(n + 1) * 512]
                        P.op('sync', lambda e, i=i, d_ap=d_ap: e.dma_start(out=d_ap, in_=qo[i][:]),
                             r=[qo[i]], w=[dst], dsem=qo[i])
                    else:
                        for hh in range(2):
                            d_ap = dst.t.ap()[fh + hh, 0:64, n * 512:(n + 1) * 512]
                            P.op('sync', lambda e, i=i, hh=hh, d_ap=d_ap: e.dma_start(
                                out=d_ap, in_=qo[i][hh * 64:(hh + 1) * 64, :]), r=[qo[i]], w=[dst], dsem=qo[i])

            pc = [P.psum(st, 'p_pc%d' % i, [128, 512]) for i in range(3)]
            vb = P.sbuf(st, 'p_vb', [128, 514], F32)
            ccs = P.sbuf(st, 'p_ccs', [128, 512], F32)
            yc = P.sbuf(st, 'p_yc', [128, 512], F32)
            for j in range(2):
                P.op('vector', lambda e: e.memset(vb[:, 0:2], 0.0), w=[vb])
                for n in range(self.NC):
                    i = it % 2
                    it += 1
                    for q3 in range(3):
                        proj_mm(pc[q3], q3 * 256 + j * 128, 128, n)
                    if n > 0:
                        P.op('vector', lambda e: e.tensor_copy(out=vb[:, 0:2], in_=vb[:, 512:514]), r=[vb], w=[vb])
                    P.op('scalar', lambda e: e.copy(out=ccs[:], in_=pc[1][:]), r=[pc[1]], w=[ccs])
                    P.op('vector', lambda e: e.tensor_tensor(out=vb[:, 2:514], in0=pc[2][:], in1=ccs[:], op=ALU.mult),
                         r=[pc[2], ccs], w=[vb])
                    P.op('vector', lambda e, j=j: e.tensor_scalar(out=yc[:], in0=vb[:, 0:512], scalar1=cw[:, j, 0:1],
                                                                 scalar2=None, op0=ALU.mult), r=[vb, cw], w=[yc])
                    P.op('vector', lambda e, j=j: e.scalar_tensor_tensor(
                        out=yc[:], in0=vb[:, 1:513], scalar=cw[:, j, 1:2], in1=yc[:], op0=ALU.mult, op1=ALU.add),
                        r=[vb, cw, yc], w=[yc])
                    P.op('vector', lambda e, j=j: e.scalar_tensor_tensor(
                        out=yc[:], in0=vb[:, 2:514], scalar=cw[:, j, 2:3], in1=yc[:], op0=ALU.mult, op1=ALU.add),
                        r=[vb, cw, yc], w=[yc])
                    P.op('vector', lambda e: e.tensor_tensor(out=yc[:], in0=pc[0][:], in1=yc[:], op=ALU.mult),
                         r=[pc[0], yc], w=[yc])
                    P.op('scalar', lambda e, i=i: e.activation(out=sq[i][:], in_=yc[:], func=AF.Square),
                         r=[yc], w=[sq[i]])
                    P.op('tensor', lambda e, i=i: e.matmul(pm[i][:], blk[:], sq[i][:], start=True, stop=True),
                         r=[blk, sq[i]], w=[pm[i]])
                    P.op('scalar', lambda e, i=i: e.activation(out=rs[i][:], in_=pm[i][:], func=AF.Ln, bias=EPS),
                         r=[pm[i]], w=[rs[i]])
                    P.op('scalar', lambda e, i=i: e.activation(out=rs[i][:], in_=rs[i][:], func=AF.Exp, scale=-0.5),
                         r=[rs[i]], w=[rs[i]])
                    P.op('vector', lambda e, i=i, j=j: e.scalar_tensor_tensor(
                        out=qo[i][:], in0=yc[:], scalar=og[:, j:j + 1], in1=rs[i][:], op0=ALU.mult, op1=ALU.mult),
                        r=[yc, og, rs[i]], w=[qo[i]])
                    d_ap = self.yT.t.ap()[j * 128:(j + 1) * 128, n * 512:(n + 1) * 512]
                    P.op('sync', lambda e, i=i, d_ap=d_ap: e.dma_start(out=d_ap, in_=qo[i][:]),
                         r=[qo[i]], w=[self.yT], dsem=qo[i])

            pf = pc[0]
            fe = P.sbuf(st, 'p_fe', [6, 512], F32)
            fc = P.sbuf(st, 'p_fc', [6, 512 + 1], F32)
            r1 = P.sbuf(st, 'p_r1', [6, 512], F32)
            pcs = [P.sbuf(st, 'p_pc%d' % i, [6, 512], BF16) for i in range(3)]
            npcs = [P.sbuf(st, 'p_npc%d' % i, [6, 512], BF16) for i in range(3)]
            ones = self.c['ones']
            onesb = self.c['onesb']
            P.op('vector', lambda e: e.memset(fc[:, 0:1], 0.0), w=[fc])
            for n in range(self.NC):
                proj_mm(pf, 3072, 6, n)
                P.op('scalar', lambda e: e.activation(out=fe[:], in_=pf[0:6, :], func=AF.Exp, bias=fb[:, 0:1],
                                                      scale=-1.0), r=[pf, fb], w=[fe])
                P.op('scalar', lambda e: e.activation(out=fe[:], in_=fe[:], func=AF.Ln, bias=1.0), r=[fe], w=[fe])
                if n > 0:
                    P.op('vector', lambda e: e.tensor_copy(out=fc[:, 0:1], in_=fc[:, 512:513]), r=[fc], w=[fc])
                P.op('vector', lambda e: e.tensor_tensor_scan(out=fc[:, 1:513], data0=ones[0:6, 0:512], data1=fe[:],
                                                              initial=fc[:, 0:1], op0=ALU.mult, op1=ALU.add),
                     r=[ones, fe, fc], w=[fc])
                P.op('vector', lambda e: e.tensor_copy(out=pcs[0][:], in_=fc[:, 1:513]), r=[fc], w=[pcs[0]])
                P.op('vector', lambda e: e.tensor_tensor(out=r1[:], in0=fc[:, 1:513], in1=pcs[0][:], op=ALU.subtract),
                     r=[fc, pcs[0]], w=[r1])
                P.op('vector', lambda e: e.tensor_copy(out=pcs[1][:], in_=r1[:]), r=[r1], w=[pcs[1]])
                P.op('vector', lambda e: e.tensor_tensor(out=r1[:], in0=r1[:], in1=pcs[1][:], op=ALU.subtract),
                     r=[r1, pcs[1]], w=[r1])
                P.op('vector', lambda e: e.tensor_copy(out=pcs[2][:], in_=r1[:]), r=[r1], w=[pcs[2]])
                for q3 in range(3):
                    P.op('vector', lambda e, q3=q3: e.tensor_scalar(out=npcs[q3][:], in0=pcs[q3][:], scalar1=-1.0,
                                                                   scalar2=None, op0=ALU.mult),
                         r=[pcs[q3]], w=[npcs[q3]])
                    sl = slice(n * 512, (n + 1) * 512)
                    dq = self.qTa.t.ap()[:, 64 + q3, sl]
                    dk = self.kTa.t.ap()[:, 67 + q3, sl]
                    P.op('sync', lambda e, q3=q3, dq=dq: e.dma_start(out=dq, in_=npcs[q3][:]),
                         r=[npcs[q3]], w=[self.qTa], dsem=npcs[q3])
                    P.op('sync', lambda e, q3=q3, dk=dk: e.dma_start(out=dk, in_=pcs[q3][:]),
                         r=[pcs[q3]], w=[self.kTa], dsem=pcs[q3])
                    dq1 = self.qTa.t.ap()[:, 67 + q3, sl]
                    dk1 = self.kTa.t.ap()[:, 64 + q3, sl]
                    P.op('sync', lambda e, dq1=dq1: e.dma_start(out=dq1, in_=onesb[0:6, 0:512]),
                         r=[onesb], w=[self.qTa], dsem=onesb)
                    P.op('sync', lambda e, dk1=dk1: e.dma_start(out=dk1, in_=onesb[0:6, 0:512]),
                         r=[onesb], w=[self.kTa], dsem=onesb)

            vo = [P.sbuf(st, 'p_vo%d' % i, [128, 768], BF16) for i in range(2)]
            for t in range(self.NT):
                i = t % 2

                def mmv(e, t=t, i=i):
                    ins = None
                    for (ps, c0) in ((pq[i], 1536), (pm[i], 2688)):
                        for k in range(8):
                            ins = e.matmul(ps[:, 0:384], hT[:, k, t * 128:(t + 1) * 128], wbf[:, k, c0:c0 + 384],
                                           start=(k == 0), stop=(k == 7))
                    return ins
                P.op('tensor', mmv, r=[wbf, hT], w=[pq[i], pm[i]])
                P.op('scalar', lambda e, i=i: e.copy(out=vo[i][:, 0:384], in_=pq[i][:, 0:384]), r=[pq[i]], w=[vo[i]])
                P.op('vector', lambda e, i=i: e.tensor_copy(out=vo[i][:, 384:768], in_=pm[i][:, 0:384]),
                     r=[pm[i]], w=[vo[i]])
                d1 = self.v_sb.t.ap()[t * 128:(t + 1) * 128, :]
                d2 = self.v_fox.t.ap()[t * 128:(t + 1) * 128, :]
                P.op('sync', lambda e, i=i, d1=d1: e.dma_start(out=d1, in_=vo[i][:, 0:384]), r=[vo[i]],
                     w=[self.v_sb], dsem=vo[i])
                P.op('sync', lambda e, i=i, d2=d2: e.dma_start(out=d2, in_=vo[i][:, 384:768]), r=[vo[i]],
                     w=[self.v_fox], dsem=vo[i])
            P.barrier()
            P.flush()


    def head_norm_part1(self, st_bufs, po, nrow):
        P = self.P
        osb, sq, pn, rs, yo = st_bufs
        P.op('scalar', lambda e: e.copy(out=osb[0:64, :], in_=po[0:64, :]), r=[po], w=[osb])
        P.op('scalar', lambda e: e.activation(out=sq[0:nrow, :], in_=po[0:nrow, :], func=AF.Square), r=[po], w=[sq])

    def head_norm_part2(self, st_bufs, nrow, lhsT_ap, gcol_ap, drow, c, eps_bias):
        P = self.P
        osb, sq, pn, rs, yo = st_bufs
        P.op('tensor', lambda e: e.matmul(pn[0:64, :], lhsT_ap, sq[0:nrow, :], start=True, stop=True),
             r=[sq], w=[pn])
        P.op('scalar', lambda e: e.activation(out=rs[0:64, :], in_=pn[0:64, :], func=AF.Ln, bias=eps_bias),
             r=[pn], w=[rs])
        P.op('scalar', lambda e: e.activation(out=rs[0:64, :], in_=rs[0:64, :], func=AF.Exp, scale=-0.5),
             r=[rs], w=[rs])
        P.op('vector', lambda e: e.scalar_tensor_tensor(out=yo[0:64, :], in0=osb[0:64, :], scalar=gcol_ap,
                                                       in1=rs[0:64, :], op0=ALU.mult, op1=ALU.mult),
             r=[osb, rs], w=[yo])
        d_ap = self.yT.t.ap()[drow:drow + 64, c * 512:(c + 1) * 512]
        P.op('sync', lambda e: e.dma_start(out=d_ap, in_=yo[0:64, :]), r=[yo], w=[self.yT], dsem=yo)

    def phase_attn(self, l):
        P = self.P
        S = self.S
        NT = self.NT
        inp = self.inp
        with ExitStack() as st:
            ogs = P.sbuf(st, 't_og', [64, 12], F32)
            src_og = inp['out_norm_g'].t.ap()[l, 256:1024].rearrange("(h d) -> d h", d=64)
            P.op('sync', lambda e: e.dma_start(out=ogs[:], in_=src_og, allow_slow_non_contiguous=True),
                 w=[ogs], dsem=ogs)
            kinds = ('sb', 'fox')
            kT = {k: [P.sbuf(st, 't_kT%s%d' % (k, i), [70, S], BF16) for i in range(2)] for k in kinds}
            qT = {k: [P.sbuf(st, 't_qT%s%d' % (k, i), [70, S], BF16) for i in range(2)] for k in kinds}
            vv = {k: [P.sbuf(st, 't_v%s%d' % (k, i), [128, NT, 65], BF16) for i in range(2)] for k in kinds}
            for i in range(2):
                P.op('vector', lambda e, i=i: e.memset(vv['fox'][i][:], 1.0), w=[vv['fox'][i]])
            pzs = [P.psum(st, 't_pzs%d' % i, [128, 512]) for i in range(2)]
            pbs = [P.psum(st, 't_pbs%d' % i, [128, 512]) for i in range(2)]
            pzf = P.psum(st, 't_pzf', [128, 512])
            po = {k: P.psum(st, 't_po' + k, [128, 512]) for k in kinds}
            pn = P.psum(st, 't_pn', [128, 512])
            e32 = [P.sbuf(st, 't_e%d' % i, [128, 512], F32) for i in range(2)]
            e32f = P.sbuf(st, 't_ef', [128, 512], F32)
            ec = [P.sbuf(st, 't_ec%d' % i, [128, 512], BF16) for i in range(2)]
            sp = [P.sbuf(st, 't_sp%d' % i, [128, 512], BF16) for i in range(2)]
            wt = {k: [P.sbuf(st, 't_w%s%d' % (k, i), [128, 512], BF16) for i in range(2)] for k in kinds}
            lacc = P.sbuf(st, 't_lacc', [128, 512], F32)
            laccb = [P.sbuf(st, 't_laccb%d' % i, [128, 512], BF16) for i in range(2)]
            nbufs = {}
            for k in kinds:
                nbufs[k] = (P.sbuf(st, 't_osb' + k, [64, 512], F32), P.sbuf(st, 't_sq' + k, [65, 512], F32), pn,
                            P.sbuf(st, 't_rs' + k, [64, 512], F32), P.sbuf(st, 't_yo' + k, [64, 512], BF16))
            negtri = self.c['negtri']
            negones = self.c['negones']
            mask_s = self.c['mask_s']
            mask_n = self.c['mask_n']
            blk = self.c['blk64']
            wn65 = self.c['wn65']
            its = []
            for c in range(self.NC):
                nkb = 4 * c + 4
                for idx, kb in enumerate(reversed(range(nkb))):
                    its.append((c, kb, idx == 0, kb == 0))
            def head_loads(h):
                slot = h % 2
                for kind in kinds:
                    K_ = 64 if kind == 'sb' else 70
                    if kind == 'sb':
                        ksrc = self.kT_sb.t.ap()[h * 64:(h + 1) * 64, :]
                        qsrc = self.qT_sb.t.ap()[h * 64:(h + 1) * 64, :]
                        vsrc = self.v_sb.t.ap()[:, h * 64:(h + 1) * 64].rearrange("(t p) d -> p t d", p=128)
                        srcs = (self.kT_sb, self.qT_sb, self.v_sb)
                    else:
                        ksrc = self.kTa.t.ap()[h]
                        qsrc = self.qTa.t.ap()[h]
                        vsrc = self.v_fox.t.ap()[:, h * 64:(h + 1) * 64].rearrange("(t p) d -> p t d", p=128)
                        srcs = (self.kTa, self.qTa, self.v_fox)
                    kt_, qt_, v_ = kT[kind][slot], qT[kind][slot], vv[kind][slot]
                    P.op('sync', lambda e, kt_=kt_, ksrc=ksrc, K_=K_: e.dma_start(out=kt_[0:K_, :], in_=ksrc),
                         r=[srcs[0]], w=[kt_], dsem=kt_)
                    P.op('sync', lambda e, qt_=qt_, qsrc=qsrc, K_=K_: e.dma_start(out=qt_[0:K_, :], in_=qsrc),
                         r=[srcs[1]], w=[qt_], dsem=qt_)
                    P.op('sync', lambda e, v_=v_, vsrc=vsrc: e.dma_start(out=v_[:, :, 0:64], in_=vsrc),
                         r=[srcs[2]], w=[v_], dsem=v_)

            head_loads(0)
            for h in range(6):
                slot = h % 2
                if h + 1 < 6:
                    head_loads(h + 1)

                def stageA(kind, n, slot=slot):
                    c, kb, first, last = its[n]
                    i = n % 2
                    j = kb - 4 * c
                    kt_, qt_ = kT[kind][slot], qT[kind][slot]
                    if kind == 'sb':
                        pz = pzs[i]
                        P.op('tensor', lambda e: e.matmul(pz[:], kt_[0:64, kb * 128:(kb + 1) * 128],
                                                          qt_[0:64, c * 512:(c + 1) * 512], start=True, stop=True),
                             r=[kt_, qt_], w=[pz])
                        P.op('scalar', lambda e: e.activation(out=e32[i][:], in_=pz[:], func=AF.Exp),
                             r=[pz], w=[e32[i]])
                        P.op('scalar', lambda e: e.activation(out=sp[i][:], in_=e32[i][:], func=AF.Ln, bias=1.0),
                             r=[e32[i]], w=[sp[i]])
                        if j >= 0:
                            P.op('gpsimd', lambda e: e.tensor_tensor(out=sp[i][:], in0=sp[i][:], in1=mask_s[:, j, :],
                                                                    op=ALU.mult), r=[sp[i], mask_s], w=[sp[i]])
                        if not last:
                            ln_ = laccb[(n + 1) % 2]
                            if first:
                                P.op('vector', lambda e: e.tensor_copy(out=lacc[:], in_=sp[i][:]), r=[sp[i]], w=[lacc])
                            else:
                                P.op('vector', lambda e: e.tensor_tensor(out=lacc[:], in0=lacc[:], in1=sp[i][:],
                                                                        op=ALU.add), r=[sp[i], lacc], w=[lacc])
                            P.op('vector', lambda e: e.tensor_copy(out=ln_[:], in_=lacc[:]), r=[lacc], w=[ln_])
                    else:
                        w_ = wt['fox'][i]
                        P.op('tensor', lambda e: e.matmul(pzf[:], kt_[0:70, kb * 128:(kb + 1) * 128],
                                                          qt_[0:70, c * 512:(c + 1) * 512], start=True, stop=True),
                             r=[kt_, qt_], w=[pzf])
                        if j >= 0:
                            P.op('vector', lambda e: e.tensor_scalar(out=e32f[:], in0=pzf[:], scalar1=60.0,
                                                                    scalar2=None, op0=ALU.min), r=[pzf], w=[e32f])
                            P.op('scalar', lambda e: e.activation(out=w_[:], in_=e32f[:], func=AF.Exp),
                                 r=[e32f], w=[w_])
                            P.op('gpsimd', lambda e: e.tensor_tensor(out=w_[:], in0=w_[:], in1=mask_n[:, j, :],
                                                                    op=ALU.mult), r=[w_, mask_n], w=[w_])
                        else:
                            P.op('scalar', lambda e: e.activation(out=w_[:], in_=pzf[:], func=AF.Exp),
                                 r=[pzf], w=[w_])

                def stageB1(n, slot=slot, h=h):
                    kind = 'sb'
                    c, kb, first, last = its[n]
                    i = n % 2
                    j = kb - 4 * c
                    w_ = wt[kind][i]
                    if True:
                        lb = laccb[n % 2]
                        pb = pbs[i]

                        def mm2(e):
                            ins = e.matmul(pb[:], negtri[:], sp[i][:], start=True, stop=first)
                            if not first:
                                ins = e.matmul(pb[:], negones[:], lb[:], start=False, stop=True)
                            return ins
                        P.op('tensor', mm2, r=[sp[i], negtri, negones] + ([] if first else [lb]), w=[pb])
                        P.op('scalar', lambda e: e.activation(out=ec[i][:], in_=pb[:], func=AF.Exp), r=[pb], w=[ec[i]])
                        if j >= 0:
                            P.op('gpsimd', lambda e: e.tensor_tensor(out=ec[i][:], in0=ec[i][:], in1=mask_s[:, j, :],
                                                                    op=ALU.mult), r=[ec[i], mask_s], w=[ec[i]])
                        P.op('vector', lambda e: e.tensor_tensor(out=w_[:], in0=e32[i][:], in1=ec[i][:], op=ALU.mult),
                             r=[e32[i], ec[i]], w=[w_])

                def stageB2(kind, n, slot=slot, h=h):
                    c, kb, first, last = its[n]
                    i = n % 2
                    v_ = vv[kind][slot]
                    pc_ = po[kind]
                    w_ = wt[kind][i]
                    nr = 64 if kind == 'sb' else 65
                    P.op('tensor', lambda e: e.matmul(pc_[0:nr, :], v_[:, kb, 0:nr], w_[:], start=first, stop=last),
                         r=[v_, w_], w=[pc_])
                    if last:
                        if kind == 'sb':
                            self.head_norm_part1(nbufs[kind], pc_, 64)
                            pending.append(lambda: self.head_norm_part2(nbufs['sb'], 64, blk[0:64, 0:64],
                                                                        ogs[:, h:h + 1], 256 + 64 * h, c, EPS))
                        else:
                            self.head_norm_part1(nbufs[kind], pc_, 65)
                            pending.append(lambda: self.head_norm_part2(nbufs['fox'], 65, wn65[:, :],
                                                                        ogs[:, 6 + h:7 + h], 640 + 64 * h, c, 0.0))

                stageA('sb', 0)
                stageA('fox', 0)
                pending = []
                for n in range(len(its)):
                    if self.sparse and n % 13 == 6:
                        self.issue_casts(l, 1)
                    stageB1(n)
                    if n + 1 < len(its):
                        stageA('sb', n + 1)
                        stageA('fox', n + 1)
                    todo, pending[:] = list(pending), []
                    for f in todo:
                        f()
                    stageB2('fox', n)
                    stageB2('sb', n)
                for f in pending:
                    f()
            if self.sparse:
                self.issue_casts(l, 2 * NE)
            P.barrier()
            P.flush()

    def phase_wout(self, l, xsrc, xdst):
        P = self.P
        inp = self.inp
        with ExitStack() as st:
            wo = P.sbuf(st, 'o_w', [128, 8, D], BF16)
            wv = inp['w_out'].t.ap()[l].rearrange("(k p) n -> p k n", p=128)
            for k in range(8):
                P.op('gpsimd', lambda e, k=k: e.dma_start(out=wo[:, k, :], in_=wv[:, k, :]), w=[wo], dsem=wo)
            g1 = self.load_bcast(st, 'o_g1', self.modv, l * 6 * D + 2 * D)
            yt = [P.sbuf(st, 'o_y%d' % i, [128, 8, 512], BF16) for i in range(2)]
            xt = [P.sbuf(st, 'o_x%d' % i, [128, D], F32) for i in range(2)]
            xo = [P.sbuf(st, 'o_xo%d' % i, [128, D], F32) for i in range(2)]
            py = [P.psum(st, 'o_p%d' % i, [128, 512]) for i in range(4)]
            yv = self.yT.t.ap().rearrange("(k p) s -> p k s", p=128)
            for c in range(self.NC):
                yb = yt[c % 2]
                P.op('sync', lambda e, yb=yb, c=c: e.dma_start(out=yb[:], in_=yv[:, :, c * 512:(c + 1) * 512]),
                     r=[self.yT], w=[yb], dsem=yb)
                for tt in range(4):
                    t = c * 4 + tt
                    i = t % 2
                    src = xsrc.t.ap()[t * 128:(t + 1) * 128, :]
                    P.op('sync', lambda e, i=i, src=src: e.dma_start(out=xt[i][:], in_=src), r=[xsrc], w=[xt[i]],
                         dsem=xt[i])
                    for half in range(2):
                        pp = py[i * 2 + half]

                        def mm(e, yb=yb, tt=tt, half=half, pp=pp):
                            ins = None
                            for k in range(8):
                                ins = e.matmul(pp[:], yb[:, k, tt * 128:(tt + 1) * 128],
                                               wo[:, k, half * 512:(half + 1) * 512], start=(k == 0), stop=(k == 7))
                            return ins
                        P.op('tensor', mm, r=[yb, wo], w=[pp])
                        sl = slice(half * 512, (half + 1) * 512)
                        P.op('vector', lambda e, i=i, pp=pp, sl=sl: e.tensor_tensor(
                            out=xo[i][:, sl], in0=pp[:], in1=g1[:, sl], op=ALU.mult), r=[pp, g1], w=[xo[i]])
                    P.op('gpsimd', lambda e, i=i: e.tensor_tensor(out=xo[i][:], in0=xo[i][:], in1=xt[i][:], op=ALU.add),
                         r=[xo[i], xt[i]], w=[xo[i]])
                    dst = xdst.t.ap()[t * 128:(t + 1) * 128, :]
                    P.op('sync', lambda e, i=i, dst=dst: e.dma_start(out=dst, in_=xo[i][:]), r=[xo[i]], w=[xdst],
                         dsem=xo[i])
            P.barrier()
            P.flush()

    def load_expert(self, l, e_, w1b, w2b, b1c, b1s):
        P = self.P
        inp = self.inp
        w1v = inp['w_mlp1'].t.ap()[l, e_].rearrange("(k p) n -> p k n", p=128)
        w2v = inp['w_mlp2'].t.ap()[l, e_].rearrange("(p i) n -> p i n", i=8)
        for k in range(8):
            P.op('gpsimd', lambda e, k=k: e.dma_start(out=w1b[:, k, :], in_=w1v[:, k, :]), w=[w1b], dsem=w1b)
        for k in range(0, 8, 2):
            P.op('gpsimd', lambda e, k=k: e.dma_start(out=w2b[:, k:k + 2, :], in_=w2v[:, k:k + 2, :]),
                 w=[w2b], dsem=w2b)
        b1v = inp['b_mlp1'].t.ap()[l, e_].rearrange("(p m) -> p m", m=16)
        P.op('sync', lambda e: e.dma_start(out=b1c[:], in_=b1v), w=[b1c], dsem=b1c)

    def ffn_block(self, B, xT, w1b, w2b, b1c, b1s, evac, mid_hook=None):
        P = self.P
        aT = B['aT']
        CSIG = float(1.0 / (1.0 + np.exp(-1.702 * 7.0)))
        for i in range(8):
            q = B['it'] % 2
            B['it'] += 1
            pg, pl = B['pg'][q], B['pl'][q]
            sg, g, ll, tt = B['sg'][q], B['g'][q], B['l'][q], B['t'][q]

            def mm1(e, i=i, pg=pg, pl=pl):
                ins = None
                for (pp, j) in ((pg, 0), (pl, 1)):
                    for k in range(8):
                        ins = e.matmul(pp[:], w1b[:, k, 2 * i + j:2 * D:16], xT[:, k, :],
                                       start=(k == 0), stop=(k == 7))
                return ins
            P.op('tensor', mm1, r=[w1b, xT], w=[pg, pl])
            P.op('vector', lambda e, i=i, g=g, pg=pg: e.tensor_scalar(out=g[:], in0=pg[:], scalar1=b1c[:, 2 * i:2 * i + 1],
                                                                     scalar2=7.0, op0=ALU.add, op1=ALU.min),
                 r=[pg, b1c], w=[g])
            P.op('scalar', lambda e, sg=sg, g=g: e.activation(out=sg[:], in_=g[:], func=AF.Sigmoid, scale=1.702),
                 r=[g], w=[sg])
            P.op('vector', lambda e, i=i, ll=ll, pl=pl: e.tensor_scalar(out=ll[:], in0=pl[:], scalar1=b1c[:, 2 * i + 1:2 * i + 2],
                                                                       scalar2=7.0, op0=ALU.add, op1=ALU.min),
                 r=[pl, b1c], w=[ll])
            P.op('vector', lambda e, ll=ll: e.tensor_scalar(out=ll[:], in0=ll[:], scalar1=-7.0, scalar2=1.0,
                                                           op0=ALU.max, op1=ALU.add), r=[ll], w=[ll])
            P.op('gpsimd', lambda e, sg=sg, g=g, tt=tt: e.tensor_tensor(out=tt[:], in0=sg[:], in1=g[:], op=ALU.mult),
                 r=[sg, g], w=[tt])
            P.op('vector', lambda e, i=i, tt=tt, ll=ll: e.tensor_tensor(out=aT[:, i, :], in0=tt[:], in1=ll[:],
                                                                       op=ALU.mult), r=[tt, ll], w=[aT])
            if i == 4 and mid_hook is not None:
                mid_hook()
        for r_ in range(4):
            for half in range(2):
                pp = B['py'][B['ity'] % len(B['py'])]
                B['ity'] += 1

                def mm2(e, r_=r_, half=half, pp=pp):
                    ins = None
                    for k in range(8):
                        ins = e.matmul(pp[:], aT[:, k, r_ * 128:(r_ + 1) * 128],
                                       w2b[:, k, half * 512:(half + 1) * 512], start=(k == 0), stop=(k == 7))
                    return ins
                P.op('tensor', mm2, r=[aT, w2b], w=[pp])
                evac(r_, half, pp)

    def ffn_bufs(self, st, npy=4):
        P = self.P
        B = {'it': 0, 'ity': 0}
        B['aT'] = P.sbuf(st, 'f_aT', [128, 8, 512], BF16)
        B['pg'] = [P.psum(st, 'f_pg%d' % i, [128, 512]) for i in range(2)]
        B['pl'] = [P.psum(st, 'f_pl%d' % i, [128, 512]) for i in range(2)]
        B['py'] = [P.psum(st, 'f_py%d' % i, [128, 512]) for i in range(npy)]
        for nm in ('sg', 'g', 'l', 't'):
            B[nm] = [P.sbuf(st, 'f_%s%d' % (nm, i), [128, 512], F32) for i in range(2)]
        return B

    def phase_moe_dense(self, l, xsrc, xdst):
        P = self.P
        inp = self.inp
        NT = self.NT
        with ExitStack() as st:
            B = self.ffn_bufs(st)
            w1b = [P.sbuf(st, 'd_w1%d' % i, [128, 8, 2 * D], BF16) for i in range(2)]
            w2b = [P.sbuf(st, 'd_w2%d' % i, [128, 8, D], BF16) for i in range(2)]
            b1c = [P.sbuf(st, 'd_b1c%d' % i, [128, 16], F32) for i in range(2)]
            b1s = [P.sbuf(st, 'd_b1s%d' % i, [128, 8], F32) for i in range(2)]
            xT = [P.sbuf(st, 'd_xT%d' % i, [128, 8, 512], BF16) for i in range(2)]
            stage = [P.sbuf(st, 'd_st%d' % i, [128, D], F32) for i in range(4)]
            hv = self.h2T_d.t.ap().rearrange("(k p) s -> p k s", p=128)
            accd = self.accd
            with ExitStack() as s2:
                b2s = P.sbuf(s2, 'd_b2', [NE, D], F32)
                P.op('sync', lambda e: e.dma_start(out=b2s[:], in_=inp['b_mlp2'].t.ap()[l]), w=[b2s], dsem=b2s)
                cT = P.sbuf(s2, 'd_cT', [NE, 128], F32)
                identf = self.c['ident_f']
                pt = B['pg'][0]
                for t in range(NT):
                    P.op('tensor', lambda e, t=t: e.transpose(pt[0:NE, 0:128], self.comb[:, t, :], identf[:]),
                         r=[self.comb, identf], w=[pt])
                    P.op('scalar', lambda e: e.copy(out=cT[:], in_=pt[0:NE, 0:128]), r=[pt], w=[cT])
                    sg_ = stage[t % 4]
                    for half in range(2):
                        pp = B['py'][half]
                        P.op('tensor', lambda e, pp=pp, half=half: e.matmul(
                            pp[:], cT[:], b2s[:, half * 512:(half + 1) * 512], start=True, stop=True),
                            r=[cT, b2s], w=[pp])
                        P.op('vector', lambda e, pp=pp, half=half, sg_=sg_: e.tensor_copy(
                            out=sg_[:, half * 512:(half + 1) * 512], in_=pp[:]), r=[pp], w=[sg_])
                    dst = accd.t.ap()[t * 128:(t + 1) * 128, :]
                    P.op('gpsimd', lambda e, sg_=sg_, dst=dst: e.dma_start(out=dst, in_=sg_[:]), r=[sg_], w=[accd],
                         dsem=sg_)
            blk = 0
            for e_ in range(NE):
                ws = e_ % 2
                self.load_expert(l, e_, w1b[ws], w2b[ws], b1c[ws], b1s[ws])
                for c in range(self.NC):
                    xb = xT[blk % 2]
                    blk += 1
                    P.op('sync', lambda e, xb=xb, c=c: e.dma_start(out=xb[:], in_=hv[:, :, c * 512:(c + 1) * 512]),
                         r=[self.h2T_d], w=[xb], dsem=xb)

                    def evac(r_, half, pp, c=c, e_=e_):
                        t = c * 4 + r_
                        sg_ = stage[r_]
                        sl = slice(half * 512, (half + 1) * 512)
                        P.op('vector', lambda e: e.tensor_scalar(out=sg_[:, sl], in0=pp[:],
                                                                 scalar1=self.comb[:, t, e_:e_ + 1], scalar2=None,
                                                                 op0=ALU.mult), r=[pp, self.comb], w=[sg_])
                        if half == 1:
                            dst = accd.t.ap()[t * 128:(t + 1) * 128, :]
                            P.op('gpsimd', lambda e: e.dma_start(out=dst, in_=sg_[:], accum_op=ALU.add),
                                 r=[sg_], w=[accd], dsem=sg_)
                    self.ffn_block(B, xb, w1b[ws], w2b[ws], b1c[ws], b1s[ws], evac)
            P.barrier()
            P.flush()
        self.phase_final(l, xsrc, xdst)

    def phase_final(self, l, xsrc, xdst):
        P = self.P
        with ExitStack() as st:
            g2 = self.load_bcast(st, 'z_g2', self.modv, l * 6 * D + 5 * D)
            xt = [P.sbuf(st, 'z_x%d' % i, [128, D], F32) for i in range(2)]
            at = [P.sbuf(st, 'z_a%d' % i, [128, D], F32) for i in range(2)]
            for t in range(self.NT):
                i = t % 2
                rows = slice(t * 128, (t + 1) * 128)
                P.op('sync', lambda e, i=i, rows=rows: e.dma_start(out=xt[i][:], in_=xsrc.t.ap()[rows, :]),
                     r=[xsrc], w=[xt[i]], dsem=xt[i])
                P.op('sync', lambda e, i=i, rows=rows: e.dma_start(out=at[i][:], in_=self.accd.t.ap()[rows, :]),
                     r=[self.accd], w=[at[i]], dsem=at[i])
                P.op('vector', lambda e, i=i: e.tensor_tensor(out=at[i][:], in0=at[i][:], in1=g2[:], op=ALU.mult),
                     r=[at[i], g2], w=[at[i]])
                P.op('gpsimd', lambda e, i=i: e.tensor_tensor(out=at[i][:], in0=at[i][:], in1=xt[i][:], op=ALU.add),
                     r=[at[i], xt[i]], w=[at[i]])
                P.op('sync', lambda e, i=i, rows=rows: e.dma_start(out=xdst.t.ap()[rows, :], in_=at[i][:]),
                     r=[at[i]], w=[xdst], dsem=at[i])
            P.barrier()
            P.flush()


    def slots_and_scatter(self, st, l, h2b_all):
        P = self.P
        NT = self.NT
        NBLK = self.NBLK
        memb, comb = self.memb, self.comb
        onesf = self.c['ones']
        stri = self.c['stri_f']
        blkstart = self.c['blkstart']
        pcn = P.psum(st, 'q_pcn', [128, 512])
        cnt = P.sbuf(st, 'q_cnt', [128, NE], F32)
        nb = P.sbuf(st, 'q_nb', [128, NE], F32)
        incl = P.sbuf(st, 'q_incl', [128, NE], F32)
        off = P.sbuf(st, 'q_off', [128, NE], F32)
        texp = P.sbuf(st, 'q_texp', [128, NBLK], F32)

        def mmc(e):
            ins = None
            for t in range(NT):
                ins = e.matmul(pcn[:, 0:NE], onesf[:, 0:128], memb[:, t, :], start=(t == 0), stop=(t == NT - 1))
            return ins
        P.op('tensor', mmc, r=[onesf, memb], w=[pcn])
        P.op('vector', lambda e: e.tensor_copy(out=cnt[:], in_=pcn[:, 0:NE]), r=[pcn], w=[cnt])
        P.op('vector', lambda e: e.tensor_scalar(out=nb[:], in0=cnt[:], scalar1=0.0, scalar2=None, op0=ALU.is_gt),
             r=[cnt], w=[nb])
        for k in range(1, self.S // TS):
            P.op('vector', lambda e, k=k: e.scalar_tensor_tensor(out=nb[:], in0=cnt[:], scalar=float(k * TS),
                                                                in1=nb[:], op0=ALU.is_gt, op1=ALU.add),
                 r=[cnt, nb], w=[nb])
        P.op('vector', lambda e: e.tensor_scalar(out=nb[:], in0=nb[:], scalar1=float(TS), scalar2=None, op0=ALU.mult),
             r=[nb], w=[nb])
        P.op('vector', lambda e: e.tensor_tensor_scan(out=incl[:], data0=onesf[:, 0:NE], data1=nb[:], initial=0.0,
                                                      op0=ALU.mult, op1=ALU.add), r=[onesf, nb], w=[incl])
        P.op('vector', lambda e: e.tensor_tensor(out=off[:], in0=incl[:], in1=nb[:], op=ALU.subtract),
             r=[incl, nb], w=[off])
        P.op('vector', lambda e: e.tensor_scalar(out=texp[:], in0=blkstart[:, 0:NBLK], scalar1=incl[:, 0:1],
                                                 scalar2=None, op0=ALU.is_ge), r=[blkstart, incl], w=[texp])
        for e_ in range(1, NE):
            P.op('vector', lambda e, e_=e_: e.scalar_tensor_tensor(out=texp[:], in0=blkstart[:, 0:NBLK],
                                                                  scalar=incl[:, e_:e_ + 1], in1=texp[:],
                                                                  op0=ALU.is_ge, op1=ALU.add),
                 r=[blkstart, incl, texp], w=[texp])
        gar = P.sbuf(st, 'q_gar', [128, NBLK], F32)
        P.op('vector', lambda e: e.tensor_scalar(out=gar[:], in0=texp[:], scalar1=float(NE) - 0.5,
                                                 scalar2=self.c['pidx'][:, 2:3], op0=ALU.is_gt, op1=ALU.mult),
             r=[texp, self.c['pidx']], w=[gar])
        P.op('vector', lambda e: e.tensor_scalar(out=texp[:], in0=texp[:], scalar1=float(NE - 1), scalar2=None,
                                                 op0=ALU.min), r=[texp], w=[texp])
        pidx = self.c['pidx']
        t1 = P.sbuf(st, 'q_t1', [128, NBLK], F32)
        ixf = P.sbuf(st, 'q_ixf', [128, NBLK, 5], F32)
        BIG = float(1 << 15)
        P.op('vector', lambda e: e.tensor_scalar(out=t1[:], in0=texp[:], scalar1=128.0, scalar2=None,
                                                 op0=ALU.mult), r=[texp], w=[t1])
        P.op('vector', lambda e: e.scalar_tensor_tensor(out=t1[:], in0=gar[:], scalar=BIG, in1=t1[:], op0=ALU.mult,
                                                       op1=ALU.add), r=[gar, t1], w=[t1])
        P.op('vector', lambda e: e.tensor_scalar(out=ixf[:, :, 4], in0=t1[:], scalar1=pidx[:, 0:1], scalar2=None,
                                                 op0=ALU.add), r=[t1, pidx], w=[ixf])
        P.op('vector', lambda e: e.tensor_scalar(out=ixf[:, :, 1], in0=ixf[:, :, 4], scalar1=2.0, scalar2=None,
                                                 op0=ALU.mult), r=[ixf], w=[ixf])
        P.op('vector', lambda e: e.tensor_scalar(out=ixf[:, :, 2], in0=ixf[:, :, 4], scalar1=2.0, scalar2=1.0,
                                                 op0=ALU.mult, op1=ALU.add), r=[ixf], w=[ixf])
        P.op('vector', lambda e: e.tensor_scalar(out=ixf[:, :, 0], in0=ixf[:, :, 4], scalar1=float(l * NE * 128),
                                                 scalar2=None, op0=ALU.add), r=[ixf], w=[ixf])
        P.op('vector', lambda e: e.tensor_scalar(out=t1[:], in0=texp[:], scalar1=float(l * NE), scalar2=None,
                                                 op0=ALU.add), r=[texp], w=[t1])
        P.op('vector', lambda e: e.scalar_tensor_tensor(out=ixf[:, :, 3], in0=gar[:], scalar=BIG, in1=t1[:],
                                                       op0=ALU.mult, op1=ALU.add), r=[gar, t1], w=[ixf])
        widx = self.widx
        P.op('vector', lambda e: e.tensor_copy(out=widx[:], in_=ixf[:]), r=[ixf], w=[widx])
        macc = P.sbuf(st, 'q_macc', [128, NE], F32)
        pp = [P.psum(st, 'q_pp%d' % i, [128, 512]) for i in range(2)]
        tmp = P.sbuf(st, 'q_tmp', [128, NE], F32)
        val = P.sbuf(st, 'q_val', [128, NE], F32)
        oh = P.sbuf(st, 'q_oh', [128, NE], F32)
        t8 = P.sbuf(st, 'q_t8', [128, 8], F32)
        sl4, g4 = self.sl4, self.g4
        for t in range(NT):
            ppt = pp[t % 2]

            def mmp(e, t=t, ppt=ppt):
                ins = e.matmul(ppt[:, 0:NE], stri[:], memb[:, t, :], start=True, stop=(t == 0))
                if t > 0:
                    ins = e.matmul(ppt[:, 0:NE], onesf[:, 0:128], macc[:], start=False, stop=True)
                return ins
            P.op('tensor', mmp, r=[stri, memb, onesf] + ([macc] if t > 0 else []), w=[ppt])
            P.op('vector', lambda e, ppt=ppt: e.tensor_tensor(out=tmp[:], in0=ppt[:, 0:NE], in1=off[:], op=ALU.add),
                 r=[ppt, off], w=[tmp])
            P.op('vector', lambda e, t=t: e.scalar_tensor_tensor(out=val[:], in0=tmp[:], scalar=1.0, in1=memb[:, t, :],
                                                                op0=ALU.add, op1=ALU.mult), r=[tmp, memb], w=[val])
            P.op('vector', lambda e: e.max(out=t8[:], in_=val[:]), r=[val], w=[t8])
            P.op('vector', lambda e, t=t: e.tensor_scalar(out=sl4[:, t, :], in0=t8[:, 0:4], scalar1=-1.0, scalar2=None,
                                                         op0=ALU.add), r=[t8], w=[sl4])
            for j in range(4):
                P.op('vector', lambda e, j=j: e.tensor_scalar(out=oh[:], in0=val[:], scalar1=t8[:, j:j + 1],
                                                             scalar2=None, op0=ALU.is_equal), r=[val, t8], w=[oh])
                P.op('vector', lambda e, t=t: e.tensor_tensor(out=oh[:], in0=oh[:], in1=comb[:, t, :], op=ALU.mult),
                     r=[oh, comb], w=[oh])
                P.op('vector', lambda e, t=t, j=j: e.reduce_sum(out=g4[:, t, j:j + 1], in_=oh[:], axis=AX.X),
                     r=[oh], w=[g4])
            if t == 0:
                P.op('vector', lambda e: e.tensor_copy(out=macc[:], in_=memb[:, 0, :]), r=[memb], w=[macc])
            else:
                P.op('vector', lambda e, t=t: e.tensor_tensor(out=macc[:], in0=macc[:], in1=memb[:, t, :], op=ALU.add),
                     r=[macc, memb], w=[macc])
            Xs = self.Xs
            for j in range(4):
                P.op('gpsimd', lambda e, t=t, j=j: e.indirect_dma_start(
                    out=Xs.t.ap(), out_offset=bass.IndirectOffsetOnAxis(ap=sl4[:, t, j:j + 1], axis=0),
                    in_=h2b_all[:, t, :], in_offset=None), r=[sl4, h2b_all], w=[Xs], dsem=h2b_all)

    def phase_moe_sparse(self, l, xsrc, xdst):
        P = self.P
        inp = self.inp
        NT = self.NT
        widx = self.widx
        wc1, wc2 = self.wc1[l], self.wc2[l]
        w1rows = wc1.t.ap().rearrange("(e p h k) n -> (e p h) (k n)", p=128, h=2, k=4)
        w2rows = wc2.t.ap().rearrange("(e p i) n -> (e p) (i n)", p=128, i=8)
        b1rows = inp['b_mlp1'].t.ap().rearrange("l e (p m) -> (l e p) m", m=16)
        b2rows = inp['b_mlp2'].t.ap().rearrange("l e n -> (l e) n")
        with ExitStack() as st:
            B = self.ffn_bufs(st, npy=3)
            w1b = [P.sbuf(st, 'd_w1%d' % i, [128, 8, 2 * D], BF16) for i in range(2)]
            w2b = [P.sbuf(st, 'd_w2%d' % i, [128, 8, D], BF16) for i in range(2)]
            b1c = [P.sbuf(st, 'd_b1c%d' % i, [128, 16], F32) for i in range(2)]
            b2bc = [P.sbuf(st, 'd_b2%d' % i, [128, D], F32) for i in range(2)]
            xs = [P.sbuf(st, 'd_xs%d' % i, [128, D], BF16) for i in range(2)]
            xT = [P.sbuf(st, 'd_xT%d' % i, [128, 8, 512], BF16) for i in range(2)]
            stage = [P.sbuf(st, 'd_st%d' % i, [128, D], F32) for i in range(4)]
            ident = self.c['ident_bf']
            pTbuf = P.psum(st, 'pTb', [128, 8, 128], BF16)
            Xs, Ys = self.Xs, self.Ys
            rg = [self.nc.gpsimd.alloc_register('bc%d_%d' % (q, l)) for q in range(4)]
            rtile = P.sbuf(st, 'd_rt', [128, 1], F32)

            def setregs(e):
                e.reg_mov(rg[0], NE * 128 * 2 - 1)
                e.reg_mov(rg[1], NL * NE * 128 - 1)
                e.reg_mov(rg[2], NL * NE - 1)
                e.reg_mov(rg[3], NE * 128 - 1)
                return e.memset(rtile[:], 0.0)
            P.op('gpsimd', setregs, w=[rtile])
            nxs = [0]

            def gathers(i):
                ws = i % 2
                wa, wb_, bc, b2 = w1b[ws], w2b[ws], b1c[ws], b2bc[ws]
                for h in range(2):
                    P.op('gpsimd', lambda e, h=h: e.indirect_dma_start(
                        out=wa[:, 4 * h:4 * h + 4, :].rearrange("p k n -> p (k n)"), out_offset=None, in_=w1rows,
                        in_offset=bass.IndirectOffsetOnAxis(ap=widx[:, i, 1 + h:2 + h], axis=0),
                        bounds_check=rg[0], oob_is_err=False), r=[widx, wc1], w=[wa], dsem=wa)
                P.op('gpsimd', lambda e: e.indirect_dma_start(
                    out=wb_[:].rearrange("p k n -> p (k n)"), out_offset=None, in_=w2rows,
                    in_offset=bass.IndirectOffsetOnAxis(ap=widx[:, i, 4:5], axis=0),
                    bounds_check=rg[3], oob_is_err=False), r=[widx, wc2], w=[wb_], dsem=wb_)
                P.op('gpsimd', lambda e: e.indirect_dma_start(
                    out=bc[:], out_offset=None, in_=b1rows,
                    in_offset=bass.IndirectOffsetOnAxis(ap=widx[:, i, 0:1], axis=0),
                    bounds_check=rg[1], oob_is_err=False), r=[widx], w=[bc], dsem=bc)
                P.op('gpsimd', lambda e: e.indirect_dma_start(
                    out=b2[:], out_offset=None, in_=b2rows,
                    in_offset=bass.IndirectOffsetOnAxis(ap=widx[:, i, 3:4], axis=0),
                    bounds_check=rg[2], oob_is_err=False), r=[widx], w=[b2], dsem=b2)

            def prep_x(i):
                xb = xT[i % 2]
                for r_ in range(4):
                    xq = xs[nxs[0] % 2]
                    nxs[0] += 1
                    rows = slice(i * TS + r_ * 128, i * TS + (r_ + 1) * 128)
                    P.op('sync', lambda e, xq=xq, rows=rows: e.dma_start(out=xq[:], in_=Xs.t.ap()[rows, :]),
                         r=[Xs], w=[xq], dsem=xq)

                    def tr(e, xq=xq):
                        ins = None
                        for k in range(8):
                            ins = e.transpose(pTbuf[:, k, :], xq[:, k:D:8], ident[:])
                        return ins
                    P.op('tensor', tr, r=[xq, ident], w=[pTbuf])
                    P.op('scalar', lambda e, xb=xb, r_=r_: e.copy(out=xb[:, :, r_ * 128:(r_ + 1) * 128],
                                                                  in_=pTbuf[:]), r=[pTbuf], w=[xb])

            gathers(0)
            prep_x(0)
            for i in range(self.NBLK):
                ws = i % 2
                wa, wb_, bc, b2 = w1b[ws], w2b[ws], b1c[ws], b2bc[ws]
                xb = xT[i % 2]
                if i + 1 < self.NBLK:
                    gathers(i + 1)

                def evac(r_, half, pp, i=i, b2=b2):
                    sg_ = stage[r_]
                    sl = slice(half * 512, (half + 1) * 512)
                    P.op('vector', lambda e: e.tensor_tensor(out=sg_[:, sl], in0=pp[:], in1=b2[:, sl], op=ALU.add),
                         r=[pp, b2], w=[sg_])
                    if half == 1:
                        rows = slice(i * TS + r_ * 128, i * TS + (r_ + 1) * 128)
                        P.op('sync', lambda e: e.dma_start(out=Ys.t.ap()[rows, :], in_=sg_[:]),
                             r=[sg_], w=[Ys], dsem=sg_)
                hook = (lambda i=i: prep_x(i + 1)) if i + 1 < self.NBLK else None
                self.ffn_block(B, xb, wa, wb_, bc, None, evac, mid_hook=hook)
            P.barrier()
            P.flush()
        self.phase_combine(l, xsrc, xdst)

    def phase_combine(self, l, xsrc, xdst):
        P = self.P
        Ys = self.Ys
        sl4, g4 = self.sl4, self.g4
        with ExitStack() as st:
            g2 = self.load_bcast(st, 'z_g2', self.modv, l * 6 * D + 5 * D)
            xt = [P.sbuf(st, 'z_x%d' % i, [128, D], F32) for i in range(2)]
            yg = [[P.sbuf(st, 'z_y%d_%d' % (i, j), [128, D], F32) for j in range(4)] for i in range(2)]
            acc = [P.sbuf(st, 'z_a%d' % i, [128, D], F32) for i in range(2)]
            for t in range(self.NT):
                i = t % 2
                rows = slice(t * 128, (t + 1) * 128)
                P.op('sync', lambda e, i=i, rows=rows: e.dma_start(out=xt[i][:], in_=xsrc.t.ap()[rows, :]),
                     r=[xsrc], w=[xt[i]], dsem=xt[i])
                for j in range(4):
                    P.op('gpsimd', lambda e, i=i, j=j, t=t: e.indirect_dma_start(
                        out=yg[i][j][:], out_offset=None, in_=Ys.t.ap(),
                        in_offset=bass.IndirectOffsetOnAxis(ap=sl4[:, t, j:j + 1], axis=0)),
                        r=[Ys, sl4], w=[yg[i][j]], dsem=yg[i][j])
                P.op('vector', lambda e, i=i, t=t: e.tensor_scalar(out=acc[i][:], in0=yg[i][0][:],
                                                                  scalar1=g4[:, t, 0:1], scalar2=None, op0=ALU.mult),
                     r=[yg[i][0], g4], w=[acc[i]])
                for j in range(1, 4):
                    P.op('vector', lambda e, i=i, t=t, j=j: e.scalar_tensor_tensor(
                        out=acc[i][:], in0=yg[i][j][:], scalar=g4[:, t, j:j + 1], in1=acc[i][:], op0=ALU.mult,
                        op1=ALU.add), r=[yg[i][j], g4, acc[i]], w=[acc[i]])
                P.op('vector', lambda e, i=i: e.tensor_tensor(out=acc[i][:], in0=acc[i][:], in1=g2[:], op=ALU.mult),
                     r=[acc[i], g2], w=[acc[i]])
                P.op('gpsimd', lambda e, i=i: e.tensor_tensor(out=acc[i][:], in0=acc[i][:], in1=xt[i][:], op=ALU.add),
                     r=[acc[i], xt[i]], w=[acc[i]])
                P.op('sync', lambda e, i=i, rows=rows: e.dma_start(out=xdst.t.ap()[rows, :], in_=acc[i][:]),
                     r=[acc[i]], w=[xdst], dsem=acc[i])
            P.barrier()
            P.flush()

    def build(self):
        P = self.P
        self.load_consts()
        self.comb = P.sbuf(self.stack, 'comb', [128, self.NT, NE], F32)
        self.memb = P.sbuf(self.stack, 'memb', [128, self.NT, NE], F32)
        self.h2T_d = P.dram("h2T_d", [D, self.S], BF16, self.sk)
        self.accd = P.dram("accd", [self.S, D], F32, self.sk)
        self.comb_d = P.dram("comb_d", [128, self.NT, NE], F32, self.sk)
        self.NBLK = NE + 4 * self.S // TS
        self.sl4 = P.sbuf(self.stack, 'sl4', [128, self.NT, 4], I32)
        self.g4 = P.sbuf(self.stack, 'g4', [128, self.NT, 4], F32)
        self.widx = P.sbuf(self.stack, 'widx', [128, self.NBLK, 5], I32)
        self.Xs = P.dram("Xs", [self.NBLK * TS, D], BF16, self.sk)
        self.Ys = P.dram("Ys", [self.NBLK * TS, D], F32, self.sk)
        self.phase_ada()
        if self.sparse:
            self.precast_weights()
        xsrc = self.inp['x']
        for l in range(self.nlayers):
            last = (l == self.nlayers - 1)
            with ExitStack() as st:
                hT = P.sbuf(st, 'hT', [128, 8, self.S], BF16)
                self.phase_norm_T(st, l, xsrc, 0, 'norm1_g', hT)
                self.phase_proj(l, hT)
            self.phase_attn(l)
            self.phase_wout(l, xsrc, self.x1)
            xdst = self.out if last else self.x2
            if self.sparse:
                with ExitStack() as st:
                    h2b_all = P.sbuf(st, 'h2b_all', [128, self.NT, D], BF16)
                    self.phase_norm_T(None, l, self.x1, 1, 'norm2_g', None, router=True, h2b_all=h2b_all)
                self.phase_moe_sparse(l, self.x1, xdst)
            else:
                self.phase_norm_T(None, l, self.x1, 1, 'norm2_g', None, hT_dram=self.h2T_d, router=True)
                self.phase_moe_dense(l, self.x1, xdst)
            xsrc = xdst
        P.barrier()
        P.flush()
        self.stack.close()
        return self.nc


_CACHE = {}


def kernel(**inputs):
    S = inputs['x'].shape[1]
    nb = inputs['x'].shape[0]
    if S not in _CACHE:
        _CACHE[S] = K(S).build()
    nc = _CACHE[S]
    consts = make_consts()
    shared = {}
    for name, _ in INPUT_SPECS:
        if name in ('x', 'c'):
            continue
        shared[name] = np.ascontiguousarray(inputs[name], dtype=np.float32)
    for k, v in consts.items():
        shared['c_' + k] = v
    in_maps = []
    for b in range(nb):
        m = dict(shared)
        m['x'] = np.ascontiguousarray(inputs['x'][b], dtype=np.float32)
        m['c'] = np.ascontiguousarray(inputs['c'][b], dtype=np.float32)
        in_maps.append(m)
    res = run_bass_kernel_spmd(nc, in_maps, core_ids=list(range(nb)))
    return np.stack([np.asarray(r['out']) for r in res.results], 0).astype(np.float32)
```

```python
import numpy as np
import ml_dtypes
from contextlib import ExitStack
import concourse.bass as bass
import concourse.mybir as mybir
from concourse.bass_utils import run_bass_kernel_spmd

F32 = mybir.dt.float32
BF16 = mybir.dt.bfloat16
I32 = mybir.dt.int32
AF = mybir.ActivationFunctionType
ALU = mybir.AluOpType
AX = mybir.AxisListType

D = 1024
NL = 2
NE = 32
DIN = 3078
EPS = 1e-6
TS = 512
NBLK_MAX = 64
ENGS = ['sync', 'scalar', 'vector', 'gpsimd', 'tensor']


class Sem:
    def __init__(self, h):
        self.h = h
        self.v = 0
        self.nobarrier = False


class Buf:
    def __init__(self, t, name):
        self.t = t
        self.name = name
        self.w = {}
        self.r = {}
        self.dsem = None

    def __getitem__(self, idx):
        return self.t[idx]


class Prog:
    def __init__(self, nc, stack):
        self.nc = nc
        self.stack = stack
        self.q = {e: [] for e in ENGS}
        self.esem = {}
        self.allsems = []
        self.waited = {e: {} for e in ENGS}
        self.nsem = 0
        self.free_dsems = []
        self.phase_bufs = []
        for e in ['scalar', 'vector', 'gpsimd', 'tensor']:
            self.esem[e] = self.newsem('e_' + e)

    def newsem(self, name):
        h = self.stack.enter_context(self.nc.semaphore(name + '_%d' % self.nsem))
        self.nsem += 1
        s = Sem(h)
        self.allsems.append(s)
        return s

    def uname(self, name):
        self.nsem += 1
        return '%s_u%d' % (name, self.nsem)

    def sbuf(self, stack, name, shape, dt):
        name = self.uname(name)
        t = stack.enter_context(self.nc.sbuf_tensor(name, list(shape), dt))
        return Buf(t, name)

    def psum(self, stack, name, shape, dt=F32):
        name = self.uname(name)
        t = stack.enter_context(self.nc.psum_tensor(name, list(shape), dt))
        return Buf(t, name)

    def dram(self, name, shape, dt, kind="Internal"):
        t = self.nc.dram_tensor(name, list(shape), dt, kind=kind)
        return Buf(t, name)

    def op(self, eng, fn, r=(), w=(), dsem=None):
        waits = {}

        def addw(d):
            for s, v in d.items():
                if waits.get(s, 0) < v:
                    waits[s] = v
        for b in r:
            addw(b.w)
        for b in w:
            addw(b.w)
            addw(b.r)
        if dsem is not None:
            if dsem.dsem is None:
                dsem.dsem = self.free_dsems.pop() if self.free_dsems else self.newsem('d')
                self.phase_bufs.append(dsem)
            sem = dsem.dsem
            amt = 16
        else:
            sem = self.esem[eng]
            amt = 1
        wl = []
        for s, v in waits.items():
            if self.waited[eng].get(s, 0) >= v:
                continue
            if s not in self.esem.values():
                v = s.v
            if self.waited[eng].get(s, 0) < v:
                self.waited[eng][s] = v
                wl.append((s, v))
        sem.v += amt
        tok = (sem, sem.v)
        self.q[eng].append((fn, wl, sem, amt))
        for b in r:
            if b.r.get(sem, 0) < sem.v:
                b.r[sem] = sem.v
        for b in w:
            b.w = dict(b.w)
            b.w[sem] = sem.v
            b.r = {}
        return tok

    def barrier(self):
        for e in ENGS:
            wl = []
            for s in self.allsems:
                if s.nobarrier:
                    continue
                if s.v > 0 and self.waited[e].get(s, 0) < s.v:
                    self.waited[e][s] = s.v
                    wl.append((s, s.v))
            self.q[e].append((None, wl, None, 0))
        for b in self.phase_bufs:
            self.free_dsems.append(b.dsem)
            b.dsem = None
        self.phase_bufs = []

    def flush(self):
        with self.nc.Block() as block:
            for e in ENGS:
                items = self.q[e]

                def body(eng, items=items):
                    for fn, wl, sem, amt in items:
                        for s, v in wl:
                            eng.wait_ge(s.h, v)
                        if fn is not None:
                            ins = fn(eng)
                            ins.then_inc(sem.h, amt)
                getattr(block, e)(body)
        self.q = {e: [] for e in ENGS}


def bcast_ap(ap1d_tensor, offset, n, parts=128):
    return bass.AP(ap1d_tensor, offset, [[0, parts], [1, n]])


def make_consts():
    c = {}
    c['ident_bf'] = np.eye(128, dtype=np.float32).astype(ml_dtypes.bfloat16)
    c['ident_f'] = np.eye(128, dtype=np.float32)
    blk = np.zeros((128, 128), np.float32)
    blk[:64, :64] = 1.0 / 64
    blk[64:, 64:] = 1.0 / 64
    c['blk64'] = blk
    wn = np.full((65, 64), 1.0 / 64, np.float32)
    wn[64, :] = EPS
    c['wn65'] = wn
    j = np.arange(128)[:, None]
    k = np.arange(128)[None, :]
    c['negtri'] = np.where(j >= k, -1.0, 0.0).astype(np.float32).astype(ml_dtypes.bfloat16)
    c['negones'] = np.full((128, 128), -1.0, np.float32).astype(ml_dtypes.bfloat16)
    p = np.arange(128)[:, None]
    col = np.arange(512)[None, :]
    ms = np.zeros((4, 128, 512), np.float32)
    mn = np.zeros((4, 128, 512), np.float32)
    for jj in range(4):
        bc = col // 128
        ms[jj] = np.where(bc < jj, 0.0, np.where(bc == jj, (p < (col % 128)), 1.0))
        mn[jj] = np.where(bc < jj, 0.0, np.where(bc == jj, (p <= (col % 128)), 1.0))
    c['stri_f'] = (j < k).astype(np.float32)
    c['pidx'] = np.stack([np.arange(128), 8 * np.arange(128), np.minimum(np.arange(128), 1)],
                         1).astype(np.float32)
    c['blkstart'] = np.broadcast_to((np.arange(NBLK_MAX, dtype=np.float32) * TS)[None, :], (128, NBLK_MAX)).copy()
    c['mask_s'] = ms.transpose(1, 0, 2).copy().astype(ml_dtypes.bfloat16)
    c['mask_n'] = mn.transpose(1, 0, 2).copy().astype(ml_dtypes.bfloat16)
    return c


CONST_SPECS = [('ident_bf', [128, 128], BF16), ('ident_f', [128, 128], F32), ('blk64', [128, 128], F32),
               ('wn65', [65, 64], F32), ('negtri', [128, 128], BF16), ('negones', [128, 128], BF16),
               ('mask_s', [128, 4, 512], BF16), ('mask_n', [128, 4, 512], BF16),
               ('stri_f', [128, 128], F32), ('blkstart', [128, NBLK_MAX], F32), ('pidx', [128, 3], F32)]

INPUT_SPECS = [
    ('x', lambda S: [S, D]), ('c', lambda S: [D]), ('norm1_g', lambda S: [NL, D]),
    ('w_ada', lambda S: [NL, D, 6 * D]), ('b_ada', lambda S: [NL, 6 * D]), ('w_in', lambda S: [NL, D, DIN]),
    ('conv_w', lambda S: [NL, 3, 256]), ('sb_q_g', lambda S: [NL, 64]), ('sb_k_g', lambda S: [NL, 64]),
    ('fox_q_g', lambda S: [NL, 64]), ('fox_k_g', lambda S: [NL, 64]), ('fox_f_b', lambda S: [NL, 6]),
    ('out_norm_g', lambda S: [NL, D]), ('w_out', lambda S: [NL, D, D]), ('norm2_g', lambda S: [NL, D]),
    ('router_w', lambda S: [NL, D, NE]), ('router_b', lambda S: [NL, NE]),
    ('w_mlp1', lambda S: [NL, NE, D, 2 * D]), ('b_mlp1', lambda S: [NL, NE, 2 * D]),
    ('w_mlp2', lambda S: [NL, NE, D, D]), ('b_mlp2', lambda S: [NL, NE, D]),
]


class K:
    def __init__(self, S, nlayers=NL, debug=False, stop_after=None, sparse=True):
        self.sparse = sparse
        self.S = S
        self.NT = S // 128
        self.NC = S // 512
        self.debug = debug
        self.nlayers = nlayers
        self.stop_after = stop_after
        nc = bass.Bass("TRN2", target_bir_lowering=False)
        self.nc = nc
        self.stack = ExitStack()
        self.P = Prog(nc, self.stack)
        P = self.P
        self.inp = {}
        for name, shp in INPUT_SPECS:
            self.inp[name] = Buf(nc.dram_tensor(name, shp(S), F32, kind="ExternalInput"), name)
        self.cst = {}
        for name, shp, dt in CONST_SPECS:
            self.cst[name] = Buf(nc.dram_tensor('c_' + name, shp, dt, kind="ExternalInput"), name)
        self.out = Buf(nc.dram_tensor("out", [S, D], F32, kind="ExternalOutput"), "out")
        sk = "ExternalOutput" if debug else "Internal"
        self.sk = sk
        self.modv = P.dram("modv", [NL, 6 * D], F32, sk)
        self.qT_sb = P.dram("qT_sb", [384, S], BF16, sk)
        self.kT_sb = P.dram("kT_sb", [384, S], BF16, sk)
        self.v_sb = P.dram("v_sb", [S, 384], BF16, sk)
        self.qTa = P.dram("qTa", [6, 70, S], BF16, sk)
        self.kTa = P.dram("kTa", [6, 70, S], BF16, sk)
        self.v_fox = P.dram("v_fox", [S, 384], BF16, sk)
        self.yT = P.dram("yT", [D, S], BF16, sk)
        self.x1 = P.dram("x1", [S, D], F32, sk)
        self.x2 = P.dram("x2", [S, D], F32, sk)

    def load_consts(self):
        P = self.P
        st = self.stack
        self.c = {}
        for name, shp, dt in CONST_SPECS:
            b = P.sbuf(st, 'k_' + name, shp, dt)
            src = self.cst[name]
            P.op('sync', lambda e, b=b, src=src: e.dma_start(out=b[:], in_=src.t.ap()), r=[src], w=[b], dsem=b)
            self.c[name] = b
        ones = P.sbuf(st, 'k_ones', [128, 512], F32)
        P.op('vector', lambda e: e.memset(ones[:], 1.0), w=[ones])
        self.c['ones'] = ones
        onesb = P.sbuf(st, 'k_onesb', [128, 512], BF16)
        P.op('vector', lambda e: e.memset(onesb[:], 1.0), w=[onesb])
        self.c['onesb'] = onesb


    def precast_weights(self):
        P = self.P
        inp = self.inp
        self.wc1, self.wc2 = [], []
        for l in range(self.nlayers):
            b1 = P.dram("wc1_%d" % l, [NE * D, 2 * D], BF16)
            b2 = P.dram("wc2_%d" % l, [NE * D, D], BF16)
            for b in (b1, b2):
                b.dsem = P.newsem('wc')
                b.dsem.nobarrier = True
            self.wc1.append(b1)
            self.wc2.append(b2)
        self.pending_casts = [[] for _ in range(self.nlayers)]
        for l in range(self.nlayers):
            for e_ in range(NE):
                src1 = inp['w_mlp1'].t.ap()[l, e_].rearrange("(a b) n -> a (b n)", b=4)
                dst1 = self.wc1[l].t.ap()[e_ * D:(e_ + 1) * D, :].rearrange("(a b) n -> a (b n)", b=4)
                self.pending_casts[l].append((src1, dst1, self.wc1[l]))
                src2 = inp['w_mlp2'].t.ap()[l, e_].rearrange("(a b) n -> a (b n)", b=8)
                dst2 = self.wc2[l].t.ap()[e_ * D:(e_ + 1) * D, :].rearrange("(a b) n -> a (b n)", b=8)
                self.pending_casts[l].append((src2, dst2, self.wc2[l]))

    def issue_casts(self, l, n):
        P = self.P
        for _ in range(n):
            if not self.pending_casts[l]:
                return
            src, dst, buf = self.pending_casts[l].pop(0)
            P.op('gpsimd', lambda e, src=src, dst=dst: e.dma_start(out=dst, in_=src), w=[buf], dsem=buf)

    def phase_ada(self):
        P = self.P
        inp = self.inp
        with ExitStack() as st:
            cT = P.sbuf(st, 'a_cT', [128, 8], F32)
            sc = P.sbuf(st, 'a_sc', [128, 8], F32)
            wb = [P.sbuf(st, 'a_w%d' % i, [128, 3072], F32) for i in range(2)]
            bada = P.sbuf(st, 'a_b', [1, 6 * D], F32)
            modrow = P.sbuf(st, 'a_mod', [1, 6 * D], F32)
            ps = [P.psum(st, 'a_ps%d' % i, [128, 512]) for i in range(6)]
            cap = inp['c'].t.ap().rearrange("(p j) -> p j", j=8)
            P.op('sync', lambda e: e.dma_start(out=cT[:], in_=cap), w=[cT], dsem=cT)
            P.op('scalar', lambda e: e.activation(out=sc[:], in_=cT[:], func=AF.Silu), r=[cT], w=[sc])
            it = 0
            for l in range(self.nlayers):
                bsrc = inp['b_ada'].t.ap()[l:l + 1, :]
                P.op('sync', lambda e, bsrc=bsrc: e.dma_start(out=bada[:], in_=bsrc), w=[bada], dsem=bada)
                wv = inp['w_ada'].t.ap()[l].rearrange("(p j) n -> p j n", j=8)
                for half in range(2):
                    for j in range(8):
                        b = wb[it % 2]
                        it += 1
                        src = wv[:, j, half * 3072:(half + 1) * 3072]
                        P.op('sync' if it % 2 else 'gpsimd',
                             lambda e, b=b, src=src: e.dma_start(out=b[:], in_=src), w=[b], dsem=b)

                        def mm(e, b=b, j=j):
                            ins = None
                            for n in range(6):
                                ins = e.matmul(ps[n][0:1, :], sc[:, j:j + 1], b[:, n * 512:(n + 1) * 512],
                                               start=(j == 0), stop=(j == 7))
                            return ins
                        P.op('tensor', mm, r=[b, sc], w=ps)
                    for n in range(6):
                        o = half * 3072 + n * 512
                        P.op('vector', lambda e, n=n, o=o: e.tensor_tensor(
                            out=modrow[0:1, o:o + 512], in0=ps[n][0:1, :], in1=bada[0:1, o:o + 512], op=ALU.add),
                            r=[ps[n], bada], w=[modrow])
                dst = self.modv.t.ap()[l:l + 1, :]
                P.op('sync', lambda e, dst=dst: e.dma_start(out=dst, in_=modrow[:]), r=[modrow], w=[self.modv],
                     dsem=modrow)
            P.barrier()
            P.flush()

    def load_bcast(self, st, name, srcbuf, offset, n=D, eng='sync'):
        P = self.P
        b = P.sbuf(st, name, [128, n], F32)
        ap = bcast_ap(srcbuf.t, offset, n)
        P.op(eng, lambda e: e.dma_start(out=b[:], in_=ap), r=[srcbuf], w=[b], dsem=b)
        return b

    def mod_tiles(self, st, l, which, gname):
        P = self.P
        gb = self.load_bcast(st, 'm_g', self.inp[gname], l * D)
        sb = self.load_bcast(st, 'm_s', self.modv, l * 6 * D + (3 * which + 1) * D, eng='gpsimd')
        tb = self.load_bcast(st, 'm_t', self.modv, l * 6 * D + (3 * which + 0) * D)
        P.op('vector', lambda e: e.scalar_tensor_tensor(out=gb[:], in0=sb[:], scalar=1.0, in1=gb[:],
                                                       op0=ALU.add, op1=ALU.mult), r=[sb, gb], w=[gb])
        return gb, tb

    def rstd_from_ssq(self, ssq, lnv, rstd, n):
        P = self.P
        P.op('scalar', lambda e: e.activation(out=lnv[:], in_=ssq[:], func=AF.Ln, bias=EPS, scale=1.0 / n),
             r=[ssq], w=[lnv])
        P.op('scalar', lambda e: e.activation(out=rstd[:], in_=lnv[:], func=AF.Exp, scale=-0.5),
             r=[lnv], w=[rstd])

    def phase_norm_T(self, st, l, xsrc, which, gname, hT, hT_dram=None, router=False, h2b_all=None):
        P = self.P
        inp = self.inp
        with ExitStack() as s2:
            if hT_dram is not None:
                stg = [P.sbuf(s2, 'n_stg%d' % i, [128, 8, 512], BF16) for i in range(2)]
            if router:
                rw = P.sbuf(s2, 'n_rw', [128, 8, NE], F32)
                src_rw = inp['router_w'].t.ap()[l].rearrange("(k p) n -> p k n", p=128)
                P.op('sync', lambda e: e.dma_start(out=rw[:], in_=src_rw), w=[rw], dsem=rw)
                rb = self.load_bcast(s2, 'n_rb', inp['router_b'], l * NE, n=NE)
                h32 = [P.sbuf(s2, 'n_h32%d' % i, [128, D], F32) for i in range(2)]
                h32T_ = [P.sbuf(s2, 'n_h32T%d' % i, [128, 8, 128], F32) for i in range(2)]
                pR = P.psum(s2, 'n_pR', [128, 8, 128], F32)
                pL_ = [P.psum(s2, 'n_pL%d' % i, [128, 512], F32) for i in range(2)]
                lg_ = [P.sbuf(s2, 'n_lg%d' % i, [128, NE], F32) for i in range(2)]
                ex_ = [P.sbuf(s2, 'n_ex%d' % i, [128, NE], F32) for i in range(2)]
                t8_ = [P.sbuf(s2, 'n_t8%d' % i, [128, 8], F32) for i in range(2)]
                nmx_ = [P.sbuf(s2, 'n_nmx%d' % i, [128, 1], F32) for i in range(2)]
                ssm_ = [P.sbuf(s2, 'n_ssm%d' % i, [128, 1], F32) for i in range(2)]
                identf = self.c['ident_f']
            G, T = self.mod_tiles(s2, l, which, gname)
            xt = [P.sbuf(s2, 'n_x%d' % i, [128, D], F32) for i in range(2)]
            junk = P.sbuf(s2, 'n_junk', [128, D], BF16)
            hn = [P.sbuf(s2, 'n_hn%d' % i, [128, D], F32) for i in range(2)]
            hb = [P.sbuf(s2, 'n_hb%d' % i, [128, D], BF16) for i in range(2)]
            ssq = [P.sbuf(s2, 'n_ssq%d' % i, [128, 1], F32) for i in range(2)]
            lnv = [P.sbuf(s2, 'n_ln%d' % i, [128, 1], F32) for i in range(2)]
            rstd = [P.sbuf(s2, 'n_rs%d' % i, [128, 1], F32) for i in range(2)]
            if h2b_all is None:
                pT = [P.psum(s2, 'n_pT%d' % i, [128, 8, 128], BF16) for i in range(2)]
            ident = self.c['ident_bf']
            for t in range(self.NT):
                i = t % 2
                src = xsrc.t.ap()[t * 128:(t + 1) * 128, :]
                P.op('sync', lambda e, i=i, src=src: e.dma_start(out=xt[i][:], in_=src), r=[xsrc], w=[xt[i]],
                     dsem=xt[i])
                P.op('scalar', lambda e, i=i: e.activation(out=junk[:], in_=xt[i][:], func=AF.Square,
                                                          accum_out=ssq[i][:]), r=[xt[i]], w=[junk, ssq[i]])
                self.rstd_from_ssq(ssq[i], lnv[i], rstd[i], D)
                P.op('vector', lambda e, i=i: e.scalar_tensor_tensor(
                    out=hn[i][:], in0=xt[i][:], scalar=rstd[i][:, 0:1], in1=G[:], op0=ALU.mult, op1=ALU.mult),
                    r=[xt[i], rstd[i], G], w=[hn[i]])
                if not router:
                    P.op('gpsimd', lambda e, i=i: e.tensor_tensor(out=hb[i][:], in0=hn[i][:], in1=T[:], op=ALU.add),
                         r=[hn[i], T], w=[hb[i]])
                else:
                    def _router(t, i, h32T, pL, lg, ex, t8, nmx, ssm):
                        P.op('gpsimd', lambda e, i=i: e.tensor_tensor(out=h32[i][:], in0=hn[i][:], in1=T[:], op=ALU.add),
                             r=[hn[i], T], w=[h32[i]])
                        if h2b_all is None:
                            P.op('scalar', lambda e, i=i: e.copy(out=hb[i][:], in_=h32[i][:]), r=[h32[i]], w=[hb[i]])
                        else:
                            P.op('scalar', lambda e, i=i, t=t: e.copy(out=h2b_all[:, t, :], in_=h32[i][:]),
                                 r=[h32[i]], w=[h2b_all])

                        def trf(e, i=i):
                            ins = None
                            for k in range(8):
                                ins = e.transpose(pR[:, k, :], h32[i][:, k * 128:(k + 1) * 128], identf[:])
                            return ins
                        P.op('tensor', trf, r=[h32[i], identf], w=[pR])
                        P.op('scalar', lambda e: e.copy(out=h32T[:], in_=pR[:]), r=[pR], w=[h32T])

                        def mmr(e):
                            ins = None
                            for k in range(8):
                                ins = e.matmul(pL[:, 0:NE], h32T[:, k, :], rw[:, k, :], start=(k == 0), stop=(k == 7))
                            return ins
                        P.op('tensor', mmr, r=[h32T, rw], w=[pL])
                        P.op('vector', lambda e: e.tensor_tensor(out=lg[:], in0=pL[:, 0:NE], in1=rb[:], op=ALU.add),
                             r=[pL, rb], w=[lg])
                        P.op('vector', lambda e: e.max(out=t8[:], in_=lg[:]), r=[lg], w=[t8])
                        mb = self.memb
                        cb = self.comb
                        P.op('vector', lambda e, t=t: e.tensor_scalar(out=mb[:, t, :], in0=lg[:], scalar1=t8[:, 3:4],
                                                                     scalar2=None, op0=ALU.is_ge), r=[lg, t8], w=[mb])
                        P.op('vector', lambda e: e.tensor_scalar(out=nmx[:], in0=t8[:, 0:1], scalar1=-1.0, scalar2=None,
                                                                 op0=ALU.mult), r=[t8], w=[nmx])
                        P.op('scalar', lambda e: e.activation(out=ex[:], in_=lg[:], func=AF.Exp, bias=nmx[:, 0:1]),
                             r=[lg, nmx], w=[ex])
                        P.op('vector', lambda e, t=t: e.tensor_tensor(out=ex[:], in0=ex[:], in1=mb[:, t, :], op=ALU.mult),
                             r=[ex, mb], w=[ex])
                        P.op('vector', lambda e: e.reduce_sum(out=ssm[:], in_=ex[:], axis=AX.X), r=[ex], w=[ssm])
                        P.op('vector', lambda e: e.reciprocal(out=ssm[:], in_=ssm[:]), r=[ssm], w=[ssm])
                        P.op('vector', lambda e, t=t: e.tensor_scalar(out=cb[:, t, :], in0=ex[:], scalar1=ssm[:, 0:1],
                                                                     scalar2=None, op0=ALU.mult), r=[ex, ssm], w=[cb])
                    _router(t, i, h32T_[i], pL_[i], lg_[i], ex_[i], t8_[i], nmx_[i], ssm_[i])
                if h2b_all is not None:
                    continue

                def tr(e, i=i):
                    ins = None
                    for k in range(8):
                        ins = e.transpose(pT[i][:, k, :], hb[i][:, k * 128:(k + 1) * 128], ident[:])
                    return ins
                P.op('tensor', tr, r=[hb[i], ident], w=[pT[i]])
                if hT_dram is None:
                    P.op('vector', lambda e, i=i, t=t: e.tensor_copy(out=hT[:, :, t * 128:(t + 1) * 128],
                                                                    in_=pT[i][:]), r=[pT[i]], w=[hT])
                else:
                    sg_ = stg[(t // 4) % 2]
                    tt = t % 4
                    P.op('vector', lambda e, i=i, tt=tt, sg_=sg_: e.tensor_copy(
                        out=sg_[:, :, tt * 128:(tt + 1) * 128], in_=pT[i][:]), r=[pT[i]], w=[sg_])
                    if tt == 3:
                        cc_ = t // 4
                        d_ap = hT_dram.t.ap().rearrange("(k p) s -> p k s", p=128)[:, :, cc_ * 512:(cc_ + 1) * 512]
                        P.op('sync', lambda e, sg_=sg_, d_ap=d_ap: e.dma_start(out=d_ap, in_=sg_[:]),
                             r=[sg_], w=[hT_dram], dsem=sg_)
            if h2b_all is not None:
                self.slots_and_scatter(s2, l, h2b_all)
            P.barrier()
            P.flush()

    def phase_proj(self, l, hT):
        P = self.P
        S = self.S
        inp = self.inp
        with ExitStack() as st:
            wbf = P.sbuf(st, 'p_w', [128, 8, DIN], BF16)
            wv = inp['w_in'].t.ap()[l].rearrange("(k p) n -> p k n", p=128)
            for k in range(8):
                P.op('gpsimd', lambda e, k=k: e.dma_start(out=wbf[:, k, :], in_=wv[:, k, :]), w=[wbf], dsem=wbf)
            gcol = {}
            for nm in ['sb_q_g', 'sb_k_g', 'fox_q_g', 'fox_k_g']:
                g = P.sbuf(st, 'p_' + nm, [128, 1], F32)
                for hh in range(2):
                    src = inp[nm].t.ap()[l].rearrange("(d o) -> d o", o=1)
                    P.op('sync', lambda e, g=g, hh=hh, src=src: e.dma_start(out=g[hh * 64:(hh + 1) * 64, :], in_=src),
                         w=[g], dsem=g)
                if nm.endswith('q_g'):
                    P.op('vector', lambda e, g=g: e.tensor_scalar(out=g[:], in0=g[:], scalar1=0.125, scalar2=None,
                                                                 op0=ALU.mult), r=[g], w=[g])
                gcol[nm] = g
            cw = P.sbuf(st, 'p_cw', [128, 2, 3], F32)
            for j in range(2):
                for i in range(3):
                    src = inp['conv_w'].t.ap()[l, i, j * 128:(j + 1) * 128].rearrange("(d o) -> d o", o=1)
                    P.op('sync', lambda e, j=j, i=i, src=src: e.dma_start(out=cw[:, j, i:i + 1], in_=src),
                         w=[cw], dsem=cw)
            og = P.sbuf(st, 'p_og', [128, 8], F32)
            src_og = inp['out_norm_g'].t.ap()[l].rearrange("(k p) -> p k", p=128)
            P.op('sync', lambda e: e.dma_start(out=og[:], in_=src_og, allow_slow_non_contiguous=True), w=[og], dsem=og)
            self.og_keep = None
            fb = P.sbuf(st, 'p_fb', [6, 1], F32)
            src_fb = inp['fox_f_b'].t.ap()[l].rearrange("(d o) -> d o", o=1)
            P.op('sync', lambda e: e.dma_start(out=fb[:], in_=src_fb), w=[fb], dsem=fb)
            P.op('vector', lambda e: e.tensor_scalar(out=fb[:], in0=fb[:], scalar1=-1.0, scalar2=None, op0=ALU.mult),
                 r=[fb], w=[fb])

            blk = self.c['blk64']
            pq = [P.psum(st, 'p_pq%d' % i, [128, 512]) for i in range(2)]
            pm = [P.psum(st, 'p_pm%d' % i, [128, 512]) for i in range(2)]
            sq = [P.sbuf(st, 'p_sq%d' % i, [128, 512], F32) for i in range(2)]
            rs = [P.sbuf(st, 'p_rs%d' % i, [128, 512], F32) for i in range(2)]
            qo = [P.sbuf(st, 'p_qo%d' % i, [128, 512], BF16) for i in range(2)]
            it = 0

            def proj_mm(ps, c0, ncols, n):
                def mm(e):
                    ins = None
                    for k in range(8):
                        ins = e.matmul(ps[0:ncols, :], wbf[:, k, c0:c0 + ncols], hT[:, k, n * 512:(n + 1) * 512],
                                       start=(k == 0), stop=(k == 7))
                    return ins
                P.op('tensor', mm, r=[wbf, hT], w=[ps])

            qk_tiles = []
            for m in range(3):
                qk_tiles.append((768 + m * 128, 'sb_q_g', self.qT_sb, m * 128, None))
                qk_tiles.append((1152 + m * 128, 'sb_k_g', self.kT_sb, m * 128, None))
                qk_tiles.append((1920 + m * 128, 'fox_q_g', self.qTa, None, 2 * m))
                qk_tiles.append((2304 + m * 128, 'fox_k_g', self.kTa, None, 2 * m))
            for (c0, gname, dst, row0, fh) in qk_tiles:
                for n in range(self.NC):
                    i = it % 2
                    it += 1
                    proj_mm(pq[i], c0, 128, n)
                    P.op('scalar', lambda e, i=i: e.activation(out=sq[i][:], in_=pq[i][:], func=AF.Square),
                         r=[pq[i]], w=[sq[i]])
                    P.op('tensor', lambda e, i=i: e.matmul(pm[i][:], blk[:], sq[i][:], start=True, stop=True),
                         r=[blk, sq[i]], w=[pm[i]])
                    P.op('scalar', lambda e, i=i: e.activation(out=rs[i][:], in_=pm[i][:], func=AF.Ln, bias=EPS),
                         r=[pm[i]], w=[rs[i]])
                    P.op('scalar', lambda e, i=i: e.activation(out=rs[i][:], in_=rs[i][:], func=AF.Exp, scale=-0.5),
                         r=[rs[i]], w=[rs[i]])
                    g = gcol[gname]
                    P.op('vector', lambda e, i=i, g=g: e.scalar_tensor_tensor(
                        out=qo[i][:], in0=pq[i][:], scalar=g[:, 0:1], in1=rs[i][:], op0=ALU.mult, op1=ALU.mult),
                        r=[pq[i], g, rs[i]], w=[qo[i]])
                    if row0 is not None:
                        d_ap = dst.t.ap()[row0:row0 + 128, n * 512:(n + 1) * 512]
                        P.op('sync', lambda e, i=i, d_ap=d_ap: e.dma_start(out=d_ap, in_=qo[i][:]),
                             r=[qo[i]], w=[dst], dsem=qo[i])
                    else:
                        for hh in range(2):
                            d_ap = dst.t.ap()[fh + hh, 0:64, n * 512:(n + 1) * 512]
                            P.op('sync', lambda e, i=i, hh=hh, d_ap=d_ap: e.dma_start(
                                out=d_ap, in_=qo[i][hh * 64:(hh + 1) * 64, :]), r=[qo[i]], w=[dst], dsem=qo[i])

            pc = [P.psum(st, 'p_pc%d' % i, [128, 512]) for i in range(3)]
            vb = P.sbuf(st, 'p_vb', [128, 514], F32)
            ccs = P.sbuf(st, 'p_ccs', [128, 512], F32)
            yc = P.sbuf(st, 'p_yc', [128, 512], F32)
            for j in range(2):
                P.op('vector', lambda e: e.memset(vb[:, 0:2], 0.0), w=[vb])
                for n in range(self.NC):
                    i = it % 2
                    it += 1
                    for q3 in range(3):
                        proj_mm(pc[q3], q3 * 256 + j * 128, 128, n)
                    if n > 0:
                        P.op('vector', lambda e: e.tensor_copy(out=vb[:, 0:2], in_=vb[:, 512:514]), r=[vb], w=[vb])
                    P.op('scalar', lambda e: e.copy(out=ccs[:], in_=pc[1][:]), r=[pc[1]], w=[ccs])
                    P.op('vector', lambda e: e.tensor_tensor(out=vb[:, 2:514], in0=pc[2][:], in1=ccs[:], op=ALU.mult),
                         r=[pc[2], ccs], w=[vb])
                    P.op('vector', lambda e, j=j: e.tensor_scalar(out=yc[:], in0=vb[:, 0:512], scalar1=cw[:, j, 0:1],
                                                                 scalar2=None, op0=ALU.mult), r=[vb, cw], w=[yc])
                    P.op('vector', lambda e, j=j: e.scalar_tensor_tensor(
                        out=yc[:], in0=vb[:, 1:513], scalar=cw[:, j, 1:2], in1=yc[:], op0=ALU.mult, op1=ALU.add),
                        r=[vb, cw, yc], w=[yc])
                    P.op('vector', lambda e, j=j: e.scalar_tensor_tensor(
                        out=yc[:], in0=vb[:, 2:514], scalar=cw[:, j, 2:3], in1=yc[:], op0=ALU.mult, op1=ALU.add),
                        r=[vb, cw, yc], w=[yc])
                    P.op('vector', lambda e: e.tensor_tensor(out=yc[:], in0=pc[0][:], in1=yc[:], op=ALU.mult),
                         r=[pc[0], yc], w=[yc])
                    P.op('scalar', lambda e, i=i: e.activation(out=sq[i][:], in_=yc[:], func=AF.Square),
                         r=[yc], w=[sq[i]])
                    P.op('tensor', lambda e, i=i: e.matmul(pm[i][:], blk[:], sq[i][:], start=True, stop=True),
                         r=[blk, sq[i]], w=[pm[i]])
                    P.op('scalar', lambda e, i=i: e.activation(out=rs[i][:], in_=pm[i][:], func=AF.Ln, bias=EPS),
                         r=[pm[i]], w=[rs[i]])
                    P.op('scalar', lambda e, i=i: e.activation(out=rs[i][:], in_=rs[i][:], func=AF.Exp, scale=-0.5),
                         r=[rs[i]], w=[rs[i]])
                    P.op('vector', lambda e, i=i, j=j: e.scalar_tensor_tensor(
                        out=qo[i][:], in0=yc[:], scalar=og[:, j:j + 1], in1=rs[i][:], op0=ALU.mult, op1=ALU.mult),
                        r=[yc, og, rs[i]], w=[qo[i]])
                    d_ap = self.yT.t.ap()[j * 128:(j + 1) * 128, n * 512:(n + 1) * 512]
                    P.op('sync', lambda e, i=i, d_ap=d_ap: e.dma_start(out=d_ap, in_=qo[i][:]),
                         r=[qo[i]], w=[self.yT], dsem=qo[i])

            pf = pc[0]
            fe = P.sbuf(st, 'p_fe', [6, 512], F32)
            fc = P.sbuf(st, 'p_fc', [6, 512 + 1], F32)
            r1 = P.sbuf(st, 'p_r1', [6, 512], F32)
            pcs = [P.sbuf(st, 'p_pc%d' % i, [6, 512], BF16) for i in range(3)]
            npcs = [P.sbuf(st, 'p_npc%d' % i, [6, 512], BF16) for i in range(3)]
            ones = self.c['ones']
            onesb = self.c['onesb']
            P.op('vector', lambda e: e.memset(fc[:, 0:1], 0.0), w=[fc])
            for n in range(self.NC):
                proj_mm(pf, 3072, 6, n)
                P.op('scalar', lambda e: e.activation(out=fe[:], in_=pf[0:6, :], func=AF.Exp, bias=fb[:, 0:1],
                                                      scale=-1.0), r=[pf, fb], w=[fe])
                P.op('scalar', lambda e: e.activation(out=fe[:], in_=fe[:], func=AF.Ln, bias=1.0), r=[fe], w=[fe])
                if n > 0:
                    P.op('vector', lambda e: e.tensor_copy(out=fc[:, 0:1], in_=fc[:, 512:513]), r=[fc], w=[fc])
                P.op('vector', lambda e: e.tensor_tensor_scan(out=fc[:, 1:513], data0=ones[0:6, 0:512], data1=fe[:],
                                                              initial=fc[:, 0:1], op0=ALU.mult, op1=ALU.add),
                     r=[ones, fe, fc], w=[fc])
                P.op('vector', lambda e: e.tensor_copy(out=pcs[0][:], in_=fc[:, 1:513]), r=[fc], w=[pcs[0]])
                P.op('vector', lambda e: e.tensor_tensor(out=r1[:], in0=fc[:, 1:513], in1=pcs[0][:], op=ALU.subtract),
                     r=[fc, pcs[0]], w=[r1])
                P.op('vector', lambda e: e.tensor_copy(out=pcs[1][:], in_=r1[:]), r=[r1], w=[pcs[1]])
                P.op('vector', lambda e: e.tensor_tensor(out=r1[:], in0=r1[:], in1=pcs[1][:], op=ALU.subtract),
                     r=[r1, pcs[1]], w=[r1])
                P.op('vector', lambda e: e.tensor_copy(out=pcs[2][:], in_=r1[:]), r=[r1], w=[pcs[2]])
                for q3 in range(3):
                    P.op('vector', lambda e, q3=q3: e.tensor_scalar(out=npcs[q3][:], in0=pcs[q3][:], scalar1=-1.0,
                                                                   scalar2=None, op0=ALU.mult),
                         r=[pcs[q3]], w=[npcs[q3]])
                    sl = slice(n * 512, (n + 1) * 512)
                    dq = self.qTa.t.ap()[:, 64 + q3, sl]
                    dk = self.kTa.t.ap()[:, 67 + q3, sl]
                    P.op('sync', lambda e, q3=q3, dq=dq: e.dma_start(out=dq, in_=npcs[q3][:]),
                         r=[npcs[q3]], w=[self.qTa], dsem=npcs[q3])
                    P.op('sync', lambda e, q3=q3, dk=dk: e.dma_start(out=dk, in_=pcs[q3][:]),
                         r=[pcs[q3]], w=[self.kTa], dsem=pcs[q3])
                    dq1 = self.qTa.t.ap()[:, 67 + q3, sl]
                    dk1 = self.kTa.t.ap()[:, 64 + q3, sl]
                    P.op('sync', lambda e, dq1=dq1: e.dma_start(out=dq1, in_=onesb[0:6, 0:512]),
                         r=[onesb], w=[self.qTa], dsem=onesb)
                    P.op('sync', lambda e, dk1=dk1: e.dma_start(out=dk1, in_=onesb[0:6, 0:512]),
                         r=[onesb], w=[self.kTa], dsem=onesb)

            vo = [P.sbuf(st, 'p_vo%d' % i, [128, 768], BF16) for i in range(2)]
            for t in range(self.NT):
                i = t % 2

                def mmv(e, t=t, i=i):
                    ins = None
                    for (ps, c0) in ((pq[i], 1536), (pm[i], 2688)):
                        for k in range(8):
                            ins = e.matmul(ps[:, 0:384], hT[:, k, t * 128:(t + 1) * 128], wbf[:, k, c0:c0 + 384],
                                           start=(k == 0), stop=(k == 7))
                    return ins
                P.op('tensor', mmv, r=[wbf, hT], w=[pq[i], pm[i]])
                P.op('scalar', lambda e, i=i: e.copy(out=vo[i][:, 0:384], in_=pq[i][:, 0:384]), r=[pq[i]], w=[vo[i]])
                P.op('vector', lambda e, i=i: e.tensor_copy(out=vo[i][:, 384:768], in_=pm[i][:, 0:384]),
                     r=[pm[i]], w=[vo[i]])
                d1 = self.v_sb.t.ap()[t * 128:(t + 1) * 128, :]
                d2 = self.v_fox.t.ap()[t * 128:(t + 1) * 128, :]
                P.op('sync', lambda e, i=i, d1=d1: e.dma_start(out=d1, in_=vo[i][:, 0:384]), r=[vo[i]],
                     w=[self.v_sb], dsem=vo[i])
                P.op('sync', lambda e, i=i, d2=d2: e.dma_start(out=d2, in_=vo[i][:, 384:768]), r=[vo[i]],
                     w=[self.v_fox], dsem=vo[i])
            P.barrier()
            P.flush()


    def head_norm_part1(self, st_bufs, po, nrow):
        P = self.P
        osb, sq, pn, rs, yo = st_bufs
        P.op('scalar', lambda e: e.copy(out=osb[0:64, :], in_=po[0:64, :]), r=[po], w=[osb])
        P.op('scalar', lambda e: e.activation(out=sq[0:nrow, :], in_=po[0:nrow, :], func=AF.Square), r=[po], w=[sq])

    def head_norm_part2(self, st_bufs, nrow, lhsT_ap, gcol_ap, drow, c, eps_bias):
        P = self.P
        osb, sq, pn, rs, yo = st_bufs
        P.op('tensor', lambda e: e.matmul(pn[0:64, :], lhsT_ap, sq[0:nrow, :], start=True, stop=True),
             r=[sq], w=[pn])
        P.op('scalar', lambda e: e.activation(out=rs[0:64, :], in_=pn[0:64, :], func=AF.Ln, bias=eps_bias),
             r=[pn], w=[rs])
        P.op('scalar', lambda e: e.activation(out=rs[0:64, :], in_=rs[0:64, :], func=AF.Exp, scale=-0.5),
             r=[rs], w=[rs])
        P.op('vector', lambda e: e.scalar_tensor_tensor(out=yo[0:64, :], in0=osb[0:64, :], scalar=gcol_ap,
                                                       in1=rs[0:64, :], op0=ALU.mult, op1=ALU.mult),
             r=[osb, rs], w=[yo])
        d_ap = self.yT.t.ap()[drow:drow + 64, c * 512:(c + 1) * 512]
        P.op('sync', lambda e: e.dma_start(out=d_ap, in_=yo[0:64, :]), r=[yo], w=[self.yT], dsem=yo)

    def phase_attn(self, l):
        P = self.P
        S = self.S
        NT = self.NT
        inp = self.inp
        with ExitStack() as st:
            ogs = P.sbuf(st, 't_og', [64, 12], F32)
            src_og = inp['out_norm_g'].t.ap()[l, 256:1024].rearrange("(h d) -> d h", d=64)
            P.op('sync', lambda e: e.dma_start(out=ogs[:], in_=src_og, allow_slow_non_contiguous=True),
                 w=[ogs], dsem=ogs)
            kinds = ('sb', 'fox')
            kT = {k: [P.sbuf(st, 't_kT%s%d' % (k, i), [70, S], BF16) for i in range(2)] for k in kinds}
            qT = {k: [P.sbuf(st, 't_qT%s%d' % (k, i), [70, S], BF16) for i in range(2)] for k in kinds}
            vv = {k: [P.sbuf(st, 't_v%s%d' % (k, i), [128, NT, 65], BF16) for i in range(2)] for k in kinds}
            for i in range(2):
                P.op('vector', lambda e, i=i: e.memset(vv['fox'][i][:], 1.0), w=[vv['fox'][i]])
            pzs = [P.psum(st, 't_pzs%d' % i, [128, 512]) for i in range(2)]
            pbs = [P.psum(st, 't_pbs%d' % i, [128, 512]) for i in range(2)]
            pzf = P.psum(st, 't_pzf', [128, 512])
            po = {k: P.psum(st, 't_po' + k, [128, 512]) for k in kinds}
            pn = P.psum(st, 't_pn', [128, 512])
            e32 = [P.sbuf(st, 't_e%d' % i, [128, 512], F32) for i in range(2)]
            e32f = P.sbuf(st, 't_ef', [128, 512], F32)
            ec = [P.sbuf(st, 't_ec%d' % i, [128, 512], BF16) for i in range(2)]
            sp = [P.sbuf(st, 't_sp%d' % i, [128, 512], BF16) for i in range(2)]
            wt = {k: [P.sbuf(st, 't_w%s%d' % (k, i), [128, 512], BF16) for i in range(2)] for k in kinds}
            lacc = P.sbuf(st, 't_lacc', [128, 512], F32)
            laccb = [P.sbuf(st, 't_laccb%d' % i, [128, 512], BF16) for i in range(2)]
            nbufs = {}
            for k in kinds:
                nbufs[k] = (P.sbuf(st, 't_osb' + k, [64, 512], F32), P.sbuf(st, 't_sq' + k, [65, 512], F32), pn,
                            P.sbuf(st, 't_rs' + k, [64, 512], F32), P.sbuf(st, 't_yo' + k, [64, 512], BF16))
            negtri = self.c['negtri']
            negones = self.c['negones']
            mask_s = self.c['mask_s']
            mask_n = self.c['mask_n']
            blk = self.c['blk64']
            wn65 = self.c['wn65']
            its = []
            for c in range(self.NC):
                nkb = 4 * c + 4
                for idx, kb in enumerate(reversed(range(nkb))):
                    its.append((c, kb, idx == 0, kb == 0))
            def head_loads(h):
                slot = h % 2
                for kind in kinds:
                    K_ = 64 if kind == 'sb' else 70
                    if kind == 'sb':
                        ksrc = self.kT_sb.t.ap()[h * 64:(h + 1) * 64, :]
                        qsrc = self.qT_sb.t.ap()[h * 64:(h + 1) * 64, :]
                        vsrc = self.v_sb.t.ap()[:, h * 64:(h + 1) * 64].rearrange("(t p) d -> p t d", p=128)
                        srcs = (self.kT_sb, self.qT_sb, self.v_sb)
                    else:
                        ksrc = self.kTa.t.ap()[h]
                        qsrc = self.qTa.t.ap()[h]
                        vsrc = self.v_fox.t.ap()[:, h * 64:(h + 1) * 64].rearrange("(t p) d -> p t d", p=128)
                        srcs = (self.kTa, self.qTa, self.v_fox)
                    kt_, qt_, v_ = kT[kind][slot], qT[kind][slot], vv[kind][slot]
                    P.op('sync', lambda e, kt_=kt_, ksrc=ksrc, K_=K_: e.dma_start(out=kt_[0:K_, :], in_=ksrc),
                         r=[srcs[0]], w=[kt_], dsem=kt_)
                    P.op('sync', lambda e, qt_=qt_, qsrc=qsrc, K_=K_: e.dma_start(out=qt_[0:K_, :], in_=qsrc),
                         r=[srcs[1]], w=[qt_], dsem=qt_)
                    P.op('sync', lambda e, v_=v_, vsrc=vsrc: e.dma_start(out=v_[:, :, 0:64], in_=vsrc),
                         r=[srcs[2]], w=[v_], dsem=v_)

            head_loads(0)
            for h in range(6):
                slot = h % 2
                if h + 1 < 6:
                    head_loads(h + 1)

                def stageA(kind, n, slot=slot):
                    c, kb, first, last = its[n]
                    i = n % 2
                    j = kb - 4 * c
                    kt_, qt_ = kT[kind][slot], qT[kind][slot]
                    if kind == 'sb':
                        pz = pzs[i]
                        P.op('tensor', lambda e: e.matmul(pz[:], kt_[0:64, kb * 128:(kb + 1) * 128],
                                                          qt_[0:64, c * 512:(c + 1) * 512], start=True, stop=True),
                             r=[kt_, qt_], w=[pz])
                        P.op('scalar', lambda e: e.activation(out=e32[i][:], in_=pz[:], func=AF.Exp),
                             r=[pz], w=[e32[i]])
                        P.op('scalar', lambda e: e.activation(out=sp[i][:], in_=e32[i][:], func=AF.Ln, bias=1.0),
                             r=[e32[i]], w=[sp[i]])
                        if j >= 0:
                            P.op('gpsimd', lambda e: e.tensor_tensor(out=sp[i][:], in0=sp[i][:], in1=mask_s[:, j, :],
                                                                    op=ALU.mult), r=[sp[i], mask_s], w=[sp[i]])
                        if not last:
                            ln_ = laccb[(n + 1) % 2]
                            if first:
                                P.op('vector', lambda e: e.tensor_copy(out=lacc[:], in_=sp[i][:]), r=[sp[i]], w=[lacc])
                            else:
                                P.op('vector', lambda e: e.tensor_tensor(out=lacc[:], in0=lacc[:], in1=sp[i][:],
                                                                        op=ALU.add), r=[sp[i], lacc], w=[lacc])
                            P.op('vector', lambda e: e.tensor_copy(out=ln_[:], in_=lacc[:]), r=[lacc], w=[ln_])
                    else:
                        w_ = wt['fox'][i]
                        P.op('tensor', lambda e: e.matmul(pzf[:], kt_[0:70, kb * 128:(kb + 1) * 128],
                                                          qt_[0:70, c * 512:(c + 1) * 512], start=True, stop=True),
                             r=[kt_, qt_], w=[pzf])
                        if j >= 0:
                            P.op('vector', lambda e: e.tensor_scalar(out=e32f[:], in0=pzf[:], scalar1=60.0,
                                                                    scalar2=None, op0=ALU.min), r=[pzf], w=[e32f])
                            P.op('scalar', lambda e: e.activation(out=w_[:], in_=e32f[:], func=AF.Exp),
                                 r=[e32f], w=[w_])
                            P.op('gpsimd', lambda e: e.tensor_tensor(out=w_[:], in0=w_[:], in1=mask_n[:, j, :],
                                                                    op=ALU.mult), r=[w_, mask_n], w=[w_])
                        else:
                            P.op('scalar', lambda e: e.activation(out=w_[:], in_=pzf[:], func=AF.Exp),
                                 r=[pzf], w=[w_])

                def stageB1(n, slot=slot, h=h):
                    kind = 'sb'
                    c, kb, first, last = its[n]
                    i = n % 2
                    j = kb - 4 * c
                    w_ = wt[kind][i]
                    if True:
                        lb = laccb[n % 2]
                        pb = pbs[i]

                        def mm2(e):
                            ins = e.matmul(pb[:], negtri[:], sp[i][:], start=True, stop=first)
                            if not first:
                                ins = e.matmul(pb[:], negones[:], lb[:], start=False, stop=True)
                            return ins
                        P.op('tensor', mm2, r=[sp[i], negtri, negones] + ([] if first else [lb]), w=[pb])
                        P.op('scalar', lambda e: e.activation(out=ec[i][:], in_=pb[:], func=AF.Exp), r=[pb], w=[ec[i]])
                        if j >= 0:
                            P.op('gpsimd', lambda e: e.tensor_tensor(out=ec[i][:], in0=ec[i][:], in1=mask_s[:, j, :],
                                                                    op=ALU.mult), r=[ec[i], mask_s], w=[ec[i]])
                        P.op('vector', lambda e: e.tensor_tensor(out=w_[:], in0=e32[i][:], in1=ec[i][:], op=ALU.mult),
                             r=[e32[i], ec[i]], w=[w_])

                def stageB2(kind, n, slot=slot, h=h):
                    c, kb, first, last = its[n]
                    i = n % 2
                    v_ = vv[kind][slot]
                    pc_ = po[kind]
                    w_ = wt[kind][i]
                    nr = 64 if kind == 'sb' else 65
                    P.op('tensor', lambda e: e.matmul(pc_[0:nr, :], v_[:, kb, 0:nr], w_[:], start=first, stop=last),
                         r=[v_, w_], w=[pc_])
                    if last:
                        if kind == 'sb':
                            self.head_norm_part1(nbufs[kind], pc_, 64)
                            pending.append(lambda: self.head_norm_part2(nbufs['sb'], 64, blk[0:64, 0:64],
                                                                        ogs[:, h:h + 1], 256 + 64 * h, c, EPS))
                        else:
                            self.head_norm_part1(nbufs[kind], pc_, 65)
                            pending.append(lambda: self.head_norm_part2(nbufs['fox'], 65, wn65[:, :],
                                                                        ogs[:, 6 + h:7 + h], 640 + 64 * h, c, 0.0))

                stageA('sb', 0)
                stageA('fox', 0)
                pending = []
                for n in range(len(its)):
                    if self.sparse and n % 13 == 6:
                        self.issue_casts(l, 1)
                    stageB1(n)
                    if n + 1 < len(its):
                        stageA('sb', n + 1)
                        stageA('fox', n + 1)
                    todo, pending[:] = list(pending), []
                    for f in todo:
                        f()
                    stageB2('fox', n)
                    stageB2('sb', n)
                for f in pending:
                    f()
            if self.sparse:
                self.issue_casts(l, 2 * NE)
            P.barrier()
            P.flush()

    def phase_wout(self, l, xsrc, xdst):
        P = self.P
        inp = self.inp
        with ExitStack() as st:
            wo = P.sbuf(st, 'o_w', [128, 8, D], BF16)
            wv = inp['w_out'].t.ap()[l].rearrange("(k p) n -> p k n", p=128)
            for k in range(8):
                P.op('gpsimd', lambda e, k=k: e.dma_start(out=wo[:, k, :], in_=wv[:, k, :]), w=[wo], dsem=wo)
            g1 = self.load_bcast(st, 'o_g1', self.modv, l * 6 * D + 2 * D)
            yt = [P.sbuf(st, 'o_y%d' % i, [128, 8, 512], BF16) for i in range(2)]
            xt = [P.sbuf(st, 'o_x%d' % i, [128, D], F32) for i in range(2)]
            xo = [P.sbuf(st, 'o_xo%d' % i, [128, D], F32) for i in range(2)]
            py = [P.psum(st, 'o_p%d' % i, [128, 512]) for i in range(4)]
            yv = self.yT.t.ap().rearrange("(k p) s -> p k s", p=128)
            def load_y(c):
                yb = yt[c % 2]
                P.op('sync', lambda e: e.dma_start(out=yb[:], in_=yv[:, :, c * 512:(c + 1) * 512]),
                     r=[self.yT], w=[yb], dsem=yb)
            load_y(0)
            for c in range(self.NC):
                yb = yt[c % 2]
                if c + 1 < self.NC:
                    load_y(c + 1)
                for tt in range(4):
                    t = c * 4 + tt
                    i = t % 2
                    src = xsrc.t.ap()[t * 128:(t + 1) * 128, :]
                    P.op('sync', lambda e, i=i, src=src: e.dma_start(out=xt[i][:], in_=src), r=[xsrc], w=[xt[i]],
                         dsem=xt[i])
                    for half in range(2):
                        pp = py[i * 2 + half]

                        def mm(e, yb=yb, tt=tt, half=half, pp=pp):
                            ins = None
                            for k in range(8):
                                ins = e.matmul(pp[:], yb[:, k, tt * 128:(tt + 1) * 128],
                                               wo[:, k, half * 512:(half + 1) * 512], start=(k == 0), stop=(k == 7))
                            return ins
                        P.op('tensor', mm, r=[yb, wo], w=[pp])
                        sl = slice(half * 512, (half + 1) * 512)
                        P.op('vector', lambda e, i=i, pp=pp, sl=sl: e.tensor_tensor(
                            out=xo[i][:, sl], in0=pp[:], in1=g1[:, sl], op=ALU.mult), r=[pp, g1], w=[xo[i]])
                    P.op('gpsimd', lambda e, i=i: e.tensor_tensor(out=xo[i][:], in0=xo[i][:], in1=xt[i][:], op=ALU.add),
                         r=[xo[i], xt[i]], w=[xo[i]])
                    dst = xdst.t.ap()[t * 128:(t + 1) * 128, :]
                    P.op('sync', lambda e, i=i, dst=dst: e.dma_start(out=dst, in_=xo[i][:]), r=[xo[i]], w=[xdst],
                         dsem=xo[i])
            P.barrier()
            P.flush()

    def load_expert(self, l, e_, w1b, w2b, b1c, b1s):
        P = self.P
        inp = self.inp
        w1v = inp['w_mlp1'].t.ap()[l, e_].rearrange("(k p) n -> p k n", p=128)
        w2v = inp['w_mlp2'].t.ap()[l, e_].rearrange("(p i) n -> p i n", i=8)
        for k in range(8):
            P.op('gpsimd', lambda e, k=k: e.dma_start(out=w1b[:, k, :], in_=w1v[:, k, :]), w=[w1b], dsem=w1b)
        for k in range(0, 8, 2):
            P.op('gpsimd', lambda e, k=k: e.dma_start(out=w2b[:, k:k + 2, :], in_=w2v[:, k:k + 2, :]),
                 w=[w2b], dsem=w2b)
        b1v = inp['b_mlp1'].t.ap()[l, e_].rearrange("(p m) -> p m", m=16)
        P.op('sync', lambda e: e.dma_start(out=b1c[:], in_=b1v), w=[b1c], dsem=b1c)

    def ffn_block(self, B, xT, w1b, w2b, b1c, b1s, evac, mid_hook=None):
        P = self.P
        aT = B['aT']
        CSIG = float(1.0 / (1.0 + np.exp(-1.702 * 7.0)))
        for i in range(8):
            q = B['it'] % 2
            B['it'] += 1
            pg, pl = B['pg'][q], B['pl'][q]
            sg, g, ll, tt = B['sg'][q], B['g'][q], B['l'][q], B['t'][q]

            def mm1(e, i=i, pg=pg, pl=pl):
                ins = None
                for (pp, j) in ((pg, 0), (pl, 1)):
                    for k in range(8):
                        ins = e.matmul(pp[:], w1b[:, k, 2 * i + j:2 * D:16], xT[:, k, :],
                                       start=(k == 0), stop=(k == 7))
                return ins
            P.op('tensor', mm1, r=[w1b, xT], w=[pg, pl])
            P.op('vector', lambda e, i=i, g=g, pg=pg: e.tensor_scalar(out=g[:], in0=pg[:], scalar1=b1c[:, 2 * i:2 * i + 1],
                                                                     scalar2=7.0, op0=ALU.add, op1=ALU.min),
                 r=[pg, b1c], w=[g])
            P.op('scalar', lambda e, sg=sg, g=g: e.activation(out=sg[:], in_=g[:], func=AF.Sigmoid, scale=1.702),
                 r=[g], w=[sg])
            P.op('vector', lambda e, i=i, ll=ll, pl=pl: e.tensor_scalar(out=ll[:], in0=pl[:], scalar1=b1c[:, 2 * i + 1:2 * i + 2],
                                                                       scalar2=7.0, op0=ALU.add, op1=ALU.min),
                 r=[pl, b1c], w=[ll])
            P.op('vector', lambda e, ll=ll: e.tensor_scalar(out=ll[:], in0=ll[:], scalar1=-7.0, scalar2=1.0,
                                                           op0=ALU.max, op1=ALU.add), r=[ll], w=[ll])
            P.op('gpsimd', lambda e, sg=sg, g=g, tt=tt: e.tensor_tensor(out=tt[:], in0=sg[:], in1=g[:], op=ALU.mult),
                 r=[sg, g], w=[tt])
            P.op('vector', lambda e, i=i, tt=tt, ll=ll: e.tensor_tensor(out=aT[:, i, :], in0=tt[:], in1=ll[:],
                                                                       op=ALU.mult), r=[tt, ll], w=[aT])
            if i == 4 and mid_hook is not None:
                mid_hook()
        for r_ in range(4):
            for half in range(2):
                pp = B['py'][B['ity'] % len(B['py'])]
                B['ity'] += 1

                def mm2(e, r_=r_, half=half, pp=pp):
                    ins = None
                    for k in range(8):
                        ins = e.matmul(pp[:], aT[:, k, r_ * 128:(r_ + 1) * 128],
                                       w2b[:, k, half * 512:(half + 1) * 512], start=(k == 0), stop=(k == 7))
                    return ins
                P.op('tensor', mm2, r=[aT, w2b], w=[pp])
                evac(r_, half, pp)

    def ffn_bufs(self, st, npy=4):
        P = self.P
        B = {'it': 0, 'ity': 0}
        B['aT'] = P.sbuf(st, 'f_aT', [128, 8, 512], BF16)
        B['pg'] = [P.psum(st, 'f_pg%d' % i, [128, 512]) for i in range(2)]
        B['pl'] = [P.psum(st, 'f_pl%d' % i, [128, 512]) for i in range(2)]
        B['py'] = [P.psum(st, 'f_py%d' % i, [128, 512]) for i in range(npy)]
        for nm in ('sg', 'g', 'l', 't'):
            B[nm] = [P.sbuf(st, 'f_%s%d' % (nm, i), [128, 512], F32) for i in range(2)]
        return B

    def phase_moe_dense(self, l, xsrc, xdst):
        P = self.P
        inp = self.inp
        NT = self.NT
        with ExitStack() as st:
            B = self.ffn_bufs(st)
            w1b = [P.sbuf(st, 'd_w1%d' % i, [128, 8, 2 * D], BF16) for i in range(2)]
            w2b = [P.sbuf(st, 'd_w2%d' % i, [128, 8, D], BF16) for i in range(2)]
            b1c = [P.sbuf(st, 'd_b1c%d' % i, [128, 16], F32) for i in range(2)]
            b1s = [P.sbuf(st, 'd_b1s%d' % i, [128, 8], F32) for i in range(2)]
            xT = [P.sbuf(st, 'd_xT%d' % i, [128, 8, 512], BF16) for i in range(2)]
            stage = [P.sbuf(st, 'd_st%d' % i, [128, D], F32) for i in range(4)]
            hv = self.h2T_d.t.ap().rearrange("(k p) s -> p k s", p=128)
            accd = self.accd
            with ExitStack() as s2:
                b2s = P.sbuf(s2, 'd_b2', [NE, D], F32)
                P.op('sync', lambda e: e.dma_start(out=b2s[:], in_=inp['b_mlp2'].t.ap()[l]), w=[b2s], dsem=b2s)
                cT = P.sbuf(s2, 'd_cT', [NE, 128], F32)
                identf = self.c['ident_f']
                pt = B['pg'][0]
                for t in range(NT):
                    P.op('tensor', lambda e, t=t: e.transpose(pt[0:NE, 0:128], self.comb[:, t, :], identf[:]),
                         r=[self.comb, identf], w=[pt])
                    P.op('scalar', lambda e: e.copy(out=cT[:], in_=pt[0:NE, 0:128]), r=[pt], w=[cT])
                    sg_ = stage[t % 4]
                    for half in range(2):
                        pp = B['py'][half]
                        P.op('tensor', lambda e, pp=pp, half=half: e.matmul(
                            pp[:], cT[:], b2s[:, half * 512:(half + 1) * 512], start=True, stop=True),
                            r=[cT, b2s], w=[pp])
                        P.op('vector', lambda e, pp=pp, half=half, sg_=sg_: e.tensor_copy(
                            out=sg_[:, half * 512:(half + 1) * 512], in_=pp[:]), r=[pp], w=[sg_])
                    dst = accd.t.ap()[t * 128:(t + 1) * 128, :]
                    P.op('gpsimd', lambda e, sg_=sg_, dst=dst: e.dma_start(out=dst, in_=sg_[:]), r=[sg_], w=[accd],
                         dsem=sg_)
            blk = 0
            for e_ in range(NE):
                ws = e_ % 2
                self.load_expert(l, e_, w1b[ws], w2b[ws], b1c[ws], b1s[ws])
                for c in range(self.NC):
                    xb = xT[blk % 2]
                    blk += 1
                    P.op('sync', lambda e, xb=xb, c=c: e.dma_start(out=xb[:], in_=hv[:, :, c * 512:(c + 1) * 512]),
                         r=[self.h2T_d], w=[xb], dsem=xb)

                    def evac(r_, half, pp, c=c, e_=e_):
                        t = c * 4 + r_
                        sg_ = stage[r_]
                        sl = slice(half * 512, (half + 1) * 512)
                        P.op('vector', lambda e: e.tensor_scalar(out=sg_[:, sl], in0=pp[:],
                                                                 scalar1=self.comb[:, t, e_:e_ + 1], scalar2=None,
                                                                 op0=ALU.mult), r=[pp, self.comb], w=[sg_])
                        if half == 1:
                            dst = accd.t.ap()[t * 128:(t + 1) * 128, :]
                            P.op('gpsimd', lambda e: e.dma_start(out=dst, in_=sg_[:], accum_op=ALU.add),
                                 r=[sg_], w=[accd], dsem=sg_)
                    self.ffn_block(B, xb, w1b[ws], w2b[ws], b1c[ws], b1s[ws], evac)
            P.barrier()
            P.flush()
        self.phase_final(l, xsrc, xdst)

    def phase_final(self, l, xsrc, xdst):
        P = self.P
        with ExitStack() as st:
            g2 = self.load_bcast(st, 'z_g2', self.modv, l * 6 * D + 5 * D)
            xt = [P.sbuf(st, 'z_x%d' % i, [128, D], F32) for i in range(2)]
            at = [P.sbuf(st, 'z_a%d' % i, [128, D], F32) for i in range(2)]
            for t in range(self.NT):
                i = t % 2
                rows = slice(t * 128, (t + 1) * 128)
                P.op('sync', lambda e, i=i, rows=rows: e.dma_start(out=xt[i][:], in_=xsrc.t.ap()[rows, :]),
                     r=[xsrc], w=[xt[i]], dsem=xt[i])
                P.op('sync', lambda e, i=i, rows=rows: e.dma_start(out=at[i][:], in_=self.accd.t.ap()[rows, :]),
                     r=[self.accd], w=[at[i]], dsem=at[i])
                P.op('vector', lambda e, i=i: e.tensor_tensor(out=at[i][:], in0=at[i][:], in1=g2[:], op=ALU.mult),
                     r=[at[i], g2], w=[at[i]])
                P.op('gpsimd', lambda e, i=i: e.tensor_tensor(out=at[i][:], in0=at[i][:], in1=xt[i][:], op=ALU.add),
                     r=[at[i], xt[i]], w=[at[i]])
                P.op('sync', lambda e, i=i, rows=rows: e.dma_start(out=xdst.t.ap()[rows, :], in_=at[i][:]),
                     r=[at[i]], w=[xdst], dsem=at[i])
            P.barrier()
            P.flush()


    def slots_and_scatter(self, st, l, h2b_all):
        P = self.P
        NT = self.NT
        NBLK = self.NBLK
        memb, comb = self.memb, self.comb
        onesf = self.c['ones']
        stri = self.c['stri_f']
        blkstart = self.c['blkstart']
        pcn = P.psum(st, 'q_pcn', [128, 512])
        cnt = P.sbuf(st, 'q_cnt', [128, NE], F32)
        nb = P.sbuf(st, 'q_nb', [128, NE], F32)
        incl = P.sbuf(st, 'q_incl', [128, NE], F32)
        off = P.sbuf(st, 'q_off', [128, NE], F32)
        texp = P.sbuf(st, 'q_texp', [128, NBLK], F32)

        def mmc(e):
            ins = None
            for t in range(NT):
                ins = e.matmul(pcn[:, 0:NE], onesf[:, 0:128], memb[:, t, :], start=(t == 0), stop=(t == NT - 1))
            return ins
        P.op('tensor', mmc, r=[onesf, memb], w=[pcn])
        P.op('vector', lambda e: e.tensor_copy(out=cnt[:], in_=pcn[:, 0:NE]), r=[pcn], w=[cnt])
        P.op('vector', lambda e: e.tensor_scalar(out=nb[:], in0=cnt[:], scalar1=0.0, scalar2=None, op0=ALU.is_gt),
             r=[cnt], w=[nb])
        for k in range(1, self.S // TS):
            P.op('vector', lambda e, k=k: e.scalar_tensor_tensor(out=nb[:], in0=cnt[:], scalar=float(k * TS),
                                                                in1=nb[:], op0=ALU.is_gt, op1=ALU.add),
                 r=[cnt, nb], w=[nb])
        P.op('vector', lambda e: e.tensor_scalar(out=nb[:], in0=nb[:], scalar1=float(TS), scalar2=None, op0=ALU.mult),
             r=[nb], w=[nb])
        P.op('vector', lambda e: e.tensor_tensor_scan(out=incl[:], data0=onesf[:, 0:NE], data1=nb[:], initial=0.0,
                                                      op0=ALU.mult, op1=ALU.add), r=[onesf, nb], w=[incl])
        P.op('vector', lambda e: e.tensor_tensor(out=off[:], in0=incl[:], in1=nb[:], op=ALU.subtract),
             r=[incl, nb], w=[off])
        P.op('vector', lambda e: e.tensor_scalar(out=texp[:], in0=blkstart[:, 0:NBLK], scalar1=incl[:, 0:1],
                                                 scalar2=None, op0=ALU.is_ge), r=[blkstart, incl], w=[texp])
        for e_ in range(1, NE):
            P.op('vector', lambda e, e_=e_: e.scalar_tensor_tensor(out=texp[:], in0=blkstart[:, 0:NBLK],
                                                                  scalar=incl[:, e_:e_ + 1], in1=texp[:],
                                                                  op0=ALU.is_ge, op1=ALU.add),
                 r=[blkstart, incl, texp], w=[texp])
        gar = P.sbuf(st, 'q_gar', [128, NBLK], F32)
        P.op('vector', lambda e: e.tensor_scalar(out=gar[:], in0=texp[:], scalar1=float(NE) - 0.5,
                                                 scalar2=self.c['pidx'][:, 2:3], op0=ALU.is_gt, op1=ALU.mult),
             r=[texp, self.c['pidx']], w=[gar])
        P.op('vector', lambda e: e.tensor_scalar(out=texp[:], in0=texp[:], scalar1=float(NE - 1), scalar2=None,
                                                 op0=ALU.min), r=[texp], w=[texp])
        pidx = self.c['pidx']
        t1 = P.sbuf(st, 'q_t1', [128, NBLK], F32)
        ixf = P.sbuf(st, 'q_ixf', [128, NBLK, 5], F32)
        BIG = float(1 << 15)
        P.op('vector', lambda e: e.tensor_scalar(out=t1[:], in0=texp[:], scalar1=128.0, scalar2=None,
                                                 op0=ALU.mult), r=[texp], w=[t1])
        P.op('vector', lambda e: e.scalar_tensor_tensor(out=t1[:], in0=gar[:], scalar=BIG, in1=t1[:], op0=ALU.mult,
                                                       op1=ALU.add), r=[gar, t1], w=[t1])
        P.op('vector', lambda e: e.tensor_scalar(out=ixf[:, :, 4], in0=t1[:], scalar1=pidx[:, 0:1], scalar2=None,
                                                 op0=ALU.add), r=[t1, pidx], w=[ixf])
        P.op('vector', lambda e: e.tensor_scalar(out=ixf[:, :, 1], in0=ixf[:, :, 4], scalar1=2.0, scalar2=None,
                                                 op0=ALU.mult), r=[ixf], w=[ixf])
        P.op('vector', lambda e: e.tensor_scalar(out=ixf[:, :, 2], in0=ixf[:, :, 4], scalar1=2.0, scalar2=1.0,
                                                 op0=ALU.mult, op1=ALU.add), r=[ixf], w=[ixf])
        P.op('vector', lambda e: e.tensor_scalar(out=ixf[:, :, 0], in0=ixf[:, :, 4], scalar1=float(l * NE * 128),
                                                 scalar2=None, op0=ALU.add), r=[ixf], w=[ixf])
        P.op('vector', lambda e: e.tensor_scalar(out=t1[:], in0=texp[:], scalar1=float(l * NE), scalar2=None,
                                                 op0=ALU.add), r=[texp], w=[t1])
        P.op('vector', lambda e: e.scalar_tensor_tensor(out=ixf[:, :, 3], in0=gar[:], scalar=BIG, in1=t1[:],
                                                       op0=ALU.mult, op1=ALU.add), r=[gar, t1], w=[ixf])
        widx = self.widx
        P.op('vector', lambda e: e.tensor_copy(out=widx[:], in_=ixf[:]), r=[ixf], w=[widx])
        macc = P.sbuf(st, 'q_macc', [128, NE], F32)
        pp = [P.psum(st, 'q_pp%d' % i, [128, 512]) for i in range(2)]
        tmp = P.sbuf(st, 'q_tmp', [128, NE], F32)
        val = P.sbuf(st, 'q_val', [128, NE], F32)
        oh = P.sbuf(st, 'q_oh', [128, NE], F32)
        t8 = P.sbuf(st, 'q_t8', [128, 8], F32)
        sl4, g4 = self.sl4, self.g4
        for t in range(NT):
            ppt = pp[t % 2]

            def mmp(e, t=t, ppt=ppt):
                ins = e.matmul(ppt[:, 0:NE], stri[:], memb[:, t, :], start=True, stop=(t == 0))
                if t > 0:
                    ins = e.matmul(ppt[:, 0:NE], onesf[:, 0:128], macc[:], start=False, stop=True)
                return ins
            P.op('tensor', mmp, r=[stri, memb, onesf] + ([macc] if t > 0 else []), w=[ppt])
            P.op('vector', lambda e, ppt=ppt: e.tensor_tensor(out=tmp[:], in0=ppt[:, 0:NE], in1=off[:], op=ALU.add),
                 r=[ppt, off], w=[tmp])
            P.op('vector', lambda e, t=t: e.scalar_tensor_tensor(out=val[:], in0=tmp[:], scalar=1.0, in1=memb[:, t, :],
                                                                op0=ALU.add, op1=ALU.mult), r=[tmp, memb], w=[val])
            P.op('vector', lambda e: e.max(out=t8[:], in_=val[:]), r=[val], w=[t8])
            P.op('vector', lambda e, t=t: e.tensor_scalar(out=sl4[:, t, :], in0=t8[:, 0:4], scalar1=-1.0, scalar2=None,
                                                         op0=ALU.add), r=[t8], w=[sl4])
            for j in range(4):
                P.op('vector', lambda e, j=j: e.tensor_scalar(out=oh[:], in0=val[:], scalar1=t8[:, j:j + 1],
                                                             scalar2=None, op0=ALU.is_equal), r=[val, t8], w=[oh])
                P.op('vector', lambda e, t=t: e.tensor_tensor(out=oh[:], in0=oh[:], in1=comb[:, t, :], op=ALU.mult),
                     r=[oh, comb], w=[oh])
                P.op('vector', lambda e, t=t, j=j: e.reduce_sum(out=g4[:, t, j:j + 1], in_=oh[:], axis=AX.X),
                     r=[oh], w=[g4])
            if t == 0:
                P.op('vector', lambda e: e.tensor_copy(out=macc[:], in_=memb[:, 0, :]), r=[memb], w=[macc])
            else:
                P.op('vector', lambda e, t=t: e.tensor_tensor(out=macc[:], in0=macc[:], in1=memb[:, t, :], op=ALU.add),
                     r=[macc, memb], w=[macc])
            Xs = self.Xs
            for j in range(4):
                P.op('gpsimd', lambda e, t=t, j=j: e.indirect_dma_start(
                    out=Xs.t.ap(), out_offset=bass.IndirectOffsetOnAxis(ap=sl4[:, t, j:j + 1], axis=0),
                    in_=h2b_all[:, t, :], in_offset=None), r=[sl4, h2b_all], w=[Xs], dsem=h2b_all)

    def phase_moe_sparse(self, l, xsrc, xdst):
        P = self.P
        inp = self.inp
        NT = self.NT
        widx = self.widx
        wc1, wc2 = self.wc1[l], self.wc2[l]
        w1rows = wc1.t.ap().rearrange("(e p h k) n -> (e p h) (k n)", p=128, h=2, k=4)
        w2rows = wc2.t.ap().rearrange("(e p i) n -> (e p) (i n)", p=128, i=8)
        b1rows = inp['b_mlp1'].t.ap().rearrange("l e (p m) -> (l e p) m", m=16)
        b2rows = inp['b_mlp2'].t.ap().rearrange("l e n -> (l e) n")
        with ExitStack() as st:
            B = self.ffn_bufs(st, npy=3)
            w1b = [P.sbuf(st, 'd_w1%d' % i, [128, 8, 2 * D], BF16) for i in range(2)]
            w2b = [P.sbuf(st, 'd_w2%d' % i, [128, 8, D], BF16) for i in range(2)]
            b1c = [P.sbuf(st, 'd_b1c%d' % i, [128, 16], F32) for i in range(2)]
            b2bc = [P.sbuf(st, 'd_b2%d' % i, [128, D], F32) for i in range(2)]
            xs = [P.sbuf(st, 'd_xs%d' % i, [128, D], BF16) for i in range(2)]
            xT = [P.sbuf(st, 'd_xT%d' % i, [128, 8, 512], BF16) for i in range(2)]
            stage = [P.sbuf(st, 'd_st%d' % i, [128, D], F32) for i in range(4)]
            ident = self.c['ident_bf']
            pTbuf = P.psum(st, 'pTb', [128, 8, 128], BF16)
            Xs, Ys = self.Xs, self.Ys
            rg = [self.nc.gpsimd.alloc_register('bc%d_%d' % (q, l)) for q in range(4)]
            rtile = P.sbuf(st, 'd_rt', [128, 1], F32)

            def setregs(e):
                e.reg_mov(rg[0], NE * 128 * 2 - 1)
                e.reg_mov(rg[1], NL * NE * 128 - 1)
                e.reg_mov(rg[2], NL * NE - 1)
                e.reg_mov(rg[3], NE * 128 - 1)
                return e.memset(rtile[:], 0.0)
            P.op('gpsimd', setregs, w=[rtile])
            nxs = [0]

            def gathers(i):
                ws = i % 2
                wa, wb_, bc, b2 = w1b[ws], w2b[ws], b1c[ws], b2bc[ws]
                for h in range(2):
                    P.op('gpsimd', lambda e, h=h: e.indirect_dma_start(
                        out=wa[:, 4 * h:4 * h + 4, :].rearrange("p k n -> p (k n)"), out_offset=None, in_=w1rows,
                        in_offset=bass.IndirectOffsetOnAxis(ap=widx[:, i, 1 + h:2 + h], axis=0),
                        bounds_check=rg[0], oob_is_err=False), r=[widx, wc1], w=[wa], dsem=wa)
                P.op('gpsimd', lambda e: e.indirect_dma_start(
                    out=wb_[:].rearrange("p k n -> p (k n)"), out_offset=None, in_=w2rows,
                    in_offset=bass.IndirectOffsetOnAxis(ap=widx[:, i, 4:5], axis=0),
                    bounds_check=rg[3], oob_is_err=False), r=[widx, wc2], w=[wb_], dsem=wb_)
                P.op('gpsimd', lambda e: e.indirect_dma_start(
                    out=bc[:], out_offset=None, in_=b1rows,
                    in_offset=bass.IndirectOffsetOnAxis(ap=widx[:, i, 0:1], axis=0),
                    bounds_check=rg[1], oob_is_err=False), r=[widx], w=[bc], dsem=bc)
                P.op('gpsimd', lambda e: e.indirect_dma_start(
                    out=b2[:], out_offset=None, in_=b2rows,
                    in_offset=bass.IndirectOffsetOnAxis(ap=widx[:, i, 3:4], axis=0),
                    bounds_check=rg[2], oob_is_err=False), r=[widx], w=[b2], dsem=b2)

            def prep_x(i):
                xb = xT[i % 2]
                for r_ in range(4):
                    xq = xs[nxs[0] % 2]
                    nxs[0] += 1
                    rows = slice(i * TS + r_ * 128, i * TS + (r_ + 1) * 128)
                    P.op('sync', lambda e, xq=xq, rows=rows: e.dma_start(out=xq[:], in_=Xs.t.ap()[rows, :]),
                         r=[Xs], w=[xq], dsem=xq)

                    def tr(e, xq=xq):
                        ins = None
                        for k in range(8):
                            ins = e.transpose(pTbuf[:, k, :], xq[:, k:D:8], ident[:])
                        return ins
                    P.op('tensor', tr, r=[xq, ident], w=[pTbuf])
                    P.op('scalar', lambda e, xb=xb, r_=r_: e.copy(out=xb[:, :, r_ * 128:(r_ + 1) * 128],
                                                                  in_=pTbuf[:]), r=[pTbuf], w=[xb])

            gathers(0)
            prep_x(0)
            for i in range(self.NBLK):
                ws = i % 2
                wa, wb_, bc, b2 = w1b[ws], w2b[ws], b1c[ws], b2bc[ws]
                xb = xT[i % 2]
                if i + 1 < self.NBLK:
                    gathers(i + 1)

                def evac(r_, half, pp, i=i, b2=b2):
                    sg_ = stage[r_]
                    sl = slice(half * 512, (half + 1) * 512)
                    P.op('vector', lambda e: e.tensor_tensor(out=sg_[:, sl], in0=pp[:], in1=b2[:, sl], op=ALU.add),
                         r=[pp, b2], w=[sg_])
                    if half == 1:
                        rows = slice(i * TS + r_ * 128, i * TS + (r_ + 1) * 128)
                        P.op('sync', lambda e: e.dma_start(out=Ys.t.ap()[rows, :], in_=sg_[:]),
                             r=[sg_], w=[Ys], dsem=sg_)
                hook = (lambda i=i: prep_x(i + 1)) if i + 1 < self.NBLK else None
                self.ffn_block(B, xb, wa, wb_, bc, None, evac, mid_hook=hook)
            P.barrier()
            P.flush()
        self.phase_combine(l, xsrc, xdst)

    def phase_combine(self, l, xsrc, xdst):
        P = self.P
        Ys = self.Ys
        sl4, g4 = self.sl4, self.g4
        with ExitStack() as st:
            g2 = self.load_bcast(st, 'z_g2', self.modv, l * 6 * D + 5 * D)
            xt = [P.sbuf(st, 'z_x%d' % i, [128, D], F32) for i in range(2)]
            yg = [[P.sbuf(st, 'z_y%d_%d' % (i, j), [128, D], F32) for j in range(4)] for i in range(3)]
            acc = [P.sbuf(st, 'z_a%d' % i, [128, D], F32) for i in range(2)]
            for t in range(self.NT):
                i = t % 2
                g3 = t % 3
                rows = slice(t * 128, (t + 1) * 128)
                P.op('sync', lambda e, i=i, rows=rows: e.dma_start(out=xt[i][:], in_=xsrc.t.ap()[rows, :]),
                     r=[xsrc], w=[xt[i]], dsem=xt[i])
                for j in range(4):
                    P.op('gpsimd', lambda e, g3=g3, j=j, t=t: e.indirect_dma_start(
                        out=yg[g3][j][:], out_offset=None, in_=Ys.t.ap(),
                        in_offset=bass.IndirectOffsetOnAxis(ap=sl4[:, t, j:j + 1], axis=0)),
                        r=[Ys, sl4], w=[yg[g3][j]], dsem=yg[g3][j])
                P.op('vector', lambda e, i=i, t=t, g3=g3: e.tensor_scalar(out=acc[i][:], in0=yg[g3][0][:],
                                                                         scalar1=g4[:, t, 0:1], scalar2=None,
                                                                         op0=ALU.mult),
                     r=[yg[g3][0], g4], w=[acc[i]])
                for j in range(1, 4):
                    P.op('vector', lambda e, i=i, t=t, j=j, g3=g3: e.scalar_tensor_tensor(
                        out=acc[i][:], in0=yg[g3][j][:], scalar=g4[:, t, j:j + 1], in1=acc[i][:], op0=ALU.mult,
                        op1=ALU.add), r=[yg[g3][j], g4, acc[i]], w=[acc[i]])
                P.op('vector', lambda e, i=i: e.tensor_tensor(out=acc[i][:], in0=acc[i][:], in1=g2[:], op=ALU.mult),
                     r=[acc[i], g2], w=[acc[i]])
                P.op('vector', lambda e, i=i: e.tensor_tensor(out=acc[i][:], in0=acc[i][:], in1=xt[i][:], op=ALU.add),
                     r=[acc[i], xt[i]], w=[acc[i]])
                P.op('sync', lambda e, i=i, rows=rows: e.dma_start(out=xdst.t.ap()[rows, :], in_=acc[i][:]),
                     r=[acc[i]], w=[xdst], dsem=acc[i])
            P.barrier()
            P.flush()

    def build(self):
        P = self.P
        self.load_consts()
        self.comb = P.sbuf(self.stack, 'comb', [128, self.NT, NE], F32)
        self.memb = P.sbuf(self.stack, 'memb', [128, self.NT, NE], F32)
        self.h2T_d = P.dram("h2T_d", [D, self.S], BF16, self.sk)
        self.accd = P.dram("accd", [self.S, D], F32, self.sk)
        self.comb_d = P.dram("comb_d", [128, self.NT, NE], F32, self.sk)
        self.NBLK = NE + 4 * self.S // TS
        self.sl4 = P.sbuf(self.stack, 'sl4', [128, self.NT, 4], I32)
        self.g4 = P.sbuf(self.stack, 'g4', [128, self.NT, 4], F32)
        self.widx = P.sbuf(self.stack, 'widx', [128, self.NBLK, 5], I32)
        self.Xs = P.dram("Xs", [self.NBLK * TS, D], BF16, self.sk)
        self.Ys = P.dram("Ys", [self.NBLK * TS, D], F32, self.sk)
        self.phase_ada()
        if self.sparse:
            self.precast_weights()
        xsrc = self.inp['x']
        for l in range(self.nlayers):
            last = (l == self.nlayers - 1)
            with ExitStack() as st:
                hT = P.sbuf(st, 'hT', [128, 8, self.S], BF16)
                self.phase_norm_T(st, l, xsrc, 0, 'norm1_g', hT)
                self.phase_proj(l, hT)
            self.phase_attn(l)
            self.phase_wout(l, xsrc, self.x1)
            xdst = self.out if last else self.x2
            if self.sparse:
                with ExitStack() as st:
                    h2b_all = P.sbuf(st, 'h2b_all', [128, self.NT, D], BF16)
                    self.phase_norm_T(None, l, self.x1, 1, 'norm2_g', None, router=True, h2b_all=h2b_all)
                self.phase_moe_sparse(l, self.x1, xdst)
            else:
                self.phase_norm_T(None, l, self.x1, 1, 'norm2_g', None, hT_dram=self.h2T_d, router=True)
                self.phase_moe_dense(l, self.x1, xdst)
            xsrc = xdst
        P.barrier()
        P.flush()
        self.stack.close()
        return self.nc


_CACHE = {}


def kernel(**inputs):
    S = inputs['x'].shape[1]
    nb = inputs['x'].shape[0]
    if S not in _CACHE:
        _CACHE[S] = K(S).build()
    nc = _CACHE[S]
    consts = make_consts()
    shared = {}
    for name, _ in INPUT_SPECS:
        if name in ('x', 'c'):
            continue
        shared[name] = np.ascontiguousarray(inputs[name], dtype=np.float32)
    for k, v in consts.items():
        shared['c_' + k] = v
    in_maps = []
    for b in range(nb):
        m = dict(shared)
        m['x'] = np.ascontiguousarray(inputs['x'][b], dtype=np.float32)
        m['c'] = np.ascontiguousarray(inputs['c'][b], dtype=np.float32)
        in_maps.append(m)
    res = run_bass_kernel_spmd(nc, in_maps, core_ids=list(range(nb)))
    return np.stack([np.asarray(r['out']) for r in res.results], 0).astype(np.float32)
```

```python
import numpy as np
import ml_dtypes
from contextlib import ExitStack
import concourse.bass as bass
import concourse.mybir as mybir
from concourse.bass_utils import run_bass_kernel_spmd

F32 = mybir.dt.float32
BF16 = mybir.dt.bfloat16
I32 = mybir.dt.int32
AF = mybir.ActivationFunctionType
ALU = mybir.AluOpType
AX = mybir.AxisListType

D = 1024
NL = 2
NE = 32
DIN = 3078
EPS = 1e-6
TS = 512
NBLK_MAX = 64
ENGS = ['sync', 'scalar', 'vector', 'gpsimd', 'tensor']


class Sem:
    def __init__(self, h):
        self.h = h
        self.v = 0
        self.nobarrier = False


class Buf:
    def __init__(self, t, name):
        self.t = t
        self.name = name
        self.w = {}
        self.r = {}
        self.dsem = None

    def __getitem__(self, idx):
        return self.t[idx]


class Prog:
    def __init__(self, nc, stack):
        self.nc = nc
        self.stack = stack
        self.q = {e: [] for e in ENGS}
        self.esem = {}
        self.allsems = []
        self.waited = {e: {} for e in ENGS}
        self.nsem = 0
        self.free_dsems = []
        self.phase_bufs = []
        for e in ['scalar', 'vector', 'gpsimd', 'tensor']:
            self.esem[e] = self.newsem('e_' + e)

    def newsem(self, name):
        h = self.stack.enter_context(self.nc.semaphore(name + '_%d' % self.nsem))
        self.nsem += 1
        s = Sem(h)
        self.allsems.append(s)
        return s

    def uname(self, name):
        self.nsem += 1
        return '%s_u%d' % (name, self.nsem)

    def sbuf(self, stack, name, shape, dt):
        name = self.uname(name)
        t = stack.enter_context(self.nc.sbuf_tensor(name, list(shape), dt))
        return Buf(t, name)

    def psum(self, stack, name, shape, dt=F32):
        name = self.uname(name)
        t = stack.enter_context(self.nc.psum_tensor(name, list(shape), dt))
        return Buf(t, name)

    def dram(self, name, shape, dt, kind="Internal"):
        t = self.nc.dram_tensor(name, list(shape), dt, kind=kind)
        return Buf(t, name)

    def op(self, eng, fn, r=(), w=(), dsem=None):
        waits = {}

        def addw(d):
            for s, v in d.items():
                if waits.get(s, 0) < v:
                    waits[s] = v
        for b in r:
            addw(b.w)
        for b in w:
            addw(b.w)
            addw(b.r)
        if dsem is not None:
            if dsem.dsem is None:
                dsem.dsem = self.free_dsems.pop() if self.free_dsems else self.newsem('d')
                self.phase_bufs.append(dsem)
            sem = dsem.dsem
            amt = 16
        else:
            sem = self.esem[eng]
            amt = 1
        wl = []
        for s, v in waits.items():
            if self.waited[eng].get(s, 0) >= v:
                continue
            if s not in self.esem.values():
                v = s.v
            if self.waited[eng].get(s, 0) < v:
                self.waited[eng][s] = v
                wl.append((s, v))
        sem.v += amt
        tok = (sem, sem.v)
        self.q[eng].append((fn, wl, sem, amt))
        for b in r:
            if b.r.get(sem, 0) < sem.v:
                b.r[sem] = sem.v
        for b in w:
            b.w = dict(b.w)
            b.w[sem] = sem.v
            b.r = {}
        return tok

    def barrier(self):
        for e in ENGS:
            wl = []
            for s in self.allsems:
                if s.nobarrier:
                    continue
                if s.v > 0 and self.waited[e].get(s, 0) < s.v:
                    self.waited[e][s] = s.v
                    wl.append((s, s.v))
            self.q[e].append((None, wl, None, 0))
        for b in self.phase_bufs:
            self.free_dsems.append(b.dsem)
            b.dsem = None
        self.phase_bufs = []

    def flush(self):
        with self.nc.Block() as block:
            for e in ENGS:
                items = self.q[e]

                def body(eng, items=items):
                    for fn, wl, sem, amt in items:
                        for s, v in wl:
                            eng.wait_ge(s.h, v)
                        if fn is not None:
                            ins = fn(eng)
                            ins.then_inc(sem.h, amt)
                getattr(block, e)(body)
        self.q = {e: [] for e in ENGS}


def bcast_ap(ap1d_tensor, offset, n, parts=128):
    return bass.AP(ap1d_tensor, offset, [[0, parts], [1, n]])


def make_consts():
    c = {}
    c['ident_bf'] = np.eye(128, dtype=np.float32).astype(ml_dtypes.bfloat16)
    c['ident_f'] = np.eye(128, dtype=np.float32)
    blk = np.zeros((128, 128), np.float32)
    blk[:64, :64] = 1.0 / 64
    blk[64:, 64:] = 1.0 / 64
    c['blk64'] = blk
    wn = np.full((65, 64), 1.0 / 64, np.float32)
    wn[64, :] = EPS
    c['wn65'] = wn
    j = np.arange(128)[:, None]
    k = np.arange(128)[None, :]
    c['negtri'] = np.where(j >= k, -1.0, 0.0).astype(np.float32).astype(ml_dtypes.bfloat16)
    c['negones'] = np.full((128, 128), -1.0, np.float32).astype(ml_dtypes.bfloat16)
    p = np.arange(128)[:, None]
    col = np.arange(512)[None, :]
    ms = np.zeros((4, 128, 512), np.float32)
    mn = np.zeros((4, 128, 512), np.float32)
    for jj in range(4):
        bc = col // 128
        ms[jj] = np.where(bc < jj, 0.0, np.where(bc == jj, (p < (col % 128)), 1.0))
        mn[jj] = np.where(bc < jj, 0.0, np.where(bc == jj, (p <= (col % 128)), 1.0))
    c['stri_f'] = (j < k).astype(np.float32)
    c['pidx'] = np.stack([np.arange(128), 8 * np.arange(128), np.minimum(np.arange(128), 1)],
                         1).astype(np.float32)
    c['blkstart'] = np.broadcast_to((np.arange(NBLK_MAX, dtype=np.float32) * TS)[None, :], (128, NBLK_MAX)).copy()
    c['mask_s'] = ms.transpose(1, 0, 2).copy().astype(ml_dtypes.bfloat16)
    c['mask_n'] = mn.transpose(1, 0, 2).copy().astype(ml_dtypes.bfloat16)
    return c


CONST_SPECS = [('ident_bf', [128, 128], BF16), ('ident_f', [128, 128], F32), ('blk64', [128, 128], F32),
               ('wn65', [65, 64], F32), ('negtri', [128, 128], BF16), ('negones', [128, 128], BF16),
               ('mask_s', [128, 4, 512], BF16), ('mask_n', [128, 4, 512], BF16),
               ('stri_f', [128, 128], F32), ('blkstart', [128, NBLK_MAX], F32), ('pidx', [128, 3], F32)]

INPUT_SPECS = [
    ('x', lambda S: [S, D]), ('c', lambda S: [D]), ('norm1_g', lambda S: [NL, D]),
    ('w_ada', lambda S: [NL, D, 6 * D]), ('b_ada', lambda S: [NL, 6 * D]), ('w_in', lambda S: [NL, D, DIN]),
    ('conv_w', lambda S: [NL, 3, 256]), ('sb_q_g', lambda S: [NL, 64]), ('sb_k_g', lambda S: [NL, 64]),
    ('fox_q_g', lambda S: [NL, 64]), ('fox_k_g', lambda S: [NL, 64]), ('fox_f_b', lambda S: [NL, 6]),
    ('out_norm_g', lambda S: [NL, D]), ('w_out', lambda S: [NL, D, D]), ('norm2_g', lambda S: [NL, D]),
    ('router_w', lambda S: [NL, D, NE]), ('router_b', lambda S: [NL, NE]),
    ('w_mlp1', lambda S: [NL, NE, D, 2 * D]), ('b_mlp1', lambda S: [NL, NE, 2 * D]),
    ('w_mlp2', lambda S: [NL, NE, D, D]), ('b_mlp2', lambda S: [NL, NE, D]),
]


class K:
    def __init__(self, S, nlayers=NL, debug=False, stop_after=None, sparse=True):
        self.sparse = sparse
        self.S = S
        self.NT = S // 128
        self.NC = S // 512
        self.debug = debug
        self.nlayers = nlayers
        self.stop_after = stop_after
        nc = bass.Bass("TRN2", target_bir_lowering=False)
        self.nc = nc
        self.stack = ExitStack()
        self.P = Prog(nc, self.stack)
        P = self.P
        self.inp = {}
        for name, shp in INPUT_SPECS:
            self.inp[name] = Buf(nc.dram_tensor(name, shp(S), F32, kind="ExternalInput"), name)
        self.cst = {}
        for name, shp, dt in CONST_SPECS:
            self.cst[name] = Buf(nc.dram_tensor('c_' + name, shp, dt, kind="ExternalInput"), name)
        self.out = Buf(nc.dram_tensor("out", [S, D], F32, kind="ExternalOutput"), "out")
        sk = "ExternalOutput" if debug else "Internal"
        self.sk = sk
        self.modv = P.dram("modv", [NL, 6 * D], F32, sk)
        self.qT_sb = P.dram("qT_sb", [384, S], BF16, sk)
        self.kT_sb = P.dram("kT_sb", [384, S], BF16, sk)
        self.v_sb = P.dram("v_sb", [S, 384], BF16, sk)
        self.qTa = P.dram("qTa", [6, 70, S], BF16, sk)
        self.kTa = P.dram("kTa", [6, 70, S], BF16, sk)
        self.v_fox = P.dram("v_fox", [S, 384], BF16, sk)
        self.yT = P.dram("yT", [D, S], BF16, sk)
        self.x1 = P.dram("x1", [S, D], F32, sk)
        self.x2 = P.dram("x2", [S, D], F32, sk)

    def load_consts(self):
        P = self.P
        st = self.stack
        self.c = {}
        for name, shp, dt in CONST_SPECS:
            b = P.sbuf(st, 'k_' + name, shp, dt)
            src = self.cst[name]
            P.op('sync', lambda e, b=b, src=src: e.dma_start(out=b[:], in_=src.t.ap()), r=[src], w=[b], dsem=b)
            self.c[name] = b
        ones = P.sbuf(st, 'k_ones', [128, 512], F32)
        P.op('vector', lambda e: e.memset(ones[:], 1.0), w=[ones])
        self.c['ones'] = ones
        onesb = P.sbuf(st, 'k_onesb', [128, 512], BF16)
        P.op('vector', lambda e: e.memset(onesb[:], 1.0), w=[onesb])
        self.c['onesb'] = onesb


    def precast_weights(self):
        P = self.P
        inp = self.inp
        self.wc1, self.wc2 = [], []
        for l in range(self.nlayers):
            b1 = P.dram("wc1_%d" % l, [NE * D, 2 * D], BF16)
            b2 = P.dram("wc2_%d" % l, [NE * D, D], BF16)
            for b in (b1, b2):
                b.dsem = P.newsem('wc')
                b.dsem.nobarrier = True
            self.wc1.append(b1)
            self.wc2.append(b2)
        self.pending_casts = [[] for _ in range(self.nlayers)]
        for l in range(self.nlayers):
            for e_ in range(NE):
                src1 = inp['w_mlp1'].t.ap()[l, e_].rearrange("(a b) n -> a (b n)", b=4)
                dst1 = self.wc1[l].t.ap()[e_ * D:(e_ + 1) * D, :].rearrange("(a b) n -> a (b n)", b=4)
                self.pending_casts[l].append((src1, dst1, self.wc1[l]))
                src2 = inp['w_mlp2'].t.ap()[l, e_].rearrange("(a b) n -> a (b n)", b=8)
                dst2 = self.wc2[l].t.ap()[e_ * D:(e_ + 1) * D, :].rearrange("(a b) n -> a (b n)", b=8)
                self.pending_casts[l].append((src2, dst2, self.wc2[l]))

    def issue_casts(self, l, n):
        P = self.P
        for _ in range(n):
            if not self.pending_casts[l]:
                return
            src, dst, buf = self.pending_casts[l].pop(0)
            P.op('gpsimd', lambda e, src=src, dst=dst: e.dma_start(out=dst, in_=src), w=[buf], dsem=buf)

    def phase_ada(self):
        P = self.P
        inp = self.inp
        with ExitStack() as st:
            cT = P.sbuf(st, 'a_cT', [128, 8], F32)
            sc = P.sbuf(st, 'a_sc', [128, 8], F32)
            wb = [P.sbuf(st, 'a_w%d' % i, [128, 3072], F32) for i in range(2)]
            bada = P.sbuf(st, 'a_b', [1, 6 * D], F32)
            modrow = P.sbuf(st, 'a_mod', [1, 6 * D], F32)
            ps = [P.psum(st, 'a_ps%d' % i, [128, 512]) for i in range(6)]
            cap = inp['c'].t.ap().rearrange("(p j) -> p j", j=8)
            P.op('sync', lambda e: e.dma_start(out=cT[:], in_=cap), w=[cT], dsem=cT)
            P.op('scalar', lambda e: e.activation(out=sc[:], in_=cT[:], func=AF.Silu), r=[cT], w=[sc])
            it = 0
            for l in range(self.nlayers):
                bsrc = inp['b_ada'].t.ap()[l:l + 1, :]
                P.op('sync', lambda e, bsrc=bsrc: e.dma_start(out=bada[:], in_=bsrc), w=[bada], dsem=bada)
                wv = inp['w_ada'].t.ap()[l].rearrange("(p j) n -> p j n", j=8)
                for half in range(2):
                    for j in range(8):
                        b = wb[it % 2]
                        it += 1
                        src = wv[:, j, half * 3072:(half + 1) * 3072]
                        P.op('sync' if it % 2 else 'gpsimd',
                             lambda e, b=b, src=src: e.dma_start(out=b[:], in_=src), w=[b], dsem=b)

                        def mm(e, b=b, j=j):
                            ins = None
                            for n in range(6):
                                ins = e.matmul(ps[n][0:1, :], sc[:, j:j + 1], b[:, n * 512:(n + 1) * 512],
                                               start=(j == 0), stop=(j == 7))
                            return ins
                        P.op('tensor', mm, r=[b, sc], w=ps)
                    for n in range(6):
                        o = half * 3072 + n * 512
                        P.op('vector', lambda e, n=n, o=o: e.tensor_tensor(
                            out=modrow[0:1, o:o + 512], in0=ps[n][0:1, :], in1=bada[0:1, o:o + 512], op=ALU.add),
                            r=[ps[n], bada], w=[modrow])
                dst = self.modv.t.ap()[l:l + 1, :]
                P.op('sync', lambda e, dst=dst: e.dma_start(out=dst, in_=modrow[:]), r=[modrow], w=[self.modv],
                     dsem=modrow)
            P.barrier()
            P.flush()

    def load_bcast(self, st, name, srcbuf, offset, n=D, eng='sync'):
        P = self.P
        b = P.sbuf(st, name, [128, n], F32)
        ap = bcast_ap(srcbuf.t, offset, n)
        P.op(eng, lambda e: e.dma_start(out=b[:], in_=ap), r=[srcbuf], w=[b], dsem=b)
        return b

    def mod_tiles(self, st, l, which, gname):
        P = self.P
        gb = self.load_bcast(st, 'm_g', self.inp[gname], l * D)
        sb = self.load_bcast(st, 'm_s', self.modv, l * 6 * D + (3 * which + 1) * D, eng='gpsimd')
        tb = self.load_bcast(st, 'm_t', self.modv, l * 6 * D + (3 * which + 0) * D)
        P.op('vector', lambda e: e.scalar_tensor_tensor(out=gb[:], in0=sb[:], scalar=1.0, in1=gb[:],
                                                       op0=ALU.add, op1=ALU.mult), r=[sb, gb], w=[gb])
        return gb, tb

    def rstd_from_ssq(self, ssq, lnv, rstd, n):
        P = self.P
        P.op('scalar', lambda e: e.activation(out=lnv[:], in_=ssq[:], func=AF.Ln, bias=EPS, scale=1.0 / n),
             r=[ssq], w=[lnv])
        P.op('scalar', lambda e: e.activation(out=rstd[:], in_=lnv[:], func=AF.Exp, scale=-0.5),
             r=[lnv], w=[rstd])

    def phase_norm_T(self, st, l, xsrc, which, gname, hT, hT_dram=None, router=False, h2b_all=None):
        P = self.P
        inp = self.inp
        with ExitStack() as s2:
            if hT_dram is not None:
                stg = [P.sbuf(s2, 'n_stg%d' % i, [128, 8, 512], BF16) for i in range(2)]
            if router:
                rw = P.sbuf(s2, 'n_rw', [128, 8, NE], F32)
                src_rw = inp['router_w'].t.ap()[l].rearrange("(k p) n -> p k n", p=128)
                P.op('sync', lambda e: e.dma_start(out=rw[:], in_=src_rw), w=[rw], dsem=rw)
                rb = self.load_bcast(s2, 'n_rb', inp['router_b'], l * NE, n=NE)
                h32 = [P.sbuf(s2, 'n_h32%d' % i, [128, D], F32) for i in range(2)]
                h32T_ = [P.sbuf(s2, 'n_h32T%d' % i, [128, 8, 128], F32) for i in range(2)]
                pR = P.psum(s2, 'n_pR', [128, 8, 128], F32)
                pL_ = [P.psum(s2, 'n_pL%d' % i, [128, 512], F32) for i in range(2)]
                lg_ = [P.sbuf(s2, 'n_lg%d' % i, [128, NE], F32) for i in range(2)]
                ex_ = [P.sbuf(s2, 'n_ex%d' % i, [128, NE], F32) for i in range(2)]
                t8_ = [P.sbuf(s2, 'n_t8%d' % i, [128, 8], F32) for i in range(2)]
                nmx_ = [P.sbuf(s2, 'n_nmx%d' % i, [128, 1], F32) for i in range(2)]
                ssm_ = [P.sbuf(s2, 'n_ssm%d' % i, [128, 1], F32) for i in range(2)]
                identf = self.c['ident_f']
            G, T = self.mod_tiles(s2, l, which, gname)
            xt = [P.sbuf(s2, 'n_x%d' % i, [128, D], F32) for i in range(2)]
            junk = P.sbuf(s2, 'n_junk', [128, D], BF16)
            hn = [P.sbuf(s2, 'n_hn%d' % i, [128, D], F32) for i in range(2)]
            hb = [P.sbuf(s2, 'n_hb%d' % i, [128, D], BF16) for i in range(2)]
            ssq = [P.sbuf(s2, 'n_ssq%d' % i, [128, 1], F32) for i in range(2)]
            lnv = [P.sbuf(s2, 'n_ln%d' % i, [128, 1], F32) for i in range(2)]
            rstd = [P.sbuf(s2, 'n_rs%d' % i, [128, 1], F32) for i in range(2)]
            if h2b_all is None:
                pT = [P.psum(s2, 'n_pT%d' % i, [128, 8, 128], BF16) for i in range(2)]
            ident = self.c['ident_bf']
            for t in range(self.NT):
                i = t % 2
                src = xsrc.t.ap()[t * 128:(t + 1) * 128, :]
                P.op('sync', lambda e, i=i, src=src: e.dma_start(out=xt[i][:], in_=src), r=[xsrc], w=[xt[i]],
                     dsem=xt[i])
                P.op('scalar', lambda e, i=i: e.activation(out=junk[:], in_=xt[i][:], func=AF.Square,
                                                          accum_out=ssq[i][:]), r=[xt[i]], w=[junk, ssq[i]])
                self.rstd_from_ssq(ssq[i], lnv[i], rstd[i], D)
                P.op('vector', lambda e, i=i: e.scalar_tensor_tensor(
                    out=hn[i][:], in0=xt[i][:], scalar=rstd[i][:, 0:1], in1=G[:], op0=ALU.mult, op1=ALU.mult),
                    r=[xt[i], rstd[i], G], w=[hn[i]])
                if not router:
                    P.op('gpsimd', lambda e, i=i: e.tensor_tensor(out=hb[i][:], in0=hn[i][:], in1=T[:], op=ALU.add),
                         r=[hn[i], T], w=[hb[i]])
                else:
                    def _router(t, i, h32T, pL, lg, ex, t8, nmx, ssm):
                        P.op('gpsimd', lambda e, i=i: e.tensor_tensor(out=h32[i][:], in0=hn[i][:], in1=T[:], op=ALU.add),
                             r=[hn[i], T], w=[h32[i]])
                        if h2b_all is None:
                            P.op('scalar', lambda e, i=i: e.copy(out=hb[i][:], in_=h32[i][:]), r=[h32[i]], w=[hb[i]])
                        else:
                            P.op('scalar', lambda e, i=i, t=t: e.copy(out=h2b_all[:, t, :], in_=h32[i][:]),
                                 r=[h32[i]], w=[h2b_all])

                        def trf(e, i=i):
                            ins = None
                            for k in range(8):
                                ins = e.transpose(pR[:, k, :], h32[i][:, k * 128:(k + 1) * 128], identf[:])
                            return ins
                        P.op('tensor', trf, r=[h32[i], identf], w=[pR])
                        P.op('scalar', lambda e: e.copy(out=h32T[:], in_=pR[:]), r=[pR], w=[h32T])

                        def mmr(e):
                            ins = None
                            for k in range(8):
                                ins = e.matmul(pL[:, 0:NE], h32T[:, k, :], rw[:, k, :], start=(k == 0), stop=(k == 7))
                            return ins
                        P.op('tensor', mmr, r=[h32T, rw], w=[pL])
                        P.op('vector', lambda e: e.tensor_tensor(out=lg[:], in0=pL[:, 0:NE], in1=rb[:], op=ALU.add),
                             r=[pL, rb], w=[lg])
                        P.op('vector', lambda e: e.max(out=t8[:], in_=lg[:]), r=[lg], w=[t8])
                        mb = self.memb
                        cb = self.comb
                        P.op('vector', lambda e, t=t: e.tensor_scalar(out=mb[:, t, :], in0=lg[:], scalar1=t8[:, 3:4],
                                                                     scalar2=None, op0=ALU.is_ge), r=[lg, t8], w=[mb])
                        P.op('vector', lambda e: e.tensor_scalar(out=nmx[:], in0=t8[:, 0:1], scalar1=-1.0, scalar2=None,
                                                                 op0=ALU.mult), r=[t8], w=[nmx])
                        P.op('scalar', lambda e: e.activation(out=ex[:], in_=lg[:], func=AF.Exp, bias=nmx[:, 0:1]),
                             r=[lg, nmx], w=[ex])
                        P.op('vector', lambda e, t=t: e.tensor_tensor(out=ex[:], in0=ex[:], in1=mb[:, t, :], op=ALU.mult),
                             r=[ex, mb], w=[ex])
                        P.op('vector', lambda e: e.reduce_sum(out=ssm[:], in_=ex[:], axis=AX.X), r=[ex], w=[ssm])
                        P.op('vector', lambda e: e.reciprocal(out=ssm[:], in_=ssm[:]), r=[ssm], w=[ssm])
                        P.op('vector', lambda e, t=t: e.tensor_scalar(out=cb[:, t, :], in0=ex[:], scalar1=ssm[:, 0:1],
                                                                     scalar2=None, op0=ALU.mult), r=[ex, ssm], w=[cb])
                    _router(t, i, h32T_[i], pL_[i], lg_[i], ex_[i], t8_[i], nmx_[i], ssm_[i])
                if h2b_all is not None:
                    continue

                def tr(e, i=i):
                    ins = None
                    for k in range(8):
                        ins = e.transpose(pT[i][:, k, :], hb[i][:, k * 128:(k + 1) * 128], ident[:])
                    return ins
                P.op('tensor', tr, r=[hb[i], ident], w=[pT[i]])
                if hT_dram is None:
                    P.op('vector', lambda e, i=i, t=t: e.tensor_copy(out=hT[:, :, t * 128:(t + 1) * 128],
                                                                    in_=pT[i][:]), r=[pT[i]], w=[hT])
                else:
                    sg_ = stg[(t // 4) % 2]
                    tt = t % 4
                    P.op('vector', lambda e, i=i, tt=tt, sg_=sg_: e.tensor_copy(
                        out=sg_[:, :, tt * 128:(tt + 1) * 128], in_=pT[i][:]), r=[pT[i]], w=[sg_])
                    if tt == 3:
                        cc_ = t // 4
                        d_ap = hT_dram.t.ap().rearrange("(k p) s -> p k s", p=128)[:, :, cc_ * 512:(cc_ + 1) * 512]
                        P.op('sync', lambda e, sg_=sg_, d_ap=d_ap: e.dma_start(out=d_ap, in_=sg_[:]),
                             r=[sg_], w=[hT_dram], dsem=sg_)
            if h2b_all is not None:
                self.slots_and_scatter(s2, l, h2b_all)
            P.barrier()
            P.flush()

    def phase_proj(self, l, hT):
        P = self.P
        S = self.S
        inp = self.inp
        with ExitStack() as st:
            wbf = P.sbuf(st, 'p_w', [128, 8, DIN], BF16)
            wv = inp['w_in'].t.ap()[l].rearrange("(k p) n -> p k n", p=128)
            for k in range(8):
                P.op('gpsimd', lambda e, k=k: e.dma_start(out=wbf[:, k, :], in_=wv[:, k, :]), w=[wbf], dsem=wbf)
            gcol = {}
            for nm in ['sb_q_g', 'sb_k_g', 'fox_q_g', 'fox_k_g']:
                g = P.sbuf(st, 'p_' + nm, [128, 1], F32)
                for hh in range(2):
                    src = inp[nm].t.ap()[l].rearrange("(d o) -> d o", o=1)
                    P.op('sync', lambda e, g=g, hh=hh, src=src: e.dma_start(out=g[hh * 64:(hh + 1) * 64, :], in_=src),
                         w=[g], dsem=g)
                if nm.endswith('q_g'):
                    P.op('vector', lambda e, g=g: e.tensor_scalar(out=g[:], in0=g[:], scalar1=0.125, scalar2=None,
                                                                 op0=ALU.mult), r=[g], w=[g])
                gcol[nm] = g
            cw = P.sbuf(st, 'p_cw', [128, 2, 3], F32)
            for j in range(2):
                for i in range(3):
                    src = inp['conv_w'].t.ap()[l, i, j * 128:(j + 1) * 128].rearrange("(d o) -> d o", o=1)
                    P.op('sync', lambda e, j=j, i=i, src=src: e.dma_start(out=cw[:, j, i:i + 1], in_=src),
                         w=[cw], dsem=cw)
            og = P.sbuf(st, 'p_og', [128, 8], F32)
            src_og = inp['out_norm_g'].t.ap()[l].rearrange("(k p) -> p k", p=128)
            P.op('sync', lambda e: e.dma_start(out=og[:], in_=src_og, allow_slow_non_contiguous=True), w=[og], dsem=og)
            self.og_keep = None
            fb = P.sbuf(st, 'p_fb', [6, 1], F32)
            src_fb = inp['fox_f_b'].t.ap()[l].rearrange("(d o) -> d o", o=1)
            P.op('sync', lambda e: e.dma_start(out=fb[:], in_=src_fb), w=[fb], dsem=fb)
            P.op('vector', lambda e: e.tensor_scalar(out=fb[:], in0=fb[:], scalar1=-1.0, scalar2=None, op0=ALU.mult),
                 r=[fb], w=[fb])

            blk = self.c['blk64']
            pq = [P.psum(st, 'p_pq%d' % i, [128, 512]) for i in range(2)]
            pm = [P.psum(st, 'p_pm%d' % i, [128, 512]) for i in range(2)]
            sq = [P.sbuf(st, 'p_sq%d' % i, [128, 512], F32) for i in range(2)]
            rs = [P.sbuf(st, 'p_rs%d' % i, [128, 512], F32) for i in range(2)]
            qo = [P.sbuf(st, 'p_qo%d' % i, [128, 512], BF16) for i in range(2)]
            it = 0

            def proj_mm(ps, c0, ncols, n):
                def mm(e):
                    ins = None
                    for k in range(8):
                        ins = e.matmul(ps[0:ncols, :], wbf[:, k, c0:c0 + ncols], hT[:, k, n * 512:(n + 1) * 512],
                                       start=(k == 0), stop=(k == 7))
                    return ins
                P.op('tensor', mm, r=[wbf, hT], w=[ps])

            qk_tiles = []
            for m in range(3):
                qk_tiles.append((768 + m * 128, 'sb_q_g', self.qT_sb, m * 128, None))
                qk_tiles.append((1152 + m * 128, 'sb_k_g', self.kT_sb, m * 128, None))
                qk_tiles.append((1920 + m * 128, 'fox_q_g', self.qTa, None, 2 * m))
                qk_tiles.append((2304 + m * 128, 'fox_k_g', self.kTa, None, 2 * m))
            for (c0, gname, dst, row0, fh) in qk_tiles:
                for n in range(self.NC):
                    i = it % 2
                    it += 1
                    proj_mm(pq[i], c0, 128, n)
                    P.op('scalar', lambda e, i=i: e.activation(out=sq[i][:], in_=pq[i][:], func=AF.Square),
                         r=[pq[i]], w=[sq[i]])
                    P.op('tensor', lambda e, i=i: e.matmul(pm[i][:], blk[:], sq[i][:], start=True, stop=True),
                         r=[blk, sq[i]], w=[pm[i]])
                    P.op('scalar', lambda e, i=i: e.activation(out=rs[i][:], in_=pm[i][:], func=AF.Ln, bias=EPS),
                         r=[pm[i]], w=[rs[i]])
                    P.op('scalar', lambda e, i=i: e.activation(out=rs[i][:], in_=rs[i][:], func=AF.Exp, scale=-0.5),
                         r=[rs[i]], w=[rs[i]])
                    g = gcol[gname]
                    P.op('vector', lambda e, i=i, g=g: e.scalar_tensor_tensor(
                        out=qo[i][:], in0=pq[i][:], scalar=g[:, 0:1], in1=rs[i][:], op0=ALU.mult, op1=ALU.mult),
                        r=[pq[i], g, rs[i]], w=[qo[i]])
                    if row0 is not None:
                        d_ap = dst.t.ap()[row0:row0 + 128, n * 512:(n + 1) * 512]
                        P.op('sync', lambda e, i=i, d_ap=d_ap: e.dma_start(out=d_ap, in_=qo[i][:]),
                             r=[qo[i]], w=[dst], dsem=qo[i])
                    else:
                        for hh in range(2):
                            d_ap = dst.t.ap()[fh + hh, 0:64, n * 512:(n + 1) * 512]
                            P.op('sync', lambda e, i=i, hh=hh, d_ap=d_ap: e.dma_start(
                                out=d_ap, in_=qo[i][hh * 64:(hh + 1) * 64, :]), r=[qo[i]], w=[dst], dsem=qo[i])

            pc = [P.psum(st, 'p_pc%d' % i, [128, 512]) for i in range(3)]
            vb = P.sbuf(st, 'p_vb', [128, 514], F32)
            ccs = P.sbuf(st, 'p_ccs', [128, 512], F32)
            yc = P.sbuf(st, 'p_yc', [128, 512], F32)
            for j in range(2):
                P.op('vector', lambda e: e.memset(vb[:, 0:2], 0.0), w=[vb])
                for n in range(self.NC):
                    i = it % 2
                    it += 1
                    for q3 in range(3):
                        proj_mm(pc[q3], q3 * 256 + j * 128, 128, n)
                    if n > 0:
                        P.op('vector', lambda e: e.tensor_copy(out=vb[:, 0:2], in_=vb[:, 512:514]), r=[vb], w=[vb])
                    P.op('scalar', lambda e: e.copy(out=ccs[:], in_=pc[1][:]), r=[pc[1]], w=[ccs])
                    P.op('vector', lambda e: e.tensor_tensor(out=vb[:, 2:514], in0=pc[2][:], in1=ccs[:], op=ALU.mult),
                         r=[pc[2], ccs], w=[vb])
                    P.op('vector', lambda e, j=j: e.tensor_scalar(out=yc[:], in0=vb[:, 0:512], scalar1=cw[:, j, 0:1],
                                                                 scalar2=None, op0=ALU.mult), r=[vb, cw], w=[yc])
                    P.op('vector', lambda e, j=j: e.scalar_tensor_tensor(
                        out=yc[:], in0=vb[:, 1:513], scalar=cw[:, j, 1:2], in1=yc[:], op0=ALU.mult, op1=ALU.add),
                        r=[vb, cw, yc], w=[yc])
                    P.op('vector', lambda e, j=j: e.scalar_tensor_tensor(
                        out=yc[:], in0=vb[:, 2:514], scalar=cw[:, j, 2:3], in1=yc[:], op0=ALU.mult, op1=ALU.add),
                        r=[vb, cw, yc], w=[yc])
                    P.op('vector', lambda e: e.tensor_tensor(out=yc[:], in0=pc[0][:], in1=yc[:], op=ALU.mult),
                         r=[pc[0], yc], w=[yc])
                    P.op('scalar', lambda e, i=i: e.activation(out=sq[i][:], in_=yc[:], func=AF.Square),
                         r=[yc], w=[sq[i]])
                    P.op('tensor', lambda e, i=i: e.matmul(pm[i][:], blk[:], sq[i][:], start=True, stop=True),
                         r=[blk, sq[i]], w=[pm[i]])
                    P.op('scalar', lambda e, i=i: e.activation(out=rs[i][:], in_=pm[i][:], func=AF.Ln, bias=EPS),
                         r=[pm[i]], w=[rs[i]])
                    P.op('scalar', lambda e, i=i: e.activation(out=rs[i][:], in_=rs[i][:], func=AF.Exp, scale=-0.5),
                         r=[rs[i]], w=[rs[i]])
                    P.op('vector', lambda e, i=i, j=j: e.scalar_tensor_tensor(
                        out=qo[i][:], in0=yc[:], scalar=og[:, j:j + 1], in1=rs[i][:], op0=ALU.mult, op1=ALU.mult),
                        r=[yc, og, rs[i]], w=[qo[i]])
                    d_ap = self.yT.t.ap()[j * 128:(j + 1) * 128, n * 512:(n + 1) * 512]
                    P.op('sync', lambda e, i=i, d_ap=d_ap: e.dma_start(out=d_ap, in_=qo[i][:]),
                         r=[qo[i]], w=[self.yT], dsem=qo[i])

            pf = pc[0]
            fe = P.sbuf(st, 'p_fe', [6, 512], F32)
            fc = P.sbuf(st, 'p_fc', [6, 512 + 1], F32)
            r1 = P.sbuf(st, 'p_r1', [6, 512], F32)
            pcs = [P.sbuf(st, 'p_pc%d' % i, [6, 512], BF16) for i in range(3)]
            npcs = [P.sbuf(st, 'p_npc%d' % i, [6, 512], BF16) for i in range(3)]
            ones = self.c['ones']
            onesb = self.c['onesb']
            P.op('vector', lambda e: e.memset(fc[:, 0:1], 0.0), w=[fc])
            for n in range(self.NC):
                proj_mm(pf, 3072, 6, n)
                P.op('scalar', lambda e: e.activation(out=fe[:], in_=pf[0:6, :], func=AF.Exp, bias=fb[:, 0:1],
                                                      scale=-1.0), r=[pf, fb], w=[fe])
                P.op('scalar', lambda e: e.activation(out=fe[:], in_=fe[:], func=AF.Ln, bias=1.0), r=[fe], w=[fe])
                if n > 0:
                    P.op('vector', lambda e: e.tensor_copy(out=fc[:, 0:1], in_=fc[:, 512:513]), r=[fc], w=[fc])
                P.op('vector', lambda e: e.tensor_tensor_scan(out=fc[:, 1:513], data0=ones[0:6, 0:512], data1=fe[:],
                                                              initial=fc[:, 0:1], op0=ALU.mult, op1=ALU.add),
                     r=[ones, fe, fc], w=[fc])
                P.op('vector', lambda e: e.tensor_copy(out=pcs[0][:], in_=fc[:, 1:513]), r=[fc], w=[pcs[0]])
                P.op('vector', lambda e: e.tensor_tensor(out=r1[:], in0=fc[:, 1:513], in1=pcs[0][:], op=ALU.subtract),
                     r=[fc, pcs[0]], w=[r1])
                P.op('vector', lambda e: e.tensor_copy(out=pcs[1][:], in_=r1[:]), r=[r1], w=[pcs[1]])
                P.op('vector', lambda e: e.tensor_tensor(out=r1[:], in0=r1[:], in1=pcs[1][:], op=ALU.subtract),
                     r=[r1, pcs[1]], w=[r1])
                P.op('vector', lambda e: e.tensor_copy(out=pcs[2][:], in_=r1[:]), r=[r1], w=[pcs[2]])
                for q3 in range(3):
                    P.op('vector', lambda e, q3=q3: e.tensor_scalar(out=npcs[q3][:], in0=pcs[q3][:], scalar1=-1.0,
                                                                   scalar2=None, op0=ALU.mult),
                         r=[pcs[q3]], w=[npcs[q3]])
                    sl = slice(n * 512, (n + 1) * 512)
                    dq = self.qTa.t.ap()[:, 64 + q3, sl]
                    dk = self.kTa.t.ap()[:, 67 + q3, sl]
                    P.op('sync', lambda e, q3=q3, dq=dq: e.dma_start(out=dq, in_=npcs[q3][:]),
                         r=[npcs[q3]], w=[self.qTa], dsem=npcs[q3])
                    P.op('sync', lambda e, q3=q3, dk=dk: e.dma_start(out=dk, in_=pcs[q3][:]),
                         r=[pcs[q3]], w=[self.kTa], dsem=pcs[q3])
                    dq1 = self.qTa.t.ap()[:, 67 + q3, sl]
                    dk1 = self.kTa.t.ap()[:, 64 + q3, sl]
                    P.op('sync', lambda e, dq1=dq1: e.dma_start(out=dq1, in_=onesb[0:6, 0:512]),
                         r=[onesb], w=[self.qTa], dsem=onesb)
                    P.op('sync', lambda e, dk1=dk1: e.dma_start(out=dk1, in_=onesb[0:6, 0:512]),
                         r=[onesb], w=[self.kTa], dsem=onesb)

            vo = [P.sbuf(st, 'p_vo%d' % i, [128, 768], BF16) for i in range(2)]
            for t in range(self.NT):
                i = t % 2

                def mmv(e, t=t, i=i):
                    ins = None
                    for (ps, c0) in ((pq[i], 1536), (pm[i], 2688)):
                        for k in range(8):
                            ins = e.matmul(ps[:, 0:384], hT[:, k, t * 128:(t + 1) * 128], wbf[:, k, c0:c0 + 384],
                                           start=(k == 0), stop=(k == 7))
                    return ins
                P.op('tensor', mmv, r=[wbf, hT], w=[pq[i], pm[i]])
                P.op('scalar', lambda e, i=i: e.copy(out=vo[i][:, 0:384], in_=pq[i][:, 0:384]), r=[pq[i]], w=[vo[i]])
                P.op('vector', lambda e, i=i: e.tensor_copy(out=vo[i][:, 384:768], in_=pm[i][:, 0:384]),
                     r=[pm[i]], w=[vo[i]])
                d1 = self.v_sb.t.ap()[t * 128:(t + 1) * 128, :]
                d2 = self.v_fox.t.ap()[t * 128:(t + 1) * 128, :]
                P.op('sync', lambda e, i=i, d1=d1: e.dma_start(out=d1, in_=vo[i][:, 0:384]), r=[vo[i]],
                     w=[self.v_sb], dsem=vo[i])
                P.op('sync', lambda e, i=i, d2=d2: e.dma_start(out=d2, in_=vo[i][:, 384:768]), r=[vo[i]],
                     w=[self.v_fox], dsem=vo[i])
            P.barrier()
            P.flush()


    def head_norm_part1(self, st_bufs, po, nrow):
        P = self.P
        osb, sq, pn, rs, yo = st_bufs
        P.op('scalar', lambda e: e.copy(out=osb[0:64, :], in_=po[0:64, :]), r=[po], w=[osb])
        P.op('scalar', lambda e: e.activation(out=sq[0:nrow, :], in_=po[0:nrow, :], func=AF.Square), r=[po], w=[sq])

    def head_norm_part2(self, st_bufs, nrow, lhsT_ap, gcol_ap, drow, c, eps_bias):
        P = self.P
        osb, sq, pn, rs, yo = st_bufs
        P.op('tensor', lambda e: e.matmul(pn[0:64, :], lhsT_ap, sq[0:nrow, :], start=True, stop=True),
             r=[sq], w=[pn])
        P.op('scalar', lambda e: e.activation(out=rs[0:64, :], in_=pn[0:64, :], func=AF.Ln, bias=eps_bias),
             r=[pn], w=[rs])
        P.op('scalar', lambda e: e.activation(out=rs[0:64, :], in_=rs[0:64, :], func=AF.Exp, scale=-0.5),
             r=[rs], w=[rs])
        P.op('vector', lambda e: e.scalar_tensor_tensor(out=yo[0:64, :], in0=osb[0:64, :], scalar=gcol_ap,
                                                       in1=rs[0:64, :], op0=ALU.mult, op1=ALU.mult),
             r=[osb, rs], w=[yo])
        d_ap = self.yT.t.ap()[drow:drow + 64, c * 512:(c + 1) * 512]
        P.op('sync', lambda e: e.dma_start(out=d_ap, in_=yo[0:64, :]), r=[yo], w=[self.yT], dsem=yo)

    def phase_attn(self, l):
        P = self.P
        S = self.S
        NT = self.NT
        inp = self.inp
        with ExitStack() as st:
            ogs = P.sbuf(st, 't_og', [64, 12], F32)
            src_og = inp['out_norm_g'].t.ap()[l, 256:1024].rearrange("(h d) -> d h", d=64)
            P.op('sync', lambda e: e.dma_start(out=ogs[:], in_=src_og, allow_slow_non_contiguous=True),
                 w=[ogs], dsem=ogs)
            kinds = ('sb', 'fox')
            kT = {k: [P.sbuf(st, 't_kT%s%d' % (k, i), [70, S], BF16) for i in range(2)] for k in kinds}
            qT = {k: [P.sbuf(st, 't_qT%s%d' % (k, i), [70, S], BF16) for i in range(2)] for k in kinds}
            vv = {k: [P.sbuf(st, 't_v%s%d' % (k, i), [128, NT, 65], BF16) for i in range(2)] for k in kinds}
            for i in range(2):
                P.op('vector', lambda e, i=i: e.memset(vv['fox'][i][:], 1.0), w=[vv['fox'][i]])
            pzs = [P.psum(st, 't_pzs%d' % i, [128, 512]) for i in range(2)]
            pbs = [P.psum(st, 't_pbs%d' % i, [128, 512]) for i in range(2)]
            pzf = P.psum(st, 't_pzf', [128, 512])
            po = {k: P.psum(st, 't_po' + k, [128, 512]) for k in kinds}
            pn = P.psum(st, 't_pn', [128, 512])
            e32 = [P.sbuf(st, 't_e%d' % i, [128, 512], F32) for i in range(2)]
            e32f = P.sbuf(st, 't_ef', [128, 512], F32)
            ec = [P.sbuf(st, 't_ec%d' % i, [128, 512], BF16) for i in range(2)]
            sp = [P.sbuf(st, 't_sp%d' % i, [128, 512], BF16) for i in range(2)]
            wt = {k: [P.sbuf(st, 't_w%s%d' % (k, i), [128, 512], BF16) for i in range(2)] for k in kinds}
            lacc = P.sbuf(st, 't_lacc', [128, 512], F32)
            laccb = [P.sbuf(st, 't_laccb%d' % i, [128, 512], BF16) for i in range(2)]
            nbufs = {}
            for k in kinds:
                nbufs[k] = (P.sbuf(st, 't_osb' + k, [64, 512], F32), P.sbuf(st, 't_sq' + k, [65, 512], F32), pn,
                            P.sbuf(st, 't_rs' + k, [64, 512], F32), P.sbuf(st, 't_yo' + k, [64, 512], BF16))
            negtri = self.c['negtri']
            negones = self.c['negones']
            mask_s = self.c['mask_s']
            mask_n = self.c['mask_n']
            blk = self.c['blk64']
            wn65 = self.c['wn65']
            its = []
            for c in range(self.NC):
                nkb = 4 * c + 4
                for idx, kb in enumerate(reversed(range(nkb))):
                    its.append((c, kb, idx == 0, kb == 0))
            def head_loads(h):
                slot = h % 2
                for kind in kinds:
                    K_ = 64 if kind == 'sb' else 70
                    if kind == 'sb':
                        ksrc = self.kT_sb.t.ap()[h * 64:(h + 1) * 64, :]
                        qsrc = self.qT_sb.t.ap()[h * 64:(h + 1) * 64, :]
                        vsrc = self.v_sb.t.ap()[:, h * 64:(h + 1) * 64].rearrange("(t p) d -> p t d", p=128)
                        srcs = (self.kT_sb, self.qT_sb, self.v_sb)
                    else:
                        ksrc = self.kTa.t.ap()[h]
                        qsrc = self.qTa.t.ap()[h]
                        vsrc = self.v_fox.t.ap()[:, h * 64:(h + 1) * 64].rearrange("(t p) d -> p t d", p=128)
                        srcs = (self.kTa, self.qTa, self.v_fox)
                    kt_, qt_, v_ = kT[kind][slot], qT[kind][slot], vv[kind][slot]
                    P.op('sync', lambda e, kt_=kt_, ksrc=ksrc, K_=K_: e.dma_start(out=kt_[0:K_, :], in_=ksrc),
                         r=[srcs[0]], w=[kt_], dsem=kt_)
                    P.op('sync', lambda e, qt_=qt_, qsrc=qsrc, K_=K_: e.dma_start(out=qt_[0:K_, :], in_=qsrc),
                         r=[srcs[1]], w=[qt_], dsem=qt_)
                    P.op('sync', lambda e, v_=v_, vsrc=vsrc: e.dma_start(out=v_[:, :, 0:64], in_=vsrc),
                         r=[srcs[2]], w=[v_], dsem=v_)

            head_loads(0)
            for h in range(6):
                slot = h % 2
                if h + 1 < 6:
                    head_loads(h + 1)

                def stageA(kind, n, slot=slot):
                    c, kb, first, last = its[n]
                    i = n % 2
                    j = kb - 4 * c
                    kt_, qt_ = kT[kind][slot], qT[kind][slot]
                    if kind == 'sb':
                        pz = pzs[i]
                        P.op('tensor', lambda e: e.matmul(pz[:], kt_[0:64, kb * 128:(kb + 1) * 128],
                                                          qt_[0:64, c * 512:(c + 1) * 512], start=True, stop=True),
                             r=[kt_, qt_], w=[pz])
                        P.op('scalar', lambda e: e.activation(out=e32[i][:], in_=pz[:], func=AF.Exp),
                             r=[pz], w=[e32[i]])
                        P.op('scalar', lambda e: e.activation(out=sp[i][:], in_=e32[i][:], func=AF.Ln, bias=1.0),
                             r=[e32[i]], w=[sp[i]])
                        if j >= 0:
                            P.op('gpsimd', lambda e: e.tensor_tensor(out=sp[i][:], in0=sp[i][:], in1=mask_s[:, j, :],
                                                                    op=ALU.mult), r=[sp[i], mask_s], w=[sp[i]])
                        if not last:
                            ln_ = laccb[(n + 1) % 2]
                            if first:
                                P.op('vector', lambda e: e.tensor_copy(out=lacc[:], in_=sp[i][:]), r=[sp[i]], w=[lacc])
                            else:
                                P.op('vector', lambda e: e.tensor_tensor(out=lacc[:], in0=lacc[:], in1=sp[i][:],
                                                                        op=ALU.add), r=[sp[i], lacc], w=[lacc])
                            P.op('vector', lambda e: e.tensor_copy(out=ln_[:], in_=lacc[:]), r=[lacc], w=[ln_])
                    else:
                        w_ = wt['fox'][i]
                        P.op('tensor', lambda e: e.matmul(pzf[:], kt_[0:70, kb * 128:(kb + 1) * 128],
                                                          qt_[0:70, c * 512:(c + 1) * 512], start=True, stop=True),
                             r=[kt_, qt_], w=[pzf])
                        if j >= 0:
                            P.op('vector', lambda e: e.tensor_scalar(out=e32f[:], in0=pzf[:], scalar1=60.0,
                                                                    scalar2=None, op0=ALU.min), r=[pzf], w=[e32f])
                            P.op('scalar', lambda e: e.activation(out=w_[:], in_=e32f[:], func=AF.Exp),
                                 r=[e32f], w=[w_])
                            P.op('gpsimd', lambda e: e.tensor_tensor(out=w_[:], in0=w_[:], in1=mask_n[:, j, :],
                                                                    op=ALU.mult), r=[w_, mask_n], w=[w_])
                        else:
                            P.op('scalar', lambda e: e.activation(out=w_[:], in_=pzf[:], func=AF.Exp),
                                 r=[pzf], w=[w_])

                def stageB1(n, slot=slot, h=h):
                    kind = 'sb'
                    c, kb, first, last = its[n]
                    i = n % 2
                    j = kb - 4 * c
                    w_ = wt[kind][i]
                    if True:
                        lb = laccb[n % 2]
                        pb = pbs[i]

                        def mm2(e):
                            ins = e.matmul(pb[:], negtri[:], sp[i][:], start=True, stop=first)
                            if not first:
                                ins = e.matmul(pb[:], negones[:], lb[:], start=False, stop=True)
                            return ins
                        P.op('tensor', mm2, r=[sp[i], negtri, negones] + ([] if first else [lb]), w=[pb])
                        P.op('scalar', lambda e: e.activation(out=ec[i][:], in_=pb[:], func=AF.Exp), r=[pb], w=[ec[i]])
                        if j >= 0:
                            P.op('gpsimd', lambda e: e.tensor_tensor(out=ec[i][:], in0=ec[i][:], in1=mask_s[:, j, :],
                                                                    op=ALU.mult), r=[ec[i], mask_s], w=[ec[i]])
                        P.op('vector', lambda e: e.tensor_tensor(out=w_[:], in0=e32[i][:], in1=ec[i][:], op=ALU.mult),
                             r=[e32[i], ec[i]], w=[w_])

                def stageB2(kind, n, slot=slot, h=h):
                    c, kb, first, last = its[n]
                    i = n % 2
                    v_ = vv[kind][slot]
                    pc_ = po[kind]
                    w_ = wt[kind][i]
                    nr = 64 if kind == 'sb' else 65
                    P.op('tensor', lambda e: e.matmul(pc_[0:nr, :], v_[:, kb, 0:nr], w_[:], start=first, stop=last),
                         r=[v_, w_], w=[pc_])
                    if last:
                        if kind == 'sb':
                            self.head_norm_part1(nbufs[kind], pc_, 64)
                            pending.append(lambda: self.head_norm_part2(nbufs['sb'], 64, blk[0:64, 0:64],
                                                                        ogs[:, h:h + 1], 256 + 64 * h, c, EPS))
                        else:
                            self.head_norm_part1(nbufs[kind], pc_, 65)
                            pending.append(lambda: self.head_norm_part2(nbufs['fox'], 65, wn65[:, :],
                                                                        ogs[:, 6 + h:7 + h], 640 + 64 * h, c, 0.0))

                stageA('sb', 0)
                stageA('fox', 0)
                pending = []
                for n in range(len(its)):
                    if self.sparse and n % 13 == 6:
                        self.issue_casts(l, 1)
                    stageB1(n)
                    if n + 1 < len(its):
                        stageA('sb', n + 1)
                        stageA('fox', n + 1)
                    todo, pending[:] = list(pending), []
                    for f in todo:
                        f()
                    stageB2('fox', n)
                    stageB2('sb', n)
                for f in pending:
                    f()
            if self.sparse:
                self.issue_casts(l, 2 * NE)
            P.barrier()
            P.flush()

    def phase_wout(self, l, xsrc, xdst):
        P = self.P
        inp = self.inp
        with ExitStack() as st:
            wo = P.sbuf(st, 'o_w', [128, 8, D], BF16)
            wv = inp['w_out'].t.ap()[l].rearrange("(k p) n -> p k n", p=128)
            for k in range(8):
                P.op('gpsimd', lambda e, k=k: e.dma_start(out=wo[:, k, :], in_=wv[:, k, :]), w=[wo], dsem=wo)
            g1 = self.load_bcast(st, 'o_g1', self.modv, l * 6 * D + 2 * D)
            yt = [P.sbuf(st, 'o_y%d' % i, [128, 8, 512], BF16) for i in range(2)]
            xt = [P.sbuf(st, 'o_x%d' % i, [128, D], F32) for i in range(2)]
            xo = [P.sbuf(st, 'o_xo%d' % i, [128, D], F32) for i in range(2)]
            py = [P.psum(st, 'o_p%d' % i, [128, 512]) for i in range(4)]
            yv = self.yT.t.ap().rearrange("(k p) s -> p k s", p=128)
            def load_y(c):
                yb = yt[c % 2]
                P.op('sync', lambda e: e.dma_start(out=yb[:], in_=yv[:, :, c * 512:(c + 1) * 512]),
                     r=[self.yT], w=[yb], dsem=yb)
            load_y(0)
            for c in range(self.NC):
                yb = yt[c % 2]
                if c + 1 < self.NC:
                    load_y(c + 1)
                for tt in range(4):
                    t = c * 4 + tt
                    i = t % 2
                    src = xsrc.t.ap()[t * 128:(t + 1) * 128, :]
                    P.op('sync', lambda e, i=i, src=src: e.dma_start(out=xt[i][:], in_=src), r=[xsrc], w=[xt[i]],
                         dsem=xt[i])
                    for half in range(2):
                        pp = py[i * 2 + half]

                        def mm(e, yb=yb, tt=tt, half=half, pp=pp):
                            ins = None
                            for k in range(8):
                                ins = e.matmul(pp[:], yb[:, k, tt * 128:(tt + 1) * 128],
                                               wo[:, k, half * 512:(half + 1) * 512], start=(k == 0), stop=(k == 7))
                            return ins
                        P.op('tensor', mm, r=[yb, wo], w=[pp])
                        sl = slice(half * 512, (half + 1) * 512)
                        P.op('vector', lambda e, i=i, pp=pp, sl=sl: e.tensor_tensor(
                            out=xo[i][:, sl], in0=pp[:], in1=g1[:, sl], op=ALU.mult), r=[pp, g1], w=[xo[i]])
                    P.op('gpsimd', lambda e, i=i: e.tensor_tensor(out=xo[i][:], in0=xo[i][:], in1=xt[i][:], op=ALU.add),
                         r=[xo[i], xt[i]], w=[xo[i]])
                    dst = xdst.t.ap()[t * 128:(t + 1) * 128, :]
                    P.op('sync', lambda e, i=i, dst=dst: e.dma_start(out=dst, in_=xo[i][:]), r=[xo[i]], w=[xdst],
                         dsem=xo[i])
            P.barrier()
            P.flush()

    def load_expert(self, l, e_, w1b, w2b, b1c, b1s):
        P = self.P
        inp = self.inp
        w1v = inp['w_mlp1'].t.ap()[l, e_].rearrange("(k p) n -> p k n", p=128)
        w2v = inp['w_mlp2'].t.ap()[l, e_].rearrange("(p i) n -> p i n", i=8)
        for k in range(8):
            P.op('gpsimd', lambda e, k=k: e.dma_start(out=w1b[:, k, :], in_=w1v[:, k, :]), w=[w1b], dsem=w1b)
        for k in range(0, 8, 2):
            P.op('gpsimd', lambda e, k=k: e.dma_start(out=w2b[:, k:k + 2, :], in_=w2v[:, k:k + 2, :]),
                 w=[w2b], dsem=w2b)
        b1v = inp['b_mlp1'].t.ap()[l, e_].rearrange("(p m) -> p m", m=16)
        P.op('sync', lambda e: e.dma_start(out=b1c[:], in_=b1v), w=[b1c], dsem=b1c)

    def ffn_block(self, B, xT, w1b, w2b, b1c, b1s, evac, mid_hook=None):
        P = self.P
        aT = B['aT']
        CSIG = float(1.0 / (1.0 + np.exp(-1.702 * 7.0)))
        for i in range(8):
            q = B['it'] % 2
            B['it'] += 1
            pg, pl = B['pg'][q], B['pl'][q]
            sg, g, ll, tt = B['sg'][q], B['g'][q], B['l'][q], B['t'][q]

            def mm1(e, i=i, pg=pg, pl=pl):
                ins = None
                for (pp, j) in ((pg, 0), (pl, 1)):
                    for k in range(8):
                        ins = e.matmul(pp[:], w1b[:, k, 2 * i + j:2 * D:16], xT[:, k, :],
                                       start=(k == 0), stop=(k == 7))
                return ins
            P.op('tensor', mm1, r=[w1b, xT], w=[pg, pl])
            P.op('vector', lambda e, i=i, g=g, pg=pg: e.tensor_scalar(out=g[:], in0=pg[:], scalar1=b1c[:, 2 * i:2 * i + 1],
                                                                     scalar2=7.0, op0=ALU.add, op1=ALU.min),
                 r=[pg, b1c], w=[g])
            P.op('scalar', lambda e, sg=sg, g=g: e.activation(out=sg[:], in_=g[:], func=AF.Sigmoid, scale=1.702),
                 r=[g], w=[sg])
            P.op('vector', lambda e, i=i, ll=ll, pl=pl: e.tensor_scalar(out=ll[:], in0=pl[:], scalar1=b1c[:, 2 * i + 1:2 * i + 2],
                                                                       scalar2=7.0, op0=ALU.add, op1=ALU.min),
                 r=[pl, b1c], w=[ll])
            P.op('vector', lambda e, ll=ll: e.tensor_scalar(out=ll[:], in0=ll[:], scalar1=-7.0, scalar2=1.0,
                                                           op0=ALU.max, op1=ALU.add), r=[ll], w=[ll])
            P.op('gpsimd', lambda e, sg=sg, g=g, tt=tt: e.tensor_tensor(out=tt[:], in0=sg[:], in1=g[:], op=ALU.mult),
                 r=[sg, g], w=[tt])
            P.op('vector', lambda e, i=i, tt=tt, ll=ll: e.tensor_tensor(out=aT[:, i, :], in0=tt[:], in1=ll[:],
                                                                       op=ALU.mult), r=[tt, ll], w=[aT])
            if i == 4 and mid_hook is not None:
                mid_hook()
        for r_ in range(4):
            for half in range(2):
                pp = B['py'][B['ity'] % len(B['py'])]
                B['ity'] += 1

                def mm2(e, r_=r_, half=half, pp=pp):
                    ins = None
                    for k in range(8):
                        ins = e.matmul(pp[:], aT[:, k, r_ * 128:(r_ + 1) * 128],
                                       w2b[:, k, half * 512:(half + 1) * 512], start=(k == 0), stop=(k == 7))
                    return ins
                P.op('tensor', mm2, r=[aT, w2b], w=[pp])
                evac(r_, half, pp)

    def ffn_bufs(self, st, npy=4):
        P = self.P
        B = {'it': 0, 'ity': 0}
        B['aT'] = P.sbuf(st, 'f_aT', [128, 8, 512], BF16)
        B['pg'] = [P.psum(st, 'f_pg%d' % i, [128, 512]) for i in range(2)]
        B['pl'] = [P.psum(st, 'f_pl%d' % i, [128, 512]) for i in range(2)]
        B['py'] = [P.psum(st, 'f_py%d' % i, [128, 512]) for i in range(npy)]
        for nm in ('sg', 'g', 'l', 't'):
            B[nm] = [P.sbuf(st, 'f_%s%d' % (nm, i), [128, 512], F32) for i in range(2)]
        return B

    def phase_moe_dense(self, l, xsrc, xdst):
        P = self.P
        inp = self.inp
        NT = self.NT
        with ExitStack() as st:
            B = self.ffn_bufs(st)
            w1b = [P.sbuf(st, 'd_w1%d' % i, [128, 8, 2 * D], BF16) for i in range(2)]
            w2b = [P.sbuf(st, 'd_w2%d' % i, [128, 8, D], BF16) for i in range(2)]
            b1c = [P.sbuf(st, 'd_b1c%d' % i, [128, 16], F32) for i in range(2)]
            b1s = [P.sbuf(st, 'd_b1s%d' % i, [128, 8], F32) for i in range(2)]
            xT = [P.sbuf(st, 'd_xT%d' % i, [128, 8, 512], BF16) for i in range(2)]
            stage = [P.sbuf(st, 'd_st%d' % i, [128, D], F32) for i in range(4)]
            hv = self.h2T_d.t.ap().rearrange("(k p) s -> p k s", p=128)
            accd = self.accd
            with ExitStack() as s2:
                b2s = P.sbuf(s2, 'd_b2', [NE, D], F32)
                P.op('sync', lambda e: e.dma_start(out=b2s[:], in_=inp['b_mlp2'].t.ap()[l]), w=[b2s], dsem=b2s)
                cT = P.sbuf(s2, 'd_cT', [NE, 128], F32)
                identf = self.c['ident_f']
                pt = B['pg'][0]
                for t in range(NT):
                    P.op('tensor', lambda e, t=t: e.transpose(pt[0:NE, 0:128], self.comb[:, t, :], identf[:]),
                         r=[self.comb, identf], w=[pt])
                    P.op('scalar', lambda e: e.copy(out=cT[:], in_=pt[0:NE, 0:128]), r=[pt], w=[cT])
                    sg_ = stage[t % 4]
                    for half in range(2):
                        pp = B['py'][half]
                        P.op('tensor', lambda e, pp=pp, half=half: e.matmul(
                            pp[:], cT[:], b2s[:, half * 512:(half + 1) * 512], start=True, stop=True),
                            r=[cT, b2s], w=[pp])
                        P.op('vector', lambda e, pp=pp, half=half, sg_=sg_: e.tensor_copy(
                            out=sg_[:, half * 512:(half + 1) * 512], in_=pp[:]), r=[pp], w=[sg_])
                    dst = accd.t.ap()[t * 128:(t + 1) * 128, :]
                    P.op('gpsimd', lambda e, sg_=sg_, dst=dst: e.dma_start(out=dst, in_=sg_[:]), r=[sg_], w=[accd],
                         dsem=sg_)
            blk = 0
            for e_ in range(NE):
                ws = e_ % 2
                self.load_expert(l, e_, w1b[ws], w2b[ws], b1c[ws], b1s[ws])
                for c in range(self.NC):
                    xb = xT[blk % 2]
                    blk += 1
                    P.op('sync', lambda e, xb=xb, c=c: e.dma_start(out=xb[:], in_=hv[:, :, c * 512:(c + 1) * 512]),
                         r=[self.h2T_d], w=[xb], dsem=xb)

                    def evac(r_, half, pp, c=c, e_=e_):
                        t = c * 4 + r_
                        sg_ = stage[r_]
                        sl = slice(half * 512, (half + 1) * 512)
                        P.op('vector', lambda e: e.tensor_scalar(out=sg_[:, sl], in0=pp[:],
                                                                 scalar1=self.comb[:, t, e_:e_ + 1], scalar2=None,
                                                                 op0=ALU.mult), r=[pp, self.comb], w=[sg_])
                        if half == 1:
                            dst = accd.t.ap()[t * 128:(t + 1) * 128, :]
                            P.op('gpsimd', lambda e: e.dma_start(out=dst, in_=sg_[:], accum_op=ALU.add),
                                 r=[sg_], w=[accd], dsem=sg_)
                    self.ffn_block(B, xb, w1b[ws], w2b[ws], b1c[ws], b1s[ws], evac)
            P.barrier()
            P.flush()
        self.phase_final(l, xsrc, xdst)

    def phase_final(self, l, xsrc, xdst):
        P = self.P
        with ExitStack() as st:
            g2 = self.load_bcast(st, 'z_g2', self.modv, l * 6 * D + 5 * D)
            xt = [P.sbuf(st, 'z_x%d' % i, [128, D], F32) for i in range(2)]
            at = [P.sbuf(st, 'z_a%d' % i, [128, D], F32) for i in range(2)]
            for t in range(self.NT):
                i = t % 2
                rows = slice(t * 128, (t + 1) * 128)
                P.op('sync', lambda e, i=i, rows=rows: e.dma_start(out=xt[i][:], in_=xsrc.t.ap()[rows, :]),
                     r=[xsrc], w=[xt[i]], dsem=xt[i])
                P.op('sync', lambda e, i=i, rows=rows: e.dma_start(out=at[i][:], in_=self.accd.t.ap()[rows, :]),
                     r=[self.accd], w=[at[i]], dsem=at[i])
                P.op('vector', lambda e, i=i: e.tensor_tensor(out=at[i][:], in0=at[i][:], in1=g2[:], op=ALU.mult),
                     r=[at[i], g2], w=[at[i]])
                P.op('gpsimd', lambda e, i=i: e.tensor_tensor(out=at[i][:], in0=at[i][:], in1=xt[i][:], op=ALU.add),
                     r=[at[i], xt[i]], w=[at[i]])
                P.op('sync', lambda e, i=i, rows=rows: e.dma_start(out=xdst.t.ap()[rows, :], in_=at[i][:]),
                     r=[at[i]], w=[xdst], dsem=at[i])
            P.barrier()
            P.flush()


    def slots_and_scatter(self, st, l, h2b_all):
        P = self.P
        NT = self.NT
        NBLK = self.NBLK
        memb, comb = self.memb, self.comb
        onesf = self.c['ones']
        stri = self.c['stri_f']
        blkstart = self.c['blkstart']
        pcn = P.psum(st, 'q_pcn', [128, 512])
        cnt = P.sbuf(st, 'q_cnt', [128, NE], F32)
        nb = P.sbuf(st, 'q_nb', [128, NE], F32)
        incl = P.sbuf(st, 'q_incl', [128, NE], F32)
        off = P.sbuf(st, 'q_off', [128, NE], F32)
        texp = P.sbuf(st, 'q_texp', [128, NBLK], F32)

        def mmc(e):
            ins = None
            for t in range(NT):
                ins = e.matmul(pcn[:, 0:NE], onesf[:, 0:128], memb[:, t, :], start=(t == 0), stop=(t == NT - 1))
            return ins
        P.op('tensor', mmc, r=[onesf, memb], w=[pcn])
        P.op('vector', lambda e: e.tensor_copy(out=cnt[:], in_=pcn[:, 0:NE]), r=[pcn], w=[cnt])
        P.op('vector', lambda e: e.tensor_scalar(out=nb[:], in0=cnt[:], scalar1=0.0, scalar2=None, op0=ALU.is_gt),
             r=[cnt], w=[nb])
        for k in range(1, self.S // TS):
            P.op('vector', lambda e, k=k: e.scalar_tensor_tensor(out=nb[:], in0=cnt[:], scalar=float(k * TS),
                                                                in1=nb[:], op0=ALU.is_gt, op1=ALU.add),
                 r=[cnt, nb], w=[nb])
        P.op('vector', lambda e: e.tensor_scalar(out=nb[:], in0=nb[:], scalar1=float(TS), scalar2=None, op0=ALU.mult),
             r=[nb], w=[nb])
        P.op('vector', lambda e: e.tensor_tensor_scan(out=incl[:], data0=onesf[:, 0:NE], data1=nb[:], initial=0.0,
                                                      op0=ALU.mult, op1=ALU.add), r=[onesf, nb], w=[incl])
        P.op('vector', lambda e: e.tensor_tensor(out=off[:], in0=incl[:], in1=nb[:], op=ALU.subtract),
             r=[incl, nb], w=[off])
        P.op('vector', lambda e: e.tensor_scalar(out=texp[:], in0=blkstart[:, 0:NBLK], scalar1=incl[:, 0:1],
                                                 scalar2=None, op0=ALU.is_ge), r=[blkstart, incl], w=[texp])
        for e_ in range(1, NE):
            P.op('vector', lambda e, e_=e_: e.scalar_tensor_tensor(out=texp[:], in0=blkstart[:, 0:NBLK],
                                                                  scalar=incl[:, e_:e_ + 1], in1=texp[:],
                                                                  op0=ALU.is_ge, op1=ALU.add),
                 r=[blkstart, incl, texp], w=[texp])
        gar = P.sbuf(st, 'q_gar', [128, NBLK], F32)
        P.op('vector', lambda e: e.tensor_scalar(out=gar[:], in0=texp[:], scalar1=float(NE) - 0.5,
                                                 scalar2=self.c['pidx'][:, 2:3], op0=ALU.is_gt, op1=ALU.mult),
             r=[texp, self.c['pidx']], w=[gar])
        P.op('vector', lambda e: e.tensor_scalar(out=texp[:], in0=texp[:], scalar1=float(NE - 1), scalar2=None,
                                                 op0=ALU.min), r=[texp], w=[texp])
        pidx = self.c['pidx']
        t1 = P.sbuf(st, 'q_t1', [128, NBLK], F32)
        ixf = P.sbuf(st, 'q_ixf', [128, NBLK, 5], F32)
        BIG = float(1 << 15)
        P.op('vector', lambda e: e.tensor_scalar(out=t1[:], in0=texp[:], scalar1=128.0, scalar2=None,
                                                 op0=ALU.mult), r=[texp], w=[t1])
        P.op('vector', lambda e: e.scalar_tensor_tensor(out=t1[:], in0=gar[:], scalar=BIG, in1=t1[:], op0=ALU.mult,
                                                       op1=ALU.add), r=[gar, t1], w=[t1])
        P.op('vector', lambda e: e.tensor_scalar(out=ixf[:, :, 4], in0=t1[:], scalar1=pidx[:, 0:1], scalar2=None,
                                                 op0=ALU.add), r=[t1, pidx], w=[ixf])
        P.op('vector', lambda e: e.tensor_scalar(out=ixf[:, :, 1], in0=ixf[:, :, 4], scalar1=2.0, scalar2=None,
                                                 op0=ALU.mult), r=[ixf], w=[ixf])
        P.op('vector', lambda e: e.tensor_scalar(out=ixf[:, :, 2], in0=ixf[:, :, 4], scalar1=2.0, scalar2=1.0,
                                                 op0=ALU.mult, op1=ALU.add), r=[ixf], w=[ixf])
        P.op('vector', lambda e: e.tensor_scalar(out=ixf[:, :, 0], in0=ixf[:, :, 4], scalar1=float(l * NE * 128),
                                                 scalar2=None, op0=ALU.add), r=[ixf], w=[ixf])
        P.op('vector', lambda e: e.tensor_scalar(out=t1[:], in0=texp[:], scalar1=float(l * NE), scalar2=None,
                                                 op0=ALU.add), r=[texp], w=[t1])
        P.op('vector', lambda e: e.scalar_tensor_tensor(out=ixf[:, :, 3], in0=gar[:], scalar=BIG, in1=t1[:],
                                                       op0=ALU.mult, op1=ALU.add), r=[gar, t1], w=[ixf])
        widx = self.widx
        P.op('vector', lambda e: e.tensor_copy(out=widx[:], in_=ixf[:]), r=[ixf], w=[widx])
        macc = P.sbuf(st, 'q_macc', [128, NE], F32)
        pp = [P.psum(st, 'q_pp%d' % i, [128, 512]) for i in range(2)]
        tmp_ = [P.sbuf(st, 'q_tmp%d' % i, [128, NE], F32) for i in range(2)]
        val_ = [P.sbuf(st, 'q_val%d' % i, [128, NE], F32) for i in range(2)]
        oh_ = [P.sbuf(st, 'q_oh%d' % i, [128, 4, NE], F32) for i in range(2)]
        t8_ = [P.sbuf(st, 'q_t8%d' % i, [128, 8], F32) for i in range(2)]
        Xs = self.Xs

        def slot_tile(t, ppt, tmp, val, oh, t8, sl4v, g4v):
            def mmp(e):
                ins = e.matmul(ppt[:, 0:NE], stri[:], memb[:, t, :], start=True, stop=(t == 0))
                if t > 0:
                    ins = e.matmul(ppt[:, 0:NE], onesf[:, 0:128], macc[:], start=False, stop=True)
                return ins
            P.op('tensor', mmp, r=[stri, memb, onesf] + ([macc] if t > 0 else []), w=[ppt])
            if t == 0:
                P.op('vector', lambda e: e.tensor_copy(out=macc[:], in_=memb[:, 0, :]), r=[memb], w=[macc])
            else:
                P.op('vector', lambda e: e.tensor_tensor(out=macc[:], in0=macc[:], in1=memb[:, t, :], op=ALU.add),
                     r=[macc, memb], w=[macc])
            P.op('vector', lambda e: e.tensor_tensor(out=tmp[:], in0=ppt[:, 0:NE], in1=off[:], op=ALU.add),
                 r=[ppt, off], w=[tmp])
            P.op('vector', lambda e: e.scalar_tensor_tensor(out=val[:], in0=tmp[:], scalar=1.0, in1=memb[:, t, :],
                                                           op0=ALU.add, op1=ALU.mult), r=[tmp, memb], w=[val])
            P.op('vector', lambda e: e.max(out=t8[:], in_=val[:]), r=[val], w=[t8])
            P.op('vector', lambda e: e.tensor_scalar(out=sl4v[:, t, :], in0=t8[:, 0:4], scalar1=-1.0, scalar2=None,
                                                     op0=ALU.add), r=[t8], w=[sl4v])
            for j in range(4):
                P.op('vector', lambda e, j=j: e.tensor_scalar(out=oh[:, j, :], in0=val[:], scalar1=t8[:, j:j + 1],
                                                             scalar2=None, op0=ALU.is_equal), r=[val, t8], w=[oh])
            for j in range(4):
                P.op('vector', lambda e, j=j: e.tensor_tensor(out=oh[:, j, :], in0=oh[:, j, :], in1=comb[:, t, :],
                                                             op=ALU.mult), r=[oh, comb], w=[oh])
            P.op('vector', lambda e: e.reduce_sum(out=g4v[:, t, :], in_=oh[:], axis=AX.X), r=[oh], w=[g4v])
            for j in range(4):
                P.op('gpsimd', lambda e, j=j: e.indirect_dma_start(
                    out=Xs.t.ap(), out_offset=bass.IndirectOffsetOnAxis(ap=sl4v[:, t, j:j + 1], axis=0),
                    in_=h2b_all[:, t, :], in_offset=None), r=[sl4v, h2b_all], w=[Xs], dsem=h2b_all)

        views = []
        for t in range(NT):
            sl4v = Buf(self.sl4.t, 'sl4v%d' % t)
            g4v = Buf(self.g4.t, 'g4v%d' % t)
            views.append((sl4v, g4v))
            i = t % 2
            slot_tile(t, pp[i], tmp_[i], val_[i], oh_[i], t8_[i], sl4v, g4v)
        for sl4v, g4v in views:
            for src, dst in ((sl4v, self.sl4), (g4v, self.g4)):
                dst.w = dict(dst.w)
                for k_, v_ in src.w.items():
                    if dst.w.get(k_, 0) < v_:
                        dst.w[k_] = v_

    def phase_moe_sparse(self, l, xsrc, xdst):
        P = self.P
        inp = self.inp
        NT = self.NT
        widx = self.widx
        wc1, wc2 = self.wc1[l], self.wc2[l]
        w1rows = wc1.t.ap().rearrange("(e p h k) n -> (e p h) (k n)", p=128, h=2, k=4)
        w2rows = wc2.t.ap().rearrange("(e p i) n -> (e p) (i n)", p=128, i=8)
        b1rows = inp['b_mlp1'].t.ap().rearrange("l e (p m) -> (l e p) m", m=16)
        b2rows = inp['b_mlp2'].t.ap().rearrange("l e n -> (l e) n")
        with ExitStack() as st:
            B = self.ffn_bufs(st, npy=3)
            w1b = [P.sbuf(st, 'd_w1%d' % i, [128, 8, 2 * D], BF16) for i in range(2)]
            w2b = [P.sbuf(st, 'd_w2%d' % i, [128, 8, D], BF16) for i in range(2)]
            b1c = [P.sbuf(st, 'd_b1c%d' % i, [128, 16], F32) for i in range(2)]
            b2bc = [P.sbuf(st, 'd_b2%d' % i, [128, D], F32) for i in range(2)]
            xs = [P.sbuf(st, 'd_xs%d' % i, [128, D], BF16) for i in range(2)]
            xT = [P.sbuf(st, 'd_xT%d' % i, [128, 8, 512], BF16) for i in range(2)]
            stage = [P.sbuf(st, 'd_st%d' % i, [128, D], F32) for i in range(4)]
            ident = self.c['ident_bf']
            pTbuf = P.psum(st, 'pTb', [128, 8, 128], BF16)
            Xs, Ys = self.Xs, self.Ys
            rg = [self.nc.gpsimd.alloc_register('bc%d_%d' % (q, l)) for q in range(4)]
            rtile = P.sbuf(st, 'd_rt', [128, 1], F32)

            def setregs(e):
                e.reg_mov(rg[0], NE * 128 * 2 - 1)
                e.reg_mov(rg[1], NL * NE * 128 - 1)
                e.reg_mov(rg[2], NL * NE - 1)
                e.reg_mov(rg[3], NE * 128 - 1)
                return e.memset(rtile[:], 0.0)
            P.op('gpsimd', setregs, w=[rtile])
            nxs = [0]

            def gathers(i):
                ws = i % 2
                wa, wb_, bc, b2 = w1b[ws], w2b[ws], b1c[ws], b2bc[ws]
                for h in range(2):
                    P.op('gpsimd', lambda e, h=h: e.indirect_dma_start(
                        out=wa[:, 4 * h:4 * h + 4, :].rearrange("p k n -> p (k n)"), out_offset=None, in_=w1rows,
                        in_offset=bass.IndirectOffsetOnAxis(ap=widx[:, i, 1 + h:2 + h], axis=0),
                        bounds_check=rg[0], oob_is_err=False), r=[widx, wc1], w=[wa], dsem=wa)
                P.op('gpsimd', lambda e: e.indirect_dma_start(
                    out=wb_[:].rearrange("p k n -> p (k n)"), out_offset=None, in_=w2rows,
                    in_offset=bass.IndirectOffsetOnAxis(ap=widx[:, i, 4:5], axis=0),
                    bounds_check=rg[3], oob_is_err=False), r=[widx, wc2], w=[wb_], dsem=wb_)
                P.op('gpsimd', lambda e: e.indirect_dma_start(
                    out=bc[:], out_offset=None, in_=b1rows,
                    in_offset=bass.IndirectOffsetOnAxis(ap=widx[:, i, 0:1], axis=0),
                    bounds_check=rg[1], oob_is_err=False), r=[widx], w=[bc], dsem=bc)
                P.op('gpsimd', lambda e: e.indirect_dma_start(
                    out=b2[:], out_offset=None, in_=b2rows,
                    in_offset=bass.IndirectOffsetOnAxis(ap=widx[:, i, 3:4], axis=0),
                    bounds_check=rg[2], oob_is_err=False), r=[widx], w=[b2], dsem=b2)

            def prep_x(i):
                xb = xT[i % 2]
                for r_ in range(4):
                    xq = xs[nxs[0] % 2]
                    nxs[0] += 1
                    rows = slice(i * TS + r_ * 128, i * TS + (r_ + 1) * 128)
                    P.op('sync', lambda e, xq=xq, rows=rows: e.dma_start(out=xq[:], in_=Xs.t.ap()[rows, :]),
                         r=[Xs], w=[xq], dsem=xq)

                    def tr(e, xq=xq):
                        ins = None
                        for k in range(8):
                            ins = e.transpose(pTbuf[:, k, :], xq[:, k:D:8], ident[:])
                        return ins
                    P.op('tensor', tr, r=[xq, ident], w=[pTbuf])
                    P.op('scalar', lambda e, xb=xb, r_=r_: e.copy(out=xb[:, :, r_ * 128:(r_ + 1) * 128],
                                                                  in_=pTbuf[:]), r=[pTbuf], w=[xb])

            gathers(0)
            prep_x(0)
            for i in range(self.NBLK):
                ws = i % 2
                wa, wb_, bc, b2 = w1b[ws], w2b[ws], b1c[ws], b2bc[ws]
                xb = xT[i % 2]
                if i + 1 < self.NBLK:
                    gathers(i + 1)

                def evac(r_, half, pp, i=i, b2=b2):
                    sg_ = stage[r_]
                    sl = slice(half * 512, (half + 1) * 512)
                    P.op('vector', lambda e: e.tensor_tensor(out=sg_[:, sl], in0=pp[:], in1=b2[:, sl], op=ALU.add),
                         r=[pp, b2], w=[sg_])
                    if half == 1:
                        rows = slice(i * TS + r_ * 128, i * TS + (r_ + 1) * 128)
                        P.op('sync', lambda e: e.dma_start(out=Ys.t.ap()[rows, :], in_=sg_[:]),
                             r=[sg_], w=[Ys], dsem=sg_)
                hook = (lambda i=i: prep_x(i + 1)) if i + 1 < self.NBLK else None
                self.ffn_block(B, xb, wa, wb_, bc, None, evac, mid_hook=hook)
            P.barrier()
            P.flush()
        self.phase_combine(l, xsrc, xdst)

    def phase_combine(self, l, xsrc, xdst):
        P = self.P
        Ys = self.Ys
        sl4, g4 = self.sl4, self.g4
        with ExitStack() as st:
            g2 = self.load_bcast(st, 'z_g2', self.modv, l * 6 * D + 5 * D)
            xt = [P.sbuf(st, 'z_x%d' % i, [128, D], F32) for i in range(2)]
            yg = [[P.sbuf(st, 'z_y%d_%d' % (i, j), [128, D], F32) for j in range(4)] for i in range(3)]
            acc = [P.sbuf(st, 'z_a%d' % i, [128, D], F32) for i in range(2)]
            for t in range(self.NT):
                i = t % 2
                g3 = t % 3
                rows = slice(t * 128, (t + 1) * 128)
                P.op('sync', lambda e, i=i, rows=rows: e.dma_start(out=xt[i][:], in_=xsrc.t.ap()[rows, :]),
                     r=[xsrc], w=[xt[i]], dsem=xt[i])
                for j in range(4):
                    P.op('gpsimd', lambda e, g3=g3, j=j, t=t: e.indirect_dma_start(
                        out=yg[g3][j][:], out_offset=None, in_=Ys.t.ap(),
                        in_offset=bass.IndirectOffsetOnAxis(ap=sl4[:, t, j:j + 1], axis=0)),
                        r=[Ys, sl4], w=[yg[g3][j]], dsem=yg[g3][j])
                P.op('vector', lambda e, i=i, t=t, g3=g3: e.tensor_scalar(out=acc[i][:], in0=yg[g3][0][:],
                                                                         scalar1=g4[:, t, 0:1], scalar2=None,
                                                                         op0=ALU.mult),
                     r=[yg[g3][0], g4], w=[acc[i]])
                for j in range(1, 4):
                    P.op('vector', lambda e, i=i, t=t, j=j, g3=g3: e.scalar_tensor_tensor(
                        out=acc[i][:], in0=yg[g3][j][:], scalar=g4[:, t, j:j + 1], in1=acc[i][:], op0=ALU.mult,
                        op1=ALU.add), r=[yg[g3][j], g4, acc[i]], w=[acc[i]])
                P.op('vector', lambda e, i=i: e.tensor_tensor(out=acc[i][:], in0=acc[i][:], in1=g2[:], op=ALU.mult),
                     r=[acc[i], g2], w=[acc[i]])
                P.op('vector', lambda e, i=i: e.tensor_tensor(out=acc[i][:], in0=acc[i][:], in1=xt[i][:], op=ALU.add),
                     r=[acc[i], xt[i]], w=[acc[i]])
                P.op('sync', lambda e, i=i, rows=rows: e.dma_start(out=xdst.t.ap()[rows, :], in_=acc[i][:]),
                     r=[acc[i]], w=[xdst], dsem=acc[i])
            P.barrier()
            P.flush()

    def build(self):
        P = self.P
        self.load_consts()
        self.comb = P.sbuf(self.stack, 'comb', [128, self.NT, NE], F32)
        self.memb = P.sbuf(self.stack, 'memb', [128, self.NT, NE], F32)
        self.h2T_d = P.dram("h2T_d", [D, self.S], BF16, self.sk)
        self.accd = P.dram("accd", [self.S, D], F32, self.sk)
        self.comb_d = P.dram("comb_d", [128, self.NT, NE], F32, self.sk)
        self.NBLK = NE + 4 * self.S // TS
        self.sl4 = P.sbuf(self.stack, 'sl4', [128, self.NT, 4], I32)
        self.g4 = P.sbuf(self.stack, 'g4', [128, self.NT, 4], F32)
        self.widx = P.sbuf(self.stack, 'widx', [128, self.NBLK, 5], I32)
        self.Xs = P.dram("Xs", [self.NBLK * TS, D], BF16, self.sk)
        self.Ys = P.dram("Ys", [self.NBLK * TS, D], F32, self.sk)
        self.phase_ada()
        if self.sparse:
            self.precast_weights()
        xsrc = self.inp['x']
        for l in range(self.nlayers):
            last = (l == self.nlayers - 1)
            with ExitStack() as st:
                hT = P.sbuf(st, 'hT', [128, 8, self.S], BF16)
                self.phase_norm_T(st, l, xsrc, 0, 'norm1_g', hT)
                self.phase_proj(l, hT)
            self.phase_attn(l)
            self.phase_wout(l, xsrc, self.x1)
            xdst = self.out if last else self.x2
            if self.sparse:
                with ExitStack() as st:
                    h2b_all = P.sbuf(st, 'h2b_all', [128, self.NT, D], BF16)
                    self.phase_norm_T(None, l, self.x1, 1, 'norm2_g', None, router=True, h2b_all=h2b_all)
                self.phase_moe_sparse(l, self.x1, xdst)
            else:
                self.phase_norm_T(None, l, self.x1, 1, 'norm2_g', None, hT_dram=self.h2T_d, router=True)
                self.phase_moe_dense(l, self.x1, xdst)
            xsrc = xdst
        P.barrier()
        P.flush()
        self.stack.close()
        return self.nc


_CACHE = {}


def kernel(**inputs):
    S = inputs['x'].shape[1]
    nb = inputs['x'].shape[0]
    if S not in _CACHE:
        _CACHE[S] = K(S).build()
    nc = _CACHE[S]
    consts = make_consts()
    shared = {}
    for name, _ in INPUT_SPECS:
        if name in ('x', 'c'):
            continue
        shared[name] = np.ascontiguousarray(inputs[name], dtype=np.float32)
    for k, v in consts.items():
        shared['c_' + k] = v
    in_maps = []
    for b in range(nb):
        m = dict(shared)
        m['x'] = np.ascontiguousarray(inputs['x'][b], dtype=np.float32)
        m['c'] = np.ascontiguousarray(inputs['c'][b], dtype=np.float32)
        in_maps.append(m)
    res = run_bass_kernel_spmd(nc, in_maps, core_ids=list(range(nb)))
    return np.stack([np.asarray(r['out']) for r in res.results], 0).astype(np.float32)
```
